# Optimizing a Trainium2 kernel written in Bass

```python
import math
import jax, jax.numpy as jnp
from jax import lax
import numpy as np

D_MODEL = 1024
BATCH = 2
SEQ = 8192
DEPTH = 1

D_MIX = D_MODEL
ATTN_WIDTH = D_MIX // 2
CONV_WIDTH = D_MIX - ATTN_WIDTH
N_DIFF_HEADS = 4
DIFF_HEAD_DIM = ATTN_WIDTH // N_DIFF_HEADS // 2
V_HEAD_DIM = 2 * DIFF_HEAD_DIM
ROT_DIM = DIFF_HEAD_DIM // 4
ROPE_THETA = 500000.0
CONV_K = 31
IN_COLS = 3 * ATTN_WIDTH + 2 * CONV_WIDTH
N_GROUPS = 4
EXPERTS_PER_GROUP = 8
N_EXPERTS = N_GROUPS * EXPERTS_PER_GROUP
EXPERT_FF = 256
TOP_K_INNER = 2
Q_BLOCK = 128
EPS = 1e-6

kernel_name = "hymba_diffattn_conformer_hmoe_adaln"


def rms_norm(x, g):
    xf = x.astype(jnp.float32)
    y = xf * lax.rsqrt(jnp.mean(xf * xf, axis=-1, keepdims=True) + EPS)
    return (y * g.astype(jnp.float32)).astype(x.dtype)


def layer_norm(x, g, b):
    xf = x.astype(jnp.float32)
    mu = jnp.mean(xf, axis=-1, keepdims=True)
    var = jnp.mean(jnp.square(xf - mu), axis=-1, keepdims=True)
    y = (xf - mu) * lax.rsqrt(var + EPS) * g.astype(jnp.float32) + b.astype(jnp.float32)
    return y.astype(x.dtype)


def rope_tables(positions):
    half = ROT_DIM // 2
    inv_freq = ROPE_THETA ** (-jnp.arange(0, ROT_DIM, 2, dtype=jnp.float32) / ROT_DIM)
    ang = positions.astype(jnp.float32)[..., None] * inv_freq
    return jnp.cos(ang)[:, :, None, None, :], jnp.sin(ang)[:, :, None, None, :]


def partial_rope(t, cos, sin):
    half = ROT_DIM // 2
    tf = t[..., :ROT_DIM].astype(jnp.float32)
    t1, t2 = tf[..., :half], tf[..., half:]
    rot = jnp.concatenate([t1 * cos - t2 * sin, t2 * cos + t1 * sin], axis=-1)
    return jnp.concatenate([rot.astype(t.dtype), t[..., ROT_DIM:]], axis=-1)


def diff_attention(q, k, v, lam, lambda_init, subln_g):
    B, S, H = q.shape[0], q.shape[1], q.shape[2]
    nb = S // Q_BLOCK
    scale = DIFF_HEAD_DIM ** -0.5
    kf = k.astype(jnp.float32)
    vf = v.astype(jnp.float32)
    qb = q.reshape(B, nb, Q_BLOCK, H, 2, DIFF_HEAD_DIM).transpose(1, 0, 2, 3, 4, 5)
    k_pos = jnp.arange(S)

    def block(args):
        q_blk, i = args
        s = jnp.einsum('bqhcd,bkhcd->bhcqk', q_blk.astype(jnp.float32), kf) * scale
        q_pos = i * Q_BLOCK + jnp.arange(Q_BLOCK)
        mask = k_pos[None, :] <= q_pos[:, None]
        p = jax.nn.softmax(jnp.where(mask, s, -jnp.inf), axis=-1)
        a = p[:, :, 0] - lam * p[:, :, 1]
        return jnp.einsum('bhqk,bkhe->bqhe', a, vf)

    o = lax.map(block, (qb, jnp.arange(nb)))
    o = o.transpose(1, 0, 2, 3, 4).reshape(B, S, H, V_HEAD_DIM).astype(q.dtype)
    o = rms_norm(o, subln_g) * (1.0 - lambda_init)
    return o.reshape(B, S, H * V_HEAD_DIM)


def conformer_conv(glu_in, w_dw, b_dw, ln_g, ln_b):
    a, gate = glu_in[..., :CONV_WIDTH], glu_in[..., CONV_WIDTH:]
    u = a * jax.nn.sigmoid(gate)
    y = lax.conv_general_dilated(u, w_dw[:, None, :], window_strides=(1,), padding=[(CONV_K - 1, 0)],
                                 dimension_numbers=('NWC', 'WIO', 'NWC'),
                                 feature_group_count=CONV_WIDTH) + b_dw
    return jax.nn.silu(layer_norm(y, ln_g, ln_b))


def hier_moe(h, w_group, b_group, w_router, b_router, w_gate, w_up, w_down):
    B, S, D = h.shape
    t = h.reshape(-1, D)
    n = t.shape[0]
    g_prob = jax.nn.softmax((t @ w_group).astype(jnp.float32) + b_group.astype(jnp.float32), axis=-1)
    g_p, g_idx = lax.top_k(g_prob, 1)
    e_logits = ((t @ w_router).astype(jnp.float32) + b_router.astype(jnp.float32)).reshape(n, N_GROUPS, EXPERTS_PER_GROUP)
    e_in = e_logits[jnp.arange(n), g_idx[:, 0]]
    e_p, e_idx = lax.top_k(jax.nn.softmax(e_in, axis=-1), TOP_K_INNER)
    e_p = e_p / jnp.sum(e_p, axis=-1, keepdims=True)
    wts = g_p * e_p
    expert_id = g_idx * EXPERTS_PER_GROUP + e_idx
    combine = jnp.sum(jax.nn.one_hot(expert_id, N_EXPERTS, dtype=jnp.float32) * wts[..., None], axis=1)
    gt = jnp.einsum('nd,edf->nef', t, w_gate)
    up = jnp.einsum('nd,edf->nef', t, w_up)
    hid = jax.nn.silu(gt) * up * combine.astype(t.dtype)[:, :, None]
    out = jnp.einsum('nef,efd->nd', hid, w_down)
    return out.reshape(B, S, D)


def _normal(k, shape, std):
    return jax.random.normal(k, shape, jnp.float32) * std


def setup_inputs(seed: int = 0) -> dict:
    key = jax.random.key(seed)
    ks = jax.random.split(key, 32)
    L, D = DEPTH, D_MODEL
    offset = jax.random.randint(ks[2], (BATCH, 1), 0, 1024, dtype=jnp.int32)
    positions = offset + jnp.arange(SEQ, dtype=jnp.int32)[None, :]
    return {
        "x": _normal(ks[0], (BATCH, SEQ, D), 1.0),
        "c": _normal(ks[1], (BATCH, D), 1.0),
        "positions": positions,
        "w_ada": _normal(ks[3], (L, D, 6 * D), D ** -0.5),
        "b_ada": _normal(ks[4], (L, 6 * D), 0.02),
        "g_mix": 1.0 + _normal(ks[5], (L, D), 0.02),
        "w_in": _normal(ks[6], (L, D, IN_COLS), D ** -0.5),
        "q_norm_g": 1.0 + _normal(ks[7], (L, DIFF_HEAD_DIM), 0.02),
        "k_norm_g": 1.0 + _normal(ks[8], (L, DIFF_HEAD_DIM), 0.02),
        "lambda_q1": _normal(ks[9], (L, DIFF_HEAD_DIM), 0.1),
        "lambda_k1": _normal(ks[10], (L, DIFF_HEAD_DIM), 0.1),
        "lambda_q2": _normal(ks[11], (L, DIFF_HEAD_DIM), 0.1),
        "lambda_k2": _normal(ks[12], (L, DIFF_HEAD_DIM), 0.1),
        "subln_g": 1.0 + _normal(ks[13], (L, V_HEAD_DIM), 0.02),
        "b_glu": _normal(ks[14], (L, 2 * CONV_WIDTH), 0.02),
        "w_dw": _normal(ks[15], (L, CONV_K, CONV_WIDTH), CONV_K ** -0.5),
        "b_dw": _normal(ks[16], (L, CONV_WIDTH), 0.02),
        "conv_ln_g": 1.0 + _normal(ks[17], (L, CONV_WIDTH), 0.02),
        "conv_ln_b": _normal(ks[18], (L, CONV_WIDTH), 0.02),
        "w_out": _normal(ks[19], (L, D_MIX, D), D_MIX ** -0.5),
        "g_ffn": 1.0 + _normal(ks[20], (L, D), 0.02),
        "w_group": _normal(ks[21], (L, D, N_GROUPS), D ** -0.5),
        "b_group": _normal(ks[22], (L, N_GROUPS), 0.01),
        "w_router": _normal(ks[23], (L, D, N_EXPERTS), D ** -0.5),
        "b_router": _normal(ks[24], (L, N_EXPERTS), 0.01),
        "w_gate": _normal(ks[25], (L, N_EXPERTS, D, EXPERT_FF), D ** -0.5),
        "w_up": _normal(ks[26], (L, N_EXPERTS, D, EXPERT_FF), D ** -0.5),
        "w_down": _normal(ks[27], (L, N_EXPERTS, EXPERT_FF, D), EXPERT_FF ** -0.5),
    }


def reference(x, c, positions, w_ada, b_ada, g_mix, w_in, q_norm_g, k_norm_g, lambda_q1, lambda_k1,
              lambda_q2, lambda_k2, subln_g, b_glu, w_dw, b_dw, conv_ln_g, conv_ln_b, w_out, g_ffn,
              w_group, b_group, w_router, b_router, w_gate, w_up, w_down):
    B, S, D = x.shape
    cos, sin = rope_tables(positions)
    c_act = jax.nn.silu(c)
    for l in range(DEPTH):
        lambda_init = 0.8 - 0.6 * math.exp(-0.3 * l)
        mod = c_act @ w_ada[l] + b_ada[l]
        sh_a, sc_a, gt_a, sh_f, sc_f, gt_f = [m[:, None, :] for m in jnp.split(mod, 6, axis=-1)]

        h = rms_norm(x, g_mix[l]) * (1.0 + sc_a) + sh_a
        p = h @ w_in[l]
        q = p[..., :ATTN_WIDTH].reshape(B, S, N_DIFF_HEADS, 2, DIFF_HEAD_DIM)
        k = p[..., ATTN_WIDTH:2 * ATTN_WIDTH].reshape(B, S, N_DIFF_HEADS, 2, DIFF_HEAD_DIM)
        v = p[..., 2 * ATTN_WIDTH:3 * ATTN_WIDTH].reshape(B, S, N_DIFF_HEADS, V_HEAD_DIM)
        glu_in = p[..., 3 * ATTN_WIDTH:] + b_glu[l]
        q = partial_rope(rms_norm(q, q_norm_g[l]), cos, sin)
        k = partial_rope(rms_norm(k, k_norm_g[l]), cos, sin)
        lam = (jnp.exp(jnp.sum(lambda_q1[l].astype(jnp.float32) * lambda_k1[l].astype(jnp.float32)))
               - jnp.exp(jnp.sum(lambda_q2[l].astype(jnp.float32) * lambda_k2[l].astype(jnp.float32)))
               + lambda_init)
        attn_out = diff_attention(q, k, v, lam, lambda_init, subln_g[l])
        conv_out = conformer_conv(glu_in, w_dw[l], b_dw[l], conv_ln_g[l], conv_ln_b[l])
        y = jnp.concatenate([attn_out, conv_out], axis=-1) @ w_out[l]
        x = x + gt_a * y

        h = rms_norm(x, g_ffn[l]) * (1.0 + sc_f) + sh_f
        x = x + gt_f * hier_moe(h, w_group[l], b_group[l], w_router[l], b_router[l],
                                w_gate[l], w_up[l], w_down[l])
    return x
```

```python
import math
import numpy as np
import ml_dtypes
import concourse.bass as bass
import concourse.mybir as mybir
from concourse.bass_utils import run_bass_kernel_spmd

F32 = mybir.dt.float32
BF16 = mybir.dt.bfloat16
I32 = mybir.dt.int32
AF = mybir.ActivationFunctionType
ALU = mybir.AluOpType
AX = mybir.AxisListType

D = 1024
KC = 8
SEQ = 8192
NBLK_B = 64
NOWN = 16
EPS = 1e-6
LAMBDA_INIT = 0.8 - 0.6 * math.exp(-0.3 * 0)
NEXP = 32
TWO_PI = 2.0 * math.pi
C1 = 6.28125
C2 = TWO_PI - C1


class Sched:
    COMPUTE = ("pe", "act", "dve", "pool")

    def __init__(self, nc):
        self.nc = nc
        self.ops = []
        self.last_w = {}
        self.readers = {}
        self.tag_count = {}
        self.bulk_tags = set()
        self.epoch = None
        self.epoch_start = 0

    def add(self, eng, fn, r=(), w=(), tag=None):
        idx = len(self.ops)
        deps = set()
        for res in r:
            if res in self.last_w:
                deps.add(self.last_w[res])
        for res in w:
            if res in self.last_w:
                deps.add(self.last_w[res])
            for i in self.readers.get(res, {}).values():
                deps.add(i)
        if self.epoch is not None:
            deps.add(self.epoch)
        op = dict(eng=eng, fn=fn, deps=deps, tag=tag, signal=False, idx=idx)
        if tag is not None:
            self.tag_count[tag] = self.tag_count.get(tag, 0) + 1
            op["tagn"] = self.tag_count[tag]
        self.ops.append(op)
        for res in w:
            self.last_w[res] = idx
            self.readers[res] = {}
        for res in r:
            key = eng if tag is None else ("dma", idx)
            self.readers.setdefault(res, {})[key] = idx
        return idx

    def barrier(self, tile):
        deps = set()
        last = {}
        for op in self.ops[self.epoch_start:]:
            if op["tag"] is None:
                last[op["eng"]] = op["idx"]
            else:
                deps.add(op["idx"])
        deps |= set(last.values())
        idx = self.add("dve", lambda e: e.memset(tile, 0.0))
        self.ops[idx]["deps"] |= deps
        self.epoch = idx
        self.epoch_start = idx

    def emit(self):
        nc = self.nc
        ops = self.ops
        for op in ops:
            for d in op["deps"]:
                dop = ops[d]
                if dop["tag"] is None and dop["eng"] == "pe" and op["eng"] == "pe" and op["tag"] is None:
                    continue
                dop["signal"] = True
        sems = {e: nc.alloc_semaphore(name="sem_" + e) for e in self.COMPUTE}
        tagsem = {t: nc.alloc_semaphore(name="semt_" + str(t)) for t in self.tag_count}
        cnt = {e: 0 for e in self.COMPUTE}
        for op in ops:
            if op["tag"] is not None:
                t = op["tag"]
                n = self.tag_count[t] if t in self.bulk_tags else op["tagn"]
                op["token"] = (tagsem[t], 16 * n, ("t", t))
            elif op["signal"]:
                cnt[op["eng"]] += 1
                op["token"] = (sems[op["eng"]], cnt[op["eng"]], ("e", op["eng"]))
        streams = {e: [] for e in ("pe", "act", "dve", "pool", "sp")}
        for op in ops:
            streams[op["eng"]].append(op)
        out_tag_total = {t: 16 * n for t, n in self.tag_count.items()}

        def run(engname, e):
            waited = {}
            for op in streams[engname]:
                need = {}
                for d in op["deps"]:
                    dop = ops[d]
                    if dop["tag"] is None and dop["eng"] == "pe" and engname == "pe" and op["tag"] is None:
                        continue
                    sem, val, key = dop["token"]
                    if need.get(key, (None, 0))[1] < val:
                        need[key] = (sem, val)
                for key, (sem, val) in need.items():
                    if waited.get(key, 0) >= val:
                        continue
                    e.wait_ge(sem, val)
                    waited[key] = val
                ins = op["fn"](e)
                if op["tag"] is not None:
                    ins.then_inc(op["token"][0], 16)
                elif op["signal"]:
                    ins.then_inc(op["token"][0], 1)
            if engname == "sp":
                for t, tot in out_tag_total.items():
                    if str(t).startswith("out"):
                        e.wait_ge(tagsem[t], tot)

        with nc.Block() as block:
            @block.tensor
            def _(e):
                run("pe", e)

            @block.scalar
            def _(e):
                run("act", e)

            @block.vector
            def _(e):
                run("dve", e)

            @block.gpsimd
            def _(e):
                run("pool", e)

            @block.sync
            def _(e):
                run("sp", e)


class Arena:
    def __init__(self, nc, words):
        self.t = nc.alloc_sbuf_tensor("arena", [128, words], F32)
        self.words = words
        self.off = 0

    def mark(self):
        return self.off

    def reset(self, m):
        self.off = m

    def alloc(self, shape, dt=F32):
        n = 1
        for s in shape[1:]:
            n *= s
        w = n if dt in (F32, I32) else (n + 1) // 2
        w = (w + 7) // 8 * 8
        assert self.off + w <= self.words, ("arena overflow", self.off, w, self.words)
        v = self.t[:, self.off:self.off + w]
        self.off += w
        if dt != F32:
            v = v.bitcast(dt)
        v = v[:, 0:n]
        if len(shape) == 3:
            v = v.rearrange("p (a b) -> p a b", a=shape[1])
        elif len(shape) == 4:
            v = v.rearrange("p (a b c) -> p a b c", a=shape[1], b=shape[2])
        return v


def build_program(stop_after=None, mini=False, mini_ns=1):
    nc = bass.Bass("TRN2", target_bir_lowering=False)
    S = Sched(nc)
    S.bulk_tags.add("const")

    def din(name, shape, dt=F32):
        return nc.dram_tensor(name, list(shape), dt, kind="ExternalInput").ap()

    xb = din("xb", [SEQ, D])
    xo = din("xo", [NOWN * 128, D])
    xh = din("xh", [4 * 128, D])
    cT_d = din("cT", [128, KC])
    posb_d = din("posb", [128, NBLK_B], I32)
    poso_d = din("poso", [128, NOWN], I32)
    w_ada = din("w_ada", [D, 6 * D])
    b_ada = din("b_ada", [1, 6 * D])
    gmix_d = din("g_mixT", [128, KC])
    gffn_d = din("g_ffnT", [128, KC])
    w_in = din("w_in", [D, 2560])
    gq_d = din("q_norm_g", [1, 64])
    gk_d = din("k_norm_g", [1, 64])
    lq1_d = din("lambda_q1", [1, 64])
    lk1_d = din("lambda_k1", [1, 64])
    lq2_d = din("lambda_q2", [1, 64])
    lk2_d = din("lambda_k2", [1, 64])
    subg_d = din("subln_g", [1, 128])
    bglu_d = din("b_gluT", [128, 8])
    wdw_d = din("w_dwT", [128, 4, 31])
    bdw_d = din("b_dwT", [128, 4])
    lng_d = din("ln_gT", [128, 4])
    lnb_d = din("ln_bT", [128, 4])
    w_out = din("w_out", [D, D])
    wr_d = din("w_rt", [D, 36])
    br_d = din("b_rt", [1, 36])
    w_gate = din("w_gate", [NEXP, D, 256])
    w_up = din("w_up", [NEXP, D, 256])
    w_down = din("w_down", [NEXP, 256, D])
    identf_d = din("ident_f", [128, 128])
    identb_d = din("ident_b", [128, 128], BF16)
    invf_d = din("inv_freq", [1, 8])
    mask_d = din("cmask", [128, 4 * 256], BF16)
    hmask_d = din("hmask", [128, 1])
    out_d = nc.dram_tensor("out", [NOWN * 128, D], F32, kind="ExternalOutput").ap()
    dbg = {}

    A = Arena(nc, 53000)
    ps = [nc.alloc_psum_tensor("ps%d" % i, [128, 512], F32) for i in range(8)]

    def psb(i):
        return ps[i][:, :].bitcast(BF16)

    ident_f = A.alloc([128, 128])
    ident_b = A.alloc([128, 128], BF16)
    cT = A.alloc([128, KC])
    cact = A.alloc([128, KC])
    gmixT = A.alloc([128, KC])
    gffnT = A.alloc([128, KC])
    sh_a = A.alloc([128, KC]); sc_a = A.alloc([128, KC]); gsc_a = A.alloc([128, KC])
    sh_f = A.alloc([128, KC]); sc_f = A.alloc([128, KC]); gsc_f = A.alloc([128, KC])
    gq_rep = A.alloc([128, 64]); gk_rep = A.alloc([128, 64])
    lvec = A.alloc([128, 4, 64])
    sg_rep = A.alloc([128, 128])
    lam_t = A.alloc([128, 8])
    bgluT = A.alloc([128, 8])
    wdwT = A.alloc([128, 4, 31])
    bdwT = A.alloc([128, 4]); lngT = A.alloc([128, 4]); lnbT = A.alloc([128, 4])
    wr = A.alloc([128, KC, 36])
    br_rep = A.alloc([128, 36])
    invf = A.alloc([128, 8])
    cmask = A.alloc([128, 4 * 256], BF16)
    hmask = A.alloc([128, 1])
    cos_b = A.alloc([128, NBLK_B, 8]); sin_b = A.alloc([128, NBLK_B, 8])
    cos_o = A.alloc([128, NOWN, 8]); sin_o = A.alloc([128, NOWN, 8])
    epsc = A.alloc([128, 1]); m8c = A.alloc([128, 1])
    ones_ln = A.alloc([128, 128])
    ATTN_OFF = A.mark()
    attnT = A.alloc([128, 4, NOWN * 128], BF16)
    small = A.alloc([128, 64])
    junk_sb = A.alloc([128, D], BF16)
    P_END = A.mark()

    def dma(out, in_, r, w, tag):
        S.add("sp", lambda e, o=out, i=in_: e.dma_start(out=o, in_=i), r=r, w=w, tag=tag)

    def act(out, in_, func, r, w, bias=None, scale=None, accum=None):
        def f(e, out=out, in_=in_, func=func, bias=bias, scale=scale, accum=accum):
            kw = {}
            if bias is not None:
                kw["bias"] = bias
            if scale is not None:
                kw["scale"] = scale
            if accum is not None:
                kw["accum_out"] = accum
            return e.activation(out, in_, func, **kw)
        S.add("act", f, r=r, w=w)

    def tsc(eng, out, in0, s1, s2, op0, op1, r, w):
        def f(e, out=out, in0=in0, s1=s1, s2=s2, op0=op0, op1=op1):
            if op1 is None:
                return e.tensor_scalar(out, in0, s1, None, op0)
            return e.tensor_scalar(out, in0, s1, s2, op0, op1)
        S.add(eng, f, r=r, w=w)

    def tt(eng, out, in0, in1, op, r, w):
        S.add(eng, lambda e, o=out, a=in0, b=in1, op=op: e.tensor_tensor(o, a, b, op), r=r, w=w)

    def stt(out, in0, sc, in1, op0, op1, r, w):
        S.add("dve", lambda e, o=out, a=in0, s=sc, b=in1, o0=op0, o1=op1:
              e.scalar_tensor_tensor(o, a, s, b, o0, o1), r=r, w=w)

    def cp(eng, out, in_, r, w):
        S.add(eng, lambda e, o=out, i=in_: e.tensor_copy(o, i), r=r, w=w)

    def mms(items, r, w):
        def f(e, items=items):
            ins = None
            for (o, l, rh, st, sp_) in items:
                ins = e.matmul(o, l, rh, start=st, stop=sp_)
            return ins
        S.add("pe", f, r=r, w=w)

    def tps(items, ident, r, w):
        def f(e, items=items, ident=ident):
            ins = None
            for (o, i) in items:
                ins = e.transpose(o, i, ident)
            return ins
        S.add("pe", f, r=r, w=w)

    def rstd_from_ss(ss_ap, out_ap, inv_n, rk, wk):
        act(out_ap, ss_ap, AF.Ln, r=[rk, "epsc"], w=[wk], bias=epsc[:, 0:1], scale=inv_n)
        act(out_ap, out_ap, AF.Exp, r=[wk], w=[wk], scale=-0.5)

    def cload(dst, src, key):
        dma(dst, src, r=[], w=[key], tag="const")

    cload(ident_f, identf_d, "ident_f")
    cload(ident_b, identb_d, "ident_b")
    cload(cT, cT_d, "cT")
    cload(gmixT, gmix_d, "gmixT")
    cload(gffnT, gffn_d, "gffnT")
    cload(gq_rep, gq_d.broadcast_to([128, 64]), "gq_rep")
    cload(gk_rep, gk_d.broadcast_to([128, 64]), "gk_rep")
    for i, dd in enumerate((lq1_d, lk1_d, lq2_d, lk2_d)):
        cload(lvec[:, i, :], dd.broadcast_to([128, 64]), "lvec%d" % i)
    cload(sg_rep, subg_d.broadcast_to([128, 128]), "sg_rep")
    cload(bgluT, bglu_d, "bgluT")
    cload(wdwT, wdw_d, "wdwT")
    cload(bdwT, bdw_d, "bdwT")
    cload(lngT, lng_d, "lngT")
    cload(lnbT, lnb_d, "lnbT")
    cload(wr, wr_d.rearrange("(k p) n -> p k n", p=128), "wr")
    cload(br_rep, br_d.broadcast_to([128, 36]), "br_rep")
    cload(invf, invf_d.broadcast_to([128, 8]), "invf")
    cload(cmask, mask_d, "cmask")
    cload(hmask, hmask_d, "hmask")
    posb_i = A.alloc([128, NBLK_B], I32)
    poso_i = A.alloc([128, NOWN], I32)
    cload(posb_i, posb_d, "posb_i")
    cload(poso_i, poso_d, "poso_i")

    S.add("dve", lambda e: e.memset(lam_t, 0.0), r=[], w=["lam0", "lam1", "lam23", "lam4", "neglam"])
    S.add("dve", lambda e: e.memset(small, 0.0), r=[], w=["small", "junk64", "ropetab", "gains"])
    S.add("dve", lambda e: e.memset(epsc, EPS), r=[], w=["epsc"])
    S.add("dve", lambda e: e.memset(m8c, -8.0), r=[], w=["m8c"])
    S.add("dve", lambda e: e.memset(ones_ln, 1.0 / 512.0), r=[], w=["ones_ln"])

    act(cact, cT, AF.Silu, r=["cT"], w=["cact"])
    tsc("dve", sg_rep, sg_rep, 1.0 - LAMBDA_INIT, None, ALU.mult, None, r=["sg_rep"], w=["sg_rep"])

    junk64 = small[:, 0:64]
    tt("dve", junk64, lvec[:, 0, :], lvec[:, 1, :], ALU.mult, r=["lvec0", "lvec1"], w=["junk64"])
    S.add("dve", lambda e: e.tensor_reduce(lam_t[:, 0:1], junk64, AX.X, ALU.add), r=["junk64"], w=["lam0"])
    tt("dve", junk64, lvec[:, 2, :], lvec[:, 3, :], ALU.mult, r=["lvec2", "lvec3", "lam0"], w=["junk64"])
    S.add("dve", lambda e: e.tensor_reduce(lam_t[:, 1:2], junk64, AX.X, ALU.add), r=["junk64"], w=["lam1"])
    act(lam_t[:, 2:4], lam_t[:, 0:2], AF.Exp, r=["lam0", "lam1"], w=["lam23"])
    tt("dve", lam_t[:, 4:5], lam_t[:, 3:4], lam_t[:, 2:3], ALU.subtract, r=["lam23"], w=["lam4"])
    tsc("dve", lam_t[:, 7:8], lam_t[:, 4:5], -LAMBDA_INIT, None, ALU.add, None, r=["lam4"], w=["neglam"])
    neglam = lam_t[:, 7:8]

    def rope_tables(pos_i, nblk, cos_t, sin_t, pfx):
        m0 = A.mark()
        posf = A.alloc([128, nblk])
        ang = A.alloc([128, nblk, 8])
        tq = A.alloc([128, nblk, 8])
        ki = A.alloc([128, nblk, 8], I32)
        kf = A.alloc([128, nblk, 8])
        rr = A.alloc([128, nblk, 8])
        mk = A.alloc([128, nblk, 8])
        cp("dve", posf, pos_i, r=[pfx + "pos_i"], w=[pfx + "posf"])
        tt("dve", ang, posf.unsqueeze(2).broadcast_to([128, nblk, 8]),
           invf.unsqueeze(1).broadcast_to([128, nblk, 8]), ALU.mult, r=[pfx + "posf", "invf"], w=[pfx + "ang"])
        for which, dst in (("s", sin_t), ("c", cos_t)):
            k0 = pfx + which
            src = ang
            if which == "c":
                tsc("dve", rr, ang, math.pi / 2.0, None, ALU.add, None, r=[pfx + "ang", pfx + "rr"], w=[pfx + "rr"])
                tsc("dve", tq, rr, 1.0 / TWO_PI, None, ALU.mult, None, r=[pfx + "rr", pfx + "tq"], w=[pfx + "tq"])
                base = rr
            else:
                tsc("dve", tq, ang, 1.0 / TWO_PI, None, ALU.mult, None, r=[pfx + "ang"], w=[pfx + "tq"])
                base = ang
            cp("dve", ki, tq, r=[pfx + "tq"], w=[pfx + "ki"])
            cp("dve", kf, ki, r=[pfx + "ki"], w=[pfx + "kf"])
            stt(rr, kf, -C1, base, ALU.mult, ALU.add, r=[pfx + "kf", pfx + "ang", pfx + "rr"], w=[pfx + "rr"])
            stt(rr, kf, -C2, rr, ALU.mult, ALU.add, r=[pfx + "kf", pfx + "rr"], w=[pfx + "rr"])
            tsc("dve", mk, rr, math.pi, -TWO_PI, ALU.is_gt, ALU.mult, r=[pfx + "rr", pfx + "mk"], w=[pfx + "mk"])
            tt("dve", rr, rr, mk, ALU.add, r=[pfx + "rr", pfx + "mk"], w=[pfx + "rr"])
            tsc("dve", mk, rr, -math.pi, TWO_PI, ALU.is_lt, ALU.mult, r=[pfx + "rr", pfx + "mk"], w=[pfx + "mk"])
            tt("dve", rr, rr, mk, ALU.add, r=[pfx + "rr", pfx + "mk"], w=[pfx + "rr"])
            act(dst, rr, AF.Sin, r=[pfx + "rr"], w=[pfx + "tab" + which])
        return [pfx + "tabs", pfx + "tabc"]

    KV0 = A.mark()
    KT = A.alloc([128, 4, SEQ], BF16)
    VA = A.alloc([128, NBLK_B, 4, 130], BF16)
    WORK0 = A.mark()
    rb = rope_tables(posb_i, NBLK_B, cos_b, sin_b, "rb_")
    ro = rope_tables(poso_i, NOWN, cos_o, sin_o, "ro_")
    rope_keys = ["rb_rr", "rb_mk", "rb_kf", "rb_ki", "rb_tq", "rb_ang", "rb_posf",
                 "ro_rr", "ro_mk", "ro_kf", "ro_ki", "ro_tq", "ro_ang", "ro_posf"]

    def ada_alloc():
        return [(A.alloc([128, KC, 512]), A.alloc([128, 512]), A.alloc([128, 512]), str(i)) for i in range(2)]

    def ada_group(g, ab, pbank):
        stage, brep, modrep, sfx = ab
        dma(stage, w_ada.rearrange("(k p) n -> p k n", p=128)[:, :, g * 512:(g + 1) * 512],
            r=[], w=["ada_stage" + sfx], tag="ada" + sfx)
        dma(brep, b_ada[0:1, g * 512:(g + 1) * 512].broadcast_to([128, 512]), r=[], w=["ada_brep" + sfx], tag="adab" + sfx)
        mms([(ps[pbank][:, :], crep[:, k, :], stage[:, k, :], k == 0, k == KC - 1) for k in range(KC)],
            r=["ada_stage" + sfx, "crep"], w=["ps%d" % pbank])
        tt("dve", modrep, ps[pbank][:, :], brep, ALU.add, r=["ps%d" % pbank, "ada_brep" + sfx], w=["ada_modrep" + sfx])

    def ada_to_cols(ab, dst, col0, pbank):
        modrep, sfx = ab[2], ab[3]
        tps([(ps[pbank][:, jj * 128:(jj + 1) * 128], modrep[:, jj * 128:(jj + 1) * 128]) for jj in range(4)],
            ident_f, r=["ada_modrep" + sfx, "ident_f"], w=["ps%d" % pbank])
        cp("dve", dst[:, col0:col0 + 4], ps[pbank][:, :].rearrange("p (j c) -> p j c", c=128)[:, :, 0],
           r=["ps%d" % pbank], w=["adacols"])

    S.barrier(small[:, 62:63])
    A.reset(WORK0)
    m_ada = A.mark()
    crep = A.alloc([128, KC, 128])
    cp("dve", crep, cact.unsqueeze(2).broadcast_to([128, KC, 128]), r=["cact"], w=["crep"])
    abufs = ada_alloc()
    for g in range(4):
        ada_group(g, abufs[g % 2], 7 - 2 * (g % 2))
        ada_to_cols(abufs[g % 2], sh_a if g < 2 else sc_a, (g % 2) * 4, 6 - 2 * (g % 2))
    tsc("dve", gsc_a, sc_a, 1.0, None, ALU.add, None, r=["adacols"], w=["gsc_a"])
    tt("dve", gsc_a, gsc_a, gmixT, ALU.mult, r=["gsc_a", "gmixT"], w=["gsc_a"])

    S.barrier(small[:, 62:63])
    A.reset(WORK0)
    WORK = A.mark()
    NXT = 3
    xt = [A.alloc([128, D]) for _ in range(NXT)]
    _cur = A.mark()
    A.reset(ATTN_OFF)
    xnb = [A.alloc([128, D], BF16) for _ in range(2)]
    hT = [A.alloc([128, KC, 128], BF16) for _ in range(2)]
    shrep = A.alloc([128, KC, 128])
    assert A.mark() <= ATTN_OFF + 4096
    A.reset(_cur)
    wkv = A.alloc([128, KC, 1024], BF16)
    shW_bf = A.alloc([128, 1024], BF16)
    c128 = A.alloc([128, 128], BF16)
    junkb = junk_sb
    ssA = [A.alloc([128, 1]) for _ in range(2)]
    rsA = [A.alloc([128, 1]) for _ in range(2)]
    ksq = [A.alloc([128, 512]) for _ in range(2)]
    kn = [A.alloc([128, 8, 64]) for _ in range(2)]
    kb16 = [A.alloc([128, 512], BF16) for _ in range(2)]
    ssk = [A.alloc([128, 8]) for _ in range(2)]
    rk = [A.alloc([128, 8]) for _ in range(2)]
    rt = [[A.alloc([128, 8, 8]) for _ in range(4)] for _ in range(2)]

    S.add("pool", lambda e: e.memset(VA[:, :, :, 128:130], 1.0), r=[], w=["VAones"])
    S.add("pool", lambda e: e.memset(c128, 1.0 / 128.0), r=[], w=["c128"])
    cp("dve", shrep, sh_a.unsqueeze(2).broadcast_to([128, KC, 128]), r=["adacols"], w=["shrep"])

    w_in_v = w_in.rearrange("(k p) n -> p k n", p=128)
    for pc in range(8):
        xi = pc % NXT
        st = xt[xi][:, :].rearrange("p (k n) -> p k n", k=KC)
        dma(st, w_in_v[:, :, 512 + pc * 128: 512 + (pc + 1) * 128], r=[], w=["xt%d" % xi], tag="xt%d" % xi)
        tt("pool", wkv[:, :, pc * 128:(pc + 1) * 128], st, gsc_a.unsqueeze(2).broadcast_to([128, KC, 128]), ALU.mult,
           r=["xt%d" % xi, "gsc_a"], w=["wkv"])
        bk = pc // 4
        mms([(ps[bk][:, (pc % 4) * 128:(pc % 4 + 1) * 128], shrep[:, k, :], st[:, k, :], k == 0, k == KC - 1)
             for k in range(KC)], r=["xt%d" % xi, "shrep"], w=["ps%d" % bk])
    cp("dve", shW_bf[:, 0:512], ps[0][:, :], r=["ps0"], w=["shW"])
    cp("dve", shW_bf[:, 512:1024], ps[1][:, :], r=["ps1"], w=["shW"])

    def qk_chain(pkey, psrc, gain_rep, cos_ap, sin_ap, sidx, evac_fn, tbank):
        i = sidx
        p3 = psrc.rearrange("p (g d) -> p g d", d=64)
        act(ksq[i], psrc, AF.Square, r=[pkey], w=["ksq"])
        S.add("dve", lambda e, o=ssk[i], a=ksq[i][:, :].rearrange("p (g d) -> p g d", d=64): e.tensor_reduce(o, a, AX.X, ALU.add),
              r=["ksq"], w=["ssk%d" % i])
        rstd_from_ss(ssk[i], rk[i], 1.0 / 64.0, "ssk%d" % i, "rk%d" % i)
        tt("dve", kn[i], p3, rk[i].unsqueeze(2).broadcast_to([128, 8, 64]), ALU.mult,
           r=[pkey, "rk%d" % i], w=["kn%d" % i])
        tt("dve", kn[i], kn[i], gain_rep.unsqueeze(1).broadcast_to([128, 8, 64]), ALU.mult,
           r=["kn%d" % i, "gains"], w=["kn%d" % i])
        k3 = kb16[i][:, :].rearrange("p (g d) -> p g d", d=64)
        cp("pool", k3, kn[i], r=["kn%d" % i], w=["kb%d" % i])
        a = kn[i][:, :, 0:8]
        b = kn[i][:, :, 8:16]
        cb = cos_ap.unsqueeze(1).broadcast_to([128, 8, 8])
        sb_ = sin_ap.unsqueeze(1).broadcast_to([128, 8, 8])
        t1, t2, t3, t4 = rt[i]
        kk = "rt%d" % i
        tt("pool", t1, a, cb, ALU.mult, r=["kn%d" % i, "ropetab"], w=[kk + "a"])
        tt("pool", t2, b, sb_, ALU.mult, r=["kn%d" % i, "ropetab"], w=[kk + "b"])
        tt("pool", k3[:, :, 0:8], t1, t2, ALU.subtract, r=[kk + "a", kk + "b"], w=["kb%d" % i])
        tt("pool", t3, b, cb, ALU.mult, r=["kn%d" % i, "ropetab"], w=[kk + "c"])
        tt("pool", t4, a, sb_, ALU.mult, r=["kn%d" % i, "ropetab"], w=[kk + "d"])
        tt("pool", k3[:, :, 8:16], t3, t4, ALU.add, r=[kk + "c", kk + "d"], w=["kb%d" % i])
        pT = psb(tbank)
        tps([(pT[:, h * 128:(h + 1) * 128], kb16[i][:, h * 128:(h + 1) * 128]) for h in range(4)],
            ident_b, r=["kb%d" % i, "ident_b"], w=["ps%d" % tbank])
        evac_fn(pT[:, 0:512])

    S.add("pool", lambda e: e.memset(small[:, 60:61], 0.0), r=rb + ro, w=["ropetab"])
    S.add("pool", lambda e: e.memset(small[:, 61:62], 0.0), r=["gq_rep", "gk_rep"], w=["gains"])

    def norm_transpose(xsrc, xk, ssv, rsv, sskey, hdst, hkeys, gsc, sh, gkeys, xout=None, xoutk=None, junk=None, junkk="ps7"):
        act(junkb, xsrc, AF.Square, r=[xk], w=["junk_sb", sskey], accum=ssv)
        rstd_from_ss(ssv, rsv, 1.0 / D, sskey, sskey + "r")
        if xout is None:
            xout, xoutk = xsrc, xk
            tsc("dve", xout, xsrc, rsv, None, ALU.mult, None, r=[xk, sskey + "r"], w=[xk])
        else:
            tsc("dve", xout, xsrc, rsv, None, ALU.mult, None, r=[xk, sskey + "r"], w=[xoutk])
        for half in range(2):
            tps([(ps[half][:, kk * 128:(kk + 1) * 128], xout[:, (half * 4 + kk) * 128:(half * 4 + kk + 1) * 128])
                 for kk in range(4)], ident_f, r=[xoutk, "ident_f"], w=["ps%d" % half])
            for kk in range(4):
                k = half * 4 + kk
                src = ps[half][:, kk * 128:(kk + 1) * 128]
                if True:
                    act(hdst[:, k, :], src, AF.Identity, r=["ps%d" % half] + gkeys, w=[hkeys[k]],
                        bias=sh[:, k:k + 1], scale=gsc[:, k:k + 1])
                else:
                    tsc("dve", hdst[:, k, :], src, gsc[:, k:k + 1], sh[:, k:k + 1], ALU.mult, ALU.add,
                        r=["ps%d" % half] + gkeys, w=[hkeys[k]])

    NT_A = NBLK_B
    if stop_after == "pro":
        NT_A = 0
    if stop_after == "A4":
        NT_A = 4
    if stop_after == "C1":
        NT_A = 8
    if mini:
        NT_A = 16 * mini_ns
    import os
    PST = (0, 1)
    PSK = (2, 3, 4)
    PSV = (5, 6)
    PST2 = 7

    def a_d0(b):
        xi = b % NXT
        dma(xt[xi], xb[b * 128:(b + 1) * 128, :], r=[], w=["xt%d" % xi], tag="xt%d" % xi)

    def a_a12(b):
        xi, i2 = b % NXT, b % 2
        act(junkb, xt[xi], AF.Square, r=["xt%d" % xi], w=["junk_sb", "ssA%d" % i2], accum=ssA[i2])
        rstd_from_ss(ssA[i2], rsA[i2], 1.0 / D, "ssA%d" % i2, "rsA%d" % i2)

    def a_v1(b):
        xi, i2 = b % NXT, b % 2
        act(xnb[i2], xt[xi], AF.Identity, r=["xt%d" % xi, "rsA%d" % i2], w=["xnb%d" % i2], scale=rsA[i2])

    def a_p1(b):
        i2 = b % 2
        pT = psb(PST[i2])
        tps([(pT[:, k * 128:(k + 1) * 128], xnb[i2][:, k * 128:(k + 1) * 128]) for k in range(KC)],
            ident_b, r=["xnb%d" % i2, "ident_b"], w=["ps%d" % PST[i2]])

    def a_ev(b):
        i2 = b % 2
        pT = psb(PST[i2])
        for half in range(2):
            cp("dve", hT[i2][:, half * 4:(half + 1) * 4, :],
               pT[:, half * 512:(half + 1) * 512].rearrange("p (k q) -> p k q", q=128),
               r=["ps%d" % PST[i2]], w=["hT%d_%d" % (i2, half)])

    def a_p2(b):
        i2 = b % 2
        pK = PSK[b % 3]
        pV = PSV[i2]
        hks = ["hT%d_0" % i2, "hT%d_1" % i2]
        mms([(ps[pK][:, :], hT[i2][:, k, :], wkv[:, k, 0:512], k == 0, False) for k in range(KC)] +
            [(ps[pK][:, :], c128, shW_bf[:, 0:512], False, True)],
            r=hks + ["wkv", "shW", "c128"], w=["ps%d" % pK])
        mms([(ps[pV][:, :], hT[i2][:, k, :], wkv[:, k, 512:1024], k == 0, False) for k in range(KC)] +
            [(ps[pV][:, :], c128, shW_bf[:, 512:1024], False, True)],
            r=hks + ["wkv", "shW", "c128"], w=["ps%d" % pV])

    def a_a45(b):
        i2 = b % 2
        pK = PSK[b % 3]
        pV = PSV[i2]
        act(ksq[i2], ps[pK][:, :], AF.Square, r=["ps%d" % pK], w=["ksq%d" % i2])
        act(VA[:, b, :, 0:128], ps[pV][:, :].rearrange("p (h e) -> p h e", e=128), AF.Copy,
            r=["ps%d" % pV, "VAones"], w=[("V", b)])

    def a_v3(b):
        i2 = b % 2
        S.add("dve", lambda e, o=ssk[i2], a=ksq[i2][:, :].rearrange("p (g d) -> p g d", d=64): e.tensor_reduce(o, a, AX.X, ALU.add),
              r=["ksq%d" % i2], w=["ssk%d" % i2])

    def a_a6(b):
        i2 = b % 2
        rstd_from_ss(ssk[i2], rk[i2], 1.0 / 64.0, "ssk%d" % i2, "rk%d" % i2)

    def a_v4(b):
        i2 = b % 2
        pK = PSK[b % 3]
        p3 = ps[pK][:, :].rearrange("p (g d) -> p g d", d=64)
        tt("dve", kn[i2], p3, rk[i2].unsqueeze(2).broadcast_to([128, 8, 64]), ALU.mult,
           r=["ps%d" % pK, "rk%d" % i2], w=["kn%d" % i2])
        tt("dve", kn[i2], kn[i2], gk_rep.unsqueeze(1).broadcast_to([128, 8, 64]), ALU.mult,
           r=["kn%d" % i2, "gains"], w=["kn%d" % i2])

    def a_g1(b):
        i = b % 2
        k3 = kb16[i][:, :].rearrange("p (g d) -> p g d", d=64)
        cp("pool", k3, kn[i], r=["kn%d" % i], w=["kb%d" % i])
        a = kn[i][:, :, 0:8]
        b_ = kn[i][:, :, 8:16]
        cb = cos_b[:, b, :].unsqueeze(1).broadcast_to([128, 8, 8])
        sb_ = sin_b[:, b, :].unsqueeze(1).broadcast_to([128, 8, 8])
        t1, t2, t3, t4 = rt[i]
        kk = "rt%d" % i
        tt("pool", t1, a, cb, ALU.mult, r=["kn%d" % i, "ropetab"], w=[kk + "a"])
        tt("pool", t2, b_, sb_, ALU.mult, r=["kn%d" % i, "ropetab"], w=[kk + "b"])
        tt("pool", k3[:, :, 0:8], t1, t2, ALU.subtract, r=[kk + "a", kk + "b"], w=["kb%d" % i])
        tt("pool", t3, b_, cb, ALU.mult, r=["kn%d" % i, "ropetab"], w=[kk + "c"])
        tt("pool", t4, a, sb_, ALU.mult, r=["kn%d" % i, "ropetab"], w=[kk + "d"])
        tt("pool", k3[:, :, 8:16], t3, t4, ALU.add, r=[kk + "c", kk + "d"], w=["kb%d" % i])

    def a_p3(b):
        i = b % 2
        pT = psb(PST2)
        tps([(pT[:, h * 128:(h + 1) * 128], kb16[i][:, h * 128:(h + 1) * 128]) for h in range(4)],
            ident_b, r=["kb%d" % i, "ident_b"], w=["ps%d" % PST2])

    def a_v5(b):
        pT = psb(PST2)
        cp("dve", KT[:, :, b * 128:(b + 1) * 128], pT[:, 0:512].rearrange("p (h q) -> p h q", q=128),
           r=["ps%d" % PST2], w=[("KT", b)])

    a_order = [(a_v5, 11), (a_p3, 10), (a_g1, 9), (a_a6, 8), (a_a45, 7), (a_v4, 8), (a_p2, 6), (a_ev, 5),
               (a_p1, 4), (a_v1, 3), (a_a12, 2), (a_v3, 7), (a_d0, 0)]
    for t in range(NT_A + 12):
        for fn_, off_ in a_order:
            b = t - off_
            if 0 <= b < NT_A:
                fn_(b)


    if stop_after in ("pro", "A", "A4"):
        if os.environ.get("MK_CUT"):
            cut = int(os.environ["MK_CUT"])
            S.ops = S.ops[:cut]
            S.last_w = {k: v for k, v in S.last_w.items() if v < cut}
            S.readers = {k: {e: i for e, i in d_.items() if i < cut} for k, d_ in S.readers.items()}
            S.tag_count = {}
            for op in S.ops:
                if op["tag"] is not None:
                    S.tag_count[op["tag"]] = S.tag_count.get(op["tag"], 0) + 1
            stop_after = "pro"
        dbg["KT"] = nc.dram_tensor("dbg_KT", [128, 4 * SEQ], BF16, kind="ExternalOutput").ap()
        dbg["VA"] = nc.dram_tensor("dbg_VA", [128, NBLK_B * 4 * 130], BF16, kind="ExternalOutput").ap()
        dbg["misc"] = nc.dram_tensor("dbg_misc", [128, 64], F32, kind="ExternalOutput").ap()
        allk = [("KT", b) for b in range(NT_A)] + [("V", b) for b in range(NT_A)]
        if stop_after in ("A", "A4"):
            S.add("dve", lambda e: e.memset(KT[:, :, NT_A * 128:], 0.0), r=[], w=["ktpad"])
            S.add("dve", lambda e: e.memset(VA[:, NT_A:, :, 0:128], 0.0), r=[], w=["vapad"])
            allk = allk + ["ktpad", "vapad"]
            dma(dbg["KT"], KT[:, :, :].rearrange("p a b -> p (a b)"), r=allk, w=[], tag="out0")
            dma(dbg["VA"], VA[:, :, :, :].rearrange("p a b c -> p (a b c)"), r=allk + ["VAones"], w=[], tag="out1")
        cp("dve", small[:, 0:8], sh_a, r=["adacols", "junk64", "neglam"], w=["small"])
        cp("dve", small[:, 8:16], gsc_a, r=["gsc_a"], w=["small"])
        cp("dve", small[:, 16:24], cos_b[:, 0, :], r=rb, w=["small"])
        cp("dve", small[:, 24:32], sin_b[:, 63, :], r=rb, w=["small"])
        cp("dve", small[:, 32:40], lam_t, r=["neglam"], w=["small"])
        dma(dbg["misc"][:, 0:40], small[:, 0:40], r=["small"], w=[], tag="out2")
        S.emit()
        return nc

    bar_tile = small[:, 62:63]
    S.barrier(bar_tile)
    A.reset(WORK)
    xq = [A.alloc([128, D]) for _ in range(2)]
    hTq = [A.alloc([128, KC, 128], BF16) for _ in range(2)]
    wq = A.alloc([128, KC, 512], BF16)
    Qbd = [A.alloc([128, 4, 2, 128], BF16) for _ in range(2)]
    NPT = 4
    PT = [A.alloc([128, 512], BF16) for _ in range(NPT)]
    ssQ = [A.alloc([128, 1]) for _ in range(2)]
    rsQ = [A.alloc([128, 1]) for _ in range(2)]
    ksq = [A.alloc([128, 512])] * 2
    kn = [A.alloc([128, 8, 64]) for _ in range(2)]
    kb16 = [A.alloc([128, 512], BF16) for _ in range(2)]
    ssk = [A.alloc([128, 8]) for _ in range(2)]
    rk = [A.alloc([128, 8]) for _ in range(2)]
    rt = [[A.alloc([128, 8, 8]) for _ in range(4)] for _ in range(2)]
    ot = [A.alloc([128, 128]) for _ in range(2)]
    ot2 = [A.alloc([128, 128]) for _ in range(2)]
    ob16 = [A.alloc([128, 128], BF16) for _ in range(2)]
    eps_ = [A.alloc([128, 8]) for _ in range(2)]
    pass

    for bq in range(2):
        S.add("pool", lambda e, q=Qbd[bq]: e.memset(q, 0.0), r=[], w=["Qbd%d" % bq])
    for pc in range(4):
        xi = pc % 2
        st = xq[xi][:, :].rearrange("p (k n) -> p k n", k=KC)
        dma(st, w_in_v[:, :, pc * 128:(pc + 1) * 128], r=[], w=["xq%d" % xi], tag="xq%d" % xi)
        cp("pool", wq[:, :, pc * 128:(pc + 1) * 128], st, r=["xq%d" % xi], w=["wq"])

    def q_stage(m, stage):
        bq = m % 2
        xk = "xq%d" % bq
        hks = ["hTq%d_%d" % (bq, k) for k in range(KC)]
        if stage == 0:
            dma(xq[bq], xo[m * 128:(m + 1) * 128, :], r=[], w=[xk], tag=xk)
            act(junkb, xq[bq], AF.Square, r=[xk], w=["junk_sb", "ssQ%d" % bq], accum=ssQ[bq])
            rstd_from_ss(ssQ[bq], rsQ[bq], 1.0 / D, "ssQ%d" % bq, "ssQ%dr" % bq)
            tsc("dve", xq[bq], xq[bq], rsQ[bq], None, ALU.mult, None, r=[xk, "ssQ%dr" % bq], w=[xk])
        elif stage == 1:
            for half in range(2):
                tps([(ps[half][:, kk * 128:(kk + 1) * 128], xq[bq][:, (half * 4 + kk) * 128:(half * 4 + kk + 1) * 128])
                     for kk in range(4)], ident_f, r=[xk, "ident_f"], w=["ps%d" % half])
            for half in range(2):
                for kk in range(4):
                    k = half * 4 + kk
                    src_ = ps[half][:, kk * 128:(kk + 1) * 128]
                    act(hTq[bq][:, k, :], src_, AF.Identity, r=["ps%d" % half, "gsc_a", "adacols"], w=[hks[k]],
                        bias=sh_a[:, k:k + 1], scale=gsc_a[:, k:k + 1])
        elif stage == 2:
            mms([(ps[0][:, :], hTq[bq][:, k, :], wq[:, k, :], k == 0, k == KC - 1) for k in range(KC)],
                r=hks + ["wq"], w=["ps0"])
            qk_front("ps0", ps[0][:, :], gq_rep, cos_o[:, m, :], sin_o[:, m, :], bq)
        else:
            pT = psb(1)
            tps([(pT[:, h * 128:(h + 1) * 128], kb16[bq][:, h * 128:(h + 1) * 128]) for h in range(4)],
                ident_b, r=["kb%d" % bq, "ident_b"], w=["ps1"])
            p3 = pT[:, 0:512].rearrange("p (h q) -> p h q", q=128)
            cp("dve", Qbd[bq][0:64, :, 0, :], p3[0:64], r=["ps1"], w=["Qbd%d" % bq])
            cp("dve", Qbd[bq][64:128, :, 1, :], p3[64:128], r=["ps1"], w=["Qbd%d" % bq])

    def qk_front(pkey, psrc, gain_rep, cos_ap, sin_ap, i):
        p3 = psrc.rearrange("p (g d) -> p g d", d=64)
        act(ksq[i], psrc, AF.Square, r=[pkey], w=["ksq"])
        S.add("dve", lambda e, o=ssk[i], a=ksq[i][:, :].rearrange("p (g d) -> p g d", d=64): e.tensor_reduce(o, a, AX.X, ALU.add),
              r=["ksq"], w=["ssk%d" % i])
        rstd_from_ss(ssk[i], rk[i], 1.0 / 64.0, "ssk%d" % i, "rk%d" % i)
        tt("dve", kn[i], p3, rk[i].unsqueeze(2).broadcast_to([128, 8, 64]), ALU.mult,
           r=[pkey, "rk%d" % i], w=["kn%d" % i])
        tt("dve", kn[i], kn[i], gain_rep.unsqueeze(1).broadcast_to([128, 8, 64]), ALU.mult,
           r=["kn%d" % i, "gains"], w=["kn%d" % i])
        k3 = kb16[i][:, :].rearrange("p (g d) -> p g d", d=64)
        cp("pool", k3, kn[i], r=["kn%d" % i], w=["kb%d" % i])
        a = kn[i][:, :, 0:8]
        b = kn[i][:, :, 8:16]
        cb = cos_ap.unsqueeze(1).broadcast_to([128, 8, 8])
        sb_ = sin_ap.unsqueeze(1).broadcast_to([128, 8, 8])
        t1, t2, t3, t4 = rt[i]
        kk = "rt%d" % i
        tt("pool", t1, a, cb, ALU.mult, r=["kn%d" % i, "ropetab"], w=[kk + "a"])
        tt("pool", t2, b, sb_, ALU.mult, r=["kn%d" % i, "ropetab"], w=[kk + "b"])
        tt("pool", k3[:, :, 0:8], t1, t2, ALU.subtract, r=[kk + "a", kk + "b"], w=["kb%d" % i])
        tt("pool", t3, b, cb, ALU.mult, r=["kn%d" % i, "ropetab"], w=[kk + "c"])
        tt("pool", t4, a, sb_, ALU.mult, r=["kn%d" % i, "ropetab"], w=[kk + "d"])
        tt("pool", k3[:, :, 8:16], t3, t4, ALU.add, r=[kk + "c", kk + "d"], w=["kb%d" % i])

    cm3 = cmask[:, :].rearrange("p (t x) -> p t x", t=4)
    NM = NOWN if stop_after != "C1" else 2
    if mini:
        NM = 4 * mini_ns
    items = []
    for m in range(NM):
        for h in range(4):
            ngrp = (4 * m + 4) // 2
            for g in range(ngrp):
                items.append((m, h, g, ngrp))
    SBK = (2, 3, 4)
    pending = []
    LOOK = 2

    def emit_S(idx):
        m, h, g, ngrp = items[idx]
        bq = m % 2
        sbk = SBK[idx % 3]
        kbs = (2 * g, 2 * g + 1)
        mms([(ps[sbk][:, i * 256:(i + 1) * 256], KT[:, h, kb * 128:(kb + 1) * 128],
              Qbd[bq][:, h, :, :], True, True) for i, kb in enumerate(kbs)],
            r=[("KT", kbs[0]), ("KT", kbs[1]), "Qbd%d" % bq], w=["ps%d" % sbk])

    def emit_rest(idx):
        m, h, g, ngrp = items[idx]
        sbk = SBK[idx % 3]
        pti = idx % NPT
        ep = (m * 4 + h) % 2
        kbs = (2 * g, 2 * g + 1)
        act(PT[pti], ps[sbk][:, :], AF.Exp, r=["ps%d" % sbk, "m8c"], w=["PT%d" % pti],
            bias=m8c[:, 0:1], scale=0.125)
        if g >= ngrp - 2:
            gb = g - (ngrp - 2)
            tt("pool", PT[pti], PT[pti], cm3[:, 2 * gb:2 * gb + 2, :], ALU.mult,
               r=["PT%d" % pti, "cmask"], w=["PT%d" % pti])
        its = []
        for i, kb in enumerate(kbs):
            for c in range(2):
                its.append((ps[5 + c][:, 0:129], PT[pti][:, i * 256 + c * 128:i * 256 + (c + 1) * 128],
                            VA[:, kb, h, 0:129], (g == 0 and i == 0), (g == ngrp - 1 and i == 1)))
        mms(its, r=["PT%d" % pti, ("V", kbs[0]), ("V", kbs[1])], w=["ps5", "ps6"])
        if g == ngrp - 1:
            epilogue_a(m, h, ep)
            pending.append((idx + 3, lambda m=m, h=h, ep=ep: epilogue_b(m, h, ep)))

    def epilogue_a(m, h, ep):
        e_ = eps_[ep]
        ek = "eps%d" % ep
        act(e_[:, 5:6], ps[5][:, 128:129], AF.Copy, r=["ps5"], w=[ek])
        act(e_[:, 6:7], ps[6][:, 128:129], AF.Copy, r=["ps6"], w=[ek])
        S.add("dve", lambda e, e_=e_: e.reciprocal(e_[:, 0:2], e_[:, 5:7]), r=[ek], w=[ek])
        tt("dve", e_[:, 2:3], e_[:, 1:2], neglam, ALU.mult, r=[ek, "neglam"], w=[ek])
        act(ot[ep], ps[5][:, 0:128], AF.Identity, r=["ps5", ek], w=["ot%d" % ep], scale=e_[:, 0:1])
        act(ot2[ep], ps[6][:, 0:128], AF.Identity, r=["ps6", ek], w=["ot2%d" % ep], scale=e_[:, 2:3])
        tt("dve", ot[ep], ot[ep], ot2[ep], ALU.add, r=["ot%d" % ep, "ot2%d" % ep], w=["ot%d" % ep])
        act(ot2[ep], ot[ep], AF.Square, r=["ot%d" % ep, "ot2%d" % ep], w=["ot2%d" % ep, ek + "s"], accum=e_[:, 3:4])
        rstd_from_ss(e_[:, 3:4], e_[:, 4:5], 1.0 / 128.0, ek + "s", ek + "r")
        act(ot2[ep], ot[ep], AF.Identity, r=["ot%d" % ep, ek + "r", "ot2%d" % ep], w=["ot2%d" % ep], scale=e_[:, 4:5])
        tt("dve", ob16[ep], ot2[ep], sg_rep, ALU.mult, r=["ot2%d" % ep, "sg_rep"], w=["ob%d" % ep])

    def epilogue_b(m, h, ep):
        tps([(psb(7)[:, ep * 128:(ep + 1) * 128], ob16[ep])], ident_b, r=["ob%d" % ep, "ident_b"], w=["ps7"])
        cp("dve", attnT[:, h, m * 128:(m + 1) * 128], psb(7)[:, ep * 128:(ep + 1) * 128], r=["ps7"], w=[("attnT", m, h)])

    for st_ in range(4):
        q_stage(0, st_)
    for i in range(min(LOOK, len(items))):
        emit_S(i)
    for idx in range(len(items)):
        m, h, g, ngrp = items[idx]
        if g == 0 and m + 1 < NM:
            q_stage(m + 1, h)
        if idx + LOOK < len(items):
            emit_S(idx + LOOK)
        while pending and pending[0][0] <= idx:
            pending.pop(0)[1]()
        emit_rest(idx)
    while pending:
        pending.pop(0)[1]()

    if stop_after in ("C", "C1"):
        dbg["attnT"] = nc.dram_tensor("dbg_attnT", [128, 4 * NOWN * 128], BF16, kind="ExternalOutput").ap()
        if NM < NOWN:
            S.add("dve", lambda e: e.memset(attnT[:, :, NM * 128:], 0.0), r=[], w=["attnpad"])
        allk = [("attnT", m, h) for m in range(NM) for h in range(4)] + ["attnpad"]
        dma(dbg["attnT"], attnT[:, :, :].rearrange("p a b -> p (a b)"), r=allk, w=[], tag="out0")
        S.emit()
        return nc

    S.barrier(bar_tile)
    A.reset(KV0)
    X = A.alloc([128, NOWN, D])
    wglu = A.alloc([128, KC, 1024], BF16)
    woutp = A.alloc([128, KC, 1024], BF16)
    D1W = A.mark()
    crep = A.alloc([128, KC, 128])
    abufs = ada_alloc()
    gta_rep = A.alloc([128, D])
    stg = [A.alloc([128, KC, 128]) for _ in range(2)]
    for s4 in range(4):
        S.bulk_tags.add("Xload%d" % s4)
        for i in range(4):
            m = 4 * s4 + i
            dma(X[:, m, :], xo[m * 128:(m + 1) * 128, :], r=[], w=[("X", m)], tag="Xload%d" % s4)
    cp("dve", crep, cact.unsqueeze(2).broadcast_to([128, KC, 128]), r=["cact"], w=["crep"])
    for g in (4, 5):
        ada_group(g, abufs[g % 2], 7 - 2 * (g % 2))
        cp("dve", gta_rep[:, (g - 4) * 512:(g - 3) * 512], abufs[g % 2][2], r=["ada_modrep%d" % (g % 2)], w=["gta_rep"])
    for pc in range(8):
        xi = pc % 2
        dma(stg[xi], w_in_v[:, :, 1536 + pc * 128:1536 + (pc + 1) * 128], r=[], w=["stg%d" % xi], tag="stg%d" % xi)
        if pc % 2 == 0:
            act(wglu[:, :, pc * 128:(pc + 1) * 128], stg[xi], AF.Copy, r=["stg%d" % xi], w=["wglu"])
        else:
            cp("pool", wglu[:, :, pc * 128:(pc + 1) * 128], stg[xi], r=["stg%d" % xi], w=["wglu"])
    w_out_v = w_out.rearrange("(k p) n -> p k n", p=128)
    for pc in range(8):
        xi = pc % 2
        dma(stg[xi], w_out_v[:, :, pc * 128:(pc + 1) * 128], r=[], w=["stg%d" % xi], tag="stg%d" % xi)
        tt("dve" if pc % 2 == 0 else "pool", woutp[:, :, pc * 128:(pc + 1) * 128], stg[xi],
           gta_rep[:, pc * 128:(pc + 1) * 128].unsqueeze(1).broadcast_to([128, KC, 128]), ALU.mult,
           r=["stg%d" % xi, "gta_rep"], w=["woutp"])
    S.barrier(bar_tile)
    A.reset(D1W)
    xhb = A.alloc([128, D])
    xn = [A.alloc([128, D]) for _ in range(2)]
    hT5 = A.alloc([128, KC, 640], BF16)
    uT = A.alloc([128, 4, 4, 160], BF16)
    sig = [A.alloc([128, 512]) for _ in range(2)]
    sigh = [A.alloc([128, 128]) for _ in range(2)]
    asb = [A.alloc([128, 512]) for _ in range(2)]
    asbh = [A.alloc([128, 128]) for _ in range(2)]
    yv = A.alloc([128, 4, 512])
    ysq = A.alloc([128, 4, 512])
    mean_sb = A.alloc([128, 512])
    m2 = A.alloc([128, 512])
    rstd_bc = A.alloc([128, 512])
    tmpv = [A.alloc([128, 512]) for _ in range(2)]
    convT = A.alloc([128, 4, 512], BF16)
    dg = [A.alloc([128, 128], BF16) for _ in range(8)]
    ssD = [A.alloc([128, 1]) for _ in range(2)]
    rsD = [A.alloc([128, 1]) for _ in range(2)]
    pass
    dgc = {"n": 0}

    NS = 4 if not mini else mini_ns
    hT5b = [hT5, A.alloc([128, KC, 640], BF16)]

    def d1_front(s4, i):
        bi = i % 2
        if i == 4:
            dma(xhb, xh[s4 * 128:(s4 + 1) * 128, :], r=[], w=["xhb"], tag="xhb")
        src, sk = (X[:, 4 * s4 + i, :], ("X", 4 * s4 + i)) if i < 4 else (xhb, "xhb")
        act(junkb, src, AF.Square, r=[sk], w=["junk_sb", "ssD%d" % bi], accum=ssD[bi])
        rstd_from_ss(ssD[bi], rsD[bi], 1.0 / D, "ssD%d" % bi, "ssD%dr" % bi)
        tsc("dve", xn[bi], src, rsD[bi], None, ALU.mult, None, r=[sk, "ssD%dr" % bi], w=["xn%d" % bi])

    def d1_back(s4, i):
        bi = i % 2
        hd = hT5b[s4 % 2][:, :, i * 128:(i + 1) * 128]
        for half in range(2):
            tps([(ps[half][:, kk * 128:(kk + 1) * 128], xn[bi][:, (half * 4 + kk) * 128:(half * 4 + kk + 1) * 128])
                 for kk in range(4)], ident_f, r=["xn%d" % bi, "ident_f"], w=["ps%d" % half])
            for kk in range(4):
                k = half * 4 + kk
                act(hd[:, k, :], ps[half][:, kk * 128:(kk + 1) * 128], AF.Identity,
                    r=["ps%d" % half, "gsc_a", "adacols"], w=["hT5_%d_%d_%d" % (s4 % 2, i, k)],
                    bias=sh_a[:, k:k + 1], scale=gsc_a[:, k:k + 1])

    for i in range(5):
        d1_front(0, i)
        d1_back(0, i)
    def d1_glu(s4):
        hT5 = hT5b[s4 % 2]
        nxt = s4 + 1 < NS
        allh = ["hT5_%d_%d_%d" % (s4 % 2, i, k) for i in range(5) for k in range(KC)]
        for cc in range(4):
            pb = cc % 2
            ba_, bg_ = bgluT[:, cc:cc + 1], bgluT[:, 4 + cc:5 + cc]
            pa, pg, ph = 2 + 3 * pb, 3 + 3 * pb, 4 + 3 * pb
            mms([(ps[pa][:, :], wglu[:, k, cc * 128:(cc + 1) * 128], hT5[:, k, 0:512], k == 0, k == KC - 1)
                 for k in range(KC)], r=allh + ["wglu"], w=["ps%d" % pa])
            mms([(ps[pg][:, :], wglu[:, k, 512 + cc * 128:512 + (cc + 1) * 128], hT5[:, k, 0:512], k == 0, k == KC - 1)
                 for k in range(KC)], r=allh + ["wglu"], w=["ps%d" % pg])
            mms([(ps[ph][:, 0:128], wglu[:, k, cc * 128:(cc + 1) * 128], hT5[:, k, 512:640], k == 0, k == KC - 1)
                 for k in range(KC)] +
                [(ps[ph][:, 128:256], wglu[:, k, 512 + cc * 128:512 + (cc + 1) * 128], hT5[:, k, 512:640], k == 0, k == KC - 1)
                 for k in range(KC)], r=allh + ["wglu"], w=["ps%d" % ph])
            act(sig[pb], ps[pg][:, :], AF.Sigmoid, r=["ps%d" % pg, "bgluT"], w=["sig%d" % pb], bias=bg_)
            act(sigh[pb], ps[ph][:, 128:256], AF.Sigmoid, r=["ps%d" % ph, "bgluT"], w=["sigh%d" % pb], bias=bg_)
            act(asb[pb], ps[pa][:, :], AF.Identity, r=["ps%d" % pa, "bgluT"], w=["asb%d" % pb], bias=ba_)
            act(asbh[pb], ps[ph][:, 0:128], AF.Identity, r=["ps%d" % ph, "bgluT"], w=["asbh%d" % pb], bias=ba_)
            tt("dve", uT[:, cc, :, 32:160], asb[pb][:, :].rearrange("p (i t) -> p i t", i=4),
               sig[pb][:, :].rearrange("p (i t) -> p i t", i=4), ALU.mult,
               r=["asb%d" % pb, "sig%d" % pb], w=["uT%d" % cc])
            tt("dve", uT[:, cc, :, 0:32], asbh[pb][:, :].rearrange("p (i t) -> p i t", i=4),
               sigh[pb][:, :].rearrange("p (i t) -> p i t", i=4), ALU.mult,
               r=["asbh%d" % pb, "sigh%d" % pb], w=["uT%d" % cc])
            if s4 == 0:
                tsc("dve", uT[:, cc, 0, 0:32], uT[:, cc, 0, 0:32], hmask[:, 0:1], None, ALU.mult, None,
                    r=["uT%d" % cc, "hmask"], w=["uT%d" % cc])

    def d1_conv(s4):
        hT5 = hT5b[s4 % 2]
        nxt = s4 + 1 < NS
        allh = ["hT5_%d_%d_%d" % (s4 % 2, i, k) for i in range(5) for k in range(KC)]
        if nxt:
            d1_front(s4 + 1, 0)
        for cc in range(4):
            cb_ = 2 + 3 * (cc % 2)
            for k in range(31):
                sl = dgc["n"] % 8
                dgc["n"] += 1
                tsc("dve", dg[sl], ident_b, wdwT[:, cc, k:k + 1], None, ALU.mult, None,
                    r=["ident_b", "wdwT"], w=["dg%d" % sl])
                mms([(ps[cb_][:, :], dg[sl], uT[:, cc, :, 2 + k:130 + k], k == 0, k == 30)],
                    r=["dg%d" % sl, "uT%d" % cc], w=["ps%d" % cb_])
            act(yv[:, cc, :], ps[cb_][:, :], AF.Identity, r=["ps%d" % cb_, "bdwT"], w=["yv%d" % cc], bias=bdwT[:, cc:cc + 1])
            act(ysq[:, cc, :], yv[:, cc, :], AF.Square, r=["yv%d" % cc], w=["ysq%d" % cc])
            if nxt:
                d1_back(s4 + 1, cc)
                d1_front(s4 + 1, cc + 1)

    def d1_ln(s4):
        hT5 = hT5b[s4 % 2]
        nxt = s4 + 1 < NS
        allh = ["hT5_%d_%d_%d" % (s4 % 2, i, k) for i in range(5) for k in range(KC)]
        mms([(ps[3][:, :], ones_ln, yv[:, cc, :], cc == 0, cc == 3) for cc in range(4)],
            r=["yv%d" % cc for cc in range(4)] + ["ones_ln"], w=["ps3"])
        mms([(ps[4][:, :], ones_ln, ysq[:, cc, :], cc == 0, cc == 3) for cc in range(4)],
            r=["ysq%d" % cc for cc in range(4)] + ["ones_ln"], w=["ps4"])
        if nxt:
            d1_back(s4 + 1, 4)
        act(mean_sb, ps[3][:, :], AF.Copy, r=["ps3"], w=["mean_sb"])
        tt("dve", m2, mean_sb, mean_sb, ALU.mult, r=["mean_sb"], w=["m2"])
        tt("dve", m2, ps[4][:, :], m2, ALU.subtract, r=["ps4", "m2"], w=["m2"])
        rstd_from_ss(m2, rstd_bc, 1.0, "m2", "rstd_bc")
        for cc in range(4):
            tb = cc % 2
            tt("dve", tmpv[tb], yv[:, cc, :], mean_sb, ALU.subtract, r=["yv%d" % cc, "mean_sb"], w=["tmpv%d" % tb])
            tt("dve", tmpv[tb], tmpv[tb], rstd_bc, ALU.mult, r=["tmpv%d" % tb, "rstd_bc"], w=["tmpv%d" % tb])
            act(convT[:, cc, :], tmpv[tb], AF.Silu, r=["tmpv%d" % tb, "lngT", "lnbT"], w=["convT%d" % cc],
                bias=lnbT[:, cc:cc + 1], scale=lngT[:, cc:cc + 1])

    def d1_out(s4):
        hT5 = hT5b[s4 % 2]
        nxt = s4 + 1 < NS
        allh = ["hT5_%d_%d_%d" % (s4 % 2, i, k) for i in range(5) for k in range(KC)]
        for i in range(4):
            m = 4 * s4 + i
            for half in range(2):
                ob_ = 5 + (i * 2 + half) % 3
                items = []
                for k in range(8):
                    lh = attnT[:, k, m * 128:(m + 1) * 128] if k < 4 else convT[:, k - 4, i * 128:(i + 1) * 128]
                    items.append((ps[ob_][:, :], lh, woutp[:, k, half * 512:(half + 1) * 512], k == 0, k == 7))
                mms(items, r=[("attnT", m, h) for h in range(4)] + ["convT%d" % c_ for c_ in range(4)] + ["woutp"],
                    w=["ps%d" % ob_])
                tt("dve", X[:, m, half * 512:(half + 1) * 512], ps[ob_][:, :], X[:, m, half * 512:(half + 1) * 512],
                   ALU.add, r=["ps%d" % ob_, ("X", m)], w=[("X", m)])


    d1_glu(0)
    for s4 in range(NS):
        d1_conv(s4)
        d1_ln(s4)
        if s4 + 1 < NS:
            d1_glu(s4 + 1)
        d1_out(s4)

    if stop_after in ("D1", "D1a"):
        dbg["X"] = nc.dram_tensor("dbg_X", [NOWN * 128, D], F32, kind="ExternalOutput").ap()
        for m in range(4 * NS):
            dma(dbg["X"][m * 128:(m + 1) * 128, :], X[:, m, :], r=[("X", m)], w=[], tag="outX")
        S.emit()
        return nc

    S.barrier(bar_tile)
    A.reset(D1W - 8192)
    h2T = A.alloc([128, KC, NOWN * 128], BF16)
    cmb = A.alloc([128, NOWN, 32])
    gtf_rep = A.alloc([128, D])
    D2W = A.mark()
    crep = A.alloc([128, KC, 128])
    abufs = ada_alloc()
    ADA_END = A.mark()
    xn = [A.alloc([128, D]) for _ in range(2)]
    h2f = [A.alloc([128, KC, 128]) for _ in range(2)]
    rs_ = [A.alloc([128, 128]) for _ in range(2)]
    ssD = [A.alloc([128, 1]) for _ in range(2)]
    rsD = [A.alloc([128, 1]) for _ in range(2)]
    cp("dve", crep, cact.unsqueeze(2).broadcast_to([128, KC, 128]), r=["cact"], w=["crep"])
    for g in range(6, 10):
        ada_group(g, abufs[g % 2], 7 - 2 * (g % 2))
        ada_to_cols(abufs[g % 2], sh_f if g < 8 else sc_f, (g % 2) * 4, 6 - 2 * (g % 2))
    tsc("dve", gsc_f, sc_f, 1.0, None, ALU.add, None, r=["adacols"], w=["gsc_f"])
    tt("dve", gsc_f, gsc_f, gffnT, ALU.mult, r=["gsc_f", "gffnT"], w=["gsc_f"])
    for g in (10, 11):
        ada_group(g, abufs[g % 2], 7 - 2 * (g % 2))
        cp("dve", gtf_rep[:, (g - 10) * 512:(g - 9) * 512], abufs[g % 2][2], r=["ada_modrep%d" % (g % 2)], w=["gtf_rep"])

    NB2 = NOWN if not mini else 4
    NRS = 5
    rs_ = rs_ + [A.alloc([128, 128]) for _ in range(NRS - 2)]

    D2END = A.mark()
    A.reset(D2W)
    stE = [A.alloc([128, 2048]) for _ in range(4)]
    Wg = [None, None]; Wu = [None, None]; Wd = [None, None]
    Wg[0] = A.alloc([128, KC, 256], BF16); Wu[0] = A.alloc([128, KC, 256], BF16); Wd[0] = A.alloc([128, 2, D], BF16)
    assert A.mark() <= ADA_END, (A.mark(), ADA_END)
    Wg[1] = A.alloc([128, KC, 256], BF16); Wu[1] = A.alloc([128, KC, 256], BF16); Wd[1] = A.alloc([128, 2, D], BF16)
    hid = [A.alloc([128, 2, 512], BF16) for _ in range(2)]
    sgl = [A.alloc([128, 512], BF16) for _ in range(2)]
    tacc = [A.alloc([128, 512]) for _ in range(2)]
    A.reset(max(A.mark(), D2END))
    stc = {"n": 0}
    ADA_GUARD = ["crep"] + [k_ + s_ for k_ in ("ada_stage", "ada_brep", "ada_modrep") for s_ in ("0", "1")]

    def load_expert(e_, guard=()):
        eb = e_ % 2
        g_ = list(guard)
        for which in range(3):
            sl = stc["n"] % 4
            stc["n"] += 1
            sk = "stE%d" % sl
            if which < 2:
                srcw = (w_gate if which == 0 else w_up)[e_].rearrange("(k p) f -> p k f", p=128)
                stv = stE[sl][:, :].rearrange("p (k f) -> p k f", k=KC)
                dma(stv, srcw, r=[], w=[sk] + g_, tag=sk)
                if which == 0:
                    cp("pool", Wg[eb], stv, r=[sk], w=["Wg%d" % eb] + g_)
                else:
                    act(Wu[eb], stv, AF.Copy, r=[sk], w=["Wu%d" % eb] + g_)
            else:
                srcw = w_down[e_].rearrange("(c p) n -> p c n", p=128)
                stv = stE[sl][:, :].rearrange("p (c n) -> p c n", c=2)
                dma(stv, srcw, r=[], w=[sk] + g_, tag=sk)
                tt("pool", Wd[eb], stv, gtf_rep.unsqueeze(1).broadcast_to([128, 2, D]), ALU.mult,
                   r=[sk, "gtf_rep"], w=["Wd%d" % eb] + g_)

    load_expert(0, guard=ADA_GUARD)

    def rviews(m):
        R = rs_[m % NRS]
        v = dict(lg=R[:, 0:36], gmax=R[:, 36:37], ngmax=R[:, 37:38], gsum=R[:, 38:39], gp=R[:, 39:40],
                 ohg=R[:, 40:44], junk4=R[:, 44:48], esel=R[:, 48:80], ein=R[:, 80:88], top8=R[:, 88:96],
                 eq2=R[:, 112:120], cmb8=R[:, 120:128])
        for i_, nm in enumerate(("dcol", "ed", "den", "w1", "w2", "w1g", "w2g")):
            v[nm] = R[:, 96 + i_:97 + i_]
        return v, "rs%d" % (m % NRS)

    def d_n1(m):
        bi = m % 2
        act(junkb, X[:, m, :], AF.Square, r=[("X", m)], w=["junk_sb", "ssD%d" % bi], accum=ssD[bi])
        rstd_from_ss(ssD[bi], rsD[bi], 1.0 / D, "ssD%d" % bi, "ssD%dr" % bi)

    def d_n2(m):
        bi = m % 2
        tsc("dve", xn[bi], X[:, m, :], rsD[bi], None, ALU.mult, None, r=[("X", m), "ssD%dr" % bi], w=["xn%d" % bi])

    def d_n3(m):
        bi = m % 2
        for half in range(2):
            tps([(ps[half][:, kk * 128:(kk + 1) * 128], xn[bi][:, (half * 4 + kk) * 128:(half * 4 + kk + 1) * 128])
                 for kk in range(4)], ident_f, r=["xn%d" % bi, "ident_f"], w=["ps%d" % half])

    def d_n4(m):
        bi = m % 2
        for half in range(2):
            for kk in range(4):
                k = half * 4 + kk
                act(h2f[bi][:, k, :], ps[half][:, kk * 128:(kk + 1) * 128], AF.Identity,
                    r=["ps%d" % half, "gsc_f", "adacols"], w=["h2f%d_%d" % (bi, k)],
                    bias=sh_f[:, k:k + 1], scale=gsc_f[:, k:k + 1])

    def d_n5(m):
        bi = m % 2
        hks = ["h2f%d_%d" % (bi, k) for k in range(KC)]
        cp("pool", h2T[:, :, m * 128:(m + 1) * 128], h2f[bi], r=hks, w=[("h2T", m)])
        mms([(ps[2][:, 0:36], h2f[bi][:, k, :], wr[:, k, :], k == 0, k == KC - 1) for k in range(KC)],
            r=hks + ["wr"], w=["ps2"])

    def d_cA(m):
        v, rk_ = rviews(m)
        tt("dve", v["lg"], ps[2][:, 0:36], br_rep, ALU.add, r=["ps2", "br_rep"], w=[rk_])
        S.add("dve", lambda e, gmax=v["gmax"], lg=v["lg"]: e.tensor_reduce(gmax, lg[:, 0:4], AX.X, ALU.max), r=[rk_], w=[rk_])
        tsc("dve", v["ngmax"], v["gmax"], -1.0, None, ALU.mult, None, r=[rk_], w=[rk_])

    def d_cB(m):
        v, rk_ = rviews(m)
        act(v["junk4"], v["lg"][:, 0:4], AF.Exp, r=[rk_], w=[rk_], bias=v["ngmax"], scale=1.0, accum=v["gsum"])

    def d_cC(m):
        v, rk_ = rviews(m)
        lg, ohg, esel, ein, top8 = v["lg"], v["ohg"], v["esel"], v["ein"], v["top8"]
        S.add("dve", lambda e, gp=v["gp"], gsum=v["gsum"]: e.reciprocal(gp, gsum), r=[rk_], w=[rk_])
        tsc("dve", ohg, lg[:, 0:4], v["gmax"], None, ALU.is_ge, None, r=[rk_], w=[rk_])
        tt("dve", esel.rearrange("p (g e) -> p g e", g=4), lg[:, 4:36].rearrange("p (g e) -> p g e", g=4),
           ohg.unsqueeze(2).broadcast_to([128, 4, 8]), ALU.mult, r=[rk_], w=[rk_])
        S.add("dve", lambda e, ein=ein, esel=esel: e.tensor_reduce(ein, esel.rearrange("p (g e) -> p e g", g=4), AX.X, ALU.add),
              r=[rk_], w=[rk_])
        S.add("dve", lambda e, top8=top8, ein=ein: e.max(top8, ein), r=[rk_], w=[rk_])
        tt("dve", v["dcol"], top8[:, 1:2], top8[:, 0:1], ALU.subtract, r=[rk_], w=[rk_])

    def d_cD(m):
        v, rk_ = rviews(m)
        act(v["ed"], v["dcol"], AF.Exp, r=[rk_], w=[rk_])

    def d_cE(m):
        v, rk_ = rviews(m)
        ed, den, w1, w2, w1g, w2g, gp = v["ed"], v["den"], v["w1"], v["w2"], v["w1g"], v["w2g"], v["gp"]
        ein, top8, cmb8, eq2, ohg = v["ein"], v["top8"], v["cmb8"], v["eq2"], v["ohg"]
        tsc("dve", den, ed, 1.0, None, ALU.add, None, r=[rk_], w=[rk_])
        S.add("dve", lambda e, w1=w1, den=den: e.reciprocal(w1, den), r=[rk_], w=[rk_])
        tt("dve", w2, ed, w1, ALU.mult, r=[rk_], w=[rk_])
        tt("dve", w1g, w1, gp, ALU.mult, r=[rk_], w=[rk_])
        tt("dve", w2g, w2, gp, ALU.mult, r=[rk_], w=[rk_])
        tsc("dve", cmb8, ein, top8[:, 0:1], None, ALU.is_equal, None, r=[rk_], w=[rk_])
        tsc("dve", cmb8, cmb8, w1g, None, ALU.mult, None, r=[rk_], w=[rk_])
        tsc("dve", eq2, ein, top8[:, 1:2], None, ALU.is_equal, None, r=[rk_], w=[rk_])
        tsc("dve", eq2, eq2, w2g, None, ALU.mult, None, r=[rk_], w=[rk_])
        tt("dve", cmb8, cmb8, eq2, ALU.add, r=[rk_], w=[rk_])
        tt("dve", cmb[:, m, :].rearrange("p (g e) -> p g e", g=4), ohg.unsqueeze(2).broadcast_to([128, 4, 8]),
           cmb8.unsqueeze(1).broadcast_to([128, 4, 8]), ALU.mult, r=[rk_], w=[("cmb", m)])

    d_order = [(d_cE, 9), (d_cD, 8), (d_cC, 7), (d_cB, 6), (d_cA, 5), (d_n5, 4), (d_n4, 3), (d_n3, 2), (d_n2, 1), (d_n1, 0)]
    for t in range(NB2 + 9):
        for fn_, off_ in d_order:
            m = t - off_
            if 0 <= m < NB2:
                fn_(m)

    if stop_after == "D2":
        dbg["cmb"] = nc.dram_tensor("dbg_cmb", [128, NOWN * 32], F32, kind="ExternalOutput").ap()
        dbg["h2T"] = nc.dram_tensor("dbg_h2T", [128, KC * NOWN * 128], BF16, kind="ExternalOutput").ap()
        if mini:
            S.add("dve", lambda e: e.memset(cmb[:, NB2:, :], 0.0), r=[], w=["cmbpad"])
            S.add("dve", lambda e: e.memset(h2T[:, :, NB2 * 128:], 0.0), r=[], w=["h2pad"])
        dma(dbg["cmb"], cmb[:, :, :].rearrange("p a b -> p (a b)"), r=[("cmb", m) for m in range(NB2)] + ["cmbpad"], w=[], tag="out0")
        dma(dbg["h2T"], h2T[:, :, :].rearrange("p a b -> p (a b)"), r=[("h2T", m) for m in range(NB2)] + ["h2pad"], w=[], tag="out1")
        S.emit()
        return nc

    S.barrier(bar_tile)
    def gate_up(e_, s4, hb):
        eb = e_ % 2
        hk = [("h2T", 4 * s4 + i) for i in range(4)]
        for fc in range(2):
            mms([(ps[2 * fc][:, :], Wg[eb][:, k, fc * 128:(fc + 1) * 128], h2T[:, k, s4 * 512:(s4 + 1) * 512], k == 0, k == KC - 1)
                 for k in range(KC)], r=hk + ["Wg%d" % eb], w=["ps%d" % (2 * fc)])
            mms([(ps[2 * fc + 1][:, :], Wu[eb][:, k, fc * 128:(fc + 1) * 128], h2T[:, k, s4 * 512:(s4 + 1) * 512], k == 0, k == KC - 1)
                 for k in range(KC)], r=hk + ["Wu%d" % eb], w=["ps%d" % (2 * fc + 1)])
            act(sgl[fc], ps[2 * fc][:, :], AF.Silu, r=["ps%d" % (2 * fc)], w=["sgl%d" % fc])
            tt("dve", hid[hb][:, fc, :], ps[2 * fc + 1][:, :], sgl[fc], ALU.mult,
               r=["ps%d" % (2 * fc + 1), "sgl%d" % fc], w=["hid%d_%d" % (hb, fc)])

    def down(e_, s4, hb):
        eb = e_ % 2
        for i in range(4):
            m = 4 * s4 + i
            for half in range(2):
                ob_ = 4 + (i * 2 + half) % 4
                mms([(ps[ob_][:, :], hid[hb][:, fc, i * 128:(i + 1) * 128], Wd[eb][:, fc, half * 512:(half + 1) * 512], fc == 0, fc == 1)
                     for fc in range(2)], r=["hid%d_0" % hb, "hid%d_1" % hb, "Wd%d" % eb], w=["ps%d" % ob_])
                tb_ = (i * 2 + half) % 2
                act(tacc[tb_], ps[ob_][:, :], AF.Identity, r=["ps%d" % ob_, ("cmb", m)], w=["tacc%d" % tb_],
                    scale=cmb[:, m, e_:e_ + 1])
                tt("dve", X[:, m, half * 512:(half + 1) * 512], X[:, m, half * 512:(half + 1) * 512], tacc[tb_],
                   ALU.add, r=["tacc%d" % tb_, ("X", m)], w=[("X", m)])

    NE = NEXP
    prev = None
    step = 0
    for e_ in range(NE):
        for s4 in range(4 if not mini else 1):
            hb = step % 2
            step += 1
            gate_up(e_, s4, hb)
            if prev is not None:
                down(*prev)
            prev = (e_, s4, hb)
            if s4 == 0 and e_ + 1 < NE:
                load_expert(e_ + 1)
    down(*prev)

    for m in range(NOWN if not mini else 4):
        dma(out_d[m * 128:(m + 1) * 128, :], X[:, m, :], r=[("X", m)], w=[], tag="outX")
    S.emit()
    return nc


def host_inputs(inputs, core):
    b, j = core // 4, core % 4
    f = np.float32
    x = np.asarray(inputs["x"], f)
    xb_ = np.ascontiguousarray(x[b])
    own_idx = np.concatenate([np.arange(512 * m + 128 * j, 512 * m + 128 * j + 128) for m in range(NOWN)])
    xo_ = np.ascontiguousarray(xb_[own_idx])
    xh_ = np.zeros((4 * 128, D), f)
    for m in range(NOWN):
        st = 512 * m + 128 * j - 32
        if st >= 0:
            xh_[m * 32:(m + 1) * 32] = xb_[st:st + 32]
    pos = np.asarray(inputs["positions"], np.int32)[b]
    posb_ = np.ascontiguousarray(pos.reshape(NBLK_B, 128).T)
    poso_ = np.ascontiguousarray(pos[own_idx].reshape(NOWN, 128).T)

    def colT(v, n):
        return np.ascontiguousarray(np.asarray(v, f).reshape(n, 128).T)

    kq = np.arange(128)
    cm = np.zeros((128, 4, 2, 128), f)
    for t in range(4):
        allowed = (t * 128 + kq[:, None]) <= (j * 128 + kq[None, :])
        cm[:, t, :, :] = allowed[:, None, :]
    d = {
        "xb": xb_, "xo": xo_, "xh": xh_,
        "cT": colT(inputs["c"][b], KC),
        "posb": posb_, "poso": poso_,
        "w_ada": np.ascontiguousarray(np.asarray(inputs["w_ada"], f)[0]),
        "b_ada": np.ascontiguousarray(np.asarray(inputs["b_ada"], f)[0:1]),
        "g_mixT": colT(inputs["g_mix"][0], KC),
        "g_ffnT": colT(inputs["g_ffn"][0], KC),
        "w_in": np.ascontiguousarray(np.asarray(inputs["w_in"], f)[0]),
        "q_norm_g": np.asarray(inputs["q_norm_g"], f)[0:1],
        "k_norm_g": np.asarray(inputs["k_norm_g"], f)[0:1],
        "lambda_q1": np.asarray(inputs["lambda_q1"], f)[0:1],
        "lambda_k1": np.asarray(inputs["lambda_k1"], f)[0:1],
        "lambda_q2": np.asarray(inputs["lambda_q2"], f)[0:1],
        "lambda_k2": np.asarray(inputs["lambda_k2"], f)[0:1],
        "subln_g": np.asarray(inputs["subln_g"], f)[0:1],
        "b_gluT": colT(inputs["b_glu"][0], 8),
        "w_dwT": np.ascontiguousarray(np.asarray(inputs["w_dw"], f)[0].T.reshape(4, 128, 31).transpose(1, 0, 2)),
        "b_dwT": colT(inputs["b_dw"][0], 4),
        "ln_gT": colT(inputs["conv_ln_g"][0], 4),
        "ln_bT": colT(inputs["conv_ln_b"][0], 4),
        "w_out": np.ascontiguousarray(np.asarray(inputs["w_out"], f)[0]),
        "w_rt": np.ascontiguousarray(np.concatenate([np.asarray(inputs["w_group"], f)[0],
                                                     np.asarray(inputs["w_router"], f)[0]], axis=1)),
        "b_rt": np.ascontiguousarray(np.concatenate([np.asarray(inputs["b_group"], f)[0],
                                                     np.asarray(inputs["b_router"], f)[0]])[None, :]),
        "w_gate": np.ascontiguousarray(np.asarray(inputs["w_gate"], f)[0]),
        "w_up": np.ascontiguousarray(np.asarray(inputs["w_up"], f)[0]),
        "w_down": np.ascontiguousarray(np.asarray(inputs["w_down"], f)[0]),
        "ident_f": np.eye(128, dtype=f),
        "ident_b": np.eye(128, dtype=f).astype(ml_dtypes.bfloat16),
        "inv_freq": (500000.0 ** (-np.arange(0, 16, 2, dtype=f) / 16.0)).astype(f)[None, :],
        "cmask": cm.reshape(128, 1024).astype(ml_dtypes.bfloat16),
        "hmask": np.full((128, 1), 0.0 if j == 0 else 1.0, f),
    }
    return d, own_idx


def kernel(**inputs):
    nc = build_program()
    in_maps = []
    owns = []
    for c in range(8):
        d, own_idx = host_inputs(inputs, c)
        in_maps.append(d)
        owns.append(own_idx)
    res = run_bass_kernel_spmd(nc, in_maps, core_ids=list(range(8)))
    out = np.zeros((2, SEQ, D), np.float32)
    for c in range(8):
        out[c // 4, owns[c]] = np.asarray(res.results[c]["out"], np.float32)
    return out
```

```python
import math
import numpy as np
import ml_dtypes
import concourse.bass as bass
import concourse.mybir as mybir
from concourse.bass_utils import run_bass_kernel_spmd

F32 = mybir.dt.float32
BF16 = mybir.dt.bfloat16
I32 = mybir.dt.int32
AF = mybir.ActivationFunctionType
ALU = mybir.AluOpType
AX = mybir.AxisListType

D = 1024
KC = 8
SEQ = 8192
NBLK_B = 64
NOWN = 16
EPS = 1e-6
LAMBDA_INIT = 0.8 - 0.6 * math.exp(-0.3 * 0)
NEXP = 32
TWO_PI = 2.0 * math.pi
C1 = 6.28125
C2 = TWO_PI - C1


class Sched:
    COMPUTE = ("pe", "act", "dve", "pool")

    def __init__(self, nc):
        self.nc = nc
        self.ops = []
        self.last_w = {}
        self.readers = {}
        self.tag_count = {}
        self.bulk_tags = set()
        self.epoch = None
        self.epoch_start = 0

    def add(self, eng, fn, r=(), w=(), tag=None):
        idx = len(self.ops)
        deps = set()
        for res in r:
            if res in self.last_w:
                deps.add(self.last_w[res])
        for res in w:
            if res in self.last_w:
                deps.add(self.last_w[res])
            for i in self.readers.get(res, {}).values():
                deps.add(i)
        if self.epoch is not None:
            deps.add(self.epoch)
        op = dict(eng=eng, fn=fn, deps=deps, tag=tag, signal=False, idx=idx)
        if tag is not None:
            self.tag_count[tag] = self.tag_count.get(tag, 0) + 1
            op["tagn"] = self.tag_count[tag]
        self.ops.append(op)
        for res in w:
            self.last_w[res] = idx
            self.readers[res] = {}
        for res in r:
            key = eng if tag is None else ("dma", idx)
            self.readers.setdefault(res, {})[key] = idx
        return idx

    def barrier(self, tile):
        deps = set()
        last = {}
        for op in self.ops[self.epoch_start:]:
            if op["tag"] is None:
                last[op["eng"]] = op["idx"]
            else:
                deps.add(op["idx"])
        deps |= set(last.values())
        idx = self.add("dve", lambda e: e.memset(tile, 0.0))
        self.ops[idx]["deps"] |= deps
        self.epoch = idx
        self.epoch_start = idx

    def emit(self):
        nc = self.nc
        ops = self.ops
        for op in ops:
            for d in op["deps"]:
                dop = ops[d]
                if dop["tag"] is None and dop["eng"] == "pe" and op["eng"] == "pe" and op["tag"] is None:
                    continue
                dop["signal"] = True
        sems = {e: nc.alloc_semaphore(name="sem_" + e) for e in self.COMPUTE}
        tagsem = {t: nc.alloc_semaphore(name="semt_" + str(t)) for t in self.tag_count}
        cnt = {e: 0 for e in self.COMPUTE}
        for op in ops:
            if op["tag"] is not None:
                t = op["tag"]
                n = self.tag_count[t] if t in self.bulk_tags else op["tagn"]
                op["token"] = (tagsem[t], 16 * n, ("t", t))
            elif op["signal"]:
                cnt[op["eng"]] += 1
                op["token"] = (sems[op["eng"]], cnt[op["eng"]], ("e", op["eng"]))
        streams = {e: [] for e in ("pe", "act", "dve", "pool", "sp")}
        for op in ops:
            streams[op["eng"]].append(op)
        out_tag_total = {t: 16 * n for t, n in self.tag_count.items()}

        def run(engname, e):
            waited = {}
            for op in streams[engname]:
                need = {}
                for d in op["deps"]:
                    dop = ops[d]
                    if dop["tag"] is None and dop["eng"] == "pe" and engname == "pe" and op["tag"] is None:
                        continue
                    sem, val, key = dop["token"]
                    if need.get(key, (None, 0))[1] < val:
                        need[key] = (sem, val)
                for key, (sem, val) in need.items():
                    if waited.get(key, 0) >= val:
                        continue
                    e.wait_ge(sem, val)
                    waited[key] = val
                ins = op["fn"](e)
                if op["tag"] is not None:
                    ins.then_inc(op["token"][0], 16)
                elif op["signal"]:
                    ins.then_inc(op["token"][0], 1)
            if engname == "sp":
                for t, tot in out_tag_total.items():
                    if str(t).startswith("out"):
                        e.wait_ge(tagsem[t], tot)

        with nc.Block() as block:
            @block.tensor
            def _(e):
                run("pe", e)

            @block.scalar
            def _(e):
                run("act", e)

            @block.vector
            def _(e):
                run("dve", e)

            @block.gpsimd
            def _(e):
                run("pool", e)

            @block.sync
            def _(e):
                run("sp", e)


class Arena:
    def __init__(self, nc, words):
        self.t = nc.alloc_sbuf_tensor("arena", [128, words], F32)
        self.words = words
        self.off = 0

    def mark(self):
        return self.off

    def reset(self, m):
        self.off = m

    def alloc(self, shape, dt=F32):
        n = 1
        for s in shape[1:]:
            n *= s
        w = n if dt in (F32, I32) else (n + 1) // 2
        w = (w + 7) // 8 * 8
        assert self.off + w <= self.words, ("arena overflow", self.off, w, self.words)
        v = self.t[:, self.off:self.off + w]
        self.off += w
        if dt != F32:
            v = v.bitcast(dt)
        v = v[:, 0:n]
        if len(shape) == 3:
            v = v.rearrange("p (a b) -> p a b", a=shape[1])
        elif len(shape) == 4:
            v = v.rearrange("p (a b c) -> p a b c", a=shape[1], b=shape[2])
        return v


def build_program(stop_after=None, mini=False, mini_ns=1):
    nc = bass.Bass("TRN2", target_bir_lowering=False)
    S = Sched(nc)
    S.bulk_tags.add("const")

    def din(name, shape, dt=F32):
        return nc.dram_tensor(name, list(shape), dt, kind="ExternalInput").ap()

    xb = din("xb", [SEQ, D])
    xo = din("xo", [NOWN * 128, D])
    xh = din("xh", [4 * 128, D])
    cT_d = din("cT", [128, KC])
    posb_d = din("posb", [128, NBLK_B], I32)
    poso_d = din("poso", [128, NOWN], I32)
    w_ada = din("w_ada", [D, 6 * D])
    b_ada = din("b_ada", [1, 6 * D])
    gmix_d = din("g_mixT", [128, KC])
    gffn_d = din("g_ffnT", [128, KC])
    w_in = din("w_in", [D, 2560])
    gq_d = din("q_norm_g", [1, 64])
    gk_d = din("k_norm_g", [1, 64])
    lq1_d = din("lambda_q1", [1, 64])
    lk1_d = din("lambda_k1", [1, 64])
    lq2_d = din("lambda_q2", [1, 64])
    lk2_d = din("lambda_k2", [1, 64])
    subg_d = din("subln_g", [1, 128])
    bglu_d = din("b_gluT", [128, 8])
    wdw_d = din("w_dwT", [128, 4, 31])
    bdw_d = din("b_dwT", [128, 4])
    lng_d = din("ln_gT", [128, 4])
    lnb_d = din("ln_bT", [128, 4])
    w_out = din("w_out", [D, D])
    wr_d = din("w_rt", [D, 36])
    br_d = din("b_rt", [1, 36])
    w_gate = din("w_gate", [NEXP, D, 256])
    w_up = din("w_up", [NEXP, D, 256])
    w_down = din("w_down", [NEXP, 256, D])
    identf_d = din("ident_f", [128, 128])
    identb_d = din("ident_b", [128, 128], BF16)
    invf_d = din("inv_freq", [1, 8])
    mask_d = din("cmask", [128, 4 * 256], BF16)
    hmask_d = din("hmask", [128, 1])
    out_d = nc.dram_tensor("out", [NOWN * 128, D], F32, kind="ExternalOutput").ap()
    dbg = {}

    A = Arena(nc, 53000)
    ps = [nc.alloc_psum_tensor("ps%d" % i, [128, 512], F32) for i in range(8)]

    def psb(i):
        return ps[i][:, :].bitcast(BF16)

    ident_f = A.alloc([128, 128])
    ident_b = A.alloc([128, 128], BF16)
    cT = A.alloc([128, KC])
    cact = A.alloc([128, KC])
    gmixT = A.alloc([128, KC])
    gffnT = A.alloc([128, KC])
    sh_a = A.alloc([128, KC]); sc_a = A.alloc([128, KC]); gsc_a = A.alloc([128, KC])
    sh_f = A.alloc([128, KC]); sc_f = A.alloc([128, KC]); gsc_f = A.alloc([128, KC])
    gq_rep = A.alloc([128, 64]); gk_rep = A.alloc([128, 64])
    lvec = A.alloc([128, 4, 64])
    sg_rep = A.alloc([128, 128])
    lam_t = A.alloc([128, 8])
    bgluT = A.alloc([128, 8])
    wdwT = A.alloc([128, 4, 31])
    bdwT = A.alloc([128, 4]); lngT = A.alloc([128, 4]); lnbT = A.alloc([128, 4])
    wr = A.alloc([128, KC, 36])
    br_rep = A.alloc([128, 36])
    invf = A.alloc([128, 8])
    cmask = A.alloc([128, 4 * 256], BF16)
    hmask = A.alloc([128, 1])
    cos_b = A.alloc([128, NBLK_B, 8]); sin_b = A.alloc([128, NBLK_B, 8])
    cos_o = A.alloc([128, NOWN, 8]); sin_o = A.alloc([128, NOWN, 8])
    epsc = A.alloc([128, 1]); m8c = A.alloc([128, 1])
    ones_ln = A.alloc([128, 128])
    ATTN_OFF = A.mark()
    attnT = A.alloc([128, 4, NOWN * 128], BF16)
    small = A.alloc([128, 64])
    junk_sb = A.alloc([128, D], BF16)
    P_END = A.mark()

    def dma(out, in_, r, w, tag):
        S.add("sp", lambda e, o=out, i=in_: e.dma_start(out=o, in_=i), r=r, w=w, tag=tag)

    def act(out, in_, func, r, w, bias=None, scale=None, accum=None):
        def f(e, out=out, in_=in_, func=func, bias=bias, scale=scale, accum=accum):
            kw = {}
            if bias is not None:
                kw["bias"] = bias
            if scale is not None:
                kw["scale"] = scale
            if accum is not None:
                kw["accum_out"] = accum
            return e.activation(out, in_, func, **kw)
        S.add("act", f, r=r, w=w)

    def tsc(eng, out, in0, s1, s2, op0, op1, r, w):
        def f(e, out=out, in0=in0, s1=s1, s2=s2, op0=op0, op1=op1):
            if op1 is None:
                return e.tensor_scalar(out, in0, s1, None, op0)
            return e.tensor_scalar(out, in0, s1, s2, op0, op1)
        S.add(eng, f, r=r, w=w)

    def tt(eng, out, in0, in1, op, r, w):
        S.add(eng, lambda e, o=out, a=in0, b=in1, op=op: e.tensor_tensor(o, a, b, op), r=r, w=w)

    def stt(out, in0, sc, in1, op0, op1, r, w):
        S.add("dve", lambda e, o=out, a=in0, s=sc, b=in1, o0=op0, o1=op1:
              e.scalar_tensor_tensor(o, a, s, b, o0, o1), r=r, w=w)

    def cp(eng, out, in_, r, w):
        S.add(eng, lambda e, o=out, i=in_: e.tensor_copy(o, i), r=r, w=w)

    def mms(items, r, w):
        def f(e, items=items):
            ins = None
            for (o, l, rh, st, sp_) in items:
                ins = e.matmul(o, l, rh, start=st, stop=sp_)
            return ins
        S.add("pe", f, r=r, w=w)

    def tps(items, ident, r, w):
        def f(e, items=items, ident=ident):
            ins = None
            for (o, i) in items:
                ins = e.transpose(o, i, ident)
            return ins
        S.add("pe", f, r=r, w=w)

    def rstd_from_ss(ss_ap, out_ap, inv_n, rk, wk):
        act(out_ap, ss_ap, AF.Ln, r=[rk, "epsc"], w=[wk], bias=epsc[:, 0:1], scale=inv_n)
        act(out_ap, out_ap, AF.Exp, r=[wk], w=[wk], scale=-0.5)

    def cload(dst, src, key):
        dma(dst, src, r=[], w=[key], tag="const")

    cload(ident_f, identf_d, "ident_f")
    cload(ident_b, identb_d, "ident_b")
    cload(cT, cT_d, "cT")
    cload(gmixT, gmix_d, "gmixT")
    cload(gffnT, gffn_d, "gffnT")
    cload(gq_rep, gq_d.broadcast_to([128, 64]), "gq_rep")
    cload(gk_rep, gk_d.broadcast_to([128, 64]), "gk_rep")
    for i, dd in enumerate((lq1_d, lk1_d, lq2_d, lk2_d)):
        cload(lvec[:, i, :], dd.broadcast_to([128, 64]), "lvec%d" % i)
    cload(sg_rep, subg_d.broadcast_to([128, 128]), "sg_rep")
    cload(bgluT, bglu_d, "bgluT")
    cload(wdwT, wdw_d, "wdwT")
    cload(bdwT, bdw_d, "bdwT")
    cload(lngT, lng_d, "lngT")
    cload(lnbT, lnb_d, "lnbT")
    cload(wr, wr_d.rearrange("(k p) n -> p k n", p=128), "wr")
    cload(br_rep, br_d.broadcast_to([128, 36]), "br_rep")
    cload(invf, invf_d.broadcast_to([128, 8]), "invf")
    cload(cmask, mask_d, "cmask")
    cload(hmask, hmask_d, "hmask")
    posb_i = A.alloc([128, NBLK_B], I32)
    poso_i = A.alloc([128, NOWN], I32)
    cload(posb_i, posb_d, "posb_i")
    cload(poso_i, poso_d, "poso_i")

    S.add("dve", lambda e: e.memset(lam_t, 0.0), r=[], w=["lam0", "lam1", "lam23", "lam4", "neglam"])
    S.add("dve", lambda e: e.memset(small, 0.0), r=[], w=["small", "junk64", "ropetab", "gains"])
    S.add("dve", lambda e: e.memset(epsc, EPS), r=[], w=["epsc"])
    S.add("dve", lambda e: e.memset(m8c, -8.0), r=[], w=["m8c"])
    S.add("dve", lambda e: e.memset(ones_ln, 1.0 / 512.0), r=[], w=["ones_ln"])

    act(cact, cT, AF.Silu, r=["cT"], w=["cact"])
    tsc("dve", sg_rep, sg_rep, 1.0 - LAMBDA_INIT, None, ALU.mult, None, r=["sg_rep"], w=["sg_rep"])

    junk64 = small[:, 0:64]
    tt("dve", junk64, lvec[:, 0, :], lvec[:, 1, :], ALU.mult, r=["lvec0", "lvec1"], w=["junk64"])
    S.add("dve", lambda e: e.tensor_reduce(lam_t[:, 0:1], junk64, AX.X, ALU.add), r=["junk64"], w=["lam0"])
    tt("dve", junk64, lvec[:, 2, :], lvec[:, 3, :], ALU.mult, r=["lvec2", "lvec3", "lam0"], w=["junk64"])
    S.add("dve", lambda e: e.tensor_reduce(lam_t[:, 1:2], junk64, AX.X, ALU.add), r=["junk64"], w=["lam1"])
    act(lam_t[:, 2:4], lam_t[:, 0:2], AF.Exp, r=["lam0", "lam1"], w=["lam23"])
    tt("dve", lam_t[:, 4:5], lam_t[:, 3:4], lam_t[:, 2:3], ALU.subtract, r=["lam23"], w=["lam4"])
    tsc("dve", lam_t[:, 7:8], lam_t[:, 4:5], -LAMBDA_INIT, None, ALU.add, None, r=["lam4"], w=["neglam"])
    neglam = lam_t[:, 7:8]

    def rope_tables(pos_i, nblk, cos_t, sin_t, pfx):
        m0 = A.mark()
        posf = A.alloc([128, nblk])
        ang = A.alloc([128, nblk, 8])
        tq = A.alloc([128, nblk, 8])
        ki = A.alloc([128, nblk, 8], I32)
        kf = A.alloc([128, nblk, 8])
        rr = A.alloc([128, nblk, 8])
        mk = A.alloc([128, nblk, 8])
        cp("dve", posf, pos_i, r=[pfx + "pos_i"], w=[pfx + "posf"])
        tt("dve", ang, posf.unsqueeze(2).broadcast_to([128, nblk, 8]),
           invf.unsqueeze(1).broadcast_to([128, nblk, 8]), ALU.mult, r=[pfx + "posf", "invf"], w=[pfx + "ang"])
        for which, dst in (("s", sin_t), ("c", cos_t)):
            k0 = pfx + which
            src = ang
            if which == "c":
                tsc("dve", rr, ang, math.pi / 2.0, None, ALU.add, None, r=[pfx + "ang", pfx + "rr"], w=[pfx + "rr"])
                tsc("dve", tq, rr, 1.0 / TWO_PI, None, ALU.mult, None, r=[pfx + "rr", pfx + "tq"], w=[pfx + "tq"])
                base = rr
            else:
                tsc("dve", tq, ang, 1.0 / TWO_PI, None, ALU.mult, None, r=[pfx + "ang"], w=[pfx + "tq"])
                base = ang
            cp("dve", ki, tq, r=[pfx + "tq"], w=[pfx + "ki"])
            cp("dve", kf, ki, r=[pfx + "ki"], w=[pfx + "kf"])
            stt(rr, kf, -C1, base, ALU.mult, ALU.add, r=[pfx + "kf", pfx + "ang", pfx + "rr"], w=[pfx + "rr"])
            stt(rr, kf, -C2, rr, ALU.mult, ALU.add, r=[pfx + "kf", pfx + "rr"], w=[pfx + "rr"])
            tsc("dve", mk, rr, math.pi, -TWO_PI, ALU.is_gt, ALU.mult, r=[pfx + "rr", pfx + "mk"], w=[pfx + "mk"])
            tt("dve", rr, rr, mk, ALU.add, r=[pfx + "rr", pfx + "mk"], w=[pfx + "rr"])
            tsc("dve", mk, rr, -math.pi, TWO_PI, ALU.is_lt, ALU.mult, r=[pfx + "rr", pfx + "mk"], w=[pfx + "mk"])
            tt("dve", rr, rr, mk, ALU.add, r=[pfx + "rr", pfx + "mk"], w=[pfx + "rr"])
            act(dst, rr, AF.Sin, r=[pfx + "rr"], w=[pfx + "tab" + which])
        return [pfx + "tabs", pfx + "tabc"]

    KV0 = A.mark()
    KT = A.alloc([128, 4, SEQ], BF16)
    VA = A.alloc([128, NBLK_B, 4, 130], BF16)
    WORK0 = A.mark()
    rb = rope_tables(posb_i, NBLK_B, cos_b, sin_b, "rb_")
    ro = rope_tables(poso_i, NOWN, cos_o, sin_o, "ro_")
    rope_keys = ["rb_rr", "rb_mk", "rb_kf", "rb_ki", "rb_tq", "rb_ang", "rb_posf",
                 "ro_rr", "ro_mk", "ro_kf", "ro_ki", "ro_tq", "ro_ang", "ro_posf"]

    def ada_alloc():
        return [(A.alloc([128, KC, 512]), A.alloc([128, 512]), A.alloc([128, 512]), str(i)) for i in range(2)]

    def ada_group(g, ab, pbank):
        stage, brep, modrep, sfx = ab
        dma(stage, w_ada.rearrange("(k p) n -> p k n", p=128)[:, :, g * 512:(g + 1) * 512],
            r=[], w=["ada_stage" + sfx], tag="ada" + sfx)
        dma(brep, b_ada[0:1, g * 512:(g + 1) * 512].broadcast_to([128, 512]), r=[], w=["ada_brep" + sfx], tag="adab" + sfx)
        mms([(ps[pbank][:, :], crep[:, k, :], stage[:, k, :], k == 0, k == KC - 1) for k in range(KC)],
            r=["ada_stage" + sfx, "crep"], w=["ps%d" % pbank])
        tt("dve", modrep, ps[pbank][:, :], brep, ALU.add, r=["ps%d" % pbank, "ada_brep" + sfx], w=["ada_modrep" + sfx])

    def ada_to_cols(ab, dst, col0, pbank):
        modrep, sfx = ab[2], ab[3]
        tps([(ps[pbank][:, jj * 128:(jj + 1) * 128], modrep[:, jj * 128:(jj + 1) * 128]) for jj in range(4)],
            ident_f, r=["ada_modrep" + sfx, "ident_f"], w=["ps%d" % pbank])
        cp("dve", dst[:, col0:col0 + 4], ps[pbank][:, :].rearrange("p (j c) -> p j c", c=128)[:, :, 0],
           r=["ps%d" % pbank], w=["adacols"])

    S.barrier(small[:, 62:63])
    A.reset(WORK0)
    m_ada = A.mark()
    crep = A.alloc([128, KC, 128])
    cp("dve", crep, cact.unsqueeze(2).broadcast_to([128, KC, 128]), r=["cact"], w=["crep"])
    abufs = ada_alloc()
    for g in range(4):
        ada_group(g, abufs[g % 2], 7 - 2 * (g % 2))
        ada_to_cols(abufs[g % 2], sh_a if g < 2 else sc_a, (g % 2) * 4, 6 - 2 * (g % 2))
    tsc("dve", gsc_a, sc_a, 1.0, None, ALU.add, None, r=["adacols"], w=["gsc_a"])
    tt("dve", gsc_a, gsc_a, gmixT, ALU.mult, r=["gsc_a", "gmixT"], w=["gsc_a"])

    S.barrier(small[:, 62:63])
    A.reset(WORK0)
    WORK = A.mark()
    NXT = 3
    xt = [A.alloc([128, D]) for _ in range(NXT)]
    _cur = A.mark()
    A.reset(ATTN_OFF)
    xnb = [A.alloc([128, D], BF16) for _ in range(2)]
    hT = [A.alloc([128, KC, 128], BF16) for _ in range(2)]
    shrep = A.alloc([128, KC, 128])
    assert A.mark() <= ATTN_OFF + 4096
    A.reset(_cur)
    wkv = A.alloc([128, KC, 1024], BF16)
    shW_bf = A.alloc([128, 1024], BF16)
    c128 = A.alloc([128, 128], BF16)
    junkb = junk_sb
    ssA = [A.alloc([128, 1]) for _ in range(2)]
    rsA = [A.alloc([128, 1]) for _ in range(2)]
    ksq = [A.alloc([128, 512]) for _ in range(2)]
    kn = [A.alloc([128, 8, 64]) for _ in range(2)]
    kb16 = [A.alloc([128, 512], BF16) for _ in range(2)]
    ssk = [A.alloc([128, 8]) for _ in range(2)]
    rk = [A.alloc([128, 8]) for _ in range(2)]
    rt = [[A.alloc([128, 8, 8]) for _ in range(4)] for _ in range(2)]

    S.add("pool", lambda e: e.memset(VA[:, :, :, 128:130], 1.0), r=[], w=["VAones"])
    S.add("pool", lambda e: e.memset(c128, 1.0 / 128.0), r=[], w=["c128"])
    cp("dve", shrep, sh_a.unsqueeze(2).broadcast_to([128, KC, 128]), r=["adacols"], w=["shrep"])

    w_in_v = w_in.rearrange("(k p) n -> p k n", p=128)
    for pc in range(8):
        xi = pc % NXT
        st = xt[xi][:, :].rearrange("p (k n) -> p k n", k=KC)
        dma(st, w_in_v[:, :, 512 + pc * 128: 512 + (pc + 1) * 128], r=[], w=["xt%d" % xi], tag="xt%d" % xi)
        tt("pool", wkv[:, :, pc * 128:(pc + 1) * 128], st, gsc_a.unsqueeze(2).broadcast_to([128, KC, 128]), ALU.mult,
           r=["xt%d" % xi, "gsc_a"], w=["wkv"])
        bk = pc // 4
        mms([(ps[bk][:, (pc % 4) * 128:(pc % 4 + 1) * 128], shrep[:, k, :], st[:, k, :], k == 0, k == KC - 1)
             for k in range(KC)], r=["xt%d" % xi, "shrep"], w=["ps%d" % bk])
    cp("dve", shW_bf[:, 0:512], ps[0][:, :], r=["ps0"], w=["shW"])
    cp("dve", shW_bf[:, 512:1024], ps[1][:, :], r=["ps1"], w=["shW"])

    def qk_chain(pkey, psrc, gain_rep, cos_ap, sin_ap, sidx, evac_fn, tbank):
        i = sidx
        p3 = psrc.rearrange("p (g d) -> p g d", d=64)
        act(ksq[i], psrc, AF.Square, r=[pkey], w=["ksq"])
        S.add("dve", lambda e, o=ssk[i], a=ksq[i][:, :].rearrange("p (g d) -> p g d", d=64): e.tensor_reduce(o, a, AX.X, ALU.add),
              r=["ksq"], w=["ssk%d" % i])
        rstd_from_ss(ssk[i], rk[i], 1.0 / 64.0, "ssk%d" % i, "rk%d" % i)
        tt("dve", kn[i], p3, rk[i].unsqueeze(2).broadcast_to([128, 8, 64]), ALU.mult,
           r=[pkey, "rk%d" % i], w=["kn%d" % i])
        tt("dve", kn[i], kn[i], gain_rep.unsqueeze(1).broadcast_to([128, 8, 64]), ALU.mult,
           r=["kn%d" % i, "gains"], w=["kn%d" % i])
        k3 = kb16[i][:, :].rearrange("p (g d) -> p g d", d=64)
        cp("pool", k3, kn[i], r=["kn%d" % i], w=["kb%d" % i])
        a = kn[i][:, :, 0:8]
        b = kn[i][:, :, 8:16]
        cb = cos_ap.unsqueeze(1).broadcast_to([128, 8, 8])
        sb_ = sin_ap.unsqueeze(1).broadcast_to([128, 8, 8])
        t1, t2, t3, t4 = rt[i]
        kk = "rt%d" % i
        tt("pool", t1, a, cb, ALU.mult, r=["kn%d" % i, "ropetab"], w=[kk + "a"])
        tt("pool", t2, b, sb_, ALU.mult, r=["kn%d" % i, "ropetab"], w=[kk + "b"])
        tt("pool", k3[:, :, 0:8], t1, t2, ALU.subtract, r=[kk + "a", kk + "b"], w=["kb%d" % i])
        tt("pool", t3, b, cb, ALU.mult, r=["kn%d" % i, "ropetab"], w=[kk + "c"])
        tt("pool", t4, a, sb_, ALU.mult, r=["kn%d" % i, "ropetab"], w=[kk + "d"])
        tt("pool", k3[:, :, 8:16], t3, t4, ALU.add, r=[kk + "c", kk + "d"], w=["kb%d" % i])
        pT = psb(tbank)
        tps([(pT[:, h * 128:(h + 1) * 128], kb16[i][:, h * 128:(h + 1) * 128]) for h in range(4)],
            ident_b, r=["kb%d" % i, "ident_b"], w=["ps%d" % tbank])
        evac_fn(pT[:, 0:512])

    S.add("pool", lambda e: e.memset(small[:, 60:61], 0.0), r=rb + ro, w=["ropetab"])
    S.add("pool", lambda e: e.memset(small[:, 61:62], 0.0), r=["gq_rep", "gk_rep"], w=["gains"])

    def norm_transpose(xsrc, xk, ssv, rsv, sskey, hdst, hkeys, gsc, sh, gkeys, xout=None, xoutk=None, junk=None, junkk="ps7"):
        act(junkb, xsrc, AF.Square, r=[xk], w=["junk_sb", sskey], accum=ssv)
        rstd_from_ss(ssv, rsv, 1.0 / D, sskey, sskey + "r")
        if xout is None:
            xout, xoutk = xsrc, xk
            tsc("dve", xout, xsrc, rsv, None, ALU.mult, None, r=[xk, sskey + "r"], w=[xk])
        else:
            tsc("dve", xout, xsrc, rsv, None, ALU.mult, None, r=[xk, sskey + "r"], w=[xoutk])
        for half in range(2):
            tps([(ps[half][:, kk * 128:(kk + 1) * 128], xout[:, (half * 4 + kk) * 128:(half * 4 + kk + 1) * 128])
                 for kk in range(4)], ident_f, r=[xoutk, "ident_f"], w=["ps%d" % half])
            for kk in range(4):
                k = half * 4 + kk
                src = ps[half][:, kk * 128:(kk + 1) * 128]
                if True:
                    act(hdst[:, k, :], src, AF.Identity, r=["ps%d" % half] + gkeys, w=[hkeys[k]],
                        bias=sh[:, k:k + 1], scale=gsc[:, k:k + 1])
                else:
                    tsc("dve", hdst[:, k, :], src, gsc[:, k:k + 1], sh[:, k:k + 1], ALU.mult, ALU.add,
                        r=["ps%d" % half] + gkeys, w=[hkeys[k]])

    NT_A = NBLK_B
    if stop_after == "pro":
        NT_A = 0
    if stop_after == "A4":
        NT_A = 4
    if stop_after == "C1":
        NT_A = 8
    if mini:
        NT_A = 16 * mini_ns
    import os
    PST = (0, 1)
    PSK = (2, 3, 4)
    PSV = (5, 6)
    PST2 = 7

    def a_d0(b):
        xi = b % NXT
        dma(xt[xi], xb[b * 128:(b + 1) * 128, :], r=[], w=["xt%d" % xi], tag="xt%d" % xi)

    def a_a12(b):
        xi, i2 = b % NXT, b % 2
        act(junkb, xt[xi], AF.Square, r=["xt%d" % xi], w=["junk_sb", "ssA%d" % i2], accum=ssA[i2])
        rstd_from_ss(ssA[i2], rsA[i2], 1.0 / D, "ssA%d" % i2, "rsA%d" % i2)

    def a_v1(b):
        xi, i2 = b % NXT, b % 2
        act(xnb[i2], xt[xi], AF.Identity, r=["xt%d" % xi, "rsA%d" % i2], w=["xnb%d" % i2], scale=rsA[i2])

    def a_p1(b):
        i2 = b % 2
        pT = psb(PST[i2])
        tps([(pT[:, k * 128:(k + 1) * 128], xnb[i2][:, k * 128:(k + 1) * 128]) for k in range(KC)],
            ident_b, r=["xnb%d" % i2, "ident_b"], w=["ps%d" % PST[i2]])

    def a_ev(b):
        i2 = b % 2
        pT = psb(PST[i2])
        for half in range(2):
            cp("dve", hT[i2][:, half * 4:(half + 1) * 4, :],
               pT[:, half * 512:(half + 1) * 512].rearrange("p (k q) -> p k q", q=128),
               r=["ps%d" % PST[i2]], w=["hT%d_%d" % (i2, half)])

    def a_p2(b):
        i2 = b % 2
        pK = PSK[b % 3]
        pV = PSV[i2]
        hks = ["hT%d_0" % i2, "hT%d_1" % i2]
        mms([(ps[pK][:, :], hT[i2][:, k, :], wkv[:, k, 0:512], k == 0, False) for k in range(KC)] +
            [(ps[pK][:, :], c128, shW_bf[:, 0:512], False, True)],
            r=hks + ["wkv", "shW", "c128"], w=["ps%d" % pK])
        mms([(ps[pV][:, :], hT[i2][:, k, :], wkv[:, k, 512:1024], k == 0, False) for k in range(KC)] +
            [(ps[pV][:, :], c128, shW_bf[:, 512:1024], False, True)],
            r=hks + ["wkv", "shW", "c128"], w=["ps%d" % pV])

    def a_a45(b):
        i2 = b % 2
        pK = PSK[b % 3]
        pV = PSV[i2]
        act(ksq[i2], ps[pK][:, :], AF.Square, r=["ps%d" % pK], w=["ksq%d" % i2])
        act(VA[:, b, :, 0:128], ps[pV][:, :].rearrange("p (h e) -> p h e", e=128), AF.Copy,
            r=["ps%d" % pV, "VAones"], w=[("V", b)])

    def a_v3(b):
        i2 = b % 2
        S.add("dve", lambda e, o=ssk[i2], a=ksq[i2][:, :].rearrange("p (g d) -> p g d", d=64): e.tensor_reduce(o, a, AX.X, ALU.add),
              r=["ksq%d" % i2], w=["ssk%d" % i2])

    def a_a6(b):
        i2 = b % 2
        rstd_from_ss(ssk[i2], rk[i2], 1.0 / 64.0, "ssk%d" % i2, "rk%d" % i2)

    def a_v4(b):
        i2 = b % 2
        pK = PSK[b % 3]
        p3 = ps[pK][:, :].rearrange("p (g d) -> p g d", d=64)
        tt("dve", kn[i2], p3, rk[i2].unsqueeze(2).broadcast_to([128, 8, 64]), ALU.mult,
           r=["ps%d" % pK, "rk%d" % i2], w=["kn%d" % i2])
        tt("dve", kn[i2], kn[i2], gk_rep.unsqueeze(1).broadcast_to([128, 8, 64]), ALU.mult,
           r=["kn%d" % i2, "gains"], w=["kn%d" % i2])

    def a_g1(b):
        i = b % 2
        k3 = kb16[i][:, :].rearrange("p (g d) -> p g d", d=64)
        cp("pool", k3, kn[i], r=["kn%d" % i], w=["kb%d" % i])
        a = kn[i][:, :, 0:8]
        b_ = kn[i][:, :, 8:16]
        cb = cos_b[:, b, :].unsqueeze(1).broadcast_to([128, 8, 8])
        sb_ = sin_b[:, b, :].unsqueeze(1).broadcast_to([128, 8, 8])
        t1, t2, t3, t4 = rt[i]
        kk = "rt%d" % i
        tt("pool", t1, a, cb, ALU.mult, r=["kn%d" % i, "ropetab"], w=[kk + "a"])
        tt("pool", t2, b_, sb_, ALU.mult, r=["kn%d" % i, "ropetab"], w=[kk + "b"])
        tt("pool", k3[:, :, 0:8], t1, t2, ALU.subtract, r=[kk + "a", kk + "b"], w=["kb%d" % i])
        tt("pool", t3, b_, cb, ALU.mult, r=["kn%d" % i, "ropetab"], w=[kk + "c"])
        tt("pool", t4, a, sb_, ALU.mult, r=["kn%d" % i, "ropetab"], w=[kk + "d"])
        tt("pool", k3[:, :, 8:16], t3, t4, ALU.add, r=[kk + "c", kk + "d"], w=["kb%d" % i])

    def a_p3(b):
        i = b % 2
        pT = psb(PST2)
        tps([(pT[:, h * 128:(h + 1) * 128], kb16[i][:, h * 128:(h + 1) * 128]) for h in range(4)],
            ident_b, r=["kb%d" % i, "ident_b"], w=["ps%d" % PST2])

    def a_v5(b):
        pT = psb(PST2)
        cp("dve", KT[:, :, b * 128:(b + 1) * 128], pT[:, 0:512].rearrange("p (h q) -> p h q", q=128),
           r=["ps%d" % PST2], w=[("KT", b)])

    a_order = [(a_v5, 11), (a_p3, 10), (a_g1, 9), (a_a6, 8), (a_a45, 7), (a_v4, 8), (a_p2, 6), (a_ev, 5),
               (a_p1, 4), (a_v1, 3), (a_a12, 2), (a_v3, 7), (a_d0, 0)]
    for t in range(NT_A + 12):
        for fn_, off_ in a_order:
            b = t - off_
            if 0 <= b < NT_A:
                fn_(b)


    if stop_after in ("pro", "A", "A4"):
        if os.environ.get("MK_CUT"):
            cut = int(os.environ["MK_CUT"])
            S.ops = S.ops[:cut]
            S.last_w = {k: v for k, v in S.last_w.items() if v < cut}
            S.readers = {k: {e: i for e, i in d_.items() if i < cut} for k, d_ in S.readers.items()}
            S.tag_count = {}
            for op in S.ops:
                if op["tag"] is not None:
                    S.tag_count[op["tag"]] = S.tag_count.get(op["tag"], 0) + 1
            stop_after = "pro"
        dbg["KT"] = nc.dram_tensor("dbg_KT", [128, 4 * SEQ], BF16, kind="ExternalOutput").ap()
        dbg["VA"] = nc.dram_tensor("dbg_VA", [128, NBLK_B * 4 * 130], BF16, kind="ExternalOutput").ap()
        dbg["misc"] = nc.dram_tensor("dbg_misc", [128, 64], F32, kind="ExternalOutput").ap()
        allk = [("KT", b) for b in range(NT_A)] + [("V", b) for b in range(NT_A)]
        if stop_after in ("A", "A4"):
            S.add("dve", lambda e: e.memset(KT[:, :, NT_A * 128:], 0.0), r=[], w=["ktpad"])
            S.add("dve", lambda e: e.memset(VA[:, NT_A:, :, 0:128], 0.0), r=[], w=["vapad"])
            allk = allk + ["ktpad", "vapad"]
            dma(dbg["KT"], KT[:, :, :].rearrange("p a b -> p (a b)"), r=allk, w=[], tag="out0")
            dma(dbg["VA"], VA[:, :, :, :].rearrange("p a b c -> p (a b c)"), r=allk + ["VAones"], w=[], tag="out1")
        cp("dve", small[:, 0:8], sh_a, r=["adacols", "junk64", "neglam"], w=["small"])
        cp("dve", small[:, 8:16], gsc_a, r=["gsc_a"], w=["small"])
        cp("dve", small[:, 16:24], cos_b[:, 0, :], r=rb, w=["small"])
        cp("dve", small[:, 24:32], sin_b[:, 63, :], r=rb, w=["small"])
        cp("dve", small[:, 32:40], lam_t, r=["neglam"], w=["small"])
        dma(dbg["misc"][:, 0:40], small[:, 0:40], r=["small"], w=[], tag="out2")
        S.emit()
        return nc

    bar_tile = small[:, 62:63]
    S.barrier(bar_tile)
    A.reset(WORK)
    xq = [A.alloc([128, D]) for _ in range(2)]
    hTq = [A.alloc([128, KC, 128], BF16) for _ in range(2)]
    wq = A.alloc([128, KC, 512], BF16)
    Qbd = [A.alloc([128, 4, 2, 128], BF16) for _ in range(2)]
    NPT = 6
    PT = [A.alloc([128, 512], BF16) for _ in range(NPT)]
    ssQ = [A.alloc([128, 1]) for _ in range(2)]
    rsQ = [A.alloc([128, 1]) for _ in range(2)]
    ksq = [A.alloc([128, 512])] * 2
    kn = [A.alloc([128, 8, 64]) for _ in range(2)]
    kb16 = [A.alloc([128, 512], BF16) for _ in range(2)]
    ssk = [A.alloc([128, 8]) for _ in range(2)]
    rk = [A.alloc([128, 8]) for _ in range(2)]
    rt = [[A.alloc([128, 8, 8]) for _ in range(4)] for _ in range(2)]
    ot = [A.alloc([128, 128]) for _ in range(2)]
    ot2 = [A.alloc([128, 128]) for _ in range(2)]
    ob16 = [A.alloc([128, 128], BF16) for _ in range(2)]
    eps_ = [A.alloc([128, 8]) for _ in range(2)]
    pass

    for bq in range(2):
        S.add("pool", lambda e, q=Qbd[bq]: e.memset(q, 0.0), r=[], w=["Qbd%d" % bq])
    for pc in range(4):
        xi = pc % 2
        st = xq[xi][:, :].rearrange("p (k n) -> p k n", k=KC)
        dma(st, w_in_v[:, :, pc * 128:(pc + 1) * 128], r=[], w=["xq%d" % xi], tag="xq%d" % xi)
        cp("pool", wq[:, :, pc * 128:(pc + 1) * 128], st, r=["xq%d" % xi], w=["wq"])

    def q_stage(m, stage):
        bq = m % 2
        xk = "xq%d" % bq
        hks = ["hTq%d_%d" % (bq, k) for k in range(KC)]
        if stage == 0:
            dma(xq[bq], xo[m * 128:(m + 1) * 128, :], r=[], w=[xk], tag=xk)
            act(junkb, xq[bq], AF.Square, r=[xk], w=["junk_sb", "ssQ%d" % bq], accum=ssQ[bq])
            rstd_from_ss(ssQ[bq], rsQ[bq], 1.0 / D, "ssQ%d" % bq, "ssQ%dr" % bq)
            tsc("dve", xq[bq], xq[bq], rsQ[bq], None, ALU.mult, None, r=[xk, "ssQ%dr" % bq], w=[xk])
        elif stage == 1:
            for half in range(2):
                tps([(ps[half][:, kk * 128:(kk + 1) * 128], xq[bq][:, (half * 4 + kk) * 128:(half * 4 + kk + 1) * 128])
                     for kk in range(4)], ident_f, r=[xk, "ident_f"], w=["ps%d" % half])
            for half in range(2):
                for kk in range(4):
                    k = half * 4 + kk
                    src_ = ps[half][:, kk * 128:(kk + 1) * 128]
                    act(hTq[bq][:, k, :], src_, AF.Identity, r=["ps%d" % half, "gsc_a", "adacols"], w=[hks[k]],
                        bias=sh_a[:, k:k + 1], scale=gsc_a[:, k:k + 1])
        elif stage == 2:
            mms([(ps[0][:, :], hTq[bq][:, k, :], wq[:, k, :], k == 0, k == KC - 1) for k in range(KC)],
                r=hks + ["wq"], w=["ps0"])
            qk_front("ps0", ps[0][:, :], gq_rep, cos_o[:, m, :], sin_o[:, m, :], bq)
        else:
            pT = psb(1)
            tps([(pT[:, h * 128:(h + 1) * 128], kb16[bq][:, h * 128:(h + 1) * 128]) for h in range(4)],
                ident_b, r=["kb%d" % bq, "ident_b"], w=["ps1"])
            p3 = pT[:, 0:512].rearrange("p (h q) -> p h q", q=128)
            cp("dve", Qbd[bq][0:64, :, 0, :], p3[0:64], r=["ps1"], w=["Qbd%d" % bq])
            cp("dve", Qbd[bq][64:128, :, 1, :], p3[64:128], r=["ps1"], w=["Qbd%d" % bq])

    def qk_front(pkey, psrc, gain_rep, cos_ap, sin_ap, i):
        p3 = psrc.rearrange("p (g d) -> p g d", d=64)
        act(ksq[i], psrc, AF.Square, r=[pkey], w=["ksq"])
        S.add("dve", lambda e, o=ssk[i], a=ksq[i][:, :].rearrange("p (g d) -> p g d", d=64): e.tensor_reduce(o, a, AX.X, ALU.add),
              r=["ksq"], w=["ssk%d" % i])
        rstd_from_ss(ssk[i], rk[i], 1.0 / 64.0, "ssk%d" % i, "rk%d" % i)
        tt("dve", kn[i], p3, rk[i].unsqueeze(2).broadcast_to([128, 8, 64]), ALU.mult,
           r=[pkey, "rk%d" % i], w=["kn%d" % i])
        tt("dve", kn[i], kn[i], gain_rep.unsqueeze(1).broadcast_to([128, 8, 64]), ALU.mult,
           r=["kn%d" % i, "gains"], w=["kn%d" % i])
        k3 = kb16[i][:, :].rearrange("p (g d) -> p g d", d=64)
        cp("pool", k3, kn[i], r=["kn%d" % i], w=["kb%d" % i])
        a = kn[i][:, :, 0:8]
        b = kn[i][:, :, 8:16]
        cb = cos_ap.unsqueeze(1).broadcast_to([128, 8, 8])
        sb_ = sin_ap.unsqueeze(1).broadcast_to([128, 8, 8])
        t1, t2, t3, t4 = rt[i]
        kk = "rt%d" % i
        tt("pool", t1, a, cb, ALU.mult, r=["kn%d" % i, "ropetab"], w=[kk + "a"])
        tt("pool", t2, b, sb_, ALU.mult, r=["kn%d" % i, "ropetab"], w=[kk + "b"])
        tt("pool", k3[:, :, 0:8], t1, t2, ALU.subtract, r=[kk + "a", kk + "b"], w=["kb%d" % i])
        tt("pool", t3, b, cb, ALU.mult, r=["kn%d" % i, "ropetab"], w=[kk + "c"])
        tt("pool", t4, a, sb_, ALU.mult, r=["kn%d" % i, "ropetab"], w=[kk + "d"])
        tt("pool", k3[:, :, 8:16], t3, t4, ALU.add, r=[kk + "c", kk + "d"], w=["kb%d" % i])

    cm3 = cmask[:, :].rearrange("p (t x) -> p t x", t=4)
    NM = NOWN if stop_after != "C1" else 2
    if mini:
        NM = 4 * mini_ns
    items = []
    for m in range(NM):
        for h in range(4):
            ngrp = (4 * m + 4) // 2
            for g in range(ngrp):
                items.append((m, h, g, ngrp))
    SBK = (2, 3, 4)
    pending = []
    LOOK = 2

    def emit_S(idx):
        m, h, g, ngrp = items[idx]
        bq = m % 2
        sbk = SBK[idx % 3]
        kbs = (2 * g, 2 * g + 1)
        mms([(ps[sbk][:, i * 256:(i + 1) * 256], KT[:, h, kb * 128:(kb + 1) * 128],
              Qbd[bq][:, h, :, :], True, True) for i, kb in enumerate(kbs)],
            r=[("KT", kbs[0]), ("KT", kbs[1]), "Qbd%d" % bq], w=["ps%d" % sbk])

    def emit_rest(idx):
        m, h, g, ngrp = items[idx]
        sbk = SBK[idx % 3]
        pti = idx % NPT
        ep = (m * 4 + h) % 2
        kbs = (2 * g, 2 * g + 1)
        act(PT[pti], ps[sbk][:, :], AF.Exp, r=["ps%d" % sbk, "m8c"], w=["PT%d" % pti],
            bias=m8c[:, 0:1], scale=0.125)
        if g >= ngrp - 2:
            gb = g - (ngrp - 2)
            tt("pool", PT[pti], PT[pti], cm3[:, 2 * gb:2 * gb + 2, :], ALU.mult,
               r=["PT%d" % pti, "cmask"], w=["PT%d" % pti])
        its = []
        for i, kb in enumerate(kbs):
            for c in range(2):
                its.append((ps[5 + c][:, 0:129], PT[pti][:, i * 256 + c * 128:i * 256 + (c + 1) * 128],
                            VA[:, kb, h, 0:129], (g == 0 and i == 0), (g == ngrp - 1 and i == 1)))
        mms(its, r=["PT%d" % pti, ("V", kbs[0]), ("V", kbs[1])], w=["ps5", "ps6"])
        if g == ngrp - 1:
            epilogue_a(m, h, ep)
            pending.append((idx + 3, lambda m=m, h=h, ep=ep: epilogue_b(m, h, ep)))

    def epilogue_a(m, h, ep):
        e_ = eps_[ep]
        ek = "eps%d" % ep
        act(e_[:, 5:6], ps[5][:, 128:129], AF.Copy, r=["ps5"], w=[ek])
        act(e_[:, 6:7], ps[6][:, 128:129], AF.Copy, r=["ps6"], w=[ek])
        S.add("dve", lambda e, e_=e_: e.reciprocal(e_[:, 0:2], e_[:, 5:7]), r=[ek], w=[ek])
        tt("dve", e_[:, 2:3], e_[:, 1:2], neglam, ALU.mult, r=[ek, "neglam"], w=[ek])
        act(ot[ep], ps[5][:, 0:128], AF.Identity, r=["ps5", ek], w=["ot%d" % ep], scale=e_[:, 0:1])
        act(ot2[ep], ps[6][:, 0:128], AF.Identity, r=["ps6", ek], w=["ot2%d" % ep], scale=e_[:, 2:3])
        tt("dve", ot[ep], ot[ep], ot2[ep], ALU.add, r=["ot%d" % ep, "ot2%d" % ep], w=["ot%d" % ep])
        act(ot2[ep], ot[ep], AF.Square, r=["ot%d" % ep, "ot2%d" % ep], w=["ot2%d" % ep, ek + "s"], accum=e_[:, 3:4])
        rstd_from_ss(e_[:, 3:4], e_[:, 4:5], 1.0 / 128.0, ek + "s", ek + "r")
        act(ot2[ep], ot[ep], AF.Identity, r=["ot%d" % ep, ek + "r", "ot2%d" % ep], w=["ot2%d" % ep], scale=e_[:, 4:5])
        tt("dve", ob16[ep], ot2[ep], sg_rep, ALU.mult, r=["ot2%d" % ep, "sg_rep"], w=["ob%d" % ep])

    def epilogue_b(m, h, ep):
        tps([(psb(7)[:, ep * 128:(ep + 1) * 128], ob16[ep])], ident_b, r=["ob%d" % ep, "ident_b"], w=["ps7"])
        cp("dve", attnT[:, h, m * 128:(m + 1) * 128], psb(7)[:, ep * 128:(ep + 1) * 128], r=["ps7"], w=[("attnT", m, h)])

    for st_ in range(4):
        q_stage(0, st_)
    for i in range(min(LOOK, len(items))):
        emit_S(i)
    for idx in range(len(items)):
        m, h, g, ngrp = items[idx]
        if g == 0 and m + 1 < NM:
            q_stage(m + 1, h)
        if idx + LOOK < len(items):
            emit_S(idx + LOOK)
        while pending and pending[0][0] <= idx:
            pending.pop(0)[1]()
        emit_rest(idx)
    while pending:
        pending.pop(0)[1]()

    if stop_after in ("C", "C1"):
        dbg["attnT"] = nc.dram_tensor("dbg_attnT", [128, 4 * NOWN * 128], BF16, kind="ExternalOutput").ap()
        if NM < NOWN:
            S.add("dve", lambda e: e.memset(attnT[:, :, NM * 128:], 0.0), r=[], w=["attnpad"])
        allk = [("attnT", m, h) for m in range(NM) for h in range(4)] + ["attnpad"]
        dma(dbg["attnT"], attnT[:, :, :].rearrange("p a b -> p (a b)"), r=allk, w=[], tag="out0")
        S.emit()
        return nc

    S.barrier(bar_tile)
    A.reset(KV0)
    X = A.alloc([128, NOWN, D])
    wglu = A.alloc([128, KC, 1024], BF16)
    woutp = A.alloc([128, KC, 1024], BF16)
    D1W = A.mark()
    crep = A.alloc([128, KC, 128])
    abufs = ada_alloc()
    gta_rep = A.alloc([128, D])
    stg = [A.alloc([128, KC, 128]) for _ in range(2)]
    for s4 in range(4):
        S.bulk_tags.add("Xload%d" % s4)
        for i in range(4):
            m = 4 * s4 + i
            dma(X[:, m, :], xo[m * 128:(m + 1) * 128, :], r=[], w=[("X", m)], tag="Xload%d" % s4)
    cp("dve", crep, cact.unsqueeze(2).broadcast_to([128, KC, 128]), r=["cact"], w=["crep"])
    for g in (4, 5):
        ada_group(g, abufs[g % 2], 7 - 2 * (g % 2))
        cp("dve", gta_rep[:, (g - 4) * 512:(g - 3) * 512], abufs[g % 2][2], r=["ada_modrep%d" % (g % 2)], w=["gta_rep"])
    for pc in range(8):
        xi = pc % 2
        dma(stg[xi], w_in_v[:, :, 1536 + pc * 128:1536 + (pc + 1) * 128], r=[], w=["stg%d" % xi], tag="stg%d" % xi)
        if pc % 2 == 0:
            act(wglu[:, :, pc * 128:(pc + 1) * 128], stg[xi], AF.Copy, r=["stg%d" % xi], w=["wglu"])
        else:
            cp("pool", wglu[:, :, pc * 128:(pc + 1) * 128], stg[xi], r=["stg%d" % xi], w=["wglu"])
    w_out_v = w_out.rearrange("(k p) n -> p k n", p=128)
    for pc in range(8):
        xi = pc % 2
        dma(stg[xi], w_out_v[:, :, pc * 128:(pc + 1) * 128], r=[], w=["stg%d" % xi], tag="stg%d" % xi)
        tt("dve" if pc % 2 == 0 else "pool", woutp[:, :, pc * 128:(pc + 1) * 128], stg[xi],
           gta_rep[:, pc * 128:(pc + 1) * 128].unsqueeze(1).broadcast_to([128, KC, 128]), ALU.mult,
           r=["stg%d" % xi, "gta_rep"], w=["woutp"])
    S.barrier(bar_tile)
    A.reset(D1W)
    xhb = A.alloc([128, D])
    xn = [A.alloc([128, D]) for _ in range(2)]
    hT5 = A.alloc([128, KC, 640], BF16)
    uT = A.alloc([128, 4, 4, 160], BF16)
    sig = [A.alloc([128, 512]) for _ in range(2)]
    sigh = [A.alloc([128, 128]) for _ in range(2)]
    asb = [A.alloc([128, 512]) for _ in range(2)]
    asbh = [A.alloc([128, 128]) for _ in range(2)]
    yv = A.alloc([128, 4, 512])
    ysq = A.alloc([128, 4, 512])
    mean_sb = A.alloc([128, 512])
    m2 = A.alloc([128, 512])
    rstd_bc = A.alloc([128, 512])
    tmpv = [A.alloc([128, 512]) for _ in range(2)]
    convT = A.alloc([128, 4, 512], BF16)
    dg = [A.alloc([128, 128], BF16) for _ in range(8)]
    ssD = [A.alloc([128, 1]) for _ in range(2)]
    rsD = [A.alloc([128, 1]) for _ in range(2)]
    pass
    dgc = {"n": 0}

    NS = 4 if not mini else mini_ns
    hT5b = [hT5, A.alloc([128, KC, 640], BF16)]

    def d1_front(s4, i):
        bi = i % 2
        if i == 4:
            dma(xhb, xh[s4 * 128:(s4 + 1) * 128, :], r=[], w=["xhb"], tag="xhb")
        src, sk = (X[:, 4 * s4 + i, :], ("X", 4 * s4 + i)) if i < 4 else (xhb, "xhb")
        act(junkb, src, AF.Square, r=[sk], w=["junk_sb", "ssD%d" % bi], accum=ssD[bi])
        rstd_from_ss(ssD[bi], rsD[bi], 1.0 / D, "ssD%d" % bi, "ssD%dr" % bi)
        tsc("dve", xn[bi], src, rsD[bi], None, ALU.mult, None, r=[sk, "ssD%dr" % bi], w=["xn%d" % bi])

    def d1_back(s4, i):
        bi = i % 2
        hd = hT5b[s4 % 2][:, :, i * 128:(i + 1) * 128]
        for half in range(2):
            tps([(ps[half][:, kk * 128:(kk + 1) * 128], xn[bi][:, (half * 4 + kk) * 128:(half * 4 + kk + 1) * 128])
                 for kk in range(4)], ident_f, r=["xn%d" % bi, "ident_f"], w=["ps%d" % half])
            for kk in range(4):
                k = half * 4 + kk
                act(hd[:, k, :], ps[half][:, kk * 128:(kk + 1) * 128], AF.Identity,
                    r=["ps%d" % half, "gsc_a", "adacols"], w=["hT5_%d_%d_%d" % (s4 % 2, i, k)],
                    bias=sh_a[:, k:k + 1], scale=gsc_a[:, k:k + 1])

    for i in range(5):
        d1_front(0, i)
        d1_back(0, i)
    def d1_glu(s4):
        hT5 = hT5b[s4 % 2]
        nxt = s4 + 1 < NS
        allh = ["hT5_%d_%d_%d" % (s4 % 2, i, k) for i in range(5) for k in range(KC)]
        for cc in range(4):
            pb = cc % 2
            ba_, bg_ = bgluT[:, cc:cc + 1], bgluT[:, 4 + cc:5 + cc]
            pa, pg, ph = 2 + 3 * pb, 3 + 3 * pb, 4 + 3 * pb
            mms([(ps[pa][:, :], wglu[:, k, cc * 128:(cc + 1) * 128], hT5[:, k, 0:512], k == 0, k == KC - 1)
                 for k in range(KC)], r=allh + ["wglu"], w=["ps%d" % pa])
            mms([(ps[pg][:, :], wglu[:, k, 512 + cc * 128:512 + (cc + 1) * 128], hT5[:, k, 0:512], k == 0, k == KC - 1)
                 for k in range(KC)], r=allh + ["wglu"], w=["ps%d" % pg])
            mms([(ps[ph][:, 0:128], wglu[:, k, cc * 128:(cc + 1) * 128], hT5[:, k, 512:640], k == 0, k == KC - 1)
                 for k in range(KC)] +
                [(ps[ph][:, 128:256], wglu[:, k, 512 + cc * 128:512 + (cc + 1) * 128], hT5[:, k, 512:640], k == 0, k == KC - 1)
                 for k in range(KC)], r=allh + ["wglu"], w=["ps%d" % ph])
            act(sig[pb], ps[pg][:, :], AF.Sigmoid, r=["ps%d" % pg, "bgluT"], w=["sig%d" % pb], bias=bg_)
            act(sigh[pb], ps[ph][:, 128:256], AF.Sigmoid, r=["ps%d" % ph, "bgluT"], w=["sigh%d" % pb], bias=bg_)
            act(asb[pb], ps[pa][:, :], AF.Identity, r=["ps%d" % pa, "bgluT"], w=["asb%d" % pb], bias=ba_)
            act(asbh[pb], ps[ph][:, 0:128], AF.Identity, r=["ps%d" % ph, "bgluT"], w=["asbh%d" % pb], bias=ba_)
            tt("dve", uT[:, cc, :, 32:160], asb[pb][:, :].rearrange("p (i t) -> p i t", i=4),
               sig[pb][:, :].rearrange("p (i t) -> p i t", i=4), ALU.mult,
               r=["asb%d" % pb, "sig%d" % pb], w=["uT%d" % cc])
            tt("dve", uT[:, cc, :, 0:32], asbh[pb][:, :].rearrange("p (i t) -> p i t", i=4),
               sigh[pb][:, :].rearrange("p (i t) -> p i t", i=4), ALU.mult,
               r=["asbh%d" % pb, "sigh%d" % pb], w=["uT%d" % cc])
            if s4 == 0:
                tsc("dve", uT[:, cc, 0, 0:32], uT[:, cc, 0, 0:32], hmask[:, 0:1], None, ALU.mult, None,
                    r=["uT%d" % cc, "hmask"], w=["uT%d" % cc])

    def d1_conv(s4):
        hT5 = hT5b[s4 % 2]
        nxt = s4 + 1 < NS
        allh = ["hT5_%d_%d_%d" % (s4 % 2, i, k) for i in range(5) for k in range(KC)]
        if nxt:
            d1_front(s4 + 1, 0)
        for cc in range(4):
            cb_ = 2 + 3 * (cc % 2)
            for k in range(31):
                sl = dgc["n"] % 8
                dgc["n"] += 1
                tsc("dve", dg[sl], ident_b, wdwT[:, cc, k:k + 1], None, ALU.mult, None,
                    r=["ident_b", "wdwT"], w=["dg%d" % sl])
                mms([(ps[cb_][:, :], dg[sl], uT[:, cc, :, 2 + k:130 + k], k == 0, k == 30)],
                    r=["dg%d" % sl, "uT%d" % cc], w=["ps%d" % cb_])
            act(yv[:, cc, :], ps[cb_][:, :], AF.Identity, r=["ps%d" % cb_, "bdwT"], w=["yv%d" % cc], bias=bdwT[:, cc:cc + 1])
            act(ysq[:, cc, :], yv[:, cc, :], AF.Square, r=["yv%d" % cc], w=["ysq%d" % cc])
            if nxt:
                d1_back(s4 + 1, cc)
                d1_front(s4 + 1, cc + 1)

    def d1_ln(s4):
        hT5 = hT5b[s4 % 2]
        nxt = s4 + 1 < NS
        allh = ["hT5_%d_%d_%d" % (s4 % 2, i, k) for i in range(5) for k in range(KC)]
        mms([(ps[3][:, :], ones_ln, yv[:, cc, :], cc == 0, cc == 3) for cc in range(4)],
            r=["yv%d" % cc for cc in range(4)] + ["ones_ln"], w=["ps3"])
        mms([(ps[4][:, :], ones_ln, ysq[:, cc, :], cc == 0, cc == 3) for cc in range(4)],
            r=["ysq%d" % cc for cc in range(4)] + ["ones_ln"], w=["ps4"])
        if nxt:
            d1_back(s4 + 1, 4)
        act(mean_sb, ps[3][:, :], AF.Copy, r=["ps3"], w=["mean_sb"])
        tt("dve", m2, mean_sb, mean_sb, ALU.mult, r=["mean_sb"], w=["m2"])
        tt("dve", m2, ps[4][:, :], m2, ALU.subtract, r=["ps4", "m2"], w=["m2"])
        rstd_from_ss(m2, rstd_bc, 1.0, "m2", "rstd_bc")
        for cc in range(4):
            tb = cc % 2
            tt("dve", tmpv[tb], yv[:, cc, :], mean_sb, ALU.subtract, r=["yv%d" % cc, "mean_sb"], w=["tmpv%d" % tb])
            tt("dve", tmpv[tb], tmpv[tb], rstd_bc, ALU.mult, r=["tmpv%d" % tb, "rstd_bc"], w=["tmpv%d" % tb])
            act(convT[:, cc, :], tmpv[tb], AF.Silu, r=["tmpv%d" % tb, "lngT", "lnbT"], w=["convT%d" % cc],
                bias=lnbT[:, cc:cc + 1], scale=lngT[:, cc:cc + 1])

    def d1_out(s4):
        hT5 = hT5b[s4 % 2]
        nxt = s4 + 1 < NS
        allh = ["hT5_%d_%d_%d" % (s4 % 2, i, k) for i in range(5) for k in range(KC)]
        for i in range(4):
            m = 4 * s4 + i
            for half in range(2):
                ob_ = 5 + (i * 2 + half) % 3
                items = []
                for k in range(8):
                    lh = attnT[:, k, m * 128:(m + 1) * 128] if k < 4 else convT[:, k - 4, i * 128:(i + 1) * 128]
                    items.append((ps[ob_][:, :], lh, woutp[:, k, half * 512:(half + 1) * 512], k == 0, k == 7))
                mms(items, r=[("attnT", m, h) for h in range(4)] + ["convT%d" % c_ for c_ in range(4)] + ["woutp"],
                    w=["ps%d" % ob_])
                tt("dve", X[:, m, half * 512:(half + 1) * 512], ps[ob_][:, :], X[:, m, half * 512:(half + 1) * 512],
                   ALU.add, r=["ps%d" % ob_, ("X", m)], w=[("X", m)])


    d1_glu(0)
    for s4 in range(NS):
        d1_conv(s4)
        d1_ln(s4)
        if s4 + 1 < NS:
            d1_glu(s4 + 1)
        d1_out(s4)

    if stop_after in ("D1", "D1a"):
        dbg["X"] = nc.dram_tensor("dbg_X", [NOWN * 128, D], F32, kind="ExternalOutput").ap()
        for m in range(4 * NS):
            dma(dbg["X"][m * 128:(m + 1) * 128, :], X[:, m, :], r=[("X", m)], w=[], tag="outX")
        S.emit()
        return nc

    S.barrier(bar_tile)
    A.reset(D1W - 8192)
    h2T = A.alloc([128, KC, NOWN * 128], BF16)
    cmb = A.alloc([128, NOWN, 32])
    gtf_rep = A.alloc([128, D])
    D2W = A.mark()
    crep = A.alloc([128, KC, 128])
    abufs = ada_alloc()
    xn = [A.alloc([128, D]) for _ in range(2)]
    h2f = [A.alloc([128, KC, 128]) for _ in range(2)]
    rs_ = [A.alloc([128, 128]) for _ in range(2)]
    ssD = [A.alloc([128, 1]) for _ in range(2)]
    rsD = [A.alloc([128, 1]) for _ in range(2)]
    cp("dve", crep, cact.unsqueeze(2).broadcast_to([128, KC, 128]), r=["cact"], w=["crep"])
    for g in range(6, 10):
        ada_group(g, abufs[g % 2], 7 - 2 * (g % 2))
        ada_to_cols(abufs[g % 2], sh_f if g < 8 else sc_f, (g % 2) * 4, 6 - 2 * (g % 2))
    tsc("dve", gsc_f, sc_f, 1.0, None, ALU.add, None, r=["adacols"], w=["gsc_f"])
    tt("dve", gsc_f, gsc_f, gffnT, ALU.mult, r=["gsc_f", "gffnT"], w=["gsc_f"])
    for g in (10, 11):
        ada_group(g, abufs[g % 2], 7 - 2 * (g % 2))
        cp("dve", gtf_rep[:, (g - 10) * 512:(g - 9) * 512], abufs[g % 2][2], r=["ada_modrep%d" % (g % 2)], w=["gtf_rep"])

    NB2 = NOWN if not mini else 4
    NRS = 5
    rs_ = rs_ + [A.alloc([128, 128]) for _ in range(NRS - 2)]

    def rviews(m):
        R = rs_[m % NRS]
        v = dict(lg=R[:, 0:36], gmax=R[:, 36:37], ngmax=R[:, 37:38], gsum=R[:, 38:39], gp=R[:, 39:40],
                 ohg=R[:, 40:44], junk4=R[:, 44:48], esel=R[:, 48:80], ein=R[:, 80:88], top8=R[:, 88:96],
                 eq2=R[:, 112:120], cmb8=R[:, 120:128])
        for i_, nm in enumerate(("dcol", "ed", "den", "w1", "w2", "w1g", "w2g")):
            v[nm] = R[:, 96 + i_:97 + i_]
        return v, "rs%d" % (m % NRS)

    def d_n1(m):
        bi = m % 2
        act(junkb, X[:, m, :], AF.Square, r=[("X", m)], w=["junk_sb", "ssD%d" % bi], accum=ssD[bi])
        rstd_from_ss(ssD[bi], rsD[bi], 1.0 / D, "ssD%d" % bi, "ssD%dr" % bi)

    def d_n2(m):
        bi = m % 2
        tsc("dve", xn[bi], X[:, m, :], rsD[bi], None, ALU.mult, None, r=[("X", m), "ssD%dr" % bi], w=["xn%d" % bi])

    def d_n3(m):
        bi = m % 2
        for half in range(2):
            tps([(ps[half][:, kk * 128:(kk + 1) * 128], xn[bi][:, (half * 4 + kk) * 128:(half * 4 + kk + 1) * 128])
                 for kk in range(4)], ident_f, r=["xn%d" % bi, "ident_f"], w=["ps%d" % half])

    def d_n4(m):
        bi = m % 2
        for half in range(2):
            for kk in range(4):
                k = half * 4 + kk
                act(h2f[bi][:, k, :], ps[half][:, kk * 128:(kk + 1) * 128], AF.Identity,
                    r=["ps%d" % half, "gsc_f", "adacols"], w=["h2f%d_%d" % (bi, k)],
                    bias=sh_f[:, k:k + 1], scale=gsc_f[:, k:k + 1])

    def d_n5(m):
        bi = m % 2
        hks = ["h2f%d_%d" % (bi, k) for k in range(KC)]
        cp("pool", h2T[:, :, m * 128:(m + 1) * 128], h2f[bi], r=hks, w=[("h2T", m)])
        mms([(ps[2][:, 0:36], h2f[bi][:, k, :], wr[:, k, :], k == 0, k == KC - 1) for k in range(KC)],
            r=hks + ["wr"], w=["ps2"])

    def d_cA(m):
        v, rk_ = rviews(m)
        tt("dve", v["lg"], ps[2][:, 0:36], br_rep, ALU.add, r=["ps2", "br_rep"], w=[rk_])
        S.add("dve", lambda e, gmax=v["gmax"], lg=v["lg"]: e.tensor_reduce(gmax, lg[:, 0:4], AX.X, ALU.max), r=[rk_], w=[rk_])
        tsc("dve", v["ngmax"], v["gmax"], -1.0, None, ALU.mult, None, r=[rk_], w=[rk_])

    def d_cB(m):
        v, rk_ = rviews(m)
        act(v["junk4"], v["lg"][:, 0:4], AF.Exp, r=[rk_], w=[rk_], bias=v["ngmax"], scale=1.0, accum=v["gsum"])

    def d_cC(m):
        v, rk_ = rviews(m)
        lg, ohg, esel, ein, top8 = v["lg"], v["ohg"], v["esel"], v["ein"], v["top8"]
        S.add("dve", lambda e, gp=v["gp"], gsum=v["gsum"]: e.reciprocal(gp, gsum), r=[rk_], w=[rk_])
        tsc("dve", ohg, lg[:, 0:4], v["gmax"], None, ALU.is_ge, None, r=[rk_], w=[rk_])
        tt("dve", esel.rearrange("p (g e) -> p g e", g=4), lg[:, 4:36].rearrange("p (g e) -> p g e", g=4),
           ohg.unsqueeze(2).broadcast_to([128, 4, 8]), ALU.mult, r=[rk_], w=[rk_])
        S.add("dve", lambda e, ein=ein, esel=esel: e.tensor_reduce(ein, esel.rearrange("p (g e) -> p e g", g=4), AX.X, ALU.add),
              r=[rk_], w=[rk_])
        S.add("dve", lambda e, top8=top8, ein=ein: e.max(top8, ein), r=[rk_], w=[rk_])
        tt("dve", v["dcol"], top8[:, 1:2], top8[:, 0:1], ALU.subtract, r=[rk_], w=[rk_])

    def d_cD(m):
        v, rk_ = rviews(m)
        act(v["ed"], v["dcol"], AF.Exp, r=[rk_], w=[rk_])

    def d_cE(m):
        v, rk_ = rviews(m)
        ed, den, w1, w2, w1g, w2g, gp = v["ed"], v["den"], v["w1"], v["w2"], v["w1g"], v["w2g"], v["gp"]
        ein, top8, cmb8, eq2, ohg = v["ein"], v["top8"], v["cmb8"], v["eq2"], v["ohg"]
        tsc("dve", den, ed, 1.0, None, ALU.add, None, r=[rk_], w=[rk_])
        S.add("dve", lambda e, w1=w1, den=den: e.reciprocal(w1, den), r=[rk_], w=[rk_])
        tt("dve", w2, ed, w1, ALU.mult, r=[rk_], w=[rk_])
        tt("dve", w1g, w1, gp, ALU.mult, r=[rk_], w=[rk_])
        tt("dve", w2g, w2, gp, ALU.mult, r=[rk_], w=[rk_])
        tsc("dve", cmb8, ein, top8[:, 0:1], None, ALU.is_equal, None, r=[rk_], w=[rk_])
        tsc("dve", cmb8, cmb8, w1g, None, ALU.mult, None, r=[rk_], w=[rk_])
        tsc("dve", eq2, ein, top8[:, 1:2], None, ALU.is_equal, None, r=[rk_], w=[rk_])
        tsc("dve", eq2, eq2, w2g, None, ALU.mult, None, r=[rk_], w=[rk_])
        tt("dve", cmb8, cmb8, eq2, ALU.add, r=[rk_], w=[rk_])
        tt("dve", cmb[:, m, :].rearrange("p (g e) -> p g e", g=4), ohg.unsqueeze(2).broadcast_to([128, 4, 8]),
           cmb8.unsqueeze(1).broadcast_to([128, 4, 8]), ALU.mult, r=[rk_], w=[("cmb", m)])

    d_order = [(d_cE, 9), (d_cD, 8), (d_cC, 7), (d_cB, 6), (d_cA, 5), (d_n5, 4), (d_n4, 3), (d_n3, 2), (d_n2, 1), (d_n1, 0)]
    for t in range(NB2 + 9):
        for fn_, off_ in d_order:
            m = t - off_
            if 0 <= m < NB2:
                fn_(m)

    if stop_after == "D2":
        dbg["cmb"] = nc.dram_tensor("dbg_cmb", [128, NOWN * 32], F32, kind="ExternalOutput").ap()
        dbg["h2T"] = nc.dram_tensor("dbg_h2T", [128, KC * NOWN * 128], BF16, kind="ExternalOutput").ap()
        if mini:
            S.add("dve", lambda e: e.memset(cmb[:, NB2:, :], 0.0), r=[], w=["cmbpad"])
            S.add("dve", lambda e: e.memset(h2T[:, :, NB2 * 128:], 0.0), r=[], w=["h2pad"])
        dma(dbg["cmb"], cmb[:, :, :].rearrange("p a b -> p (a b)"), r=[("cmb", m) for m in range(NB2)] + ["cmbpad"], w=[], tag="out0")
        dma(dbg["h2T"], h2T[:, :, :].rearrange("p a b -> p (a b)"), r=[("h2T", m) for m in range(NB2)] + ["h2pad"], w=[], tag="out1")
        S.emit()
        return nc

    S.barrier(bar_tile)
    A.reset(D2W)
    Wg = [A.alloc([128, KC, 256], BF16) for _ in range(2)]
    Wu = [A.alloc([128, KC, 256], BF16) for _ in range(2)]
    Wd = [A.alloc([128, 2, D], BF16) for _ in range(2)]
    stE = [A.alloc([128, 2048]) for _ in range(4)]
    hid = [A.alloc([128, 2, 512], BF16) for _ in range(2)]
    sgl = [A.alloc([128, 512], BF16) for _ in range(2)]
    tacc = [A.alloc([128, 512]) for _ in range(2)]
    pass
    stc = {"n": 0}

    def load_expert(e_):
        eb = e_ % 2
        for which in range(3):
            sl = stc["n"] % 4
            stc["n"] += 1
            sk = "stE%d" % sl
            if which < 2:
                srcw = (w_gate if which == 0 else w_up)[e_].rearrange("(k p) f -> p k f", p=128)
                stv = stE[sl][:, :].rearrange("p (k f) -> p k f", k=KC)
                dma(stv, srcw, r=[], w=[sk], tag=sk)
                if which == 0:
                    cp("pool", Wg[eb], stv, r=[sk], w=["Wg%d" % eb])
                else:
                    act(Wu[eb], stv, AF.Copy, r=[sk], w=["Wu%d" % eb])
            else:
                srcw = w_down[e_].rearrange("(c p) n -> p c n", p=128)
                stv = stE[sl][:, :].rearrange("p (c n) -> p c n", c=2)
                dma(stv, srcw, r=[], w=[sk], tag=sk)
                tt("pool", Wd[eb], stv, gtf_rep.unsqueeze(1).broadcast_to([128, 2, D]), ALU.mult,
                   r=[sk, "gtf_rep"], w=["Wd%d" % eb])

    def gate_up(e_, s4, hb):
        eb = e_ % 2
        hk = [("h2T", 4 * s4 + i) for i in range(4)]
        for fc in range(2):
            mms([(ps[2 * fc][:, :], Wg[eb][:, k, fc * 128:(fc + 1) * 128], h2T[:, k, s4 * 512:(s4 + 1) * 512], k == 0, k == KC - 1)
                 for k in range(KC)], r=hk + ["Wg%d" % eb], w=["ps%d" % (2 * fc)])
            mms([(ps[2 * fc + 1][:, :], Wu[eb][:, k, fc * 128:(fc + 1) * 128], h2T[:, k, s4 * 512:(s4 + 1) * 512], k == 0, k == KC - 1)
                 for k in range(KC)], r=hk + ["Wu%d" % eb], w=["ps%d" % (2 * fc + 1)])
            act(sgl[fc], ps[2 * fc][:, :], AF.Silu, r=["ps%d" % (2 * fc)], w=["sgl%d" % fc])
            tt("dve", hid[hb][:, fc, :], ps[2 * fc + 1][:, :], sgl[fc], ALU.mult,
               r=["ps%d" % (2 * fc + 1), "sgl%d" % fc], w=["hid%d_%d" % (hb, fc)])

    def down(e_, s4, hb):
        eb = e_ % 2
        for i in range(4):
            m = 4 * s4 + i
            for half in range(2):
                ob_ = 4 + (i * 2 + half) % 4
                mms([(ps[ob_][:, :], hid[hb][:, fc, i * 128:(i + 1) * 128], Wd[eb][:, fc, half * 512:(half + 1) * 512], fc == 0, fc == 1)
                     for fc in range(2)], r=["hid%d_0" % hb, "hid%d_1" % hb, "Wd%d" % eb], w=["ps%d" % ob_])
                tb_ = (i * 2 + half) % 2
                act(tacc[tb_], ps[ob_][:, :], AF.Identity, r=["ps%d" % ob_, ("cmb", m)], w=["tacc%d" % tb_],
                    scale=cmb[:, m, e_:e_ + 1])
                tt("dve", X[:, m, half * 512:(half + 1) * 512], X[:, m, half * 512:(half + 1) * 512], tacc[tb_],
                   ALU.add, r=["tacc%d" % tb_, ("X", m)], w=[("X", m)])

    NE = NEXP
    load_expert(0)
    prev = None
    step = 0
    for e_ in range(NE):
        for s4 in range(4 if not mini else 1):
            hb = step % 2
            step += 1
            gate_up(e_, s4, hb)
            if prev is not None:
                down(*prev)
            prev = (e_, s4, hb)
            if s4 == 0 and e_ + 1 < NE:
                load_expert(e_ + 1)
    down(*prev)

    for m in range(NOWN if not mini else 4):
        dma(out_d[m * 128:(m + 1) * 128, :], X[:, m, :], r=[("X", m)], w=[], tag="outX")
    S.emit()
    return nc


def host_inputs(inputs, core):
    b, j = core // 4, core % 4
    f = np.float32
    x = np.asarray(inputs["x"], f)
    xb_ = np.ascontiguousarray(x[b])
    own_idx = np.concatenate([np.arange(512 * m + 128 * j, 512 * m + 128 * j + 128) for m in range(NOWN)])
    xo_ = np.ascontiguousarray(xb_[own_idx])
    xh_ = np.zeros((4 * 128, D), f)
    for m in range(NOWN):
        st = 512 * m + 128 * j - 32
        if st >= 0:
            xh_[m * 32:(m + 1) * 32] = xb_[st:st + 32]
    pos = np.asarray(inputs["positions"], np.int32)[b]
    posb_ = np.ascontiguousarray(pos.reshape(NBLK_B, 128).T)
    poso_ = np.ascontiguousarray(pos[own_idx].reshape(NOWN, 128).T)

    def colT(v, n):
        return np.ascontiguousarray(np.asarray(v, f).reshape(n, 128).T)

    kq = np.arange(128)
    cm = np.zeros((128, 4, 2, 128), f)
    for t in range(4):
        allowed = (t * 128 + kq[:, None]) <= (j * 128 + kq[None, :])
        cm[:, t, :, :] = allowed[:, None, :]
    d = {
        "xb": xb_, "xo": xo_, "xh": xh_,
        "cT": colT(inputs["c"][b], KC),
        "posb": posb_, "poso": poso_,
        "w_ada": np.ascontiguousarray(np.asarray(inputs["w_ada"], f)[0]),
        "b_ada": np.ascontiguousarray(np.asarray(inputs["b_ada"], f)[0:1]),
        "g_mixT": colT(inputs["g_mix"][0], KC),
        "g_ffnT": colT(inputs["g_ffn"][0], KC),
        "w_in": np.ascontiguousarray(np.asarray(inputs["w_in"], f)[0]),
        "q_norm_g": np.asarray(inputs["q_norm_g"], f)[0:1],
        "k_norm_g": np.asarray(inputs["k_norm_g"], f)[0:1],
        "lambda_q1": np.asarray(inputs["lambda_q1"], f)[0:1],
        "lambda_k1": np.asarray(inputs["lambda_k1"], f)[0:1],
        "lambda_q2": np.asarray(inputs["lambda_q2"], f)[0:1],
        "lambda_k2": np.asarray(inputs["lambda_k2"], f)[0:1],
        "subln_g": np.asarray(inputs["subln_g"], f)[0:1],
        "b_gluT": colT(inputs["b_glu"][0], 8),
        "w_dwT": np.ascontiguousarray(np.asarray(inputs["w_dw"], f)[0].T.reshape(4, 128, 31).transpose(1, 0, 2)),
        "b_dwT": colT(inputs["b_dw"][0], 4),
        "ln_gT": colT(inputs["conv_ln_g"][0], 4),
        "ln_bT": colT(inputs["conv_ln_b"][0], 4),
        "w_out": np.ascontiguousarray(np.asarray(inputs["w_out"], f)[0]),
        "w_rt": np.ascontiguousarray(np.concatenate([np.asarray(inputs["w_group"], f)[0],
                                                     np.asarray(inputs["w_router"], f)[0]], axis=1)),
        "b_rt": np.ascontiguousarray(np.concatenate([np.asarray(inputs["b_group"], f)[0],
                                                     np.asarray(inputs["b_router"], f)[0]])[None, :]),
        "w_gate": np.ascontiguousarray(np.asarray(inputs["w_gate"], f)[0]),
        "w_up": np.ascontiguousarray(np.asarray(inputs["w_up"], f)[0]),
        "w_down": np.ascontiguousarray(np.asarray(inputs["w_down"], f)[0]),
        "ident_f": np.eye(128, dtype=f),
        "ident_b": np.eye(128, dtype=f).astype(ml_dtypes.bfloat16),
        "inv_freq": (500000.0 ** (-np.arange(0, 16, 2, dtype=f) / 16.0)).astype(f)[None, :],
        "cmask": cm.reshape(128, 1024).astype(ml_dtypes.bfloat16),
        "hmask": np.full((128, 1), 0.0 if j == 0 else 1.0, f),
    }
    return d, own_idx


def kernel(**inputs):
    nc = build_program()
    in_maps = []
    owns = []
    for c in range(8):
        d, own_idx = host_inputs(inputs, c)
        in_maps.append(d)
        owns.append(own_idx)
    res = run_bass_kernel_spmd(nc, in_maps, core_ids=list(range(8)))
    out = np.zeros((2, SEQ, D), np.float32)
    for c in range(8):
        out[c // 4, owns[c]] = np.asarray(res.results[c]["out"], np.float32)
    return out
```

```python
import math
import numpy as np
import ml_dtypes
import concourse.bass as bass
import concourse.mybir as mybir
from concourse.bass_utils import run_bass_kernel_spmd

F32 = mybir.dt.float32
BF16 = mybir.dt.bfloat16
I32 = mybir.dt.int32
AF = mybir.ActivationFunctionType
ALU = mybir.AluOpType
AX = mybir.AxisListType

D = 1024
KC = 8
SEQ = 8192
NBLK_B = 64
NOWN = 16
EPS = 1e-6
LAMBDA_INIT = 0.8 - 0.6 * math.exp(-0.3 * 0)
NEXP = 32
TWO_PI = 2.0 * math.pi
C1 = 6.28125
C2 = TWO_PI - C1


class Sched:
    COMPUTE = ("pe", "act", "dve", "pool")

    def __init__(self, nc):
        self.nc = nc
        self.ops = []
        self.last_w = {}
        self.readers = {}
        self.tag_count = {}
        self.bulk_tags = set()
        self.epoch = None
        self.epoch_start = 0

    def add(self, eng, fn, r=(), w=(), tag=None):
        idx = len(self.ops)
        deps = set()
        for res in r:
            if res in self.last_w:
                deps.add(self.last_w[res])
        for res in w:
            if res in self.last_w:
                deps.add(self.last_w[res])
            for i in self.readers.get(res, {}).values():
                deps.add(i)
        if self.epoch is not None:
            deps.add(self.epoch)
        op = dict(eng=eng, fn=fn, deps=deps, tag=tag, signal=False, idx=idx)
        if tag is not None:
            self.tag_count[tag] = self.tag_count.get(tag, 0) + 1
            op["tagn"] = self.tag_count[tag]
        self.ops.append(op)
        for res in w:
            self.last_w[res] = idx
            self.readers[res] = {}
        for res in r:
            key = eng if tag is None else ("dma", idx)
            self.readers.setdefault(res, {})[key] = idx
        return idx

    def barrier(self, tile):
        deps = set()
        last = {}
        for op in self.ops[self.epoch_start:]:
            if op["tag"] is None:
                last[op["eng"]] = op["idx"]
            else:
                deps.add(op["idx"])
        deps |= set(last.values())
        idx = self.add("dve", lambda e: e.memset(tile, 0.0))
        self.ops[idx]["deps"] |= deps
        self.epoch = idx
        self.epoch_start = idx

    def emit(self):
        nc = self.nc
        ops = self.ops
        for op in ops:
            for d in op["deps"]:
                dop = ops[d]
                if dop["tag"] is None and dop["eng"] == "pe" and op["eng"] == "pe" and op["tag"] is None:
                    continue
                dop["signal"] = True
        sems = {e: nc.alloc_semaphore(name="sem_" + e) for e in self.COMPUTE}
        tagsem = {t: nc.alloc_semaphore(name="semt_" + str(t)) for t in self.tag_count}
        cnt = {e: 0 for e in self.COMPUTE}
        for op in ops:
            if op["tag"] is not None:
                t = op["tag"]
                n = self.tag_count[t] if t in self.bulk_tags else op["tagn"]
                op["token"] = (tagsem[t], 16 * n, ("t", t))
            elif op["signal"]:
                cnt[op["eng"]] += 1
                op["token"] = (sems[op["eng"]], cnt[op["eng"]], ("e", op["eng"]))
        streams = {e: [] for e in ("pe", "act", "dve", "pool", "sp")}
        for op in ops:
            streams[op["eng"]].append(op)
        out_tag_total = {t: 16 * n for t, n in self.tag_count.items()}

        def run(engname, e):
            waited = {}
            for op in streams[engname]:
                need = {}
                for d in op["deps"]:
                    dop = ops[d]
                    if dop["tag"] is None and dop["eng"] == "pe" and engname == "pe" and op["tag"] is None:
                        continue
                    sem, val, key = dop["token"]
                    if need.get(key, (None, 0))[1] < val:
                        need[key] = (sem, val)
                for key, (sem, val) in need.items():
                    if waited.get(key, 0) >= val:
                        continue
                    e.wait_ge(sem, val)
                    waited[key] = val
                ins = op["fn"](e)
                if op["tag"] is not None:
                    ins.then_inc(op["token"][0], 16)
                elif op["signal"]:
                    ins.then_inc(op["token"][0], 1)
            if engname == "sp":
                for t, tot in out_tag_total.items():
                    if str(t).startswith("out"):
                        e.wait_ge(tagsem[t], tot)

        with nc.Block() as block:
            @block.tensor
            def _(e):
                run("pe", e)

            @block.scalar
            def _(e):
                run("act", e)

            @block.vector
            def _(e):
                run("dve", e)

            @block.gpsimd
            def _(e):
                run("pool", e)

            @block.sync
            def _(e):
                run("sp", e)


class Arena:
    def __init__(self, nc, words):
        self.t = nc.alloc_sbuf_tensor("arena", [128, words], F32)
        self.words = words
        self.off = 0

    def mark(self):
        return self.off

    def reset(self, m):
        self.off = m

    def alloc(self, shape, dt=F32):
        n = 1
        for s in shape[1:]:
            n *= s
        w = n if dt in (F32, I32) else (n + 1) // 2
        w = (w + 7) // 8 * 8
        assert self.off + w <= self.words, ("arena overflow", self.off, w, self.words)
        v = self.t[:, self.off:self.off + w]
        self.off += w
        if dt != F32:
            v = v.bitcast(dt)
        v = v[:, 0:n]
        if len(shape) == 3:
            v = v.rearrange("p (a b) -> p a b", a=shape[1])
        elif len(shape) == 4:
            v = v.rearrange("p (a b c) -> p a b c", a=shape[1], b=shape[2])
        return v


def build_program(stop_after=None, mini=False, mini_ns=1):
    nc = bass.Bass("TRN2", target_bir_lowering=False)
    S = Sched(nc)
    S.bulk_tags.add("const")

    def din(name, shape, dt=F32):
        return nc.dram_tensor(name, list(shape), dt, kind="ExternalInput").ap()

    xb = din("xb", [SEQ, D])
    xo = din("xo", [NOWN * 128, D])
    xh = din("xh", [4 * 128, D])
    cT_d = din("cT", [128, KC])
    posb_d = din("posb", [128, NBLK_B], I32)
    poso_d = din("poso", [128, NOWN], I32)
    w_ada = din("w_ada", [D, 6 * D])
    b_ada = din("b_ada", [1, 6 * D])
    gmix_d = din("g_mixT", [128, KC])
    gffn_d = din("g_ffnT", [128, KC])
    w_in = din("w_in", [D, 2560])
    gq_d = din("q_norm_g", [1, 64])
    gk_d = din("k_norm_g", [1, 64])
    lq1_d = din("lambda_q1", [1, 64])
    lk1_d = din("lambda_k1", [1, 64])
    lq2_d = din("lambda_q2", [1, 64])
    lk2_d = din("lambda_k2", [1, 64])
    subg_d = din("subln_g", [1, 128])
    bglu_d = din("b_gluT", [128, 8])
    wdw_d = din("w_dwT", [128, 4, 31])
    bdw_d = din("b_dwT", [128, 4])
    lng_d = din("ln_gT", [128, 4])
    lnb_d = din("ln_bT", [128, 4])
    w_out = din("w_out", [D, D])
    wr_d = din("w_rt", [D, 36])
    br_d = din("b_rt", [1, 36])
    w_gate = din("w_gate", [NEXP, D, 256])
    w_up = din("w_up", [NEXP, D, 256])
    w_down = din("w_down", [NEXP, 256, D])
    identf_d = din("ident_f", [128, 128])
    identb_d = din("ident_b", [128, 128], BF16)
    invf_d = din("inv_freq", [1, 8])
    mask_d = din("cmask", [128, 4 * 256], BF16)
    hmask_d = din("hmask", [128, 1])
    out_d = nc.dram_tensor("out", [NOWN * 128, D], F32, kind="ExternalOutput").ap()
    dbg = {}

    A = Arena(nc, 53000)
    ps = [nc.alloc_psum_tensor("ps%d" % i, [128, 512], F32) for i in range(8)]

    def psb(i):
        return ps[i][:, :].bitcast(BF16)

    ident_f = A.alloc([128, 128])
    ident_b = A.alloc([128, 128], BF16)
    cT = A.alloc([128, KC])
    cact = A.alloc([128, KC])
    gmixT = A.alloc([128, KC])
    gffnT = A.alloc([128, KC])
    sh_a = A.alloc([128, KC]); sc_a = A.alloc([128, KC]); gsc_a = A.alloc([128, KC])
    sh_f = A.alloc([128, KC]); sc_f = A.alloc([128, KC]); gsc_f = A.alloc([128, KC])
    gq_rep = A.alloc([128, 64]); gk_rep = A.alloc([128, 64])
    lvec = A.alloc([128, 4, 64])
    sg_rep = A.alloc([128, 128])
    lam_t = A.alloc([128, 8])
    bgluT = A.alloc([128, 8])
    wdwT = A.alloc([128, 4, 31])
    bdwT = A.alloc([128, 4]); lngT = A.alloc([128, 4]); lnbT = A.alloc([128, 4])
    wr = A.alloc([128, KC, 36])
    br_rep = A.alloc([128, 36])
    invf = A.alloc([128, 8])
    cmask = A.alloc([128, 4 * 256], BF16)
    hmask = A.alloc([128, 1])
    cos_b = A.alloc([128, NBLK_B, 8]); sin_b = A.alloc([128, NBLK_B, 8])
    cos_o = A.alloc([128, NOWN, 8]); sin_o = A.alloc([128, NOWN, 8])
    epsc = A.alloc([128, 1]); m8c = A.alloc([128, 1])
    ones_ln = A.alloc([128, 128])
    ATTN_OFF = A.mark()
    attnT = A.alloc([128, 4, NOWN * 128], BF16)
    small = A.alloc([128, 64])
    junk_sb = A.alloc([128, D], BF16)
    P_END = A.mark()

    def dma(out, in_, r, w, tag):
        S.add("sp", lambda e, o=out, i=in_: e.dma_start(out=o, in_=i), r=r, w=w, tag=tag)

    def act(out, in_, func, r, w, bias=None, scale=None, accum=None):
        def f(e, out=out, in_=in_, func=func, bias=bias, scale=scale, accum=accum):
            kw = {}
            if bias is not None:
                kw["bias"] = bias
            if scale is not None:
                kw["scale"] = scale
            if accum is not None:
                kw["accum_out"] = accum
            return e.activation(out, in_, func, **kw)
        S.add("act", f, r=r, w=w)

    def tsc(eng, out, in0, s1, s2, op0, op1, r, w):
        def f(e, out=out, in0=in0, s1=s1, s2=s2, op0=op0, op1=op1):
            if op1 is None:
                return e.tensor_scalar(out, in0, s1, None, op0)
            return e.tensor_scalar(out, in0, s1, s2, op0, op1)
        S.add(eng, f, r=r, w=w)

    def tt(eng, out, in0, in1, op, r, w):
        S.add(eng, lambda e, o=out, a=in0, b=in1, op=op: e.tensor_tensor(o, a, b, op), r=r, w=w)

    def stt(out, in0, sc, in1, op0, op1, r, w):
        S.add("dve", lambda e, o=out, a=in0, s=sc, b=in1, o0=op0, o1=op1:
              e.scalar_tensor_tensor(o, a, s, b, o0, o1), r=r, w=w)

    def cp(eng, out, in_, r, w):
        S.add(eng, lambda e, o=out, i=in_: e.tensor_copy(o, i), r=r, w=w)

    def mms(items, r, w):
        def f(e, items=items):
            ins = None
            for (o, l, rh, st, sp_) in items:
                ins = e.matmul(o, l, rh, start=st, stop=sp_)
            return ins
        S.add("pe", f, r=r, w=w)

    def tps(items, ident, r, w):
        def f(e, items=items, ident=ident):
            ins = None
            for (o, i) in items:
                ins = e.transpose(o, i, ident)
            return ins
        S.add("pe", f, r=r, w=w)

    def rstd_from_ss(ss_ap, out_ap, inv_n, rk, wk):
        act(out_ap, ss_ap, AF.Ln, r=[rk, "epsc"], w=[wk], bias=epsc[:, 0:1], scale=inv_n)
        act(out_ap, out_ap, AF.Exp, r=[wk], w=[wk], scale=-0.5)

    def cload(dst, src, key):
        dma(dst, src, r=[], w=[key], tag="const")

    cload(ident_f, identf_d, "ident_f")
    cload(ident_b, identb_d, "ident_b")
    cload(cT, cT_d, "cT")
    cload(gmixT, gmix_d, "gmixT")
    cload(gffnT, gffn_d, "gffnT")
    cload(gq_rep, gq_d.broadcast_to([128, 64]), "gq_rep")
    cload(gk_rep, gk_d.broadcast_to([128, 64]), "gk_rep")
    for i, dd in enumerate((lq1_d, lk1_d, lq2_d, lk2_d)):
        cload(lvec[:, i, :], dd.broadcast_to([128, 64]), "lvec%d" % i)
    cload(sg_rep, subg_d.broadcast_to([128, 128]), "sg_rep")
    cload(bgluT, bglu_d, "bgluT")
    cload(wdwT, wdw_d, "wdwT")
    cload(bdwT, bdw_d, "bdwT")
    cload(lngT, lng_d, "lngT")
    cload(lnbT, lnb_d, "lnbT")
    cload(wr, wr_d.rearrange("(k p) n -> p k n", p=128), "wr")
    cload(br_rep, br_d.broadcast_to([128, 36]), "br_rep")
    cload(invf, invf_d.broadcast_to([128, 8]), "invf")
    cload(cmask, mask_d, "cmask")
    cload(hmask, hmask_d, "hmask")
    posb_i = A.alloc([128, NBLK_B], I32)
    poso_i = A.alloc([128, NOWN], I32)
    cload(posb_i, posb_d, "posb_i")
    cload(poso_i, poso_d, "poso_i")

    S.add("dve", lambda e: e.memset(lam_t, 0.0), r=[], w=["lam0", "lam1", "lam23", "lam4", "neglam"])
    S.add("dve", lambda e: e.memset(small, 0.0), r=[], w=["small", "junk64", "ropetab", "gains"])
    S.add("dve", lambda e: e.memset(epsc, EPS), r=[], w=["epsc"])
    S.add("dve", lambda e: e.memset(m8c, -8.0), r=[], w=["m8c"])
    S.add("dve", lambda e: e.memset(ones_ln, 1.0 / 512.0), r=[], w=["ones_ln"])

    act(cact, cT, AF.Silu, r=["cT"], w=["cact"])
    tsc("dve", sg_rep, sg_rep, 1.0 - LAMBDA_INIT, None, ALU.mult, None, r=["sg_rep"], w=["sg_rep"])

    junk64 = small[:, 0:64]
    tt("dve", junk64, lvec[:, 0, :], lvec[:, 1, :], ALU.mult, r=["lvec0", "lvec1"], w=["junk64"])
    S.add("dve", lambda e: e.tensor_reduce(lam_t[:, 0:1], junk64, AX.X, ALU.add), r=["junk64"], w=["lam0"])
    tt("dve", junk64, lvec[:, 2, :], lvec[:, 3, :], ALU.mult, r=["lvec2", "lvec3", "lam0"], w=["junk64"])
    S.add("dve", lambda e: e.tensor_reduce(lam_t[:, 1:2], junk64, AX.X, ALU.add), r=["junk64"], w=["lam1"])
    act(lam_t[:, 2:4], lam_t[:, 0:2], AF.Exp, r=["lam0", "lam1"], w=["lam23"])
    tt("dve", lam_t[:, 4:5], lam_t[:, 3:4], lam_t[:, 2:3], ALU.subtract, r=["lam23"], w=["lam4"])
    tsc("dve", lam_t[:, 7:8], lam_t[:, 4:5], -LAMBDA_INIT, None, ALU.add, None, r=["lam4"], w=["neglam"])
    neglam = lam_t[:, 7:8]

    def rope_tables(pos_i, nblk, cos_t, sin_t, pfx):
        m0 = A.mark()
        posf = A.alloc([128, nblk])
        ang = A.alloc([128, nblk, 8])
        tq = A.alloc([128, nblk, 8])
        ki = A.alloc([128, nblk, 8], I32)
        kf = A.alloc([128, nblk, 8])
        rr = A.alloc([128, nblk, 8])
        mk = A.alloc([128, nblk, 8])
        cp("dve", posf, pos_i, r=[pfx + "pos_i"], w=[pfx + "posf"])
        tt("dve", ang, posf.unsqueeze(2).broadcast_to([128, nblk, 8]),
           invf.unsqueeze(1).broadcast_to([128, nblk, 8]), ALU.mult, r=[pfx + "posf", "invf"], w=[pfx + "ang"])
        for which, dst in (("s", sin_t), ("c", cos_t)):
            k0 = pfx + which
            src = ang
            if which == "c":
                tsc("dve", rr, ang, math.pi / 2.0, None, ALU.add, None, r=[pfx + "ang", pfx + "rr"], w=[pfx + "rr"])
                tsc("dve", tq, rr, 1.0 / TWO_PI, None, ALU.mult, None, r=[pfx + "rr", pfx + "tq"], w=[pfx + "tq"])
                base = rr
            else:
                tsc("dve", tq, ang, 1.0 / TWO_PI, None, ALU.mult, None, r=[pfx + "ang"], w=[pfx + "tq"])
                base = ang
            cp("dve", ki, tq, r=[pfx + "tq"], w=[pfx + "ki"])
            cp("dve", kf, ki, r=[pfx + "ki"], w=[pfx + "kf"])
            stt(rr, kf, -C1, base, ALU.mult, ALU.add, r=[pfx + "kf", pfx + "ang", pfx + "rr"], w=[pfx + "rr"])
            stt(rr, kf, -C2, rr, ALU.mult, ALU.add, r=[pfx + "kf", pfx + "rr"], w=[pfx + "rr"])
            tsc("dve", mk, rr, math.pi, -TWO_PI, ALU.is_gt, ALU.mult, r=[pfx + "rr", pfx + "mk"], w=[pfx + "mk"])
            tt("dve", rr, rr, mk, ALU.add, r=[pfx + "rr", pfx + "mk"], w=[pfx + "rr"])
            tsc("dve", mk, rr, -math.pi, TWO_PI, ALU.is_lt, ALU.mult, r=[pfx + "rr", pfx + "mk"], w=[pfx + "mk"])
            tt("dve", rr, rr, mk, ALU.add, r=[pfx + "rr", pfx + "mk"], w=[pfx + "rr"])
            act(dst, rr, AF.Sin, r=[pfx + "rr"], w=[pfx + "tab" + which])
        return [pfx + "tabs", pfx + "tabc"]

    KV0 = A.mark()
    KT = A.alloc([128, 4, SEQ], BF16)
    VA = A.alloc([128, NBLK_B, 4, 130], BF16)
    WORK0 = A.mark()
    rb = rope_tables(posb_i, NBLK_B, cos_b, sin_b, "rb_")
    ro = rope_tables(poso_i, NOWN, cos_o, sin_o, "ro_")
    rope_keys = ["rb_rr", "rb_mk", "rb_kf", "rb_ki", "rb_tq", "rb_ang", "rb_posf",
                 "ro_rr", "ro_mk", "ro_kf", "ro_ki", "ro_tq", "ro_ang", "ro_posf"]

    def ada_alloc():
        return [(A.alloc([128, KC, 512]), A.alloc([128, 512]), A.alloc([128, 512]), str(i)) for i in range(2)]

    def ada_group(g, ab, pbank):
        stage, brep, modrep, sfx = ab
        dma(stage, w_ada.rearrange("(k p) n -> p k n", p=128)[:, :, g * 512:(g + 1) * 512],
            r=[], w=["ada_stage" + sfx], tag="ada" + sfx)
        dma(brep, b_ada[0:1, g * 512:(g + 1) * 512].broadcast_to([128, 512]), r=[], w=["ada_brep" + sfx], tag="adab" + sfx)
        mms([(ps[pbank][:, :], crep[:, k, :], stage[:, k, :], k == 0, k == KC - 1) for k in range(KC)],
            r=["ada_stage" + sfx, "crep"], w=["ps%d" % pbank])
        tt("dve", modrep, ps[pbank][:, :], brep, ALU.add, r=["ps%d" % pbank, "ada_brep" + sfx], w=["ada_modrep" + sfx])

    def ada_to_cols(ab, dst, col0, pbank):
        modrep, sfx = ab[2], ab[3]
        tps([(ps[pbank][:, jj * 128:(jj + 1) * 128], modrep[:, jj * 128:(jj + 1) * 128]) for jj in range(4)],
            ident_f, r=["ada_modrep" + sfx, "ident_f"], w=["ps%d" % pbank])
        cp("dve", dst[:, col0:col0 + 4], ps[pbank][:, :].rearrange("p (j c) -> p j c", c=128)[:, :, 0],
           r=["ps%d" % pbank], w=["adacols"])

    S.barrier(small[:, 62:63])
    A.reset(WORK0)
    m_ada = A.mark()
    crep = A.alloc([128, KC, 128])
    cp("dve", crep, cact.unsqueeze(2).broadcast_to([128, KC, 128]), r=["cact"], w=["crep"])
    abufs = ada_alloc()
    for g in range(4):
        ada_group(g, abufs[g % 2], 7 - 2 * (g % 2))
        ada_to_cols(abufs[g % 2], sh_a if g < 2 else sc_a, (g % 2) * 4, 6 - 2 * (g % 2))
    tsc("dve", gsc_a, sc_a, 1.0, None, ALU.add, None, r=["adacols"], w=["gsc_a"])
    tt("dve", gsc_a, gsc_a, gmixT, ALU.mult, r=["gsc_a", "gmixT"], w=["gsc_a"])

    S.barrier(small[:, 62:63])
    A.reset(WORK0)
    WORK = A.mark()
    NXT = 3
    xt = [A.alloc([128, D]) for _ in range(NXT)]
    _cur = A.mark()
    A.reset(ATTN_OFF)
    xnb = [A.alloc([128, D], BF16) for _ in range(2)]
    hT = [A.alloc([128, KC, 128], BF16) for _ in range(2)]
    shrep = A.alloc([128, KC, 128])
    assert A.mark() <= ATTN_OFF + 4096
    A.reset(_cur)
    wkv = A.alloc([128, KC, 1024], BF16)
    shW_bf = A.alloc([128, 1024], BF16)
    c128 = A.alloc([128, 128], BF16)
    junkb = junk_sb
    ssA = [A.alloc([128, 1]) for _ in range(2)]
    rsA = [A.alloc([128, 1]) for _ in range(2)]
    ksq = [A.alloc([128, 512]) for _ in range(2)]
    kn = [A.alloc([128, 8, 64]) for _ in range(2)]
    kb16 = [A.alloc([128, 512], BF16) for _ in range(2)]
    ssk = [A.alloc([128, 8]) for _ in range(2)]
    rk = [A.alloc([128, 8]) for _ in range(2)]
    rt = [[A.alloc([128, 8, 8]) for _ in range(4)] for _ in range(2)]

    S.add("pool", lambda e: e.memset(VA[:, :, :, 128:130], 1.0), r=[], w=["VAones"])
    S.add("pool", lambda e: e.memset(c128, 1.0 / 128.0), r=[], w=["c128"])
    cp("dve", shrep, sh_a.unsqueeze(2).broadcast_to([128, KC, 128]), r=["adacols"], w=["shrep"])

    w_in_v = w_in.rearrange("(k p) n -> p k n", p=128)
    for pc in range(8):
        xi = pc % NXT
        st = xt[xi][:, :].rearrange("p (k n) -> p k n", k=KC)
        dma(st, w_in_v[:, :, 512 + pc * 128: 512 + (pc + 1) * 128], r=[], w=["xt%d" % xi], tag="xt%d" % xi)
        tt("pool", wkv[:, :, pc * 128:(pc + 1) * 128], st, gsc_a.unsqueeze(2).broadcast_to([128, KC, 128]), ALU.mult,
           r=["xt%d" % xi, "gsc_a"], w=["wkv"])
        bk = pc // 4
        mms([(ps[bk][:, (pc % 4) * 128:(pc % 4 + 1) * 128], shrep[:, k, :], st[:, k, :], k == 0, k == KC - 1)
             for k in range(KC)], r=["xt%d" % xi, "shrep"], w=["ps%d" % bk])
    cp("dve", shW_bf[:, 0:512], ps[0][:, :], r=["ps0"], w=["shW"])
    cp("dve", shW_bf[:, 512:1024], ps[1][:, :], r=["ps1"], w=["shW"])

    def qk_chain(pkey, psrc, gain_rep, cos_ap, sin_ap, sidx, evac_fn, tbank):
        i = sidx
        p3 = psrc.rearrange("p (g d) -> p g d", d=64)
        act(ksq[i], psrc, AF.Square, r=[pkey], w=["ksq"])
        S.add("dve", lambda e, o=ssk[i], a=ksq[i][:, :].rearrange("p (g d) -> p g d", d=64): e.tensor_reduce(o, a, AX.X, ALU.add),
              r=["ksq"], w=["ssk%d" % i])
        rstd_from_ss(ssk[i], rk[i], 1.0 / 64.0, "ssk%d" % i, "rk%d" % i)
        tt("dve", kn[i], p3, rk[i].unsqueeze(2).broadcast_to([128, 8, 64]), ALU.mult,
           r=[pkey, "rk%d" % i], w=["kn%d" % i])
        tt("dve", kn[i], kn[i], gain_rep.unsqueeze(1).broadcast_to([128, 8, 64]), ALU.mult,
           r=["kn%d" % i, "gains"], w=["kn%d" % i])
        k3 = kb16[i][:, :].rearrange("p (g d) -> p g d", d=64)
        cp("pool", k3, kn[i], r=["kn%d" % i], w=["kb%d" % i])
        a = kn[i][:, :, 0:8]
        b = kn[i][:, :, 8:16]
        cb = cos_ap.unsqueeze(1).broadcast_to([128, 8, 8])
        sb_ = sin_ap.unsqueeze(1).broadcast_to([128, 8, 8])
        t1, t2, t3, t4 = rt[i]
        kk = "rt%d" % i
        tt("pool", t1, a, cb, ALU.mult, r=["kn%d" % i, "ropetab"], w=[kk + "a"])
        tt("pool", t2, b, sb_, ALU.mult, r=["kn%d" % i, "ropetab"], w=[kk + "b"])
        tt("pool", k3[:, :, 0:8], t1, t2, ALU.subtract, r=[kk + "a", kk + "b"], w=["kb%d" % i])
        tt("pool", t3, b, cb, ALU.mult, r=["kn%d" % i, "ropetab"], w=[kk + "c"])
        tt("pool", t4, a, sb_, ALU.mult, r=["kn%d" % i, "ropetab"], w=[kk + "d"])
        tt("pool", k3[:, :, 8:16], t3, t4, ALU.add, r=[kk + "c", kk + "d"], w=["kb%d" % i])
        pT = psb(tbank)
        tps([(pT[:, h * 128:(h + 1) * 128], kb16[i][:, h * 128:(h + 1) * 128]) for h in range(4)],
            ident_b, r=["kb%d" % i, "ident_b"], w=["ps%d" % tbank])
        evac_fn(pT[:, 0:512])

    S.add("pool", lambda e: e.memset(small[:, 60:61], 0.0), r=rb + ro, w=["ropetab"])
    S.add("pool", lambda e: e.memset(small[:, 61:62], 0.0), r=["gq_rep", "gk_rep"], w=["gains"])

    def norm_transpose(xsrc, xk, ssv, rsv, sskey, hdst, hkeys, gsc, sh, gkeys, xout=None, xoutk=None, junk=None, junkk="ps7"):
        act(junkb, xsrc, AF.Square, r=[xk], w=["junk_sb", sskey], accum=ssv)
        rstd_from_ss(ssv, rsv, 1.0 / D, sskey, sskey + "r")
        if xout is None:
            xout, xoutk = xsrc, xk
            tsc("dve", xout, xsrc, rsv, None, ALU.mult, None, r=[xk, sskey + "r"], w=[xk])
        else:
            tsc("dve", xout, xsrc, rsv, None, ALU.mult, None, r=[xk, sskey + "r"], w=[xoutk])
        for half in range(2):
            tps([(ps[half][:, kk * 128:(kk + 1) * 128], xout[:, (half * 4 + kk) * 128:(half * 4 + kk + 1) * 128])
                 for kk in range(4)], ident_f, r=[xoutk, "ident_f"], w=["ps%d" % half])
            for kk in range(4):
                k = half * 4 + kk
                src = ps[half][:, kk * 128:(kk + 1) * 128]
                if True:
                    act(hdst[:, k, :], src, AF.Identity, r=["ps%d" % half] + gkeys, w=[hkeys[k]],
                        bias=sh[:, k:k + 1], scale=gsc[:, k:k + 1])
                else:
                    tsc("dve", hdst[:, k, :], src, gsc[:, k:k + 1], sh[:, k:k + 1], ALU.mult, ALU.add,
                        r=["ps%d" % half] + gkeys, w=[hkeys[k]])

    NT_A = NBLK_B
    if stop_after == "pro":
        NT_A = 0
    if stop_after == "A4":
        NT_A = 4
    if stop_after == "C1":
        NT_A = 8
    if mini:
        NT_A = 16 * mini_ns
    import os
    PST = (0, 1)
    PSK = (2, 3, 4)
    PSV = (5, 6)
    PST2 = 7

    def a_d0(b):
        xi = b % NXT
        dma(xt[xi], xb[b * 128:(b + 1) * 128, :], r=[], w=["xt%d" % xi], tag="xt%d" % xi)

    def a_a12(b):
        xi, i2 = b % NXT, b % 2
        act(junkb, xt[xi], AF.Square, r=["xt%d" % xi], w=["junk_sb", "ssA%d" % i2], accum=ssA[i2])
        rstd_from_ss(ssA[i2], rsA[i2], 1.0 / D, "ssA%d" % i2, "rsA%d" % i2)

    def a_v1(b):
        xi, i2 = b % NXT, b % 2
        act(xnb[i2], xt[xi], AF.Identity, r=["xt%d" % xi, "rsA%d" % i2], w=["xnb%d" % i2], scale=rsA[i2])

    def a_p1(b):
        i2 = b % 2
        pT = psb(PST[i2])
        tps([(pT[:, k * 128:(k + 1) * 128], xnb[i2][:, k * 128:(k + 1) * 128]) for k in range(KC)],
            ident_b, r=["xnb%d" % i2, "ident_b"], w=["ps%d" % PST[i2]])

    def a_ev(b):
        i2 = b % 2
        pT = psb(PST[i2])
        for half in range(2):
            cp("dve", hT[i2][:, half * 4:(half + 1) * 4, :],
               pT[:, half * 512:(half + 1) * 512].rearrange("p (k q) -> p k q", q=128),
               r=["ps%d" % PST[i2]], w=["hT%d_%d" % (i2, half)])

    def a_p2(b):
        i2 = b % 2
        pK = PSK[b % 3]
        pV = PSV[i2]
        hks = ["hT%d_0" % i2, "hT%d_1" % i2]
        mms([(ps[pK][:, :], hT[i2][:, k, :], wkv[:, k, 0:512], k == 0, False) for k in range(KC)] +
            [(ps[pK][:, :], c128, shW_bf[:, 0:512], False, True)],
            r=hks + ["wkv", "shW", "c128"], w=["ps%d" % pK])
        mms([(ps[pV][:, :], hT[i2][:, k, :], wkv[:, k, 512:1024], k == 0, False) for k in range(KC)] +
            [(ps[pV][:, :], c128, shW_bf[:, 512:1024], False, True)],
            r=hks + ["wkv", "shW", "c128"], w=["ps%d" % pV])

    def a_a45(b):
        i2 = b % 2
        pK = PSK[b % 3]
        pV = PSV[i2]
        act(ksq[i2], ps[pK][:, :], AF.Square, r=["ps%d" % pK], w=["ksq%d" % i2])
        act(VA[:, b, :, 0:128], ps[pV][:, :].rearrange("p (h e) -> p h e", e=128), AF.Copy,
            r=["ps%d" % pV, "VAones"], w=[("V", b)])

    def a_v3(b):
        i2 = b % 2
        S.add("dve", lambda e, o=ssk[i2], a=ksq[i2][:, :].rearrange("p (g d) -> p g d", d=64): e.tensor_reduce(o, a, AX.X, ALU.add),
              r=["ksq%d" % i2], w=["ssk%d" % i2])

    def a_a6(b):
        i2 = b % 2
        rstd_from_ss(ssk[i2], rk[i2], 1.0 / 64.0, "ssk%d" % i2, "rk%d" % i2)

    def a_v4(b):
        i2 = b % 2
        pK = PSK[b % 3]
        p3 = ps[pK][:, :].rearrange("p (g d) -> p g d", d=64)
        tt("dve", kn[i2], p3, rk[i2].unsqueeze(2).broadcast_to([128, 8, 64]), ALU.mult,
           r=["ps%d" % pK, "rk%d" % i2], w=["kn%d" % i2])
        tt("dve", kn[i2], kn[i2], gk_rep.unsqueeze(1).broadcast_to([128, 8, 64]), ALU.mult,
           r=["kn%d" % i2, "gains"], w=["kn%d" % i2])

    def a_g1(b):
        i = b % 2
        k3 = kb16[i][:, :].rearrange("p (g d) -> p g d", d=64)
        cp("pool", k3, kn[i], r=["kn%d" % i], w=["kb%d" % i])
        a = kn[i][:, :, 0:8]
        b_ = kn[i][:, :, 8:16]
        cb = cos_b[:, b, :].unsqueeze(1).broadcast_to([128, 8, 8])
        sb_ = sin_b[:, b, :].unsqueeze(1).broadcast_to([128, 8, 8])
        t1, t2, t3, t4 = rt[i]
        kk = "rt%d" % i
        tt("pool", t1, a, cb, ALU.mult, r=["kn%d" % i, "ropetab"], w=[kk + "a"])
        tt("pool", t2, b_, sb_, ALU.mult, r=["kn%d" % i, "ropetab"], w=[kk + "b"])
        tt("pool", k3[:, :, 0:8], t1, t2, ALU.subtract, r=[kk + "a", kk + "b"], w=["kb%d" % i])
        tt("pool", t3, b_, cb, ALU.mult, r=["kn%d" % i, "ropetab"], w=[kk + "c"])
        tt("pool", t4, a, sb_, ALU.mult, r=["kn%d" % i, "ropetab"], w=[kk + "d"])
        tt("pool", k3[:, :, 8:16], t3, t4, ALU.add, r=[kk + "c", kk + "d"], w=["kb%d" % i])

    def a_p3(b):
        i = b % 2
        pT = psb(PST2)
        tps([(pT[:, h * 128:(h + 1) * 128], kb16[i][:, h * 128:(h + 1) * 128]) for h in range(4)],
            ident_b, r=["kb%d" % i, "ident_b"], w=["ps%d" % PST2])

    def a_v5(b):
        pT = psb(PST2)
        cp("dve", KT[:, :, b * 128:(b + 1) * 128], pT[:, 0:512].rearrange("p (h q) -> p h q", q=128),
           r=["ps%d" % PST2], w=[("KT", b)])

    a_order = [(a_v5, 11), (a_p3, 10), (a_g1, 9), (a_a6, 8), (a_a45, 7), (a_v4, 8), (a_p2, 6), (a_ev, 5),
               (a_p1, 4), (a_v1, 3), (a_a12, 2), (a_v3, 7), (a_d0, 0)]
    for t in range(NT_A + 12):
        for fn_, off_ in a_order:
            b = t - off_
            if 0 <= b < NT_A:
                fn_(b)


    if stop_after in ("pro", "A", "A4"):
        if os.environ.get("MK_CUT"):
            cut = int(os.environ["MK_CUT"])
            S.ops = S.ops[:cut]
            S.last_w = {k: v for k, v in S.last_w.items() if v < cut}
            S.readers = {k: {e: i for e, i in d_.items() if i < cut} for k, d_ in S.readers.items()}
            S.tag_count = {}
            for op in S.ops:
                if op["tag"] is not None:
                    S.tag_count[op["tag"]] = S.tag_count.get(op["tag"], 0) + 1
            stop_after = "pro"
        dbg["KT"] = nc.dram_tensor("dbg_KT", [128, 4 * SEQ], BF16, kind="ExternalOutput").ap()
        dbg["VA"] = nc.dram_tensor("dbg_VA", [128, NBLK_B * 4 * 130], BF16, kind="ExternalOutput").ap()
        dbg["misc"] = nc.dram_tensor("dbg_misc", [128, 64], F32, kind="ExternalOutput").ap()
        allk = [("KT", b) for b in range(NT_A)] + [("V", b) for b in range(NT_A)]
        if stop_after in ("A", "A4"):
            S.add("dve", lambda e: e.memset(KT[:, :, NT_A * 128:], 0.0), r=[], w=["ktpad"])
            S.add("dve", lambda e: e.memset(VA[:, NT_A:, :, 0:128], 0.0), r=[], w=["vapad"])
            allk = allk + ["ktpad", "vapad"]
            dma(dbg["KT"], KT[:, :, :].rearrange("p a b -> p (a b)"), r=allk, w=[], tag="out0")
            dma(dbg["VA"], VA[:, :, :, :].rearrange("p a b c -> p (a b c)"), r=allk + ["VAones"], w=[], tag="out1")
        cp("dve", small[:, 0:8], sh_a, r=["adacols", "junk64", "neglam"], w=["small"])
        cp("dve", small[:, 8:16], gsc_a, r=["gsc_a"], w=["small"])
        cp("dve", small[:, 16:24], cos_b[:, 0, :], r=rb, w=["small"])
        cp("dve", small[:, 24:32], sin_b[:, 63, :], r=rb, w=["small"])
        cp("dve", small[:, 32:40], lam_t, r=["neglam"], w=["small"])
        dma(dbg["misc"][:, 0:40], small[:, 0:40], r=["small"], w=[], tag="out2")
        S.emit()
        return nc

    bar_tile = small[:, 62:63]
    S.barrier(bar_tile)
    A.reset(WORK)
    xq = [A.alloc([128, D]) for _ in range(2)]
    hTq = [A.alloc([128, KC, 128], BF16) for _ in range(2)]
    wq = A.alloc([128, KC, 512], BF16)
    Qbd = [A.alloc([128, 4, 2, 128], BF16) for _ in range(2)]
    NPT = 4
    PT = [A.alloc([128, 512], BF16) for _ in range(NPT)]
    ssQ = [A.alloc([128, 1]) for _ in range(2)]
    rsQ = [A.alloc([128, 1]) for _ in range(2)]
    ksq = [A.alloc([128, 512])] * 2
    kn = [A.alloc([128, 8, 64]) for _ in range(2)]
    kb16 = [A.alloc([128, 512], BF16) for _ in range(2)]
    ssk = [A.alloc([128, 8]) for _ in range(2)]
    rk = [A.alloc([128, 8]) for _ in range(2)]
    rt = [[A.alloc([128, 8, 8]) for _ in range(4)] for _ in range(2)]
    ot = [A.alloc([128, 128]) for _ in range(2)]
    ot2 = [A.alloc([128, 128]) for _ in range(2)]
    ob16 = [A.alloc([128, 128], BF16) for _ in range(2)]
    eps_ = [A.alloc([128, 8]) for _ in range(2)]
    pass

    for bq in range(2):
        S.add("pool", lambda e, q=Qbd[bq]: e.memset(q, 0.0), r=[], w=["Qbd%d" % bq])
    for pc in range(4):
        xi = pc % 2
        st = xq[xi][:, :].rearrange("p (k n) -> p k n", k=KC)
        dma(st, w_in_v[:, :, pc * 128:(pc + 1) * 128], r=[], w=["xq%d" % xi], tag="xq%d" % xi)
        cp("pool", wq[:, :, pc * 128:(pc + 1) * 128], st, r=["xq%d" % xi], w=["wq"])

    def q_stage(m, stage):
        bq = m % 2
        xk = "xq%d" % bq
        hks = ["hTq%d_%d" % (bq, k) for k in range(KC)]
        if stage == 0:
            dma(xq[bq], xo[m * 128:(m + 1) * 128, :], r=[], w=[xk], tag=xk)
            act(junkb, xq[bq], AF.Square, r=[xk], w=["junk_sb", "ssQ%d" % bq], accum=ssQ[bq])
            rstd_from_ss(ssQ[bq], rsQ[bq], 1.0 / D, "ssQ%d" % bq, "ssQ%dr" % bq)
            tsc("dve", xq[bq], xq[bq], rsQ[bq], None, ALU.mult, None, r=[xk, "ssQ%dr" % bq], w=[xk])
        elif stage == 1:
            for half in range(2):
                tps([(ps[half][:, kk * 128:(kk + 1) * 128], xq[bq][:, (half * 4 + kk) * 128:(half * 4 + kk + 1) * 128])
                     for kk in range(4)], ident_f, r=[xk, "ident_f"], w=["ps%d" % half])
            for half in range(2):
                for kk in range(4):
                    k = half * 4 + kk
                    src_ = ps[half][:, kk * 128:(kk + 1) * 128]
                    act(hTq[bq][:, k, :], src_, AF.Identity, r=["ps%d" % half, "gsc_a", "adacols"], w=[hks[k]],
                        bias=sh_a[:, k:k + 1], scale=gsc_a[:, k:k + 1])
        elif stage == 2:
            mms([(ps[0][:, :], hTq[bq][:, k, :], wq[:, k, :], k == 0, k == KC - 1) for k in range(KC)],
                r=hks + ["wq"], w=["ps0"])
            qk_front("ps0", ps[0][:, :], gq_rep, cos_o[:, m, :], sin_o[:, m, :], bq)
        else:
            pT = psb(1)
            tps([(pT[:, h * 128:(h + 1) * 128], kb16[bq][:, h * 128:(h + 1) * 128]) for h in range(4)],
                ident_b, r=["kb%d" % bq, "ident_b"], w=["ps1"])
            p3 = pT[:, 0:512].rearrange("p (h q) -> p h q", q=128)
            cp("dve", Qbd[bq][0:64, :, 0, :], p3[0:64], r=["ps1"], w=["Qbd%d" % bq])
            cp("dve", Qbd[bq][64:128, :, 1, :], p3[64:128], r=["ps1"], w=["Qbd%d" % bq])

    def qk_front(pkey, psrc, gain_rep, cos_ap, sin_ap, i):
        p3 = psrc.rearrange("p (g d) -> p g d", d=64)
        act(ksq[i], psrc, AF.Square, r=[pkey], w=["ksq"])
        S.add("dve", lambda e, o=ssk[i], a=ksq[i][:, :].rearrange("p (g d) -> p g d", d=64): e.tensor_reduce(o, a, AX.X, ALU.add),
              r=["ksq"], w=["ssk%d" % i])
        rstd_from_ss(ssk[i], rk[i], 1.0 / 64.0, "ssk%d" % i, "rk%d" % i)
        tt("dve", kn[i], p3, rk[i].unsqueeze(2).broadcast_to([128, 8, 64]), ALU.mult,
           r=[pkey, "rk%d" % i], w=["kn%d" % i])
        tt("dve", kn[i], kn[i], gain_rep.unsqueeze(1).broadcast_to([128, 8, 64]), ALU.mult,
           r=["kn%d" % i, "gains"], w=["kn%d" % i])
        k3 = kb16[i][:, :].rearrange("p (g d) -> p g d", d=64)
        cp("pool", k3, kn[i], r=["kn%d" % i], w=["kb%d" % i])
        a = kn[i][:, :, 0:8]
        b = kn[i][:, :, 8:16]
        cb = cos_ap.unsqueeze(1).broadcast_to([128, 8, 8])
        sb_ = sin_ap.unsqueeze(1).broadcast_to([128, 8, 8])
        t1, t2, t3, t4 = rt[i]
        kk = "rt%d" % i
        tt("pool", t1, a, cb, ALU.mult, r=["kn%d" % i, "ropetab"], w=[kk + "a"])
        tt("pool", t2, b, sb_, ALU.mult, r=["kn%d" % i, "ropetab"], w=[kk + "b"])
        tt("pool", k3[:, :, 0:8], t1, t2, ALU.subtract, r=[kk + "a", kk + "b"], w=["kb%d" % i])
        tt("pool", t3, b, cb, ALU.mult, r=["kn%d" % i, "ropetab"], w=[kk + "c"])
        tt("pool", t4, a, sb_, ALU.mult, r=["kn%d" % i, "ropetab"], w=[kk + "d"])
        tt("pool", k3[:, :, 8:16], t3, t4, ALU.add, r=[kk + "c", kk + "d"], w=["kb%d" % i])

    cm3 = cmask[:, :].rearrange("p (t x) -> p t x", t=4)
    NM = NOWN if stop_after != "C1" else 2
    if mini:
        NM = 4 * mini_ns
    items = []
    for m in range(NM):
        for h in range(4):
            ngrp = (4 * m + 4) // 2
            for g in range(ngrp):
                items.append((m, h, g, ngrp))
    SBK = (2, 3, 4)
    pending = []
    LOOK = 2

    def emit_S(idx):
        m, h, g, ngrp = items[idx]
        bq = m % 2
        sbk = SBK[idx % 3]
        kbs = (2 * g, 2 * g + 1)
        mms([(ps[sbk][:, i * 256:(i + 1) * 256], KT[:, h, kb * 128:(kb + 1) * 128],
              Qbd[bq][:, h, :, :], True, True) for i, kb in enumerate(kbs)],
            r=[("KT", kbs[0]), ("KT", kbs[1]), "Qbd%d" % bq], w=["ps%d" % sbk])

    def emit_rest(idx):
        m, h, g, ngrp = items[idx]
        sbk = SBK[idx % 3]
        pti = idx % NPT
        ep = (m * 4 + h) % 2
        kbs = (2 * g, 2 * g + 1)
        act(PT[pti], ps[sbk][:, :], AF.Exp, r=["ps%d" % sbk, "m8c"], w=["PT%d" % pti],
            bias=m8c[:, 0:1], scale=0.125)
        if g >= ngrp - 2:
            gb = g - (ngrp - 2)
            tt("dve", PT[pti], PT[pti], cm3[:, 2 * gb:2 * gb + 2, :], ALU.mult,
               r=["PT%d" % pti, "cmask"], w=["PT%d" % pti])
        its = []
        for i, kb in enumerate(kbs):
            for c in range(2):
                its.append((ps[5 + c][:, 0:129], PT[pti][:, i * 256 + c * 128:i * 256 + (c + 1) * 128],
                            VA[:, kb, h, 0:129], (g == 0 and i == 0), (g == ngrp - 1 and i == 1)))
        mms(its, r=["PT%d" % pti, ("V", kbs[0]), ("V", kbs[1])], w=["ps5", "ps6"])
        if g == ngrp - 1:
            epilogue_a(m, h, ep)
            pending.append((idx + 3, lambda m=m, h=h, ep=ep: epilogue_b(m, h, ep)))

    def epilogue_a(m, h, ep):
        e_ = eps_[ep]
        ek = "eps%d" % ep
        act(e_[:, 5:6], ps[5][:, 128:129], AF.Copy, r=["ps5"], w=[ek])
        act(e_[:, 6:7], ps[6][:, 128:129], AF.Copy, r=["ps6"], w=[ek])
        S.add("dve", lambda e, e_=e_: e.reciprocal(e_[:, 0:2], e_[:, 5:7]), r=[ek], w=[ek])
        tt("dve", e_[:, 2:3], e_[:, 1:2], neglam, ALU.mult, r=[ek, "neglam"], w=[ek])
        act(ot[ep], ps[5][:, 0:128], AF.Identity, r=["ps5", ek], w=["ot%d" % ep], scale=e_[:, 0:1])
        act(ot2[ep], ps[6][:, 0:128], AF.Identity, r=["ps6", ek], w=["ot2%d" % ep], scale=e_[:, 2:3])
        tt("dve", ot[ep], ot[ep], ot2[ep], ALU.add, r=["ot%d" % ep, "ot2%d" % ep], w=["ot%d" % ep])
        act(ot2[ep], ot[ep], AF.Square, r=["ot%d" % ep, "ot2%d" % ep], w=["ot2%d" % ep, ek + "s"], accum=e_[:, 3:4])
        rstd_from_ss(e_[:, 3:4], e_[:, 4:5], 1.0 / 128.0, ek + "s", ek + "r")
        act(ot2[ep], ot[ep], AF.Identity, r=["ot%d" % ep, ek + "r", "ot2%d" % ep], w=["ot2%d" % ep], scale=e_[:, 4:5])
        tt("dve", ob16[ep], ot2[ep], sg_rep, ALU.mult, r=["ot2%d" % ep, "sg_rep"], w=["ob%d" % ep])

    def epilogue_b(m, h, ep):
        tps([(psb(7)[:, ep * 128:(ep + 1) * 128], ob16[ep])], ident_b, r=["ob%d" % ep, "ident_b"], w=["ps7"])
        cp("dve", attnT[:, h, m * 128:(m + 1) * 128], psb(7)[:, ep * 128:(ep + 1) * 128], r=["ps7"], w=[("attnT", m, h)])

    for st_ in range(4):
        q_stage(0, st_)
    for i in range(min(LOOK, len(items))):
        emit_S(i)
    for idx in range(len(items)):
        m, h, g, ngrp = items[idx]
        if g == 0 and m + 1 < NM:
            q_stage(m + 1, h)
        if idx + LOOK < len(items):
            emit_S(idx + LOOK)
        while pending and pending[0][0] <= idx:
            pending.pop(0)[1]()
        emit_rest(idx)
    while pending:
        pending.pop(0)[1]()

    if stop_after in ("C", "C1"):
        dbg["attnT"] = nc.dram_tensor("dbg_attnT", [128, 4 * NOWN * 128], BF16, kind="ExternalOutput").ap()
        if NM < NOWN:
            S.add("dve", lambda e: e.memset(attnT[:, :, NM * 128:], 0.0), r=[], w=["attnpad"])
        allk = [("attnT", m, h) for m in range(NM) for h in range(4)] + ["attnpad"]
        dma(dbg["attnT"], attnT[:, :, :].rearrange("p a b -> p (a b)"), r=allk, w=[], tag="out0")
        S.emit()
        return nc

    S.barrier(bar_tile)
    A.reset(KV0)
    X = A.alloc([128, NOWN, D])
    wglu = A.alloc([128, KC, 1024], BF16)
    woutp = A.alloc([128, KC, 1024], BF16)
    D1W = A.mark()
    crep = A.alloc([128, KC, 128])
    abufs = ada_alloc()
    gta_rep = A.alloc([128, D])
    stg = [A.alloc([128, KC, 128]) for _ in range(2)]
    for s4 in range(4):
        S.bulk_tags.add("Xload%d" % s4)
        for i in range(4):
            m = 4 * s4 + i
            dma(X[:, m, :], xo[m * 128:(m + 1) * 128, :], r=[], w=[("X", m)], tag="Xload%d" % s4)
    cp("dve", crep, cact.unsqueeze(2).broadcast_to([128, KC, 128]), r=["cact"], w=["crep"])
    for g in (4, 5):
        ada_group(g, abufs[g % 2], 7 - 2 * (g % 2))
        cp("dve", gta_rep[:, (g - 4) * 512:(g - 3) * 512], abufs[g % 2][2], r=["ada_modrep%d" % (g % 2)], w=["gta_rep"])
    for pc in range(8):
        xi = pc % 2
        dma(stg[xi], w_in_v[:, :, 1536 + pc * 128:1536 + (pc + 1) * 128], r=[], w=["stg%d" % xi], tag="stg%d" % xi)
        if pc % 2 == 0:
            act(wglu[:, :, pc * 128:(pc + 1) * 128], stg[xi], AF.Copy, r=["stg%d" % xi], w=["wglu"])
        else:
            cp("pool", wglu[:, :, pc * 128:(pc + 1) * 128], stg[xi], r=["stg%d" % xi], w=["wglu"])
    w_out_v = w_out.rearrange("(k p) n -> p k n", p=128)
    for pc in range(8):
        xi = pc % 2
        dma(stg[xi], w_out_v[:, :, pc * 128:(pc + 1) * 128], r=[], w=["stg%d" % xi], tag="stg%d" % xi)
        tt("dve" if pc % 2 == 0 else "pool", woutp[:, :, pc * 128:(pc + 1) * 128], stg[xi],
           gta_rep[:, pc * 128:(pc + 1) * 128].unsqueeze(1).broadcast_to([128, KC, 128]), ALU.mult,
           r=["stg%d" % xi, "gta_rep"], w=["woutp"])
    S.barrier(bar_tile)
    A.reset(D1W)
    xhb = A.alloc([128, D])
    xn = [A.alloc([128, D]) for _ in range(2)]
    hT5 = A.alloc([128, KC, 640], BF16)
    uT = A.alloc([128, 4, 4, 160], BF16)
    sig = [A.alloc([128, 512]) for _ in range(2)]
    sigh = [A.alloc([128, 128]) for _ in range(2)]
    asb = [A.alloc([128, 512]) for _ in range(2)]
    asbh = [A.alloc([128, 128]) for _ in range(2)]
    yv = A.alloc([128, 4, 512])
    ysq = A.alloc([128, 4, 512])
    mean_sb = A.alloc([128, 512])
    m2 = A.alloc([128, 512])
    rstd_bc = A.alloc([128, 512])
    tmpv = [A.alloc([128, 512]) for _ in range(2)]
    convT = A.alloc([128, 4, 512], BF16)
    dg = [A.alloc([128, 128], BF16) for _ in range(8)]
    ssD = [A.alloc([128, 1]) for _ in range(2)]
    rsD = [A.alloc([128, 1]) for _ in range(2)]
    pass
    dgc = {"n": 0}

    NS = 4 if not mini else mini_ns
    hT5b = [hT5, A.alloc([128, KC, 640], BF16)]

    def d1_front(s4, i):
        bi = i % 2
        if i == 4:
            dma(xhb, xh[s4 * 128:(s4 + 1) * 128, :], r=[], w=["xhb"], tag="xhb")
        src, sk = (X[:, 4 * s4 + i, :], ("X", 4 * s4 + i)) if i < 4 else (xhb, "xhb")
        act(junkb, src, AF.Square, r=[sk], w=["junk_sb", "ssD%d" % bi], accum=ssD[bi])
        rstd_from_ss(ssD[bi], rsD[bi], 1.0 / D, "ssD%d" % bi, "ssD%dr" % bi)
        tsc("dve", xn[bi], src, rsD[bi], None, ALU.mult, None, r=[sk, "ssD%dr" % bi], w=["xn%d" % bi])

    def d1_back(s4, i):
        bi = i % 2
        hd = hT5b[s4 % 2][:, :, i * 128:(i + 1) * 128]
        for half in range(2):
            tps([(ps[half][:, kk * 128:(kk + 1) * 128], xn[bi][:, (half * 4 + kk) * 128:(half * 4 + kk + 1) * 128])
                 for kk in range(4)], ident_f, r=["xn%d" % bi, "ident_f"], w=["ps%d" % half])
            for kk in range(4):
                k = half * 4 + kk
                act(hd[:, k, :], ps[half][:, kk * 128:(kk + 1) * 128], AF.Identity,
                    r=["ps%d" % half, "gsc_a", "adacols"], w=["hT5_%d_%d_%d" % (s4 % 2, i, k)],
                    bias=sh_a[:, k:k + 1], scale=gsc_a[:, k:k + 1])

    for i in range(5):
        d1_front(0, i)
        d1_back(0, i)
    def d1_glu(s4):
        hT5 = hT5b[s4 % 2]
        nxt = s4 + 1 < NS
        allh = ["hT5_%d_%d_%d" % (s4 % 2, i, k) for i in range(5) for k in range(KC)]
        for cc in range(4):
            pb = cc % 2
            ba_, bg_ = bgluT[:, cc:cc + 1], bgluT[:, 4 + cc:5 + cc]
            pa, pg, ph = 2 + 3 * pb, 3 + 3 * pb, 4 + 3 * pb
            mms([(ps[pa][:, :], wglu[:, k, cc * 128:(cc + 1) * 128], hT5[:, k, 0:512], k == 0, k == KC - 1)
                 for k in range(KC)], r=allh + ["wglu"], w=["ps%d" % pa])
            mms([(ps[pg][:, :], wglu[:, k, 512 + cc * 128:512 + (cc + 1) * 128], hT5[:, k, 0:512], k == 0, k == KC - 1)
                 for k in range(KC)], r=allh + ["wglu"], w=["ps%d" % pg])
            mms([(ps[ph][:, 0:128], wglu[:, k, cc * 128:(cc + 1) * 128], hT5[:, k, 512:640], k == 0, k == KC - 1)
                 for k in range(KC)] +
                [(ps[ph][:, 128:256], wglu[:, k, 512 + cc * 128:512 + (cc + 1) * 128], hT5[:, k, 512:640], k == 0, k == KC - 1)
                 for k in range(KC)], r=allh + ["wglu"], w=["ps%d" % ph])
            act(sig[pb], ps[pg][:, :], AF.Sigmoid, r=["ps%d" % pg, "bgluT"], w=["sig%d" % pb], bias=bg_)
            act(sigh[pb], ps[ph][:, 128:256], AF.Sigmoid, r=["ps%d" % ph, "bgluT"], w=["sigh%d" % pb], bias=bg_)
            act(asb[pb], ps[pa][:, :], AF.Identity, r=["ps%d" % pa, "bgluT"], w=["asb%d" % pb], bias=ba_)
            act(asbh[pb], ps[ph][:, 0:128], AF.Identity, r=["ps%d" % ph, "bgluT"], w=["asbh%d" % pb], bias=ba_)
            tt("dve", uT[:, cc, :, 32:160], asb[pb][:, :].rearrange("p (i t) -> p i t", i=4),
               sig[pb][:, :].rearrange("p (i t) -> p i t", i=4), ALU.mult,
               r=["asb%d" % pb, "sig%d" % pb], w=["uT%d" % cc])
            tt("dve", uT[:, cc, :, 0:32], asbh[pb][:, :].rearrange("p (i t) -> p i t", i=4),
               sigh[pb][:, :].rearrange("p (i t) -> p i t", i=4), ALU.mult,
               r=["asbh%d" % pb, "sigh%d" % pb], w=["uT%d" % cc])
            if s4 == 0:
                tsc("dve", uT[:, cc, 0, 0:32], uT[:, cc, 0, 0:32], hmask[:, 0:1], None, ALU.mult, None,
                    r=["uT%d" % cc, "hmask"], w=["uT%d" % cc])

    def d1_conv(s4):
        hT5 = hT5b[s4 % 2]
        nxt = s4 + 1 < NS
        allh = ["hT5_%d_%d_%d" % (s4 % 2, i, k) for i in range(5) for k in range(KC)]
        if nxt:
            d1_front(s4 + 1, 0)
        for cc in range(4):
            cb_ = 2 + 3 * (cc % 2)
            for k in range(31):
                sl = dgc["n"] % 8
                dgc["n"] += 1
                tsc("dve", dg[sl], ident_b, wdwT[:, cc, k:k + 1], None, ALU.mult, None,
                    r=["ident_b", "wdwT"], w=["dg%d" % sl])
                mms([(ps[cb_][:, :], dg[sl], uT[:, cc, :, 2 + k:130 + k], k == 0, k == 30)],
                    r=["dg%d" % sl, "uT%d" % cc], w=["ps%d" % cb_])
            act(yv[:, cc, :], ps[cb_][:, :], AF.Identity, r=["ps%d" % cb_, "bdwT"], w=["yv%d" % cc], bias=bdwT[:, cc:cc + 1])
            act(ysq[:, cc, :], yv[:, cc, :], AF.Square, r=["yv%d" % cc], w=["ysq%d" % cc])
            if nxt:
                d1_back(s4 + 1, cc)
                d1_front(s4 + 1, cc + 1)

    def d1_ln(s4):
        hT5 = hT5b[s4 % 2]
        nxt = s4 + 1 < NS
        allh = ["hT5_%d_%d_%d" % (s4 % 2, i, k) for i in range(5) for k in range(KC)]
        mms([(ps[3][:, :], ones_ln, yv[:, cc, :], cc == 0, cc == 3) for cc in range(4)],
            r=["yv%d" % cc for cc in range(4)] + ["ones_ln"], w=["ps3"])
        mms([(ps[4][:, :], ones_ln, ysq[:, cc, :], cc == 0, cc == 3) for cc in range(4)],
            r=["ysq%d" % cc for cc in range(4)] + ["ones_ln"], w=["ps4"])
        if nxt:
            d1_back(s4 + 1, 4)
        act(mean_sb, ps[3][:, :], AF.Copy, r=["ps3"], w=["mean_sb"])
        tt("dve", m2, mean_sb, mean_sb, ALU.mult, r=["mean_sb"], w=["m2"])
        tt("dve", m2, ps[4][:, :], m2, ALU.subtract, r=["ps4", "m2"], w=["m2"])
        rstd_from_ss(m2, rstd_bc, 1.0, "m2", "rstd_bc")
        for cc in range(4):
            tb = cc % 2
            tt("dve", tmpv[tb], yv[:, cc, :], mean_sb, ALU.subtract, r=["yv%d" % cc, "mean_sb"], w=["tmpv%d" % tb])
            tt("dve", tmpv[tb], tmpv[tb], rstd_bc, ALU.mult, r=["tmpv%d" % tb, "rstd_bc"], w=["tmpv%d" % tb])
            act(convT[:, cc, :], tmpv[tb], AF.Silu, r=["tmpv%d" % tb, "lngT", "lnbT"], w=["convT%d" % cc],
                bias=lnbT[:, cc:cc + 1], scale=lngT[:, cc:cc + 1])

    def d1_out(s4):
        hT5 = hT5b[s4 % 2]
        nxt = s4 + 1 < NS
        allh = ["hT5_%d_%d_%d" % (s4 % 2, i, k) for i in range(5) for k in range(KC)]
        for i in range(4):
            m = 4 * s4 + i
            for half in range(2):
                ob_ = 5 + (i * 2 + half) % 3
                items = []
                for k in range(8):
                    lh = attnT[:, k, m * 128:(m + 1) * 128] if k < 4 else convT[:, k - 4, i * 128:(i + 1) * 128]
                    items.append((ps[ob_][:, :], lh, woutp[:, k, half * 512:(half + 1) * 512], k == 0, k == 7))
                mms(items, r=[("attnT", m, h) for h in range(4)] + ["convT%d" % c_ for c_ in range(4)] + ["woutp"],
                    w=["ps%d" % ob_])
                tt("dve", X[:, m, half * 512:(half + 1) * 512], ps[ob_][:, :], X[:, m, half * 512:(half + 1) * 512],
                   ALU.add, r=["ps%d" % ob_, ("X", m)], w=[("X", m)])


    d1_glu(0)
    for s4 in range(NS):
        d1_conv(s4)
        d1_ln(s4)
        if s4 + 1 < NS:
            d1_glu(s4 + 1)
        d1_out(s4)

    if stop_after in ("D1", "D1a"):
        dbg["X"] = nc.dram_tensor("dbg_X", [NOWN * 128, D], F32, kind="ExternalOutput").ap()
        for m in range(4 * NS):
            dma(dbg["X"][m * 128:(m + 1) * 128, :], X[:, m, :], r=[("X", m)], w=[], tag="outX")
        S.emit()
        return nc

    S.barrier(bar_tile)
    A.reset(D1W - 8192)
    h2T = A.alloc([128, KC, NOWN * 128], BF16)
    cmb = A.alloc([128, NOWN, 32])
    gtf_rep = A.alloc([128, D])
    D2W = A.mark()
    crep = A.alloc([128, KC, 128])
    abufs = ada_alloc()
    xn = [A.alloc([128, D]) for _ in range(2)]
    h2f = [A.alloc([128, KC, 128]) for _ in range(2)]
    rs_ = [A.alloc([128, 128]) for _ in range(2)]
    ssD = [A.alloc([128, 1]) for _ in range(2)]
    rsD = [A.alloc([128, 1]) for _ in range(2)]
    cp("dve", crep, cact.unsqueeze(2).broadcast_to([128, KC, 128]), r=["cact"], w=["crep"])
    for g in range(6, 10):
        ada_group(g, abufs[g % 2], 7 - 2 * (g % 2))
        ada_to_cols(abufs[g % 2], sh_f if g < 8 else sc_f, (g % 2) * 4, 6 - 2 * (g % 2))
    tsc("dve", gsc_f, sc_f, 1.0, None, ALU.add, None, r=["adacols"], w=["gsc_f"])
    tt("dve", gsc_f, gsc_f, gffnT, ALU.mult, r=["gsc_f", "gffnT"], w=["gsc_f"])
    for g in (10, 11):
        ada_group(g, abufs[g % 2], 7 - 2 * (g % 2))
        cp("dve", gtf_rep[:, (g - 10) * 512:(g - 9) * 512], abufs[g % 2][2], r=["ada_modrep%d" % (g % 2)], w=["gtf_rep"])

    NB2 = NOWN if not mini else 4
    NRS = 5
    rs_ = rs_ + [A.alloc([128, 128]) for _ in range(NRS - 2)]

    def rviews(m):
        R = rs_[m % NRS]
        v = dict(lg=R[:, 0:36], gmax=R[:, 36:37], ngmax=R[:, 37:38], gsum=R[:, 38:39], gp=R[:, 39:40],
                 ohg=R[:, 40:44], junk4=R[:, 44:48], esel=R[:, 48:80], ein=R[:, 80:88], top8=R[:, 88:96],
                 eq2=R[:, 112:120], cmb8=R[:, 120:128])
        for i_, nm in enumerate(("dcol", "ed", "den", "w1", "w2", "w1g", "w2g")):
            v[nm] = R[:, 96 + i_:97 + i_]
        return v, "rs%d" % (m % NRS)

    def d_n1(m):
        bi = m % 2
        act(junkb, X[:, m, :], AF.Square, r=[("X", m)], w=["junk_sb", "ssD%d" % bi], accum=ssD[bi])
        rstd_from_ss(ssD[bi], rsD[bi], 1.0 / D, "ssD%d" % bi, "ssD%dr" % bi)

    def d_n2(m):
        bi = m % 2
        tsc("dve", xn[bi], X[:, m, :], rsD[bi], None, ALU.mult, None, r=[("X", m), "ssD%dr" % bi], w=["xn%d" % bi])

    def d_n3(m):
        bi = m % 2
        for half in range(2):
            tps([(ps[half][:, kk * 128:(kk + 1) * 128], xn[bi][:, (half * 4 + kk) * 128:(half * 4 + kk + 1) * 128])
                 for kk in range(4)], ident_f, r=["xn%d" % bi, "ident_f"], w=["ps%d" % half])

    def d_n4(m):
        bi = m % 2
        for half in range(2):
            for kk in range(4):
                k = half * 4 + kk
                act(h2f[bi][:, k, :], ps[half][:, kk * 128:(kk + 1) * 128], AF.Identity,
                    r=["ps%d" % half, "gsc_f", "adacols"], w=["h2f%d_%d" % (bi, k)],
                    bias=sh_f[:, k:k + 1], scale=gsc_f[:, k:k + 1])

    def d_n5(m):
        bi = m % 2
        hks = ["h2f%d_%d" % (bi, k) for k in range(KC)]
        cp("pool", h2T[:, :, m * 128:(m + 1) * 128], h2f[bi], r=hks, w=[("h2T", m)])
        mms([(ps[2][:, 0:36], h2f[bi][:, k, :], wr[:, k, :], k == 0, k == KC - 1) for k in range(KC)],
            r=hks + ["wr"], w=["ps2"])

    def d_cA(m):
        v, rk_ = rviews(m)
        tt("dve", v["lg"], ps[2][:, 0:36], br_rep, ALU.add, r=["ps2", "br_rep"], w=[rk_])
        S.add("dve", lambda e, gmax=v["gmax"], lg=v["lg"]: e.tensor_reduce(gmax, lg[:, 0:4], AX.X, ALU.max), r=[rk_], w=[rk_])
        tsc("dve", v["ngmax"], v["gmax"], -1.0, None, ALU.mult, None, r=[rk_], w=[rk_])

    def d_cB(m):
        v, rk_ = rviews(m)
        act(v["junk4"], v["lg"][:, 0:4], AF.Exp, r=[rk_], w=[rk_], bias=v["ngmax"], scale=1.0, accum=v["gsum"])

    def d_cC(m):
        v, rk_ = rviews(m)
        lg, ohg, esel, ein, top8 = v["lg"], v["ohg"], v["esel"], v["ein"], v["top8"]
        S.add("dve", lambda e, gp=v["gp"], gsum=v["gsum"]: e.reciprocal(gp, gsum), r=[rk_], w=[rk_])
        tsc("dve", ohg, lg[:, 0:4], v["gmax"], None, ALU.is_ge, None, r=[rk_], w=[rk_])
        tt("dve", esel.rearrange("p (g e) -> p g e", g=4), lg[:, 4:36].rearrange("p (g e) -> p g e", g=4),
           ohg.unsqueeze(2).broadcast_to([128, 4, 8]), ALU.mult, r=[rk_], w=[rk_])
        S.add("dve", lambda e, ein=ein, esel=esel: e.tensor_reduce(ein, esel.rearrange("p (g e) -> p e g", g=4), AX.X, ALU.add),
              r=[rk_], w=[rk_])
        S.add("dve", lambda e, top8=top8, ein=ein: e.max(top8, ein), r=[rk_], w=[rk_])
        tt("dve", v["dcol"], top8[:, 1:2], top8[:, 0:1], ALU.subtract, r=[rk_], w=[rk_])

    def d_cD(m):
        v, rk_ = rviews(m)
        act(v["ed"], v["dcol"], AF.Exp, r=[rk_], w=[rk_])

    def d_cE(m):
        v, rk_ = rviews(m)
        ed, den, w1, w2, w1g, w2g, gp = v["ed"], v["den"], v["w1"], v["w2"], v["w1g"], v["w2g"], v["gp"]
        ein, top8, cmb8, eq2, ohg = v["ein"], v["top8"], v["cmb8"], v["eq2"], v["ohg"]
        tsc("dve", den, ed, 1.0, None, ALU.add, None, r=[rk_], w=[rk_])
        S.add("dve", lambda e, w1=w1, den=den: e.reciprocal(w1, den), r=[rk_], w=[rk_])
        tt("dve", w2, ed, w1, ALU.mult, r=[rk_], w=[rk_])
        tt("dve", w1g, w1, gp, ALU.mult, r=[rk_], w=[rk_])
        tt("dve", w2g, w2, gp, ALU.mult, r=[rk_], w=[rk_])
        tsc("dve", cmb8, ein, top8[:, 0:1], None, ALU.is_equal, None, r=[rk_], w=[rk_])
        tsc("dve", cmb8, cmb8, w1g, None, ALU.mult, None, r=[rk_], w=[rk_])
        tsc("dve", eq2, ein, top8[:, 1:2], None, ALU.is_equal, None, r=[rk_], w=[rk_])
        tsc("dve", eq2, eq2, w2g, None, ALU.mult, None, r=[rk_], w=[rk_])
        tt("dve", cmb8, cmb8, eq2, ALU.add, r=[rk_], w=[rk_])
        tt("dve", cmb[:, m, :].rearrange("p (g e) -> p g e", g=4), ohg.unsqueeze(2).broadcast_to([128, 4, 8]),
           cmb8.unsqueeze(1).broadcast_to([128, 4, 8]), ALU.mult, r=[rk_], w=[("cmb", m)])

    d_order = [(d_cE, 9), (d_cD, 8), (d_cC, 7), (d_cB, 6), (d_cA, 5), (d_n5, 4), (d_n4, 3), (d_n3, 2), (d_n2, 1), (d_n1, 0)]
    for t in range(NB2 + 9):
        for fn_, off_ in d_order:
            m = t - off_
            if 0 <= m < NB2:
                fn_(m)

    if stop_after == "D2":
        dbg["cmb"] = nc.dram_tensor("dbg_cmb", [128, NOWN * 32], F32, kind="ExternalOutput").ap()
        dbg["h2T"] = nc.dram_tensor("dbg_h2T", [128, KC * NOWN * 128], BF16, kind="ExternalOutput").ap()
        if mini:
            S.add("dve", lambda e: e.memset(cmb[:, NB2:, :], 0.0), r=[], w=["cmbpad"])
            S.add("dve", lambda e: e.memset(h2T[:, :, NB2 * 128:], 0.0), r=[], w=["h2pad"])
        dma(dbg["cmb"], cmb[:, :, :].rearrange("p a b -> p (a b)"), r=[("cmb", m) for m in range(NB2)] + ["cmbpad"], w=[], tag="out0")
        dma(dbg["h2T"], h2T[:, :, :].rearrange("p a b -> p (a b)"), r=[("h2T", m) for m in range(NB2)] + ["h2pad"], w=[], tag="out1")
        S.emit()
        return nc

    S.barrier(bar_tile)
    A.reset(D2W)
    Wg = [A.alloc([128, KC, 256], BF16) for _ in range(2)]
    Wu = [A.alloc([128, KC, 256], BF16) for _ in range(2)]
    Wd = [A.alloc([128, 2, D], BF16) for _ in range(2)]
    stE = [A.alloc([128, 2048]) for _ in range(4)]
    hid = [A.alloc([128, 2, 512], BF16) for _ in range(2)]
    sgl = [A.alloc([128, 512], BF16) for _ in range(2)]
    tacc = [A.alloc([128, 512]) for _ in range(2)]
    pass
    stc = {"n": 0}

    def load_expert(e_):
        eb = e_ % 2
        for which in range(3):
            sl = stc["n"] % 4
            stc["n"] += 1
            sk = "stE%d" % sl
            if which < 2:
                srcw = (w_gate if which == 0 else w_up)[e_].rearrange("(k p) f -> p k f", p=128)
                stv = stE[sl][:, :].rearrange("p (k f) -> p k f", k=KC)
                dma(stv, srcw, r=[], w=[sk], tag=sk)
                if which == 0:
                    cp("pool", Wg[eb], stv, r=[sk], w=["Wg%d" % eb])
                else:
                    act(Wu[eb], stv, AF.Copy, r=[sk], w=["Wu%d" % eb])
            else:
                srcw = w_down[e_].rearrange("(c p) n -> p c n", p=128)
                stv = stE[sl][:, :].rearrange("p (c n) -> p c n", c=2)
                dma(stv, srcw, r=[], w=[sk], tag=sk)
                tt("pool", Wd[eb], stv, gtf_rep.unsqueeze(1).broadcast_to([128, 2, D]), ALU.mult,
                   r=[sk, "gtf_rep"], w=["Wd%d" % eb])

    def gate_up(e_, s4, hb):
        eb = e_ % 2
        hk = [("h2T", 4 * s4 + i) for i in range(4)]
        for fc in range(2):
            mms([(ps[2 * fc][:, :], Wg[eb][:, k, fc * 128:(fc + 1) * 128], h2T[:, k, s4 * 512:(s4 + 1) * 512], k == 0, k == KC - 1)
                 for k in range(KC)], r=hk + ["Wg%d" % eb], w=["ps%d" % (2 * fc)])
            mms([(ps[2 * fc + 1][:, :], Wu[eb][:, k, fc * 128:(fc + 1) * 128], h2T[:, k, s4 * 512:(s4 + 1) * 512], k == 0, k == KC - 1)
                 for k in range(KC)], r=hk + ["Wu%d" % eb], w=["ps%d" % (2 * fc + 1)])
            act(sgl[fc], ps[2 * fc][:, :], AF.Silu, r=["ps%d" % (2 * fc)], w=["sgl%d" % fc])
            tt("dve", hid[hb][:, fc, :], ps[2 * fc + 1][:, :], sgl[fc], ALU.mult,
               r=["ps%d" % (2 * fc + 1), "sgl%d" % fc], w=["hid%d_%d" % (hb, fc)])

    def down(e_, s4, hb):
        eb = e_ % 2
        for i in range(4):
            m = 4 * s4 + i
            for half in range(2):
                ob_ = 4 + (i * 2 + half) % 4
                mms([(ps[ob_][:, :], hid[hb][:, fc, i * 128:(i + 1) * 128], Wd[eb][:, fc, half * 512:(half + 1) * 512], fc == 0, fc == 1)
                     for fc in range(2)], r=["hid%d_0" % hb, "hid%d_1" % hb, "Wd%d" % eb], w=["ps%d" % ob_])
                tb_ = (i * 2 + half) % 2
                act(tacc[tb_], ps[ob_][:, :], AF.Identity, r=["ps%d" % ob_, ("cmb", m)], w=["tacc%d" % tb_],
                    scale=cmb[:, m, e_:e_ + 1])
                tt("dve", X[:, m, half * 512:(half + 1) * 512], X[:, m, half * 512:(half + 1) * 512], tacc[tb_],
                   ALU.add, r=["tacc%d" % tb_, ("X", m)], w=[("X", m)])

    NE = NEXP
    load_expert(0)
    prev = None
    step = 0
    for e_ in range(NE):
        for s4 in range(4 if not mini else 1):
            hb = step % 2
            step += 1
            gate_up(e_, s4, hb)
            if prev is not None:
                down(*prev)
            prev = (e_, s4, hb)
            if s4 == 0 and e_ + 1 < NE:
                load_expert(e_ + 1)
    down(*prev)

    for m in range(NOWN if not mini else 4):
        dma(out_d[m * 128:(m + 1) * 128, :], X[:, m, :], r=[("X", m)], w=[], tag="outX")
    S.emit()
    return nc


def host_inputs(inputs, core):
    b, j = core // 4, core % 4
    f = np.float32
    x = np.asarray(inputs["x"], f)
    xb_ = np.ascontiguousarray(x[b])
    own_idx = np.concatenate([np.arange(512 * m + 128 * j, 512 * m + 128 * j + 128) for m in range(NOWN)])
    xo_ = np.ascontiguousarray(xb_[own_idx])
    xh_ = np.zeros((4 * 128, D), f)
    for m in range(NOWN):
        st = 512 * m + 128 * j - 32
        if st >= 0:
            xh_[m * 32:(m + 1) * 32] = xb_[st:st + 32]
    pos = np.asarray(inputs["positions"], np.int32)[b]
    posb_ = np.ascontiguousarray(pos.reshape(NBLK_B, 128).T)
    poso_ = np.ascontiguousarray(pos[own_idx].reshape(NOWN, 128).T)

    def colT(v, n):
        return np.ascontiguousarray(np.asarray(v, f).reshape(n, 128).T)

    kq = np.arange(128)
    cm = np.zeros((128, 4, 2, 128), f)
    for t in range(4):
        allowed = (t * 128 + kq[:, None]) <= (j * 128 + kq[None, :])
        cm[:, t, :, :] = allowed[:, None, :]
    d = {
        "xb": xb_, "xo": xo_, "xh": xh_,
        "cT": colT(inputs["c"][b], KC),
        "posb": posb_, "poso": poso_,
        "w_ada": np.ascontiguousarray(np.asarray(inputs["w_ada"], f)[0]),
        "b_ada": np.ascontiguousarray(np.asarray(inputs["b_ada"], f)[0:1]),
        "g_mixT": colT(inputs["g_mix"][0], KC),
        "g_ffnT": colT(inputs["g_ffn"][0], KC),
        "w_in": np.ascontiguousarray(np.asarray(inputs["w_in"], f)[0]),
        "q_norm_g": np.asarray(inputs["q_norm_g"], f)[0:1],
        "k_norm_g": np.asarray(inputs["k_norm_g"], f)[0:1],
        "lambda_q1": np.asarray(inputs["lambda_q1"], f)[0:1],
        "lambda_k1": np.asarray(inputs["lambda_k1"], f)[0:1],
        "lambda_q2": np.asarray(inputs["lambda_q2"], f)[0:1],
        "lambda_k2": np.asarray(inputs["lambda_k2"], f)[0:1],
        "subln_g": np.asarray(inputs["subln_g"], f)[0:1],
        "b_gluT": colT(inputs["b_glu"][0], 8),
        "w_dwT": np.ascontiguousarray(np.asarray(inputs["w_dw"], f)[0].T.reshape(4, 128, 31).transpose(1, 0, 2)),
        "b_dwT": colT(inputs["b_dw"][0], 4),
        "ln_gT": colT(inputs["conv_ln_g"][0], 4),
        "ln_bT": colT(inputs["conv_ln_b"][0], 4),
        "w_out": np.ascontiguousarray(np.asarray(inputs["w_out"], f)[0]),
        "w_rt": np.ascontiguousarray(np.concatenate([np.asarray(inputs["w_group"], f)[0],
                                                     np.asarray(inputs["w_router"], f)[0]], axis=1)),
        "b_rt": np.ascontiguousarray(np.concatenate([np.asarray(inputs["b_group"], f)[0],
                                                     np.asarray(inputs["b_router"], f)[0]])[None, :]),
        "w_gate": np.ascontiguousarray(np.asarray(inputs["w_gate"], f)[0]),
        "w_up": np.ascontiguousarray(np.asarray(inputs["w_up"], f)[0]),
        "w_down": np.ascontiguousarray(np.asarray(inputs["w_down"], f)[0]),
        "ident_f": np.eye(128, dtype=f),
        "ident_b": np.eye(128, dtype=f).astype(ml_dtypes.bfloat16),
        "inv_freq": (500000.0 ** (-np.arange(0, 16, 2, dtype=f) / 16.0)).astype(f)[None, :],
        "cmask": cm.reshape(128, 1024).astype(ml_dtypes.bfloat16),
        "hmask": np.full((128, 1), 0.0 if j == 0 else 1.0, f),
    }
    return d, own_idx


def kernel(**inputs):
    nc = build_program()
    in_maps = []
    owns = []
    for c in range(8):
        d, own_idx = host_inputs(inputs, c)
        in_maps.append(d)
        owns.append(own_idx)
    res = run_bass_kernel_spmd(nc, in_maps, core_ids=list(range(8)))
    out = np.zeros((2, SEQ, D), np.float32)
    for c in range(8):
        out[c // 4, owns[c]] = np.asarray(res.results[c]["out"], np.float32)
    return out
```

```python
import math
import numpy as np
import ml_dtypes
import concourse.bass as bass
import concourse.mybir as mybir
from concourse.bass_utils import run_bass_kernel_spmd

F32 = mybir.dt.float32
BF16 = mybir.dt.bfloat16
I32 = mybir.dt.int32
AF = mybir.ActivationFunctionType
ALU = mybir.AluOpType
AX = mybir.AxisListType

D = 1024
KC = 8
SEQ = 8192
NBLK_B = 64
NOWN = 16
EPS = 1e-6
LAMBDA_INIT = 0.8 - 0.6 * math.exp(-0.3 * 0)
NEXP = 32
TWO_PI = 2.0 * math.pi
C1 = 6.28125
C2 = TWO_PI - C1


class Sched:
    COMPUTE = ("pe", "act", "dve", "pool")

    def __init__(self, nc):
        self.nc = nc
        self.ops = []
        self.last_w = {}
        self.readers = {}
        self.tag_count = {}
        self.bulk_tags = set()
        self.epoch = None
        self.epoch_start = 0

    def add(self, eng, fn, r=(), w=(), tag=None):
        idx = len(self.ops)
        deps = set()
        for res in r:
            if res in self.last_w:
                deps.add(self.last_w[res])
        for res in w:
            if res in self.last_w:
                deps.add(self.last_w[res])
            for i in self.readers.get(res, {}).values():
                deps.add(i)
        if self.epoch is not None:
            deps.add(self.epoch)
        op = dict(eng=eng, fn=fn, deps=deps, tag=tag, signal=False, idx=idx)
        if tag is not None:
            self.tag_count[tag] = self.tag_count.get(tag, 0) + 1
            op["tagn"] = self.tag_count[tag]
        self.ops.append(op)
        for res in w:
            self.last_w[res] = idx
            self.readers[res] = {}
        for res in r:
            key = eng if tag is None else ("dma", idx)
            self.readers.setdefault(res, {})[key] = idx
        return idx

    def barrier(self, tile):
        deps = set()
        last = {}
        for op in self.ops[self.epoch_start:]:
            if op["tag"] is None:
                last[op["eng"]] = op["idx"]
            else:
                deps.add(op["idx"])
        deps |= set(last.values())
        idx = self.add("dve", lambda e: e.memset(tile, 0.0))
        self.ops[idx]["deps"] |= deps
        self.epoch = idx
        self.epoch_start = idx

    def emit(self):
        nc = self.nc
        ops = self.ops
        for op in ops:
            for d in op["deps"]:
                dop = ops[d]
                if dop["tag"] is None and dop["eng"] == "pe" and op["eng"] == "pe" and op["tag"] is None:
                    continue
                dop["signal"] = True
        sems = {e: nc.alloc_semaphore(name="sem_" + e) for e in self.COMPUTE}
        tagsem = {t: nc.alloc_semaphore(name="semt_" + str(t)) for t in self.tag_count}
        cnt = {e: 0 for e in self.COMPUTE}
        for op in ops:
            if op["tag"] is not None:
                t = op["tag"]
                n = self.tag_count[t] if t in self.bulk_tags else op["tagn"]
                op["token"] = (tagsem[t], 16 * n, ("t", t))
            elif op["signal"]:
                cnt[op["eng"]] += 1
                op["token"] = (sems[op["eng"]], cnt[op["eng"]], ("e", op["eng"]))
        streams = {e: [] for e in ("pe", "act", "dve", "pool", "sp")}
        for op in ops:
            streams[op["eng"]].append(op)
        out_tag_total = {t: 16 * n for t, n in self.tag_count.items()}

        def run(engname, e):
            waited = {}
            for op in streams[engname]:
                need = {}
                for d in op["deps"]:
                    dop = ops[d]
                    if dop["tag"] is None and dop["eng"] == "pe" and engname == "pe" and op["tag"] is None:
                        continue
                    sem, val, key = dop["token"]
                    if need.get(key, (None, 0))[1] < val:
                        need[key] = (sem, val)
                for key, (sem, val) in need.items():
                    if waited.get(key, 0) >= val:
                        continue
                    e.wait_ge(sem, val)
                    waited[key] = val
                ins = op["fn"](e)
                if op["tag"] is not None:
                    ins.then_inc(op["token"][0], 16)
                elif op["signal"]:
                    ins.then_inc(op["token"][0], 1)
            if engname == "sp":
                for t, tot in out_tag_total.items():
                    if str(t).startswith("out"):
                        e.wait_ge(tagsem[t], tot)

        with nc.Block() as block:
            @block.tensor
            def _(e):
                run("pe", e)

            @block.scalar
            def _(e):
                run("act", e)

            @block.vector
            def _(e):
                run("dve", e)

            @block.gpsimd
            def _(e):
                run("pool", e)

            @block.sync
            def _(e):
                run("sp", e)


class Arena:
    def __init__(self, nc, words):
        self.t = nc.alloc_sbuf_tensor("arena", [128, words], F32)
        self.words = words
        self.off = 0

    def mark(self):
        return self.off

    def reset(self, m):
        self.off = m

    def alloc(self, shape, dt=F32):
        n = 1
        for s in shape[1:]:
            n *= s
        w = n if dt in (F32, I32) else (n + 1) // 2
        w = (w + 7) // 8 * 8
        assert self.off + w <= self.words, ("arena overflow", self.off, w, self.words)
        v = self.t[:, self.off:self.off + w]
        self.off += w
        if dt != F32:
            v = v.bitcast(dt)
        v = v[:, 0:n]
        if len(shape) == 3:
            v = v.rearrange("p (a b) -> p a b", a=shape[1])
        elif len(shape) == 4:
            v = v.rearrange("p (a b c) -> p a b c", a=shape[1], b=shape[2])
        return v


def build_program(stop_after=None, mini=False, mini_ns=1):
    nc = bass.Bass("TRN2", target_bir_lowering=False)
    S = Sched(nc)
    S.bulk_tags.add("const")

    def din(name, shape, dt=F32):
        return nc.dram_tensor(name, list(shape), dt, kind="ExternalInput").ap()

    xb = din("xb", [SEQ, D])
    xo = din("xo", [NOWN * 128, D])
    xh = din("xh", [4 * 128, D])
    cT_d = din("cT", [128, KC])
    posb_d = din("posb", [128, NBLK_B], I32)
    poso_d = din("poso", [128, NOWN], I32)
    w_ada = din("w_ada", [D, 6 * D])
    b_ada = din("b_ada", [1, 6 * D])
    gmix_d = din("g_mixT", [128, KC])
    gffn_d = din("g_ffnT", [128, KC])
    w_in = din("w_in", [D, 2560])
    gq_d = din("q_norm_g", [1, 64])
    gk_d = din("k_norm_g", [1, 64])
    lq1_d = din("lambda_q1", [1, 64])
    lk1_d = din("lambda_k1", [1, 64])
    lq2_d = din("lambda_q2", [1, 64])
    lk2_d = din("lambda_k2", [1, 64])
    subg_d = din("subln_g", [1, 128])
    bglu_d = din("b_gluT", [128, 8])
    wdw_d = din("w_dwT", [128, 4, 31])
    bdw_d = din("b_dwT", [128, 4])
    lng_d = din("ln_gT", [128, 4])
    lnb_d = din("ln_bT", [128, 4])
    w_out = din("w_out", [D, D])
    wr_d = din("w_rt", [D, 36])
    br_d = din("b_rt", [1, 36])
    w_gate = din("w_gate", [NEXP, D, 256])
    w_up = din("w_up", [NEXP, D, 256])
    w_down = din("w_down", [NEXP, 256, D])
    identf_d = din("ident_f", [128, 128])
    identb_d = din("ident_b", [128, 128], BF16)
    invf_d = din("inv_freq", [1, 8])
    mask_d = din("cmask", [128, 4 * 256], BF16)
    hmask_d = din("hmask", [128, 1])
    out_d = nc.dram_tensor("out", [NOWN * 128, D], F32, kind="ExternalOutput").ap()
    dbg = {}

    A = Arena(nc, 53000)
    ps = [nc.alloc_psum_tensor("ps%d" % i, [128, 512], F32) for i in range(8)]

    def psb(i):
        return ps[i][:, :].bitcast(BF16)

    ident_f = A.alloc([128, 128])
    ident_b = A.alloc([128, 128], BF16)
    cT = A.alloc([128, KC])
    cact = A.alloc([128, KC])
    gmixT = A.alloc([128, KC])
    gffnT = A.alloc([128, KC])
    sh_a = A.alloc([128, KC]); sc_a = A.alloc([128, KC]); gsc_a = A.alloc([128, KC])
    sh_f = A.alloc([128, KC]); sc_f = A.alloc([128, KC]); gsc_f = A.alloc([128, KC])
    gq_rep = A.alloc([128, 64]); gk_rep = A.alloc([128, 64])
    lvec = A.alloc([128, 4, 64])
    sg_rep = A.alloc([128, 128])
    lam_t = A.alloc([128, 8])
    bgluT = A.alloc([128, 8])
    wdwT = A.alloc([128, 4, 31])
    bdwT = A.alloc([128, 4]); lngT = A.alloc([128, 4]); lnbT = A.alloc([128, 4])
    wr = A.alloc([128, KC, 36])
    br_rep = A.alloc([128, 36])
    invf = A.alloc([128, 8])
    cmask = A.alloc([128, 4 * 256], BF16)
    hmask = A.alloc([128, 1])
    cos_b = A.alloc([128, NBLK_B, 8]); sin_b = A.alloc([128, NBLK_B, 8])
    cos_o = A.alloc([128, NOWN, 8]); sin_o = A.alloc([128, NOWN, 8])
    epsc = A.alloc([128, 1]); m8c = A.alloc([128, 1])
    ones_ln = A.alloc([128, 128])
    ATTN_OFF = A.mark()
    attnT = A.alloc([128, 4, NOWN * 128], BF16)
    small = A.alloc([128, 64])
    junk_sb = A.alloc([128, D], BF16)
    P_END = A.mark()

    def dma(out, in_, r, w, tag):
        S.add("sp", lambda e, o=out, i=in_: e.dma_start(out=o, in_=i), r=r, w=w, tag=tag)

    def act(out, in_, func, r, w, bias=None, scale=None, accum=None):
        def f(e, out=out, in_=in_, func=func, bias=bias, scale=scale, accum=accum):
            kw = {}
            if bias is not None:
                kw["bias"] = bias
            if scale is not None:
                kw["scale"] = scale
            if accum is not None:
                kw["accum_out"] = accum
            return e.activation(out, in_, func, **kw)
        S.add("act", f, r=r, w=w)

    def tsc(eng, out, in0, s1, s2, op0, op1, r, w):
        def f(e, out=out, in0=in0, s1=s1, s2=s2, op0=op0, op1=op1):
            if op1 is None:
                return e.tensor_scalar(out, in0, s1, None, op0)
            return e.tensor_scalar(out, in0, s1, s2, op0, op1)
        S.add(eng, f, r=r, w=w)

    def tt(eng, out, in0, in1, op, r, w):
        S.add(eng, lambda e, o=out, a=in0, b=in1, op=op: e.tensor_tensor(o, a, b, op), r=r, w=w)

    def stt(out, in0, sc, in1, op0, op1, r, w):
        S.add("dve", lambda e, o=out, a=in0, s=sc, b=in1, o0=op0, o1=op1:
              e.scalar_tensor_tensor(o, a, s, b, o0, o1), r=r, w=w)

    def cp(eng, out, in_, r, w):
        S.add(eng, lambda e, o=out, i=in_: e.tensor_copy(o, i), r=r, w=w)

    def mms(items, r, w):
        def f(e, items=items):
            ins = None
            for (o, l, rh, st, sp_) in items:
                ins = e.matmul(o, l, rh, start=st, stop=sp_)
            return ins
        S.add("pe", f, r=r, w=w)

    def tps(items, ident, r, w):
        def f(e, items=items, ident=ident):
            ins = None
            for (o, i) in items:
                ins = e.transpose(o, i, ident)
            return ins
        S.add("pe", f, r=r, w=w)

    def rstd_from_ss(ss_ap, out_ap, inv_n, rk, wk):
        act(out_ap, ss_ap, AF.Ln, r=[rk, "epsc"], w=[wk], bias=epsc[:, 0:1], scale=inv_n)
        act(out_ap, out_ap, AF.Exp, r=[wk], w=[wk], scale=-0.5)

    def cload(dst, src, key):
        dma(dst, src, r=[], w=[key], tag="const")

    cload(ident_f, identf_d, "ident_f")
    cload(ident_b, identb_d, "ident_b")
    cload(cT, cT_d, "cT")
    cload(gmixT, gmix_d, "gmixT")
    cload(gffnT, gffn_d, "gffnT")
    cload(gq_rep, gq_d.broadcast_to([128, 64]), "gq_rep")
    cload(gk_rep, gk_d.broadcast_to([128, 64]), "gk_rep")
    for i, dd in enumerate((lq1_d, lk1_d, lq2_d, lk2_d)):
        cload(lvec[:, i, :], dd.broadcast_to([128, 64]), "lvec%d" % i)
    cload(sg_rep, subg_d.broadcast_to([128, 128]), "sg_rep")
    cload(bgluT, bglu_d, "bgluT")
    cload(wdwT, wdw_d, "wdwT")
    cload(bdwT, bdw_d, "bdwT")
    cload(lngT, lng_d, "lngT")
    cload(lnbT, lnb_d, "lnbT")
    cload(wr, wr_d.rearrange("(k p) n -> p k n", p=128), "wr")
    cload(br_rep, br_d.broadcast_to([128, 36]), "br_rep")
    cload(invf, invf_d.broadcast_to([128, 8]), "invf")
    cload(cmask, mask_d, "cmask")
    cload(hmask, hmask_d, "hmask")
    posb_i = A.alloc([128, NBLK_B], I32)
    poso_i = A.alloc([128, NOWN], I32)
    cload(posb_i, posb_d, "posb_i")
    cload(poso_i, poso_d, "poso_i")

    S.add("dve", lambda e: e.memset(lam_t, 0.0), r=[], w=["lam0", "lam1", "lam23", "lam4", "neglam"])
    S.add("dve", lambda e: e.memset(small, 0.0), r=[], w=["small", "junk64", "ropetab", "gains"])
    S.add("dve", lambda e: e.memset(epsc, EPS), r=[], w=["epsc"])
    S.add("dve", lambda e: e.memset(m8c, -8.0), r=[], w=["m8c"])
    S.add("dve", lambda e: e.memset(ones_ln, 1.0 / 512.0), r=[], w=["ones_ln"])

    act(cact, cT, AF.Silu, r=["cT"], w=["cact"])
    tsc("dve", sg_rep, sg_rep, 1.0 - LAMBDA_INIT, None, ALU.mult, None, r=["sg_rep"], w=["sg_rep"])

    junk64 = small[:, 0:64]
    tt("dve", junk64, lvec[:, 0, :], lvec[:, 1, :], ALU.mult, r=["lvec0", "lvec1"], w=["junk64"])
    S.add("dve", lambda e: e.tensor_reduce(lam_t[:, 0:1], junk64, AX.X, ALU.add), r=["junk64"], w=["lam0"])
    tt("dve", junk64, lvec[:, 2, :], lvec[:, 3, :], ALU.mult, r=["lvec2", "lvec3", "lam0"], w=["junk64"])
    S.add("dve", lambda e: e.tensor_reduce(lam_t[:, 1:2], junk64, AX.X, ALU.add), r=["junk64"], w=["lam1"])
    act(lam_t[:, 2:4], lam_t[:, 0:2], AF.Exp, r=["lam0", "lam1"], w=["lam23"])
    tt("dve", lam_t[:, 4:5], lam_t[:, 3:4], lam_t[:, 2:3], ALU.subtract, r=["lam23"], w=["lam4"])
    tsc("dve", lam_t[:, 7:8], lam_t[:, 4:5], -LAMBDA_INIT, None, ALU.add, None, r=["lam4"], w=["neglam"])
    neglam = lam_t[:, 7:8]

    def rope_tables(pos_i, nblk, cos_t, sin_t, pfx):
        m0 = A.mark()
        posf = A.alloc([128, nblk])
        ang = A.alloc([128, nblk, 8])
        tq = A.alloc([128, nblk, 8])
        ki = A.alloc([128, nblk, 8], I32)
        kf = A.alloc([128, nblk, 8])
        rr = A.alloc([128, nblk, 8])
        mk = A.alloc([128, nblk, 8])
        cp("dve", posf, pos_i, r=[pfx + "pos_i"], w=[pfx + "posf"])
        tt("dve", ang, posf.unsqueeze(2).broadcast_to([128, nblk, 8]),
           invf.unsqueeze(1).broadcast_to([128, nblk, 8]), ALU.mult, r=[pfx + "posf", "invf"], w=[pfx + "ang"])
        for which, dst in (("s", sin_t), ("c", cos_t)):
            k0 = pfx + which
            src = ang
            if which == "c":
                tsc("dve", rr, ang, math.pi / 2.0, None, ALU.add, None, r=[pfx + "ang", pfx + "rr"], w=[pfx + "rr"])
                tsc("dve", tq, rr, 1.0 / TWO_PI, None, ALU.mult, None, r=[pfx + "rr", pfx + "tq"], w=[pfx + "tq"])
                base = rr
            else:
                tsc("dve", tq, ang, 1.0 / TWO_PI, None, ALU.mult, None, r=[pfx + "ang"], w=[pfx + "tq"])
                base = ang
            cp("dve", ki, tq, r=[pfx + "tq"], w=[pfx + "ki"])
            cp("dve", kf, ki, r=[pfx + "ki"], w=[pfx + "kf"])
            stt(rr, kf, -C1, base, ALU.mult, ALU.add, r=[pfx + "kf", pfx + "ang", pfx + "rr"], w=[pfx + "rr"])
            stt(rr, kf, -C2, rr, ALU.mult, ALU.add, r=[pfx + "kf", pfx + "rr"], w=[pfx + "rr"])
            tsc("dve", mk, rr, math.pi, -TWO_PI, ALU.is_gt, ALU.mult, r=[pfx + "rr", pfx + "mk"], w=[pfx + "mk"])
            tt("dve", rr, rr, mk, ALU.add, r=[pfx + "rr", pfx + "mk"], w=[pfx + "rr"])
            tsc("dve", mk, rr, -math.pi, TWO_PI, ALU.is_lt, ALU.mult, r=[pfx + "rr", pfx + "mk"], w=[pfx + "mk"])
            tt("dve", rr, rr, mk, ALU.add, r=[pfx + "rr", pfx + "mk"], w=[pfx + "rr"])
            act(dst, rr, AF.Sin, r=[pfx + "rr"], w=[pfx + "tab" + which])
        return [pfx + "tabs", pfx + "tabc"]

    KV0 = A.mark()
    KT = A.alloc([128, 4, SEQ], BF16)
    VA = A.alloc([128, NBLK_B, 4, 130], BF16)
    WORK0 = A.mark()
    rb = rope_tables(posb_i, NBLK_B, cos_b, sin_b, "rb_")
    ro = rope_tables(poso_i, NOWN, cos_o, sin_o, "ro_")
    rope_keys = ["rb_rr", "rb_mk", "rb_kf", "rb_ki", "rb_tq", "rb_ang", "rb_posf",
                 "ro_rr", "ro_mk", "ro_kf", "ro_ki", "ro_tq", "ro_ang", "ro_posf"]

    def ada_alloc():
        return [(A.alloc([128, KC, 512]), A.alloc([128, 512]), A.alloc([128, 512]), str(i)) for i in range(2)]

    def ada_group(g, ab, pbank):
        stage, brep, modrep, sfx = ab
        dma(stage, w_ada.rearrange("(k p) n -> p k n", p=128)[:, :, g * 512:(g + 1) * 512],
            r=[], w=["ada_stage" + sfx], tag="ada" + sfx)
        dma(brep, b_ada[0:1, g * 512:(g + 1) * 512].broadcast_to([128, 512]), r=[], w=["ada_brep" + sfx], tag="adab" + sfx)
        mms([(ps[pbank][:, :], crep[:, k, :], stage[:, k, :], k == 0, k == KC - 1) for k in range(KC)],
            r=["ada_stage" + sfx, "crep"], w=["ps%d" % pbank])
        tt("dve", modrep, ps[pbank][:, :], brep, ALU.add, r=["ps%d" % pbank, "ada_brep" + sfx], w=["ada_modrep" + sfx])

    def ada_to_cols(ab, dst, col0, pbank):
        modrep, sfx = ab[2], ab[3]
        tps([(ps[pbank][:, jj * 128:(jj + 1) * 128], modrep[:, jj * 128:(jj + 1) * 128]) for jj in range(4)],
            ident_f, r=["ada_modrep" + sfx, "ident_f"], w=["ps%d" % pbank])
        cp("dve", dst[:, col0:col0 + 4], ps[pbank][:, :].rearrange("p (j c) -> p j c", c=128)[:, :, 0],
           r=["ps%d" % pbank], w=["adacols"])

    S.barrier(small[:, 62:63])
    A.reset(WORK0)
    m_ada = A.mark()
    crep = A.alloc([128, KC, 128])
    cp("dve", crep, cact.unsqueeze(2).broadcast_to([128, KC, 128]), r=["cact"], w=["crep"])
    abufs = ada_alloc()
    for g in range(4):
        ada_group(g, abufs[g % 2], 7 - 2 * (g % 2))
        ada_to_cols(abufs[g % 2], sh_a if g < 2 else sc_a, (g % 2) * 4, 6 - 2 * (g % 2))
    tsc("dve", gsc_a, sc_a, 1.0, None, ALU.add, None, r=["adacols"], w=["gsc_a"])
    tt("dve", gsc_a, gsc_a, gmixT, ALU.mult, r=["gsc_a", "gmixT"], w=["gsc_a"])

    S.barrier(small[:, 62:63])
    A.reset(WORK0)
    WORK = A.mark()
    NXT = 3
    xt = [A.alloc([128, D]) for _ in range(NXT)]
    _cur = A.mark()
    A.reset(ATTN_OFF)
    xnb = [A.alloc([128, D], BF16) for _ in range(2)]
    hT = [A.alloc([128, KC, 128], BF16) for _ in range(2)]
    shrep = A.alloc([128, KC, 128])
    assert A.mark() <= ATTN_OFF + 4096
    A.reset(_cur)
    wkv = A.alloc([128, KC, 1024], BF16)
    shW_bf = A.alloc([128, 1024], BF16)
    c128 = A.alloc([128, 128], BF16)
    junkb = junk_sb
    ssA = [A.alloc([128, 1]) for _ in range(2)]
    rsA = [A.alloc([128, 1]) for _ in range(2)]
    ksq = [A.alloc([128, 512]) for _ in range(2)]
    kn = [A.alloc([128, 8, 64]) for _ in range(2)]
    kb16 = [A.alloc([128, 512], BF16) for _ in range(2)]
    ssk = [A.alloc([128, 8]) for _ in range(2)]
    rk = [A.alloc([128, 8]) for _ in range(2)]
    rt = [[A.alloc([128, 8, 8]) for _ in range(4)] for _ in range(2)]

    S.add("pool", lambda e: e.memset(VA[:, :, :, 128:130], 1.0), r=[], w=["VAones"])
    S.add("pool", lambda e: e.memset(c128, 1.0 / 128.0), r=[], w=["c128"])
    cp("dve", shrep, sh_a.unsqueeze(2).broadcast_to([128, KC, 128]), r=["adacols"], w=["shrep"])

    w_in_v = w_in.rearrange("(k p) n -> p k n", p=128)
    for pc in range(8):
        xi = pc % NXT
        st = xt[xi][:, :].rearrange("p (k n) -> p k n", k=KC)
        dma(st, w_in_v[:, :, 512 + pc * 128: 512 + (pc + 1) * 128], r=[], w=["xt%d" % xi], tag="xt%d" % xi)
        tt("pool", wkv[:, :, pc * 128:(pc + 1) * 128], st, gsc_a.unsqueeze(2).broadcast_to([128, KC, 128]), ALU.mult,
           r=["xt%d" % xi, "gsc_a"], w=["wkv"])
        bk = pc // 4
        mms([(ps[bk][:, (pc % 4) * 128:(pc % 4 + 1) * 128], shrep[:, k, :], st[:, k, :], k == 0, k == KC - 1)
             for k in range(KC)], r=["xt%d" % xi, "shrep"], w=["ps%d" % bk])
    cp("dve", shW_bf[:, 0:512], ps[0][:, :], r=["ps0"], w=["shW"])
    cp("dve", shW_bf[:, 512:1024], ps[1][:, :], r=["ps1"], w=["shW"])

    def qk_chain(pkey, psrc, gain_rep, cos_ap, sin_ap, sidx, evac_fn, tbank):
        i = sidx
        p3 = psrc.rearrange("p (g d) -> p g d", d=64)
        act(ksq[i], psrc, AF.Square, r=[pkey], w=["ksq"])
        S.add("dve", lambda e, o=ssk[i], a=ksq[i][:, :].rearrange("p (g d) -> p g d", d=64): e.tensor_reduce(o, a, AX.X, ALU.add),
              r=["ksq"], w=["ssk%d" % i])
        rstd_from_ss(ssk[i], rk[i], 1.0 / 64.0, "ssk%d" % i, "rk%d" % i)
        tt("dve", kn[i], p3, rk[i].unsqueeze(2).broadcast_to([128, 8, 64]), ALU.mult,
           r=[pkey, "rk%d" % i], w=["kn%d" % i])
        tt("dve", kn[i], kn[i], gain_rep.unsqueeze(1).broadcast_to([128, 8, 64]), ALU.mult,
           r=["kn%d" % i, "gains"], w=["kn%d" % i])
        k3 = kb16[i][:, :].rearrange("p (g d) -> p g d", d=64)
        cp("pool", k3, kn[i], r=["kn%d" % i], w=["kb%d" % i])
        a = kn[i][:, :, 0:8]
        b = kn[i][:, :, 8:16]
        cb = cos_ap.unsqueeze(1).broadcast_to([128, 8, 8])
        sb_ = sin_ap.unsqueeze(1).broadcast_to([128, 8, 8])
        t1, t2, t3, t4 = rt[i]
        kk = "rt%d" % i
        tt("pool", t1, a, cb, ALU.mult, r=["kn%d" % i, "ropetab"], w=[kk + "a"])
        tt("pool", t2, b, sb_, ALU.mult, r=["kn%d" % i, "ropetab"], w=[kk + "b"])
        tt("pool", k3[:, :, 0:8], t1, t2, ALU.subtract, r=[kk + "a", kk + "b"], w=["kb%d" % i])
        tt("pool", t3, b, cb, ALU.mult, r=["kn%d" % i, "ropetab"], w=[kk + "c"])
        tt("pool", t4, a, sb_, ALU.mult, r=["kn%d" % i, "ropetab"], w=[kk + "d"])
        tt("pool", k3[:, :, 8:16], t3, t4, ALU.add, r=[kk + "c", kk + "d"], w=["kb%d" % i])
        pT = psb(tbank)
        tps([(pT[:, h * 128:(h + 1) * 128], kb16[i][:, h * 128:(h + 1) * 128]) for h in range(4)],
            ident_b, r=["kb%d" % i, "ident_b"], w=["ps%d" % tbank])
        evac_fn(pT[:, 0:512])

    S.add("pool", lambda e: e.memset(small[:, 60:61], 0.0), r=rb + ro, w=["ropetab"])
    S.add("pool", lambda e: e.memset(small[:, 61:62], 0.0), r=["gq_rep", "gk_rep"], w=["gains"])

    def norm_transpose(xsrc, xk, ssv, rsv, sskey, hdst, hkeys, gsc, sh, gkeys, xout=None, xoutk=None, junk=None, junkk="ps7"):
        act(junkb, xsrc, AF.Square, r=[xk], w=["junk_sb", sskey], accum=ssv)
        rstd_from_ss(ssv, rsv, 1.0 / D, sskey, sskey + "r")
        if xout is None:
            xout, xoutk = xsrc, xk
            tsc("dve", xout, xsrc, rsv, None, ALU.mult, None, r=[xk, sskey + "r"], w=[xk])
        else:
            tsc("dve", xout, xsrc, rsv, None, ALU.mult, None, r=[xk, sskey + "r"], w=[xoutk])
        for half in range(2):
            tps([(ps[half][:, kk * 128:(kk + 1) * 128], xout[:, (half * 4 + kk) * 128:(half * 4 + kk + 1) * 128])
                 for kk in range(4)], ident_f, r=[xoutk, "ident_f"], w=["ps%d" % half])
            for kk in range(4):
                k = half * 4 + kk
                src = ps[half][:, kk * 128:(kk + 1) * 128]
                if True:
                    act(hdst[:, k, :], src, AF.Identity, r=["ps%d" % half] + gkeys, w=[hkeys[k]],
                        bias=sh[:, k:k + 1], scale=gsc[:, k:k + 1])
                else:
                    tsc("dve", hdst[:, k, :], src, gsc[:, k:k + 1], sh[:, k:k + 1], ALU.mult, ALU.add,
                        r=["ps%d" % half] + gkeys, w=[hkeys[k]])

    NT_A = NBLK_B
    if stop_after == "pro":
        NT_A = 0
    if stop_after == "A4":
        NT_A = 4
    if stop_after == "C1":
        NT_A = 8
    if mini:
        NT_A = 16 * mini_ns
    import os
    PST = (0, 1)
    PSK = (2, 3, 4)
    PSV = (5, 6)
    PST2 = 7

    def a_d0(b):
        xi = b % NXT
        dma(xt[xi], xb[b * 128:(b + 1) * 128, :], r=[], w=["xt%d" % xi], tag="xt%d" % xi)

    def a_a12(b):
        xi, i2 = b % NXT, b % 2
        act(junkb, xt[xi], AF.Square, r=["xt%d" % xi], w=["junk_sb", "ssA%d" % i2], accum=ssA[i2])
        rstd_from_ss(ssA[i2], rsA[i2], 1.0 / D, "ssA%d" % i2, "rsA%d" % i2)

    def a_v1(b):
        xi, i2 = b % NXT, b % 2
        act(xnb[i2], xt[xi], AF.Identity, r=["xt%d" % xi, "rsA%d" % i2], w=["xnb%d" % i2], scale=rsA[i2])

    def a_p1(b):
        i2 = b % 2
        pT = psb(PST[i2])
        tps([(pT[:, k * 128:(k + 1) * 128], xnb[i2][:, k * 128:(k + 1) * 128]) for k in range(KC)],
            ident_b, r=["xnb%d" % i2, "ident_b"], w=["ps%d" % PST[i2]])

    def a_ev(b):
        i2 = b % 2
        pT = psb(PST[i2])
        for half in range(2):
            cp("dve", hT[i2][:, half * 4:(half + 1) * 4, :],
               pT[:, half * 512:(half + 1) * 512].rearrange("p (k q) -> p k q", q=128),
               r=["ps%d" % PST[i2]], w=["hT%d_%d" % (i2, half)])

    def a_p2(b):
        i2 = b % 2
        pK = PSK[b % 3]
        pV = PSV[i2]
        hks = ["hT%d_0" % i2, "hT%d_1" % i2]
        mms([(ps[pK][:, :], hT[i2][:, k, :], wkv[:, k, 0:512], k == 0, False) for k in range(KC)] +
            [(ps[pK][:, :], c128, shW_bf[:, 0:512], False, True)],
            r=hks + ["wkv", "shW", "c128"], w=["ps%d" % pK])
        mms([(ps[pV][:, :], hT[i2][:, k, :], wkv[:, k, 512:1024], k == 0, False) for k in range(KC)] +
            [(ps[pV][:, :], c128, shW_bf[:, 512:1024], False, True)],
            r=hks + ["wkv", "shW", "c128"], w=["ps%d" % pV])

    def a_a45(b):
        i2 = b % 2
        pK = PSK[b % 3]
        pV = PSV[i2]
        act(ksq[i2], ps[pK][:, :], AF.Square, r=["ps%d" % pK], w=["ksq%d" % i2])
        act(VA[:, b, :, 0:128], ps[pV][:, :].rearrange("p (h e) -> p h e", e=128), AF.Copy,
            r=["ps%d" % pV, "VAones"], w=[("V", b)])

    def a_v3(b):
        i2 = b % 2
        S.add("dve", lambda e, o=ssk[i2], a=ksq[i2][:, :].rearrange("p (g d) -> p g d", d=64): e.tensor_reduce(o, a, AX.X, ALU.add),
              r=["ksq%d" % i2], w=["ssk%d" % i2])

    def a_a6(b):
        i2 = b % 2
        rstd_from_ss(ssk[i2], rk[i2], 1.0 / 64.0, "ssk%d" % i2, "rk%d" % i2)

    def a_v4(b):
        i2 = b % 2
        pK = PSK[b % 3]
        p3 = ps[pK][:, :].rearrange("p (g d) -> p g d", d=64)
        tt("dve", kn[i2], p3, rk[i2].unsqueeze(2).broadcast_to([128, 8, 64]), ALU.mult,
           r=["ps%d" % pK, "rk%d" % i2], w=["kn%d" % i2])
        tt("dve", kn[i2], kn[i2], gk_rep.unsqueeze(1).broadcast_to([128, 8, 64]), ALU.mult,
           r=["kn%d" % i2, "gains"], w=["kn%d" % i2])

    def a_g1(b):
        i = b % 2
        k3 = kb16[i][:, :].rearrange("p (g d) -> p g d", d=64)
        cp("pool", k3, kn[i], r=["kn%d" % i], w=["kb%d" % i])
        a = kn[i][:, :, 0:8]
        b_ = kn[i][:, :, 8:16]
        cb = cos_b[:, b, :].unsqueeze(1).broadcast_to([128, 8, 8])
        sb_ = sin_b[:, b, :].unsqueeze(1).broadcast_to([128, 8, 8])
        t1, t2, t3, t4 = rt[i]
        kk = "rt%d" % i
        tt("pool", t1, a, cb, ALU.mult, r=["kn%d" % i, "ropetab"], w=[kk + "a"])
        tt("pool", t2, b_, sb_, ALU.mult, r=["kn%d" % i, "ropetab"], w=[kk + "b"])
        tt("pool", k3[:, :, 0:8], t1, t2, ALU.subtract, r=[kk + "a", kk + "b"], w=["kb%d" % i])
        tt("pool", t3, b_, cb, ALU.mult, r=["kn%d" % i, "ropetab"], w=[kk + "c"])
        tt("pool", t4, a, sb_, ALU.mult, r=["kn%d" % i, "ropetab"], w=[kk + "d"])
        tt("pool", k3[:, :, 8:16], t3, t4, ALU.add, r=[kk + "c", kk + "d"], w=["kb%d" % i])

    def a_p3(b):
        i = b % 2
        pT = psb(PST2)
        tps([(pT[:, h * 128:(h + 1) * 128], kb16[i][:, h * 128:(h + 1) * 128]) for h in range(4)],
            ident_b, r=["kb%d" % i, "ident_b"], w=["ps%d" % PST2])

    def a_v5(b):
        pT = psb(PST2)
        cp("dve", KT[:, :, b * 128:(b + 1) * 128], pT[:, 0:512].rearrange("p (h q) -> p h q", q=128),
           r=["ps%d" % PST2], w=[("KT", b)])

    a_order = [(a_v5, 11), (a_p3, 10), (a_g1, 9), (a_a6, 8), (a_a45, 7), (a_v4, 8), (a_p2, 6), (a_ev, 5),
               (a_p1, 4), (a_v1, 3), (a_a12, 2), (a_v3, 7), (a_d0, 0)]
    for t in range(NT_A + 12):
        for fn_, off_ in a_order:
            b = t - off_
            if 0 <= b < NT_A:
                fn_(b)


    if stop_after in ("pro", "A", "A4"):
        if os.environ.get("MK_CUT"):
            cut = int(os.environ["MK_CUT"])
            S.ops = S.ops[:cut]
            S.last_w = {k: v for k, v in S.last_w.items() if v < cut}
            S.readers = {k: {e: i for e, i in d_.items() if i < cut} for k, d_ in S.readers.items()}
            S.tag_count = {}
            for op in S.ops:
                if op["tag"] is not None:
                    S.tag_count[op["tag"]] = S.tag_count.get(op["tag"], 0) + 1
            stop_after = "pro"
        dbg["KT"] = nc.dram_tensor("dbg_KT", [128, 4 * SEQ], BF16, kind="ExternalOutput").ap()
        dbg["VA"] = nc.dram_tensor("dbg_VA", [128, NBLK_B * 4 * 130], BF16, kind="ExternalOutput").ap()
        dbg["misc"] = nc.dram_tensor("dbg_misc", [128, 64], F32, kind="ExternalOutput").ap()
        allk = [("KT", b) for b in range(NT_A)] + [("V", b) for b in range(NT_A)]
        if stop_after in ("A", "A4"):
            S.add("dve", lambda e: e.memset(KT[:, :, NT_A * 128:], 0.0), r=[], w=["ktpad"])
            S.add("dve", lambda e: e.memset(VA[:, NT_A:, :, 0:128], 0.0), r=[], w=["vapad"])
            allk = allk + ["ktpad", "vapad"]
            dma(dbg["KT"], KT[:, :, :].rearrange("p a b -> p (a b)"), r=allk, w=[], tag="out0")
            dma(dbg["VA"], VA[:, :, :, :].rearrange("p a b c -> p (a b c)"), r=allk + ["VAones"], w=[], tag="out1")
        cp("dve", small[:, 0:8], sh_a, r=["adacols", "junk64", "neglam"], w=["small"])
        cp("dve", small[:, 8:16], gsc_a, r=["gsc_a"], w=["small"])
        cp("dve", small[:, 16:24], cos_b[:, 0, :], r=rb, w=["small"])
        cp("dve", small[:, 24:32], sin_b[:, 63, :], r=rb, w=["small"])
        cp("dve", small[:, 32:40], lam_t, r=["neglam"], w=["small"])
        dma(dbg["misc"][:, 0:40], small[:, 0:40], r=["small"], w=[], tag="out2")
        S.emit()
        return nc

    bar_tile = small[:, 62:63]
    S.barrier(bar_tile)
    A.reset(WORK)
    xq = [A.alloc([128, D]) for _ in range(2)]
    hTq = [A.alloc([128, KC, 128], BF16) for _ in range(2)]
    wq = A.alloc([128, KC, 512], BF16)
    Qbd = [A.alloc([128, 4, 2, 128], BF16) for _ in range(2)]
    NPT = 4
    PT = [A.alloc([128, 512], BF16) for _ in range(NPT)]
    ssQ = [A.alloc([128, 1]) for _ in range(2)]
    rsQ = [A.alloc([128, 1]) for _ in range(2)]
    ksq = [A.alloc([128, 512])] * 2
    kn = [A.alloc([128, 8, 64]) for _ in range(2)]
    kb16 = [A.alloc([128, 512], BF16) for _ in range(2)]
    ssk = [A.alloc([128, 8]) for _ in range(2)]
    rk = [A.alloc([128, 8]) for _ in range(2)]
    rt = [[A.alloc([128, 8, 8]) for _ in range(4)] for _ in range(2)]
    ot = [A.alloc([128, 128]) for _ in range(2)]
    ot2 = [A.alloc([128, 128]) for _ in range(2)]
    ob16 = [A.alloc([128, 128], BF16) for _ in range(2)]
    eps_ = [A.alloc([128, 8]) for _ in range(2)]
    pass

    for bq in range(2):
        S.add("pool", lambda e, q=Qbd[bq]: e.memset(q, 0.0), r=[], w=["Qbd%d" % bq])
    for pc in range(4):
        xi = pc % 2
        st = xq[xi][:, :].rearrange("p (k n) -> p k n", k=KC)
        dma(st, w_in_v[:, :, pc * 128:(pc + 1) * 128], r=[], w=["xq%d" % xi], tag="xq%d" % xi)
        cp("pool", wq[:, :, pc * 128:(pc + 1) * 128], st, r=["xq%d" % xi], w=["wq"])

    def q_stage(m, stage):
        bq = m % 2
        xk = "xq%d" % bq
        hks = ["hTq%d_%d" % (bq, k) for k in range(KC)]
        if stage == 0:
            dma(xq[bq], xo[m * 128:(m + 1) * 128, :], r=[], w=[xk], tag=xk)
            act(junkb, xq[bq], AF.Square, r=[xk], w=["junk_sb", "ssQ%d" % bq], accum=ssQ[bq])
            rstd_from_ss(ssQ[bq], rsQ[bq], 1.0 / D, "ssQ%d" % bq, "ssQ%dr" % bq)
            tsc("dve", xq[bq], xq[bq], rsQ[bq], None, ALU.mult, None, r=[xk, "ssQ%dr" % bq], w=[xk])
        elif stage == 1:
            for half in range(2):
                tps([(ps[half][:, kk * 128:(kk + 1) * 128], xq[bq][:, (half * 4 + kk) * 128:(half * 4 + kk + 1) * 128])
                     for kk in range(4)], ident_f, r=[xk, "ident_f"], w=["ps%d" % half])
            for half in range(2):
                for kk in range(4):
                    k = half * 4 + kk
                    src_ = ps[half][:, kk * 128:(kk + 1) * 128]
                    act(hTq[bq][:, k, :], src_, AF.Identity, r=["ps%d" % half, "gsc_a", "adacols"], w=[hks[k]],
                        bias=sh_a[:, k:k + 1], scale=gsc_a[:, k:k + 1])
        elif stage == 2:
            mms([(ps[0][:, :], hTq[bq][:, k, :], wq[:, k, :], k == 0, k == KC - 1) for k in range(KC)],
                r=hks + ["wq"], w=["ps0"])
            qk_front("ps0", ps[0][:, :], gq_rep, cos_o[:, m, :], sin_o[:, m, :], bq)
        else:
            pT = psb(1)
            tps([(pT[:, h * 128:(h + 1) * 128], kb16[bq][:, h * 128:(h + 1) * 128]) for h in range(4)],
                ident_b, r=["kb%d" % bq, "ident_b"], w=["ps1"])
            p3 = pT[:, 0:512].rearrange("p (h q) -> p h q", q=128)
            cp("dve", Qbd[bq][0:64, :, 0, :], p3[0:64], r=["ps1"], w=["Qbd%d" % bq])
            cp("dve", Qbd[bq][64:128, :, 1, :], p3[64:128], r=["ps1"], w=["Qbd%d" % bq])

    def qk_front(pkey, psrc, gain_rep, cos_ap, sin_ap, i):
        p3 = psrc.rearrange("p (g d) -> p g d", d=64)
        act(ksq[i], psrc, AF.Square, r=[pkey], w=["ksq"])
        S.add("dve", lambda e, o=ssk[i], a=ksq[i][:, :].rearrange("p (g d) -> p g d", d=64): e.tensor_reduce(o, a, AX.X, ALU.add),
              r=["ksq"], w=["ssk%d" % i])
        rstd_from_ss(ssk[i], rk[i], 1.0 / 64.0, "ssk%d" % i, "rk%d" % i)
        tt("dve", kn[i], p3, rk[i].unsqueeze(2).broadcast_to([128, 8, 64]), ALU.mult,
           r=[pkey, "rk%d" % i], w=["kn%d" % i])
        tt("dve", kn[i], kn[i], gain_rep.unsqueeze(1).broadcast_to([128, 8, 64]), ALU.mult,
           r=["kn%d" % i, "gains"], w=["kn%d" % i])
        k3 = kb16[i][:, :].rearrange("p (g d) -> p g d", d=64)
        cp("pool", k3, kn[i], r=["kn%d" % i], w=["kb%d" % i])
        a = kn[i][:, :, 0:8]
        b = kn[i][:, :, 8:16]
        cb = cos_ap.unsqueeze(1).broadcast_to([128, 8, 8])
        sb_ = sin_ap.unsqueeze(1).broadcast_to([128, 8, 8])
        t1, t2, t3, t4 = rt[i]
        kk = "rt%d" % i
        tt("pool", t1, a, cb, ALU.mult, r=["kn%d" % i, "ropetab"], w=[kk + "a"])
        tt("pool", t2, b, sb_, ALU.mult, r=["kn%d" % i, "ropetab"], w=[kk + "b"])
        tt("pool", k3[:, :, 0:8], t1, t2, ALU.subtract, r=[kk + "a", kk + "b"], w=["kb%d" % i])
        tt("pool", t3, b, cb, ALU.mult, r=["kn%d" % i, "ropetab"], w=[kk + "c"])
        tt("pool", t4, a, sb_, ALU.mult, r=["kn%d" % i, "ropetab"], w=[kk + "d"])
        tt("pool", k3[:, :, 8:16], t3, t4, ALU.add, r=[kk + "c", kk + "d"], w=["kb%d" % i])

    cm3 = cmask[:, :].rearrange("p (t x) -> p t x", t=4)
    NM = NOWN if stop_after != "C1" else 2
    if mini:
        NM = 4 * mini_ns
    items = []
    for m in range(NM):
        for h in range(4):
            ngrp = (4 * m + 4) // 2
            for g in range(ngrp):
                items.append((m, h, g, ngrp))
    SBK = (2, 3, 4)
    pending = []
    LOOK = 2

    def emit_S(idx):
        m, h, g, ngrp = items[idx]
        bq = m % 2
        sbk = SBK[idx % 3]
        kbs = (2 * g, 2 * g + 1)
        mms([(ps[sbk][:, i * 256:(i + 1) * 256], KT[:, h, kb * 128:(kb + 1) * 128],
              Qbd[bq][:, h, :, :], True, True) for i, kb in enumerate(kbs)],
            r=[("KT", kbs[0]), ("KT", kbs[1]), "Qbd%d" % bq], w=["ps%d" % sbk])

    def emit_rest(idx):
        m, h, g, ngrp = items[idx]
        sbk = SBK[idx % 3]
        pti = idx % NPT
        ep = (m * 4 + h) % 2
        kbs = (2 * g, 2 * g + 1)
        act(PT[pti], ps[sbk][:, :], AF.Exp, r=["ps%d" % sbk, "m8c"], w=["PT%d" % pti],
            bias=m8c[:, 0:1], scale=0.125)
        if g >= ngrp - 2:
            gb = g - (ngrp - 2)
            tt("dve", PT[pti], PT[pti], cm3[:, 2 * gb:2 * gb + 2, :], ALU.mult,
               r=["PT%d" % pti, "cmask"], w=["PT%d" % pti])
        its = []
        for i, kb in enumerate(kbs):
            for c in range(2):
                its.append((ps[5 + c][:, 0:129], PT[pti][:, i * 256 + c * 128:i * 256 + (c + 1) * 128],
                            VA[:, kb, h, 0:129], (g == 0 and i == 0), (g == ngrp - 1 and i == 1)))
        mms(its, r=["PT%d" % pti, ("V", kbs[0]), ("V", kbs[1])], w=["ps5", "ps6"])
        if g == ngrp - 1:
            epilogue_a(m, h, ep)
            pending.append((idx + 2, lambda m=m, h=h, ep=ep: epilogue_a2(m, h, ep)))
            pending.append((idx + 4, lambda m=m, h=h, ep=ep: epilogue_b(m, h, ep)))

    def epilogue_a(m, h, ep):
        e_ = eps_[ep]
        ek = "eps%d" % ep
        act(e_[:, 5:6], ps[5][:, 128:129], AF.Copy, r=["ps5"], w=[ek])
        act(e_[:, 6:7], ps[6][:, 128:129], AF.Copy, r=["ps6"], w=[ek])
        act(ot[ep], ps[5][:, 0:128], AF.Copy, r=["ps5"], w=["ot%d" % ep])
        act(ot2[ep], ps[6][:, 0:128], AF.Copy, r=["ps6"], w=["ot2%d" % ep])
        S.add("dve", lambda e, e_=e_: e.reciprocal(e_[:, 0:2], e_[:, 5:7]), r=[ek], w=[ek])
        tt("dve", e_[:, 2:3], e_[:, 1:2], neglam, ALU.mult, r=[ek, "neglam"], w=[ek])
        tsc("dve", ot[ep], ot[ep], e_[:, 0:1], None, ALU.mult, None, r=["ot%d" % ep, ek], w=["ot%d" % ep])
        tsc("dve", ot2[ep], ot2[ep], e_[:, 2:3], None, ALU.mult, None, r=["ot2%d" % ep, ek], w=["ot2%d" % ep])
        tt("dve", ot[ep], ot[ep], ot2[ep], ALU.add, r=["ot%d" % ep, "ot2%d" % ep], w=["ot%d" % ep])

    def epilogue_a2(m, h, ep):
        e_ = eps_[ep]
        ek = "eps%d" % ep
        act(ot2[ep], ot[ep], AF.Square, r=["ot%d" % ep, "ot2%d" % ep], w=["ot2%d" % ep, ek + "s"], accum=e_[:, 3:4])
        rstd_from_ss(e_[:, 3:4], e_[:, 4:5], 1.0 / 128.0, ek + "s", ek + "r")
        act(ot2[ep], ot[ep], AF.Identity, r=["ot%d" % ep, ek + "r", "ot2%d" % ep], w=["ot2%d" % ep], scale=e_[:, 4:5])
        tt("dve", ob16[ep], ot2[ep], sg_rep, ALU.mult, r=["ot2%d" % ep, "sg_rep"], w=["ob%d" % ep])

    def epilogue_b(m, h, ep):
        tps([(psb(7)[:, ep * 128:(ep + 1) * 128], ob16[ep])], ident_b, r=["ob%d" % ep, "ident_b"], w=["ps7"])
        cp("dve", attnT[:, h, m * 128:(m + 1) * 128], psb(7)[:, ep * 128:(ep + 1) * 128], r=["ps7"], w=[("attnT", m, h)])

    for st_ in range(4):
        q_stage(0, st_)
    for i in range(min(LOOK, len(items))):
        emit_S(i)
    for idx in range(len(items)):
        m, h, g, ngrp = items[idx]
        if g == 0 and m + 1 < NM:
            q_stage(m + 1, h)
        if idx + LOOK < len(items):
            emit_S(idx + LOOK)
        while pending and pending[0][0] <= idx:
            pending.pop(0)[1]()
        emit_rest(idx)
    while pending:
        pending.pop(0)[1]()

    if stop_after in ("C", "C1"):
        dbg["attnT"] = nc.dram_tensor("dbg_attnT", [128, 4 * NOWN * 128], BF16, kind="ExternalOutput").ap()
        if NM < NOWN:
            S.add("dve", lambda e: e.memset(attnT[:, :, NM * 128:], 0.0), r=[], w=["attnpad"])
        allk = [("attnT", m, h) for m in range(NM) for h in range(4)] + ["attnpad"]
        dma(dbg["attnT"], attnT[:, :, :].rearrange("p a b -> p (a b)"), r=allk, w=[], tag="out0")
        S.emit()
        return nc

    S.barrier(bar_tile)
    A.reset(KV0)
    X = A.alloc([128, NOWN, D])
    wglu = A.alloc([128, KC, 1024], BF16)
    woutp = A.alloc([128, KC, 1024], BF16)
    D1W = A.mark()
    crep = A.alloc([128, KC, 128])
    abufs = ada_alloc()
    gta_rep = A.alloc([128, D])
    stg = [A.alloc([128, KC, 128]) for _ in range(2)]
    for s4 in range(4):
        S.bulk_tags.add("Xload%d" % s4)
        for i in range(4):
            m = 4 * s4 + i
            dma(X[:, m, :], xo[m * 128:(m + 1) * 128, :], r=[], w=[("X", m)], tag="Xload%d" % s4)
    cp("dve", crep, cact.unsqueeze(2).broadcast_to([128, KC, 128]), r=["cact"], w=["crep"])
    for g in (4, 5):
        ada_group(g, abufs[g % 2], 7 - 2 * (g % 2))
        cp("dve", gta_rep[:, (g - 4) * 512:(g - 3) * 512], abufs[g % 2][2], r=["ada_modrep%d" % (g % 2)], w=["gta_rep"])
    for pc in range(8):
        xi = pc % 2
        dma(stg[xi], w_in_v[:, :, 1536 + pc * 128:1536 + (pc + 1) * 128], r=[], w=["stg%d" % xi], tag="stg%d" % xi)
        if pc % 2 == 0:
            act(wglu[:, :, pc * 128:(pc + 1) * 128], stg[xi], AF.Copy, r=["stg%d" % xi], w=["wglu"])
        else:
            cp("pool", wglu[:, :, pc * 128:(pc + 1) * 128], stg[xi], r=["stg%d" % xi], w=["wglu"])
    w_out_v = w_out.rearrange("(k p) n -> p k n", p=128)
    for pc in range(8):
        xi = pc % 2
        dma(stg[xi], w_out_v[:, :, pc * 128:(pc + 1) * 128], r=[], w=["stg%d" % xi], tag="stg%d" % xi)
        tt("dve" if pc % 2 == 0 else "pool", woutp[:, :, pc * 128:(pc + 1) * 128], stg[xi],
           gta_rep[:, pc * 128:(pc + 1) * 128].unsqueeze(1).broadcast_to([128, KC, 128]), ALU.mult,
           r=["stg%d" % xi, "gta_rep"], w=["woutp"])
    S.barrier(bar_tile)
    A.reset(D1W)
    xhb = A.alloc([128, D])
    xn = [A.alloc([128, D]) for _ in range(2)]
    hT5 = A.alloc([128, KC, 640], BF16)
    uT = A.alloc([128, 4, 4, 160], BF16)
    sig = [A.alloc([128, 512]) for _ in range(2)]
    sigh = [A.alloc([128, 128]) for _ in range(2)]
    asb = [A.alloc([128, 512]) for _ in range(2)]
    asbh = [A.alloc([128, 128]) for _ in range(2)]
    yv = A.alloc([128, 4, 512])
    ysq = A.alloc([128, 4, 512])
    mean_sb = A.alloc([128, 512])
    m2 = A.alloc([128, 512])
    rstd_bc = A.alloc([128, 512])
    tmpv = [A.alloc([128, 512]) for _ in range(2)]
    convT = A.alloc([128, 4, 512], BF16)
    dg = [A.alloc([128, 128], BF16) for _ in range(8)]
    ssD = [A.alloc([128, 1]) for _ in range(2)]
    rsD = [A.alloc([128, 1]) for _ in range(2)]
    pass
    dgc = {"n": 0}

    NS = 4 if not mini else mini_ns
    hT5b = [hT5, A.alloc([128, KC, 640], BF16)]

    def d1_front(s4, i):
        bi = i % 2
        if i == 4:
            dma(xhb, xh[s4 * 128:(s4 + 1) * 128, :], r=[], w=["xhb"], tag="xhb")
        src, sk = (X[:, 4 * s4 + i, :], ("X", 4 * s4 + i)) if i < 4 else (xhb, "xhb")
        act(junkb, src, AF.Square, r=[sk], w=["junk_sb", "ssD%d" % bi], accum=ssD[bi])
        rstd_from_ss(ssD[bi], rsD[bi], 1.0 / D, "ssD%d" % bi, "ssD%dr" % bi)
        tsc("dve", xn[bi], src, rsD[bi], None, ALU.mult, None, r=[sk, "ssD%dr" % bi], w=["xn%d" % bi])

    def d1_back(s4, i):
        bi = i % 2
        hd = hT5b[s4 % 2][:, :, i * 128:(i + 1) * 128]
        for half in range(2):
            tps([(ps[half][:, kk * 128:(kk + 1) * 128], xn[bi][:, (half * 4 + kk) * 128:(half * 4 + kk + 1) * 128])
                 for kk in range(4)], ident_f, r=["xn%d" % bi, "ident_f"], w=["ps%d" % half])
            for kk in range(4):
                k = half * 4 + kk
                act(hd[:, k, :], ps[half][:, kk * 128:(kk + 1) * 128], AF.Identity,
                    r=["ps%d" % half, "gsc_a", "adacols"], w=["hT5_%d_%d_%d" % (s4 % 2, i, k)],
                    bias=sh_a[:, k:k + 1], scale=gsc_a[:, k:k + 1])

    for i in range(5):
        d1_front(0, i)
        d1_back(0, i)
    def d1_glu(s4):
        hT5 = hT5b[s4 % 2]
        nxt = s4 + 1 < NS
        allh = ["hT5_%d_%d_%d" % (s4 % 2, i, k) for i in range(5) for k in range(KC)]
        for cc in range(4):
            pb = cc % 2
            ba_, bg_ = bgluT[:, cc:cc + 1], bgluT[:, 4 + cc:5 + cc]
            pa, pg, ph = 2 + 3 * pb, 3 + 3 * pb, 4 + 3 * pb
            mms([(ps[pa][:, :], wglu[:, k, cc * 128:(cc + 1) * 128], hT5[:, k, 0:512], k == 0, k == KC - 1)
                 for k in range(KC)], r=allh + ["wglu"], w=["ps%d" % pa])
            mms([(ps[pg][:, :], wglu[:, k, 512 + cc * 128:512 + (cc + 1) * 128], hT5[:, k, 0:512], k == 0, k == KC - 1)
                 for k in range(KC)], r=allh + ["wglu"], w=["ps%d" % pg])
            mms([(ps[ph][:, 0:128], wglu[:, k, cc * 128:(cc + 1) * 128], hT5[:, k, 512:640], k == 0, k == KC - 1)
                 for k in range(KC)] +
                [(ps[ph][:, 128:256], wglu[:, k, 512 + cc * 128:512 + (cc + 1) * 128], hT5[:, k, 512:640], k == 0, k == KC - 1)
                 for k in range(KC)], r=allh + ["wglu"], w=["ps%d" % ph])
            act(sig[pb], ps[pg][:, :], AF.Sigmoid, r=["ps%d" % pg, "bgluT"], w=["sig%d" % pb], bias=bg_)
            act(sigh[pb], ps[ph][:, 128:256], AF.Sigmoid, r=["ps%d" % ph, "bgluT"], w=["sigh%d" % pb], bias=bg_)
            act(asb[pb], ps[pa][:, :], AF.Identity, r=["ps%d" % pa, "bgluT"], w=["asb%d" % pb], bias=ba_)
            act(asbh[pb], ps[ph][:, 0:128], AF.Identity, r=["ps%d" % ph, "bgluT"], w=["asbh%d" % pb], bias=ba_)
            tt("dve", uT[:, cc, :, 32:160], asb[pb][:, :].rearrange("p (i t) -> p i t", i=4),
               sig[pb][:, :].rearrange("p (i t) -> p i t", i=4), ALU.mult,
               r=["asb%d" % pb, "sig%d" % pb], w=["uT%d" % cc])
            tt("dve", uT[:, cc, :, 0:32], asbh[pb][:, :].rearrange("p (i t) -> p i t", i=4),
               sigh[pb][:, :].rearrange("p (i t) -> p i t", i=4), ALU.mult,
               r=["asbh%d" % pb, "sigh%d" % pb], w=["uT%d" % cc])
            if s4 == 0:
                tsc("dve", uT[:, cc, 0, 0:32], uT[:, cc, 0, 0:32], hmask[:, 0:1], None, ALU.mult, None,
                    r=["uT%d" % cc, "hmask"], w=["uT%d" % cc])

    def d1_conv(s4):
        hT5 = hT5b[s4 % 2]
        nxt = s4 + 1 < NS
        allh = ["hT5_%d_%d_%d" % (s4 % 2, i, k) for i in range(5) for k in range(KC)]
        if nxt:
            d1_front(s4 + 1, 0)
        for cc in range(4):
            cb_ = 2 + 3 * (cc % 2)
            for k in range(31):
                sl = dgc["n"] % 8
                dgc["n"] += 1
                tsc("dve", dg[sl], ident_b, wdwT[:, cc, k:k + 1], None, ALU.mult, None,
                    r=["ident_b", "wdwT"], w=["dg%d" % sl])
                mms([(ps[cb_][:, :], dg[sl], uT[:, cc, :, 2 + k:130 + k], k == 0, k == 30)],
                    r=["dg%d" % sl, "uT%d" % cc], w=["ps%d" % cb_])
            act(yv[:, cc, :], ps[cb_][:, :], AF.Identity, r=["ps%d" % cb_, "bdwT"], w=["yv%d" % cc], bias=bdwT[:, cc:cc + 1])
            act(ysq[:, cc, :], yv[:, cc, :], AF.Square, r=["yv%d" % cc], w=["ysq%d" % cc])
            if nxt:
                d1_back(s4 + 1, cc)
                d1_front(s4 + 1, cc + 1)

    def d1_ln(s4):
        hT5 = hT5b[s4 % 2]
        nxt = s4 + 1 < NS
        allh = ["hT5_%d_%d_%d" % (s4 % 2, i, k) for i in range(5) for k in range(KC)]
        mms([(ps[3][:, :], ones_ln, yv[:, cc, :], cc == 0, cc == 3) for cc in range(4)],
            r=["yv%d" % cc for cc in range(4)] + ["ones_ln"], w=["ps3"])
        mms([(ps[4][:, :], ones_ln, ysq[:, cc, :], cc == 0, cc == 3) for cc in range(4)],
            r=["ysq%d" % cc for cc in range(4)] + ["ones_ln"], w=["ps4"])
        if nxt:
            d1_back(s4 + 1, 4)
        act(mean_sb, ps[3][:, :], AF.Copy, r=["ps3"], w=["mean_sb"])
        tt("dve", m2, mean_sb, mean_sb, ALU.mult, r=["mean_sb"], w=["m2"])
        tt("dve", m2, ps[4][:, :], m2, ALU.subtract, r=["ps4", "m2"], w=["m2"])
        rstd_from_ss(m2, rstd_bc, 1.0, "m2", "rstd_bc")
        for cc in range(4):
            tb = cc % 2
            tt("dve", tmpv[tb], yv[:, cc, :], mean_sb, ALU.subtract, r=["yv%d" % cc, "mean_sb"], w=["tmpv%d" % tb])
            tt("dve", tmpv[tb], tmpv[tb], rstd_bc, ALU.mult, r=["tmpv%d" % tb, "rstd_bc"], w=["tmpv%d" % tb])
            act(convT[:, cc, :], tmpv[tb], AF.Silu, r=["tmpv%d" % tb, "lngT", "lnbT"], w=["convT%d" % cc],
                bias=lnbT[:, cc:cc + 1], scale=lngT[:, cc:cc + 1])

    def d1_out(s4):
        hT5 = hT5b[s4 % 2]
        nxt = s4 + 1 < NS
        allh = ["hT5_%d_%d_%d" % (s4 % 2, i, k) for i in range(5) for k in range(KC)]
        for i in range(4):
            m = 4 * s4 + i
            for half in range(2):
                ob_ = 5 + (i * 2 + half) % 3
                items = []
                for k in range(8):
                    lh = attnT[:, k, m * 128:(m + 1) * 128] if k < 4 else convT[:, k - 4, i * 128:(i + 1) * 128]
                    items.append((ps[ob_][:, :], lh, woutp[:, k, half * 512:(half + 1) * 512], k == 0, k == 7))
                mms(items, r=[("attnT", m, h) for h in range(4)] + ["convT%d" % c_ for c_ in range(4)] + ["woutp"],
                    w=["ps%d" % ob_])
                tt("dve", X[:, m, half * 512:(half + 1) * 512], ps[ob_][:, :], X[:, m, half * 512:(half + 1) * 512],
                   ALU.add, r=["ps%d" % ob_, ("X", m)], w=[("X", m)])


    d1_glu(0)
    for s4 in range(NS):
        d1_conv(s4)
        d1_ln(s4)
        if s4 + 1 < NS:
            d1_glu(s4 + 1)
        d1_out(s4)

    if stop_after in ("D1", "D1a"):
        dbg["X"] = nc.dram_tensor("dbg_X", [NOWN * 128, D], F32, kind="ExternalOutput").ap()
        for m in range(4 * NS):
            dma(dbg["X"][m * 128:(m + 1) * 128, :], X[:, m, :], r=[("X", m)], w=[], tag="outX")
        S.emit()
        return nc

    S.barrier(bar_tile)
    A.reset(D1W - 8192)
    h2T = A.alloc([128, KC, NOWN * 128], BF16)
    cmb = A.alloc([128, NOWN, 32])
    gtf_rep = A.alloc([128, D])
    D2W = A.mark()
    crep = A.alloc([128, KC, 128])
    abufs = ada_alloc()
    xn = [A.alloc([128, D]) for _ in range(2)]
    h2f = [A.alloc([128, KC, 128]) for _ in range(2)]
    rs_ = [A.alloc([128, 128]) for _ in range(2)]
    ssD = [A.alloc([128, 1]) for _ in range(2)]
    rsD = [A.alloc([128, 1]) for _ in range(2)]
    cp("dve", crep, cact.unsqueeze(2).broadcast_to([128, KC, 128]), r=["cact"], w=["crep"])
    for g in range(6, 10):
        ada_group(g, abufs[g % 2], 7 - 2 * (g % 2))
        ada_to_cols(abufs[g % 2], sh_f if g < 8 else sc_f, (g % 2) * 4, 6 - 2 * (g % 2))
    tsc("dve", gsc_f, sc_f, 1.0, None, ALU.add, None, r=["adacols"], w=["gsc_f"])
    tt("dve", gsc_f, gsc_f, gffnT, ALU.mult, r=["gsc_f", "gffnT"], w=["gsc_f"])
    for g in (10, 11):
        ada_group(g, abufs[g % 2], 7 - 2 * (g % 2))
        cp("dve", gtf_rep[:, (g - 10) * 512:(g - 9) * 512], abufs[g % 2][2], r=["ada_modrep%d" % (g % 2)], w=["gtf_rep"])

    NB2 = NOWN if not mini else 4
    NRS = 5
    rs_ = rs_ + [A.alloc([128, 128]) for _ in range(NRS - 2)]

    def rviews(m):
        R = rs_[m % NRS]
        v = dict(lg=R[:, 0:36], gmax=R[:, 36:37], ngmax=R[:, 37:38], gsum=R[:, 38:39], gp=R[:, 39:40],
                 ohg=R[:, 40:44], junk4=R[:, 44:48], esel=R[:, 48:80], ein=R[:, 80:88], top8=R[:, 88:96],
                 eq2=R[:, 112:120], cmb8=R[:, 120:128])
        for i_, nm in enumerate(("dcol", "ed", "den", "w1", "w2", "w1g", "w2g")):
            v[nm] = R[:, 96 + i_:97 + i_]
        return v, "rs%d" % (m % NRS)

    def d_n1(m):
        bi = m % 2
        act(junkb, X[:, m, :], AF.Square, r=[("X", m)], w=["junk_sb", "ssD%d" % bi], accum=ssD[bi])
        rstd_from_ss(ssD[bi], rsD[bi], 1.0 / D, "ssD%d" % bi, "ssD%dr" % bi)

    def d_n2(m):
        bi = m % 2
        tsc("dve", xn[bi], X[:, m, :], rsD[bi], None, ALU.mult, None, r=[("X", m), "ssD%dr" % bi], w=["xn%d" % bi])

    def d_n3(m):
        bi = m % 2
        for half in range(2):
            tps([(ps[half][:, kk * 128:(kk + 1) * 128], xn[bi][:, (half * 4 + kk) * 128:(half * 4 + kk + 1) * 128])
                 for kk in range(4)], ident_f, r=["xn%d" % bi, "ident_f"], w=["ps%d" % half])

    def d_n4(m):
        bi = m % 2
        for half in range(2):
            for kk in range(4):
                k = half * 4 + kk
                act(h2f[bi][:, k, :], ps[half][:, kk * 128:(kk + 1) * 128], AF.Identity,
                    r=["ps%d" % half, "gsc_f", "adacols"], w=["h2f%d_%d" % (bi, k)],
                    bias=sh_f[:, k:k + 1], scale=gsc_f[:, k:k + 1])

    def d_n5(m):
        bi = m % 2
        hks = ["h2f%d_%d" % (bi, k) for k in range(KC)]
        cp("pool", h2T[:, :, m * 128:(m + 1) * 128], h2f[bi], r=hks, w=[("h2T", m)])
        mms([(ps[2][:, 0:36], h2f[bi][:, k, :], wr[:, k, :], k == 0, k == KC - 1) for k in range(KC)],
            r=hks + ["wr"], w=["ps2"])

    def d_cA(m):
        v, rk_ = rviews(m)
        tt("dve", v["lg"], ps[2][:, 0:36], br_rep, ALU.add, r=["ps2", "br_rep"], w=[rk_])
        S.add("dve", lambda e, gmax=v["gmax"], lg=v["lg"]: e.tensor_reduce(gmax, lg[:, 0:4], AX.X, ALU.max), r=[rk_], w=[rk_])
        tsc("dve", v["ngmax"], v["gmax"], -1.0, None, ALU.mult, None, r=[rk_], w=[rk_])

    def d_cB(m):
        v, rk_ = rviews(m)
        act(v["junk4"], v["lg"][:, 0:4], AF.Exp, r=[rk_], w=[rk_], bias=v["ngmax"], scale=1.0, accum=v["gsum"])

    def d_cC(m):
        v, rk_ = rviews(m)
        lg, ohg, esel, ein, top8 = v["lg"], v["ohg"], v["esel"], v["ein"], v["top8"]
        S.add("dve", lambda e, gp=v["gp"], gsum=v["gsum"]: e.reciprocal(gp, gsum), r=[rk_], w=[rk_])
        tsc("dve", ohg, lg[:, 0:4], v["gmax"], None, ALU.is_ge, None, r=[rk_], w=[rk_])
        tt("dve", esel.rearrange("p (g e) -> p g e", g=4), lg[:, 4:36].rearrange("p (g e) -> p g e", g=4),
           ohg.unsqueeze(2).broadcast_to([128, 4, 8]), ALU.mult, r=[rk_], w=[rk_])
        S.add("dve", lambda e, ein=ein, esel=esel: e.tensor_reduce(ein, esel.rearrange("p (g e) -> p e g", g=4), AX.X, ALU.add),
              r=[rk_], w=[rk_])
        S.add("dve", lambda e, top8=top8, ein=ein: e.max(top8, ein), r=[rk_], w=[rk_])
        tt("dve", v["dcol"], top8[:, 1:2], top8[:, 0:1], ALU.subtract, r=[rk_], w=[rk_])

    def d_cD(m):
        v, rk_ = rviews(m)
        act(v["ed"], v["dcol"], AF.Exp, r=[rk_], w=[rk_])

    def d_cE(m):
        v, rk_ = rviews(m)
        ed, den, w1, w2, w1g, w2g, gp = v["ed"], v["den"], v["w1"], v["w2"], v["w1g"], v["w2g"], v["gp"]
        ein, top8, cmb8, eq2, ohg = v["ein"], v["top8"], v["cmb8"], v["eq2"], v["ohg"]
        tsc("dve", den, ed, 1.0, None, ALU.add, None, r=[rk_], w=[rk_])
        S.add("dve", lambda e, w1=w1, den=den: e.reciprocal(w1, den), r=[rk_], w=[rk_])
        tt("dve", w2, ed, w1, ALU.mult, r=[rk_], w=[rk_])
        tt("dve", w1g, w1, gp, ALU.mult, r=[rk_], w=[rk_])
        tt("dve", w2g, w2, gp, ALU.mult, r=[rk_], w=[rk_])
        tsc("dve", cmb8, ein, top8[:, 0:1], None, ALU.is_equal, None, r=[rk_], w=[rk_])
        tsc("dve", cmb8, cmb8, w1g, None, ALU.mult, None, r=[rk_], w=[rk_])
        tsc("dve", eq2, ein, top8[:, 1:2], None, ALU.is_equal, None, r=[rk_], w=[rk_])
        tsc("dve", eq2, eq2, w2g, None, ALU.mult, None, r=[rk_], w=[rk_])
        tt("dve", cmb8, cmb8, eq2, ALU.add, r=[rk_], w=[rk_])
        tt("dve", cmb[:, m, :].rearrange("p (g e) -> p g e", g=4), ohg.unsqueeze(2).broadcast_to([128, 4, 8]),
           cmb8.unsqueeze(1).broadcast_to([128, 4, 8]), ALU.mult, r=[rk_], w=[("cmb", m)])

    d_order = [(d_cE, 9), (d_cD, 8), (d_cC, 7), (d_cB, 6), (d_cA, 5), (d_n5, 4), (d_n4, 3), (d_n3, 2), (d_n2, 1), (d_n1, 0)]
    for t in range(NB2 + 9):
        for fn_, off_ in d_order:
            m = t - off_
            if 0 <= m < NB2:
                fn_(m)

    if stop_after == "D2":
        dbg["cmb"] = nc.dram_tensor("dbg_cmb", [128, NOWN * 32], F32, kind="ExternalOutput").ap()
        dbg["h2T"] = nc.dram_tensor("dbg_h2T", [128, KC * NOWN * 128], BF16, kind="ExternalOutput").ap()
        if mini:
            S.add("dve", lambda e: e.memset(cmb[:, NB2:, :], 0.0), r=[], w=["cmbpad"])
            S.add("dve", lambda e: e.memset(h2T[:, :, NB2 * 128:], 0.0), r=[], w=["h2pad"])
        dma(dbg["cmb"], cmb[:, :, :].rearrange("p a b -> p (a b)"), r=[("cmb", m) for m in range(NB2)] + ["cmbpad"], w=[], tag="out0")
        dma(dbg["h2T"], h2T[:, :, :].rearrange("p a b -> p (a b)"), r=[("h2T", m) for m in range(NB2)] + ["h2pad"], w=[], tag="out1")
        S.emit()
        return nc

    S.barrier(bar_tile)
    A.reset(D2W)
    Wg = [A.alloc([128, KC, 256], BF16) for _ in range(2)]
    Wu = [A.alloc([128, KC, 256], BF16) for _ in range(2)]
    Wd = [A.alloc([128, 2, D], BF16) for _ in range(2)]
    stE = [A.alloc([128, 2048]) for _ in range(4)]
    hid = [A.alloc([128, 2, 512], BF16) for _ in range(2)]
    sgl = [A.alloc([128, 512], BF16) for _ in range(2)]
    tacc = [A.alloc([128, 512]) for _ in range(2)]
    pass
    stc = {"n": 0}

    def load_expert(e_):
        eb = e_ % 2
        for which in range(3):
            sl = stc["n"] % 4
            stc["n"] += 1
            sk = "stE%d" % sl
            if which < 2:
                srcw = (w_gate if which == 0 else w_up)[e_].rearrange("(k p) f -> p k f", p=128)
                stv = stE[sl][:, :].rearrange("p (k f) -> p k f", k=KC)
                dma(stv, srcw, r=[], w=[sk], tag=sk)
                if which == 0:
                    cp("pool", Wg[eb], stv, r=[sk], w=["Wg%d" % eb])
                else:
                    act(Wu[eb], stv, AF.Copy, r=[sk], w=["Wu%d" % eb])
            else:
                srcw = w_down[e_].rearrange("(c p) n -> p c n", p=128)
                stv = stE[sl][:, :].rearrange("p (c n) -> p c n", c=2)
                dma(stv, srcw, r=[], w=[sk], tag=sk)
                tt("pool", Wd[eb], stv, gtf_rep.unsqueeze(1).broadcast_to([128, 2, D]), ALU.mult,
                   r=[sk, "gtf_rep"], w=["Wd%d" % eb])

    def gate_up(e_, s4, hb):
        eb = e_ % 2
        hk = [("h2T", 4 * s4 + i) for i in range(4)]
        for fc in range(2):
            mms([(ps[2 * fc][:, :], Wg[eb][:, k, fc * 128:(fc + 1) * 128], h2T[:, k, s4 * 512:(s4 + 1) * 512], k == 0, k == KC - 1)
                 for k in range(KC)], r=hk + ["Wg%d" % eb], w=["ps%d" % (2 * fc)])
            mms([(ps[2 * fc + 1][:, :], Wu[eb][:, k, fc * 128:(fc + 1) * 128], h2T[:, k, s4 * 512:(s4 + 1) * 512], k == 0, k == KC - 1)
                 for k in range(KC)], r=hk + ["Wu%d" % eb], w=["ps%d" % (2 * fc + 1)])
            act(sgl[fc], ps[2 * fc][:, :], AF.Silu, r=["ps%d" % (2 * fc)], w=["sgl%d" % fc])
            tt("dve", hid[hb][:, fc, :], ps[2 * fc + 1][:, :], sgl[fc], ALU.mult,
               r=["ps%d" % (2 * fc + 1), "sgl%d" % fc], w=["hid%d_%d" % (hb, fc)])

    def down(e_, s4, hb):
        eb = e_ % 2
        for i in range(4):
            m = 4 * s4 + i
            for half in range(2):
                ob_ = 4 + (i * 2 + half) % 4
                mms([(ps[ob_][:, :], hid[hb][:, fc, i * 128:(i + 1) * 128], Wd[eb][:, fc, half * 512:(half + 1) * 512], fc == 0, fc == 1)
                     for fc in range(2)], r=["hid%d_0" % hb, "hid%d_1" % hb, "Wd%d" % eb], w=["ps%d" % ob_])
                tb_ = (i * 2 + half) % 2
                act(tacc[tb_], ps[ob_][:, :], AF.Identity, r=["ps%d" % ob_, ("cmb", m)], w=["tacc%d" % tb_],
                    scale=cmb[:, m, e_:e_ + 1])
                tt("dve", X[:, m, half * 512:(half + 1) * 512], X[:, m, half * 512:(half + 1) * 512], tacc[tb_],
                   ALU.add, r=["tacc%d" % tb_, ("X", m)], w=[("X", m)])

    NE = NEXP
    load_expert(0)
    prev = None
    step = 0
    for e_ in range(NE):
        for s4 in range(4 if not mini else 1):
            hb = step % 2
            step += 1
            gate_up(e_, s4, hb)
            if prev is not None:
                down(*prev)
            prev = (e_, s4, hb)
            if s4 == 0 and e_ + 1 < NE:
                load_expert(e_ + 1)
    down(*prev)

    for m in range(NOWN if not mini else 4):
        dma(out_d[m * 128:(m + 1) * 128, :], X[:, m, :], r=[("X", m)], w=[], tag="outX")
    S.emit()
    return nc


def host_inputs(inputs, core):
    b, j = core // 4, core % 4
    f = np.float32
    x = np.asarray(inputs["x"], f)
    xb_ = np.ascontiguousarray(x[b])
    own_idx = np.concatenate([np.arange(512 * m + 128 * j, 512 * m + 128 * j + 128) for m in range(NOWN)])
    xo_ = np.ascontiguousarray(xb_[own_idx])
    xh_ = np.zeros((4 * 128, D), f)
    for m in range(NOWN):
        st = 512 * m + 128 * j - 32
        if st >= 0:
            xh_[m * 32:(m + 1) * 32] = xb_[st:st + 32]
    pos = np.asarray(inputs["positions"], np.int32)[b]
    posb_ = np.ascontiguousarray(pos.reshape(NBLK_B, 128).T)
    poso_ = np.ascontiguousarray(pos[own_idx].reshape(NOWN, 128).T)

    def colT(v, n):
        return np.ascontiguousarray(np.asarray(v, f).reshape(n, 128).T)

    kq = np.arange(128)
    cm = np.zeros((128, 4, 2, 128), f)
    for t in range(4):
        allowed = (t * 128 + kq[:, None]) <= (j * 128 + kq[None, :])
        cm[:, t, :, :] = allowed[:, None, :]
    d = {
        "xb": xb_, "xo": xo_, "xh": xh_,
        "cT": colT(inputs["c"][b], KC),
        "posb": posb_, "poso": poso_,
        "w_ada": np.ascontiguousarray(np.asarray(inputs["w_ada"], f)[0]),
        "b_ada": np.ascontiguousarray(np.asarray(inputs["b_ada"], f)[0:1]),
        "g_mixT": colT(inputs["g_mix"][0], KC),
        "g_ffnT": colT(inputs["g_ffn"][0], KC),
        "w_in": np.ascontiguousarray(np.asarray(inputs["w_in"], f)[0]),
        "q_norm_g": np.asarray(inputs["q_norm_g"], f)[0:1],
        "k_norm_g": np.asarray(inputs["k_norm_g"], f)[0:1],
        "lambda_q1": np.asarray(inputs["lambda_q1"], f)[0:1],
        "lambda_k1": np.asarray(inputs["lambda_k1"], f)[0:1],
        "lambda_q2": np.asarray(inputs["lambda_q2"], f)[0:1],
        "lambda_k2": np.asarray(inputs["lambda_k2"], f)[0:1],
        "subln_g": np.asarray(inputs["subln_g"], f)[0:1],
        "b_gluT": colT(inputs["b_glu"][0], 8),
        "w_dwT": np.ascontiguousarray(np.asarray(inputs["w_dw"], f)[0].T.reshape(4, 128, 31).transpose(1, 0, 2)),
        "b_dwT": colT(inputs["b_dw"][0], 4),
        "ln_gT": colT(inputs["conv_ln_g"][0], 4),
        "ln_bT": colT(inputs["conv_ln_b"][0], 4),
        "w_out": np.ascontiguousarray(np.asarray(inputs["w_out"], f)[0]),
        "w_rt": np.ascontiguousarray(np.concatenate([np.asarray(inputs["w_group"], f)[0],
                                                     np.asarray(inputs["w_router"], f)[0]], axis=1)),
        "b_rt": np.ascontiguousarray(np.concatenate([np.asarray(inputs["b_group"], f)[0],
                                                     np.asarray(inputs["b_router"], f)[0]])[None, :]),
        "w_gate": np.ascontiguousarray(np.asarray(inputs["w_gate"], f)[0]),
        "w_up": np.ascontiguousarray(np.asarray(inputs["w_up"], f)[0]),
        "w_down": np.ascontiguousarray(np.asarray(inputs["w_down"], f)[0]),
        "ident_f": np.eye(128, dtype=f),
        "ident_b": np.eye(128, dtype=f).astype(ml_dtypes.bfloat16),
        "inv_freq": (500000.0 ** (-np.arange(0, 16, 2, dtype=f) / 16.0)).astype(f)[None, :],
        "cmask": cm.reshape(128, 1024).astype(ml_dtypes.bfloat16),
        "hmask": np.full((128, 1), 0.0 if j == 0 else 1.0, f),
    }
    return d, own_idx


def kernel(**inputs):
    nc = build_program()
    in_maps = []
    owns = []
    for c in range(8):
        d, own_idx = host_inputs(inputs, c)
        in_maps.append(d)
        owns.append(own_idx)
    res = run_bass_kernel_spmd(nc, in_maps, core_ids=list(range(8)))
    out = np.zeros((2, SEQ, D), np.float32)
    for c in range(8):
        out[c // 4, owns[c]] = np.asarray(res.results[c]["out"], np.float32)
    return out
```

```python
import math
import numpy as np
import ml_dtypes
import concourse.bass as bass
import concourse.mybir as mybir
from concourse.bass_utils import run_bass_kernel_spmd

F32 = mybir.dt.float32
BF16 = mybir.dt.bfloat16
I32 = mybir.dt.int32
AF = mybir.ActivationFunctionType
ALU = mybir.AluOpType
AX = mybir.AxisListType

D = 1024
KC = 8
SEQ = 8192
NBLK_B = 64
NOWN = 16
EPS = 1e-6
LAMBDA_INIT = 0.8 - 0.6 * math.exp(-0.3 * 0)
NEXP = 32
TWO_PI = 2.0 * math.pi
C1 = 6.28125
C2 = TWO_PI - C1


class Sched:
    COMPUTE = ("pe", "act", "dve", "pool")

    def __init__(self, nc):
        self.nc = nc
        self.ops = []
        self.last_w = {}
        self.readers = {}
        self.tag_count = {}
        self.bulk_tags = set()
        self.epoch = None
        self.epoch_start = 0

    def add(self, eng, fn, r=(), w=(), tag=None):
        idx = len(self.ops)
        deps = set()
        for res in r:
            if res in self.last_w:
                deps.add(self.last_w[res])
        for res in w:
            if res in self.last_w:
                deps.add(self.last_w[res])
            for i in self.readers.get(res, {}).values():
                deps.add(i)
        if self.epoch is not None:
            deps.add(self.epoch)
        op = dict(eng=eng, fn=fn, deps=deps, tag=tag, signal=False, idx=idx)
        if tag is not None:
            self.tag_count[tag] = self.tag_count.get(tag, 0) + 1
            op["tagn"] = self.tag_count[tag]
        self.ops.append(op)
        for res in w:
            self.last_w[res] = idx
            self.readers[res] = {}
        for res in r:
            key = eng if tag is None else ("dma", idx)
            self.readers.setdefault(res, {})[key] = idx
        return idx

    def barrier(self, tile):
        deps = set()
        last = {}
        for op in self.ops[self.epoch_start:]:
            if op["tag"] is None:
                last[op["eng"]] = op["idx"]
            else:
                deps.add(op["idx"])
        deps |= set(last.values())
        idx = self.add("dve", lambda e: e.memset(tile, 0.0))
        self.ops[idx]["deps"] |= deps
        self.epoch = idx
        self.epoch_start = idx

    def emit(self):
        nc = self.nc
        ops = self.ops
        for op in ops:
            for d in op["deps"]:
                dop = ops[d]
                if dop["tag"] is None and dop["eng"] == "pe" and op["eng"] == "pe" and op["tag"] is None:
                    continue
                dop["signal"] = True
        sems = {e: nc.alloc_semaphore(name="sem_" + e) for e in self.COMPUTE}
        tagsem = {t: nc.alloc_semaphore(name="semt_" + str(t)) for t in self.tag_count}
        cnt = {e: 0 for e in self.COMPUTE}
        for op in ops:
            if op["tag"] is not None:
                t = op["tag"]
                n = self.tag_count[t] if t in self.bulk_tags else op["tagn"]
                op["token"] = (tagsem[t], 16 * n, ("t", t))
            elif op["signal"]:
                cnt[op["eng"]] += 1
                op["token"] = (sems[op["eng"]], cnt[op["eng"]], ("e", op["eng"]))
        streams = {e: [] for e in ("pe", "act", "dve", "pool", "sp")}
        for op in ops:
            streams[op["eng"]].append(op)
        out_tag_total = {t: 16 * n for t, n in self.tag_count.items()}

        def run(engname, e):
            waited = {}
            for op in streams[engname]:
                need = {}
                for d in op["deps"]:
                    dop = ops[d]
                    if dop["tag"] is None and dop["eng"] == "pe" and engname == "pe" and op["tag"] is None:
                        continue
                    sem, val, key = dop["token"]
                    if need.get(key, (None, 0))[1] < val:
                        need[key] = (sem, val)
                for key, (sem, val) in need.items():
                    if waited.get(key, 0) >= val:
                        continue
                    e.wait_ge(sem, val)
                    waited[key] = val
                ins = op["fn"](e)
                if op["tag"] is not None:
                    ins.then_inc(op["token"][0], 16)
                elif op["signal"]:
                    ins.then_inc(op["token"][0], 1)
            if engname == "sp":
                for t, tot in out_tag_total.items():
                    if str(t).startswith("out"):
                        e.wait_ge(tagsem[t], tot)

        with nc.Block() as block:
            @block.tensor
            def _(e):
                run("pe", e)

            @block.scalar
            def _(e):
                run("act", e)

            @block.vector
            def _(e):
                run("dve", e)

            @block.gpsimd
            def _(e):
                run("pool", e)

            @block.sync
            def _(e):
                run("sp", e)


class Arena:
    def __init__(self, nc, words):
        self.t = nc.alloc_sbuf_tensor("arena", [128, words], F32)
        self.words = words
        self.off = 0

    def mark(self):
        return self.off

    def reset(self, m):
        self.off = m

    def alloc(self, shape, dt=F32):
        n = 1
        for s in shape[1:]:
            n *= s
        w = n if dt in (F32, I32) else (n + 1) // 2
        w = (w + 7) // 8 * 8
        assert self.off + w <= self.words, ("arena overflow", self.off, w, self.words)
        v = self.t[:, self.off:self.off + w]
        self.off += w
        if dt != F32:
            v = v.bitcast(dt)
        v = v[:, 0:n]
        if len(shape) == 3:
            v = v.rearrange("p (a b) -> p a b", a=shape[1])
        elif len(shape) == 4:
            v = v.rearrange("p (a b c) -> p a b c", a=shape[1], b=shape[2])
        return v


def build_program(stop_after=None, mini=False, mini_ns=1):
    nc = bass.Bass("TRN2", target_bir_lowering=False)
    S = Sched(nc)
    S.bulk_tags.add("const")

    def din(name, shape, dt=F32):
        return nc.dram_tensor(name, list(shape), dt, kind="ExternalInput").ap()

    xb = din("xb", [SEQ, D])
    xo = din("xo", [NOWN * 128, D])
    xh = din("xh", [4 * 128, D])
    cT_d = din("cT", [128, KC])
    posb_d = din("posb", [128, NBLK_B], I32)
    poso_d = din("poso", [128, NOWN], I32)
    w_ada = din("w_ada", [D, 6 * D])
    b_ada = din("b_ada", [1, 6 * D])
    gmix_d = din("g_mixT", [128, KC])
    gffn_d = din("g_ffnT", [128, KC])
    w_in = din("w_in", [D, 2560])
    gq_d = din("q_norm_g", [1, 64])
    gk_d = din("k_norm_g", [1, 64])
    lq1_d = din("lambda_q1", [1, 64])
    lk1_d = din("lambda_k1", [1, 64])
    lq2_d = din("lambda_q2", [1, 64])
    lk2_d = din("lambda_k2", [1, 64])
    subg_d = din("subln_g", [1, 128])
    bglu_d = din("b_gluT", [128, 8])
    wdw_d = din("w_dwT", [128, 4, 31])
    bdw_d = din("b_dwT", [128, 4])
    lng_d = din("ln_gT", [128, 4])
    lnb_d = din("ln_bT", [128, 4])
    w_out = din("w_out", [D, D])
    wr_d = din("w_rt", [D, 36])
    br_d = din("b_rt", [1, 36])
    w_gate = din("w_gate", [NEXP, D, 256])
    w_up = din("w_up", [NEXP, D, 256])
    w_down = din("w_down", [NEXP, 256, D])
    identf_d = din("ident_f", [128, 128])
    identb_d = din("ident_b", [128, 128], BF16)
    invf_d = din("inv_freq", [1, 8])
    mask_d = din("cmask", [128, 4 * 256], BF16)
    hmask_d = din("hmask", [128, 1])
    out_d = nc.dram_tensor("out", [NOWN * 128, D], F32, kind="ExternalOutput").ap()
    dbg = {}

    A = Arena(nc, 53000)
    ps = [nc.alloc_psum_tensor("ps%d" % i, [128, 512], F32) for i in range(8)]

    def psb(i):
        return ps[i][:, :].bitcast(BF16)

    ident_f = A.alloc([128, 128])
    ident_b = A.alloc([128, 128], BF16)
    cT = A.alloc([128, KC])
    cact = A.alloc([128, KC])
    gmixT = A.alloc([128, KC])
    gffnT = A.alloc([128, KC])
    sh_a = A.alloc([128, KC]); sc_a = A.alloc([128, KC]); gsc_a = A.alloc([128, KC])
    sh_f = A.alloc([128, KC]); sc_f = A.alloc([128, KC]); gsc_f = A.alloc([128, KC])
    gq_rep = A.alloc([128, 64]); gk_rep = A.alloc([128, 64])
    lvec = A.alloc([128, 4, 64])
    sg_rep = A.alloc([128, 128])
    lam_t = A.alloc([128, 8])
    bgluT = A.alloc([128, 8])
    wdwT = A.alloc([128, 4, 31])
    bdwT = A.alloc([128, 4]); lngT = A.alloc([128, 4]); lnbT = A.alloc([128, 4])
    wr = A.alloc([128, KC, 36])
    br_rep = A.alloc([128, 36])
    invf = A.alloc([128, 8])
    cmask = A.alloc([128, 4 * 256], BF16)
    hmask = A.alloc([128, 1])
    cos_b = A.alloc([128, NBLK_B, 8]); sin_b = A.alloc([128, NBLK_B, 8])
    cos_o = A.alloc([128, NOWN, 8]); sin_o = A.alloc([128, NOWN, 8])
    epsc = A.alloc([128, 1]); m8c = A.alloc([128, 1])
    ones_ln = A.alloc([128, 128])
    ATTN_OFF = A.mark()
    attnT = A.alloc([128, 4, NOWN * 128], BF16)
    small = A.alloc([128, 64])
    junk_sb = A.alloc([128, D], BF16)
    P_END = A.mark()

    def dma(out, in_, r, w, tag):
        S.add("sp", lambda e, o=out, i=in_: e.dma_start(out=o, in_=i), r=r, w=w, tag=tag)

    def act(out, in_, func, r, w, bias=None, scale=None, accum=None):
        def f(e, out=out, in_=in_, func=func, bias=bias, scale=scale, accum=accum):
            kw = {}
            if bias is not None:
                kw["bias"] = bias
            if scale is not None:
                kw["scale"] = scale
            if accum is not None:
                kw["accum_out"] = accum
            return e.activation(out, in_, func, **kw)
        S.add("act", f, r=r, w=w)

    def tsc(eng, out, in0, s1, s2, op0, op1, r, w):
        def f(e, out=out, in0=in0, s1=s1, s2=s2, op0=op0, op1=op1):
            if op1 is None:
                return e.tensor_scalar(out, in0, s1, None, op0)
            return e.tensor_scalar(out, in0, s1, s2, op0, op1)
        S.add(eng, f, r=r, w=w)

    def tt(eng, out, in0, in1, op, r, w):
        S.add(eng, lambda e, o=out, a=in0, b=in1, op=op: e.tensor_tensor(o, a, b, op), r=r, w=w)

    def stt(out, in0, sc, in1, op0, op1, r, w):
        S.add("dve", lambda e, o=out, a=in0, s=sc, b=in1, o0=op0, o1=op1:
              e.scalar_tensor_tensor(o, a, s, b, o0, o1), r=r, w=w)

    def cp(eng, out, in_, r, w):
        S.add(eng, lambda e, o=out, i=in_: e.tensor_copy(o, i), r=r, w=w)

    def mms(items, r, w):
        def f(e, items=items):
            ins = None
            for (o, l, rh, st, sp_) in items:
                ins = e.matmul(o, l, rh, start=st, stop=sp_)
            return ins
        S.add("pe", f, r=r, w=w)

    def tps(items, ident, r, w):
        def f(e, items=items, ident=ident):
            ins = None
            for (o, i) in items:
                ins = e.transpose(o, i, ident)
            return ins
        S.add("pe", f, r=r, w=w)

    def rstd_from_ss(ss_ap, out_ap, inv_n, rk, wk):
        act(out_ap, ss_ap, AF.Ln, r=[rk, "epsc"], w=[wk], bias=epsc[:, 0:1], scale=inv_n)
        act(out_ap, out_ap, AF.Exp, r=[wk], w=[wk], scale=-0.5)

    def cload(dst, src, key):
        dma(dst, src, r=[], w=[key], tag="const")

    cload(ident_f, identf_d, "ident_f")
    cload(ident_b, identb_d, "ident_b")
    cload(cT, cT_d, "cT")
    cload(gmixT, gmix_d, "gmixT")
    cload(gffnT, gffn_d, "gffnT")
    cload(gq_rep, gq_d.broadcast_to([128, 64]), "gq_rep")
    cload(gk_rep, gk_d.broadcast_to([128, 64]), "gk_rep")
    for i, dd in enumerate((lq1_d, lk1_d, lq2_d, lk2_d)):
        cload(lvec[:, i, :], dd.broadcast_to([128, 64]), "lvec%d" % i)
    cload(sg_rep, subg_d.broadcast_to([128, 128]), "sg_rep")
    cload(bgluT, bglu_d, "bgluT")
    cload(wdwT, wdw_d, "wdwT")
    cload(bdwT, bdw_d, "bdwT")
    cload(lngT, lng_d, "lngT")
    cload(lnbT, lnb_d, "lnbT")
    cload(wr, wr_d.rearrange("(k p) n -> p k n", p=128), "wr")
    cload(br_rep, br_d.broadcast_to([128, 36]), "br_rep")
    cload(invf, invf_d.broadcast_to([128, 8]), "invf")
    cload(cmask, mask_d, "cmask")
    cload(hmask, hmask_d, "hmask")
    posb_i = A.alloc([128, NBLK_B], I32)
    poso_i = A.alloc([128, NOWN], I32)
    cload(posb_i, posb_d, "posb_i")
    cload(poso_i, poso_d, "poso_i")

    S.add("dve", lambda e: e.memset(lam_t, 0.0), r=[], w=["lam0", "lam1", "lam23", "lam4", "neglam"])
    S.add("dve", lambda e: e.memset(small, 0.0), r=[], w=["small", "junk64", "ropetab", "gains"])
    S.add("dve", lambda e: e.memset(epsc, EPS), r=[], w=["epsc"])
    S.add("dve", lambda e: e.memset(m8c, -8.0), r=[], w=["m8c"])
    S.add("dve", lambda e: e.memset(ones_ln, 1.0 / 512.0), r=[], w=["ones_ln"])

    act(cact, cT, AF.Silu, r=["cT"], w=["cact"])
    tsc("dve", sg_rep, sg_rep, 1.0 - LAMBDA_INIT, None, ALU.mult, None, r=["sg_rep"], w=["sg_rep"])

    junk64 = small[:, 0:64]
    tt("dve", junk64, lvec[:, 0, :], lvec[:, 1, :], ALU.mult, r=["lvec0", "lvec1"], w=["junk64"])
    S.add("dve", lambda e: e.tensor_reduce(lam_t[:, 0:1], junk64, AX.X, ALU.add), r=["junk64"], w=["lam0"])
    tt("dve", junk64, lvec[:, 2, :], lvec[:, 3, :], ALU.mult, r=["lvec2", "lvec3", "lam0"], w=["junk64"])
    S.add("dve", lambda e: e.tensor_reduce(lam_t[:, 1:2], junk64, AX.X, ALU.add), r=["junk64"], w=["lam1"])
    act(lam_t[:, 2:4], lam_t[:, 0:2], AF.Exp, r=["lam0", "lam1"], w=["lam23"])
    tt("dve", lam_t[:, 4:5], lam_t[:, 3:4], lam_t[:, 2:3], ALU.subtract, r=["lam23"], w=["lam4"])
    tsc("dve", lam_t[:, 7:8], lam_t[:, 4:5], -LAMBDA_INIT, None, ALU.add, None, r=["lam4"], w=["neglam"])
    neglam = lam_t[:, 7:8]

    def rope_tables(pos_i, nblk, cos_t, sin_t, pfx):
        m0 = A.mark()
        posf = A.alloc([128, nblk])
        ang = A.alloc([128, nblk, 8])
        tq = A.alloc([128, nblk, 8])
        ki = A.alloc([128, nblk, 8], I32)
        kf = A.alloc([128, nblk, 8])
        rr = A.alloc([128, nblk, 8])
        mk = A.alloc([128, nblk, 8])
        cp("dve", posf, pos_i, r=[pfx + "pos_i"], w=[pfx + "posf"])
        tt("dve", ang, posf.unsqueeze(2).broadcast_to([128, nblk, 8]),
           invf.unsqueeze(1).broadcast_to([128, nblk, 8]), ALU.mult, r=[pfx + "posf", "invf"], w=[pfx + "ang"])
        for which, dst in (("s", sin_t), ("c", cos_t)):
            k0 = pfx + which
            src = ang
            if which == "c":
                tsc("dve", rr, ang, math.pi / 2.0, None, ALU.add, None, r=[pfx + "ang", pfx + "rr"], w=[pfx + "rr"])
                tsc("dve", tq, rr, 1.0 / TWO_PI, None, ALU.mult, None, r=[pfx + "rr", pfx + "tq"], w=[pfx + "tq"])
                base = rr
            else:
                tsc("dve", tq, ang, 1.0 / TWO_PI, None, ALU.mult, None, r=[pfx + "ang"], w=[pfx + "tq"])
                base = ang
            cp("dve", ki, tq, r=[pfx + "tq"], w=[pfx + "ki"])
            cp("dve", kf, ki, r=[pfx + "ki"], w=[pfx + "kf"])
            stt(rr, kf, -C1, base, ALU.mult, ALU.add, r=[pfx + "kf", pfx + "ang", pfx + "rr"], w=[pfx + "rr"])
            stt(rr, kf, -C2, rr, ALU.mult, ALU.add, r=[pfx + "kf", pfx + "rr"], w=[pfx + "rr"])
            tsc("dve", mk, rr, math.pi, -TWO_PI, ALU.is_gt, ALU.mult, r=[pfx + "rr", pfx + "mk"], w=[pfx + "mk"])
            tt("dve", rr, rr, mk, ALU.add, r=[pfx + "rr", pfx + "mk"], w=[pfx + "rr"])
            tsc("dve", mk, rr, -math.pi, TWO_PI, ALU.is_lt, ALU.mult, r=[pfx + "rr", pfx + "mk"], w=[pfx + "mk"])
            tt("dve", rr, rr, mk, ALU.add, r=[pfx + "rr", pfx + "mk"], w=[pfx + "rr"])
            act(dst, rr, AF.Sin, r=[pfx + "rr"], w=[pfx + "tab" + which])
        return [pfx + "tabs", pfx + "tabc"]

    KV0 = A.mark()
    KT = A.alloc([128, 4, SEQ], BF16)
    VA = A.alloc([128, NBLK_B, 4, 130], BF16)
    WORK0 = A.mark()
    rb = rope_tables(posb_i, NBLK_B, cos_b, sin_b, "rb_")
    ro = rope_tables(poso_i, NOWN, cos_o, sin_o, "ro_")
    rope_keys = ["rb_rr", "rb_mk", "rb_kf", "rb_ki", "rb_tq", "rb_ang", "rb_posf",
                 "ro_rr", "ro_mk", "ro_kf", "ro_ki", "ro_tq", "ro_ang", "ro_posf"]

    def ada_alloc():
        return [(A.alloc([128, KC, 512]), A.alloc([128, 512]), A.alloc([128, 512]), str(i)) for i in range(2)]

    def ada_group(g, ab, pbank):
        stage, brep, modrep, sfx = ab
        dma(stage, w_ada.rearrange("(k p) n -> p k n", p=128)[:, :, g * 512:(g + 1) * 512],
            r=[], w=["ada_stage" + sfx], tag="ada" + sfx)
        dma(brep, b_ada[0:1, g * 512:(g + 1) * 512].broadcast_to([128, 512]), r=[], w=["ada_brep" + sfx], tag="adab" + sfx)
        mms([(ps[pbank][:, :], crep[:, k, :], stage[:, k, :], k == 0, k == KC - 1) for k in range(KC)],
            r=["ada_stage" + sfx, "crep"], w=["ps%d" % pbank])
        tt("dve", modrep, ps[pbank][:, :], brep, ALU.add, r=["ps%d" % pbank, "ada_brep" + sfx], w=["ada_modrep" + sfx])

    def ada_to_cols(ab, dst, col0, pbank):
        modrep, sfx = ab[2], ab[3]
        tps([(ps[pbank][:, jj * 128:(jj + 1) * 128], modrep[:, jj * 128:(jj + 1) * 128]) for jj in range(4)],
            ident_f, r=["ada_modrep" + sfx, "ident_f"], w=["ps%d" % pbank])
        cp("dve", dst[:, col0:col0 + 4], ps[pbank][:, :].rearrange("p (j c) -> p j c", c=128)[:, :, 0],
           r=["ps%d" % pbank], w=["adacols"])

    S.barrier(small[:, 62:63])
    A.reset(WORK0)
    m_ada = A.mark()
    crep = A.alloc([128, KC, 128])
    cp("dve", crep, cact.unsqueeze(2).broadcast_to([128, KC, 128]), r=["cact"], w=["crep"])
    abufs = ada_alloc()
    for g in range(4):
        ada_group(g, abufs[g % 2], 7 - 2 * (g % 2))
        ada_to_cols(abufs[g % 2], sh_a if g < 2 else sc_a, (g % 2) * 4, 6 - 2 * (g % 2))
    tsc("dve", gsc_a, sc_a, 1.0, None, ALU.add, None, r=["adacols"], w=["gsc_a"])
    tt("dve", gsc_a, gsc_a, gmixT, ALU.mult, r=["gsc_a", "gmixT"], w=["gsc_a"])

    S.barrier(small[:, 62:63])
    A.reset(WORK0)
    WORK = A.mark()
    NXT = 3
    xt = [A.alloc([128, D]) for _ in range(NXT)]
    _cur = A.mark()
    A.reset(ATTN_OFF)
    xnb = [A.alloc([128, D], BF16) for _ in range(2)]
    hT = [A.alloc([128, KC, 128], BF16) for _ in range(2)]
    shrep = A.alloc([128, KC, 128])
    assert A.mark() <= ATTN_OFF + 4096
    A.reset(_cur)
    wkv = A.alloc([128, KC, 1024], BF16)
    shW_bf = A.alloc([128, 1024], BF16)
    c128 = A.alloc([128, 128], BF16)
    junkb = junk_sb
    ssA = [A.alloc([128, 1]) for _ in range(2)]
    rsA = [A.alloc([128, 1]) for _ in range(2)]
    ksq = [A.alloc([128, 512]) for _ in range(2)]
    kn = [A.alloc([128, 8, 64]) for _ in range(2)]
    kb16 = [A.alloc([128, 512], BF16) for _ in range(2)]
    ssk = [A.alloc([128, 8]) for _ in range(2)]
    rk = [A.alloc([128, 8]) for _ in range(2)]
    rt = [[A.alloc([128, 8, 8]) for _ in range(4)] for _ in range(2)]

    S.add("pool", lambda e: e.memset(VA[:, :, :, 128:130], 1.0), r=[], w=["VAones"])
    S.add("pool", lambda e: e.memset(c128, 1.0 / 128.0), r=[], w=["c128"])
    cp("dve", shrep, sh_a.unsqueeze(2).broadcast_to([128, KC, 128]), r=["adacols"], w=["shrep"])

    w_in_v = w_in.rearrange("(k p) n -> p k n", p=128)
    for pc in range(8):
        xi = pc % NXT
        st = xt[xi][:, :].rearrange("p (k n) -> p k n", k=KC)
        dma(st, w_in_v[:, :, 512 + pc * 128: 512 + (pc + 1) * 128], r=[], w=["xt%d" % xi], tag="xt%d" % xi)
        tt("pool", wkv[:, :, pc * 128:(pc + 1) * 128], st, gsc_a.unsqueeze(2).broadcast_to([128, KC, 128]), ALU.mult,
           r=["xt%d" % xi, "gsc_a"], w=["wkv"])
        bk = pc // 4
        mms([(ps[bk][:, (pc % 4) * 128:(pc % 4 + 1) * 128], shrep[:, k, :], st[:, k, :], k == 0, k == KC - 1)
             for k in range(KC)], r=["xt%d" % xi, "shrep"], w=["ps%d" % bk])
    cp("dve", shW_bf[:, 0:512], ps[0][:, :], r=["ps0"], w=["shW"])
    cp("dve", shW_bf[:, 512:1024], ps[1][:, :], r=["ps1"], w=["shW"])

    def qk_chain(pkey, psrc, gain_rep, cos_ap, sin_ap, sidx, evac_fn, tbank):
        i = sidx
        p3 = psrc.rearrange("p (g d) -> p g d", d=64)
        act(ksq[i], psrc, AF.Square, r=[pkey], w=["ksq"])
        S.add("dve", lambda e, o=ssk[i], a=ksq[i][:, :].rearrange("p (g d) -> p g d", d=64): e.tensor_reduce(o, a, AX.X, ALU.add),
              r=["ksq"], w=["ssk%d" % i])
        rstd_from_ss(ssk[i], rk[i], 1.0 / 64.0, "ssk%d" % i, "rk%d" % i)
        tt("dve", kn[i], p3, rk[i].unsqueeze(2).broadcast_to([128, 8, 64]), ALU.mult,
           r=[pkey, "rk%d" % i], w=["kn%d" % i])
        tt("dve", kn[i], kn[i], gain_rep.unsqueeze(1).broadcast_to([128, 8, 64]), ALU.mult,
           r=["kn%d" % i, "gains"], w=["kn%d" % i])
        k3 = kb16[i][:, :].rearrange("p (g d) -> p g d", d=64)
        cp("pool", k3, kn[i], r=["kn%d" % i], w=["kb%d" % i])
        a = kn[i][:, :, 0:8]
        b = kn[i][:, :, 8:16]
        cb = cos_ap.unsqueeze(1).broadcast_to([128, 8, 8])
        sb_ = sin_ap.unsqueeze(1).broadcast_to([128, 8, 8])
        t1, t2, t3, t4 = rt[i]
        kk = "rt%d" % i
        tt("pool", t1, a, cb, ALU.mult, r=["kn%d" % i, "ropetab"], w=[kk + "a"])
        tt("pool", t2, b, sb_, ALU.mult, r=["kn%d" % i, "ropetab"], w=[kk + "b"])
        tt("pool", k3[:, :, 0:8], t1, t2, ALU.subtract, r=[kk + "a", kk + "b"], w=["kb%d" % i])
        tt("pool", t3, b, cb, ALU.mult, r=["kn%d" % i, "ropetab"], w=[kk + "c"])
        tt("pool", t4, a, sb_, ALU.mult, r=["kn%d" % i, "ropetab"], w=[kk + "d"])
        tt("pool", k3[:, :, 8:16], t3, t4, ALU.add, r=[kk + "c", kk + "d"], w=["kb%d" % i])
        pT = psb(tbank)
        tps([(pT[:, h * 128:(h + 1) * 128], kb16[i][:, h * 128:(h + 1) * 128]) for h in range(4)],
            ident_b, r=["kb%d" % i, "ident_b"], w=["ps%d" % tbank])
        evac_fn(pT[:, 0:512])

    S.add("pool", lambda e: e.memset(small[:, 60:61], 0.0), r=rb + ro, w=["ropetab"])
    S.add("pool", lambda e: e.memset(small[:, 61:62], 0.0), r=["gq_rep", "gk_rep"], w=["gains"])

    def norm_transpose(xsrc, xk, ssv, rsv, sskey, hdst, hkeys, gsc, sh, gkeys, xout=None, xoutk=None, junk=None, junkk="ps7"):
        act(junkb, xsrc, AF.Square, r=[xk], w=["junk_sb", sskey], accum=ssv)
        rstd_from_ss(ssv, rsv, 1.0 / D, sskey, sskey + "r")
        if xout is None:
            xout, xoutk = xsrc, xk
            tsc("dve", xout, xsrc, rsv, None, ALU.mult, None, r=[xk, sskey + "r"], w=[xk])
        else:
            tsc("dve", xout, xsrc, rsv, None, ALU.mult, None, r=[xk, sskey + "r"], w=[xoutk])
        for half in range(2):
            tps([(ps[half][:, kk * 128:(kk + 1) * 128], xout[:, (half * 4 + kk) * 128:(half * 4 + kk + 1) * 128])
                 for kk in range(4)], ident_f, r=[xoutk, "ident_f"], w=["ps%d" % half])
            for kk in range(4):
                k = half * 4 + kk
                src = ps[half][:, kk * 128:(kk + 1) * 128]
                if True:
                    act(hdst[:, k, :], src, AF.Identity, r=["ps%d" % half] + gkeys, w=[hkeys[k]],
                        bias=sh[:, k:k + 1], scale=gsc[:, k:k + 1])
                else:
                    tsc("dve", hdst[:, k, :], src, gsc[:, k:k + 1], sh[:, k:k + 1], ALU.mult, ALU.add,
                        r=["ps%d" % half] + gkeys, w=[hkeys[k]])

    NT_A = NBLK_B
    if stop_after == "pro":
        NT_A = 0
    if stop_after == "A4":
        NT_A = 4
    if stop_after == "C1":
        NT_A = 8
    if mini:
        NT_A = 16 * mini_ns
    import os
    PST = (0, 1)
    PSK = (2, 3, 4)
    PSV = (5, 6)
    PST2 = 7

    def a_d0(b):
        xi = b % NXT
        dma(xt[xi], xb[b * 128:(b + 1) * 128, :], r=[], w=["xt%d" % xi], tag="xt%d" % xi)

    def a_a12(b):
        xi, i2 = b % NXT, b % 2
        act(junkb, xt[xi], AF.Square, r=["xt%d" % xi], w=["junk_sb", "ssA%d" % i2], accum=ssA[i2])
        rstd_from_ss(ssA[i2], rsA[i2], 1.0 / D, "ssA%d" % i2, "rsA%d" % i2)

    def a_v1(b):
        xi, i2 = b % NXT, b % 2
        act(xnb[i2], xt[xi], AF.Identity, r=["xt%d" % xi, "rsA%d" % i2], w=["xnb%d" % i2], scale=rsA[i2])

    def a_p1(b):
        i2 = b % 2
        pT = psb(PST[i2])
        tps([(pT[:, k * 128:(k + 1) * 128], xnb[i2][:, k * 128:(k + 1) * 128]) for k in range(KC)],
            ident_b, r=["xnb%d" % i2, "ident_b"], w=["ps%d" % PST[i2]])

    def a_ev(b):
        i2 = b % 2
        pT = psb(PST[i2])
        for half in range(2):
            cp("dve", hT[i2][:, half * 4:(half + 1) * 4, :],
               pT[:, half * 512:(half + 1) * 512].rearrange("p (k q) -> p k q", q=128),
               r=["ps%d" % PST[i2]], w=["hT%d_%d" % (i2, half)])

    def a_p2(b):
        i2 = b % 2
        pK = PSK[b % 3]
        pV = PSV[i2]
        hks = ["hT%d_0" % i2, "hT%d_1" % i2]
        mms([(ps[pK][:, :], hT[i2][:, k, :], wkv[:, k, 0:512], k == 0, False) for k in range(KC)] +
            [(ps[pK][:, :], c128, shW_bf[:, 0:512], False, True)],
            r=hks + ["wkv", "shW", "c128"], w=["ps%d" % pK])
        mms([(ps[pV][:, :], hT[i2][:, k, :], wkv[:, k, 512:1024], k == 0, False) for k in range(KC)] +
            [(ps[pV][:, :], c128, shW_bf[:, 512:1024], False, True)],
            r=hks + ["wkv", "shW", "c128"], w=["ps%d" % pV])

    def a_a45(b):
        i2 = b % 2
        pK = PSK[b % 3]
        pV = PSV[i2]
        act(ksq[i2], ps[pK][:, :], AF.Square, r=["ps%d" % pK], w=["ksq%d" % i2])
        act(VA[:, b, :, 0:128], ps[pV][:, :].rearrange("p (h e) -> p h e", e=128), AF.Copy,
            r=["ps%d" % pV, "VAones"], w=[("V", b)])

    def a_v3(b):
        i2 = b % 2
        S.add("dve", lambda e, o=ssk[i2], a=ksq[i2][:, :].rearrange("p (g d) -> p g d", d=64): e.tensor_reduce(o, a, AX.X, ALU.add),
              r=["ksq%d" % i2], w=["ssk%d" % i2])

    def a_a6(b):
        i2 = b % 2
        rstd_from_ss(ssk[i2], rk[i2], 1.0 / 64.0, "ssk%d" % i2, "rk%d" % i2)

    def a_v4(b):
        i2 = b % 2
        pK = PSK[b % 3]
        p3 = ps[pK][:, :].rearrange("p (g d) -> p g d", d=64)
        tt("dve", kn[i2], p3, rk[i2].unsqueeze(2).broadcast_to([128, 8, 64]), ALU.mult,
           r=["ps%d" % pK, "rk%d" % i2], w=["kn%d" % i2])
        tt("dve", kn[i2], kn[i2], gk_rep.unsqueeze(1).broadcast_to([128, 8, 64]), ALU.mult,
           r=["kn%d" % i2, "gains"], w=["kn%d" % i2])

    def a_g1(b):
        i = b % 2
        k3 = kb16[i][:, :].rearrange("p (g d) -> p g d", d=64)
        cp("pool", k3, kn[i], r=["kn%d" % i], w=["kb%d" % i])
        a = kn[i][:, :, 0:8]
        b_ = kn[i][:, :, 8:16]
        cb = cos_b[:, b, :].unsqueeze(1).broadcast_to([128, 8, 8])
        sb_ = sin_b[:, b, :].unsqueeze(1).broadcast_to([128, 8, 8])
        t1, t2, t3, t4 = rt[i]
        kk = "rt%d" % i
        tt("pool", t1, a, cb, ALU.mult, r=["kn%d" % i, "ropetab"], w=[kk + "a"])
        tt("pool", t2, b_, sb_, ALU.mult, r=["kn%d" % i, "ropetab"], w=[kk + "b"])
        tt("pool", k3[:, :, 0:8], t1, t2, ALU.subtract, r=[kk + "a", kk + "b"], w=["kb%d" % i])
        tt("pool", t3, b_, cb, ALU.mult, r=["kn%d" % i, "ropetab"], w=[kk + "c"])
        tt("pool", t4, a, sb_, ALU.mult, r=["kn%d" % i, "ropetab"], w=[kk + "d"])
        tt("pool", k3[:, :, 8:16], t3, t4, ALU.add, r=[kk + "c", kk + "d"], w=["kb%d" % i])

    def a_p3(b):
        i = b % 2
        pT = psb(PST2)
        tps([(pT[:, h * 128:(h + 1) * 128], kb16[i][:, h * 128:(h + 1) * 128]) for h in range(4)],
            ident_b, r=["kb%d" % i, "ident_b"], w=["ps%d" % PST2])

    def a_v5(b):
        pT = psb(PST2)
        cp("dve", KT[:, :, b * 128:(b + 1) * 128], pT[:, 0:512].rearrange("p (h q) -> p h q", q=128),
           r=["ps%d" % PST2], w=[("KT", b)])

    a_order = [(a_v5, 11), (a_p3, 10), (a_g1, 9), (a_a6, 8), (a_a45, 7), (a_v4, 8), (a_p2, 6), (a_ev, 5),
               (a_p1, 4), (a_v1, 3), (a_a12, 2), (a_v3, 7), (a_d0, 0)]
    for t in range(NT_A + 12):
        for fn_, off_ in a_order:
            b = t - off_
            if 0 <= b < NT_A:
                fn_(b)


    if stop_after in ("pro", "A", "A4"):
        if os.environ.get("MK_CUT"):
            cut = int(os.environ["MK_CUT"])
            S.ops = S.ops[:cut]
            S.last_w = {k: v for k, v in S.last_w.items() if v < cut}
            S.readers = {k: {e: i for e, i in d_.items() if i < cut} for k, d_ in S.readers.items()}
            S.tag_count = {}
            for op in S.ops:
                if op["tag"] is not None:
                    S.tag_count[op["tag"]] = S.tag_count.get(op["tag"], 0) + 1
            stop_after = "pro"
        dbg["KT"] = nc.dram_tensor("dbg_KT", [128, 4 * SEQ], BF16, kind="ExternalOutput").ap()
        dbg["VA"] = nc.dram_tensor("dbg_VA", [128, NBLK_B * 4 * 130], BF16, kind="ExternalOutput").ap()
        dbg["misc"] = nc.dram_tensor("dbg_misc", [128, 64], F32, kind="ExternalOutput").ap()
        allk = [("KT", b) for b in range(NT_A)] + [("V", b) for b in range(NT_A)]
        if stop_after in ("A", "A4"):
            S.add("dve", lambda e: e.memset(KT[:, :, NT_A * 128:], 0.0), r=[], w=["ktpad"])
            S.add("dve", lambda e: e.memset(VA[:, NT_A:, :, 0:128], 0.0), r=[], w=["vapad"])
            allk = allk + ["ktpad", "vapad"]
            dma(dbg["KT"], KT[:, :, :].rearrange("p a b -> p (a b)"), r=allk, w=[], tag="out0")
            dma(dbg["VA"], VA[:, :, :, :].rearrange("p a b c -> p (a b c)"), r=allk + ["VAones"], w=[], tag="out1")
        cp("dve", small[:, 0:8], sh_a, r=["adacols", "junk64", "neglam"], w=["small"])
        cp("dve", small[:, 8:16], gsc_a, r=["gsc_a"], w=["small"])
        cp("dve", small[:, 16:24], cos_b[:, 0, :], r=rb, w=["small"])
        cp("dve", small[:, 24:32], sin_b[:, 63, :], r=rb, w=["small"])
        cp("dve", small[:, 32:40], lam_t, r=["neglam"], w=["small"])
        dma(dbg["misc"][:, 0:40], small[:, 0:40], r=["small"], w=[], tag="out2")
        S.emit()
        return nc

    bar_tile = small[:, 62:63]
    S.barrier(bar_tile)
    A.reset(WORK)
    xq = [A.alloc([128, D]) for _ in range(2)]
    hTq = [A.alloc([128, KC, 128], BF16) for _ in range(2)]
    wq = A.alloc([128, KC, 512], BF16)
    Qbd = [A.alloc([128, 4, 2, 128], BF16) for _ in range(2)]
    NPT = 4
    PT = [A.alloc([128, 512], BF16) for _ in range(NPT)]
    ssQ = [A.alloc([128, 1]) for _ in range(2)]
    rsQ = [A.alloc([128, 1]) for _ in range(2)]
    ksq = [A.alloc([128, 512])] * 2
    kn = [A.alloc([128, 8, 64]) for _ in range(2)]
    kb16 = [A.alloc([128, 512], BF16) for _ in range(2)]
    ssk = [A.alloc([128, 8]) for _ in range(2)]
    rk = [A.alloc([128, 8]) for _ in range(2)]
    rt = [[A.alloc([128, 8, 8]) for _ in range(4)] for _ in range(2)]
    ot = [A.alloc([128, 128]) for _ in range(2)]
    ot2 = [A.alloc([128, 128]) for _ in range(2)]
    ob16 = [A.alloc([128, 128], BF16) for _ in range(2)]
    eps_ = [A.alloc([128, 8]) for _ in range(2)]
    pass

    for bq in range(2):
        S.add("pool", lambda e, q=Qbd[bq]: e.memset(q, 0.0), r=[], w=["Qbd%d" % bq])
    for pc in range(4):
        xi = pc % 2
        st = xq[xi][:, :].rearrange("p (k n) -> p k n", k=KC)
        dma(st, w_in_v[:, :, pc * 128:(pc + 1) * 128], r=[], w=["xq%d" % xi], tag="xq%d" % xi)
        cp("pool", wq[:, :, pc * 128:(pc + 1) * 128], st, r=["xq%d" % xi], w=["wq"])

    def q_stage(m, stage):
        bq = m % 2
        xk = "xq%d" % bq
        hks = ["hTq%d_%d" % (bq, k) for k in range(KC)]
        if stage == 0:
            dma(xq[bq], xo[m * 128:(m + 1) * 128, :], r=[], w=[xk], tag=xk)
            act(junkb, xq[bq], AF.Square, r=[xk], w=["junk_sb", "ssQ%d" % bq], accum=ssQ[bq])
            rstd_from_ss(ssQ[bq], rsQ[bq], 1.0 / D, "ssQ%d" % bq, "ssQ%dr" % bq)
            tsc("dve", xq[bq], xq[bq], rsQ[bq], None, ALU.mult, None, r=[xk, "ssQ%dr" % bq], w=[xk])
        elif stage == 1:
            for half in range(2):
                tps([(ps[half][:, kk * 128:(kk + 1) * 128], xq[bq][:, (half * 4 + kk) * 128:(half * 4 + kk + 1) * 128])
                     for kk in range(4)], ident_f, r=[xk, "ident_f"], w=["ps%d" % half])
            for half in range(2):
                for kk in range(4):
                    k = half * 4 + kk
                    src_ = ps[half][:, kk * 128:(kk + 1) * 128]
                    act(hTq[bq][:, k, :], src_, AF.Identity, r=["ps%d" % half, "gsc_a", "adacols"], w=[hks[k]],
                        bias=sh_a[:, k:k + 1], scale=gsc_a[:, k:k + 1])
        elif stage == 2:
            mms([(ps[0][:, :], hTq[bq][:, k, :], wq[:, k, :], k == 0, k == KC - 1) for k in range(KC)],
                r=hks + ["wq"], w=["ps0"])
            qk_front("ps0", ps[0][:, :], gq_rep, cos_o[:, m, :], sin_o[:, m, :], bq)
        else:
            pT = psb(1)
            tps([(pT[:, h * 128:(h + 1) * 128], kb16[bq][:, h * 128:(h + 1) * 128]) for h in range(4)],
                ident_b, r=["kb%d" % bq, "ident_b"], w=["ps1"])
            p3 = pT[:, 0:512].rearrange("p (h q) -> p h q", q=128)
            cp("dve", Qbd[bq][0:64, :, 0, :], p3[0:64], r=["ps1"], w=["Qbd%d" % bq])
            cp("dve", Qbd[bq][64:128, :, 1, :], p3[64:128], r=["ps1"], w=["Qbd%d" % bq])

    def qk_front(pkey, psrc, gain_rep, cos_ap, sin_ap, i):
        p3 = psrc.rearrange("p (g d) -> p g d", d=64)
        act(ksq[i], psrc, AF.Square, r=[pkey], w=["ksq"])
        S.add("dve", lambda e, o=ssk[i], a=ksq[i][:, :].rearrange("p (g d) -> p g d", d=64): e.tensor_reduce(o, a, AX.X, ALU.add),
              r=["ksq"], w=["ssk%d" % i])
        rstd_from_ss(ssk[i], rk[i], 1.0 / 64.0, "ssk%d" % i, "rk%d" % i)
        tt("dve", kn[i], p3, rk[i].unsqueeze(2).broadcast_to([128, 8, 64]), ALU.mult,
           r=[pkey, "rk%d" % i], w=["kn%d" % i])
        tt("dve", kn[i], kn[i], gain_rep.unsqueeze(1).broadcast_to([128, 8, 64]), ALU.mult,
           r=["kn%d" % i, "gains"], w=["kn%d" % i])
        k3 = kb16[i][:, :].rearrange("p (g d) -> p g d", d=64)
        cp("pool", k3, kn[i], r=["kn%d" % i], w=["kb%d" % i])
        a = kn[i][:, :, 0:8]
        b = kn[i][:, :, 8:16]
        cb = cos_ap.unsqueeze(1).broadcast_to([128, 8, 8])
        sb_ = sin_ap.unsqueeze(1).broadcast_to([128, 8, 8])
        t1, t2, t3, t4 = rt[i]
        kk = "rt%d" % i
        tt("pool", t1, a, cb, ALU.mult, r=["kn%d" % i, "ropetab"], w=[kk + "a"])
        tt("pool", t2, b, sb_, ALU.mult, r=["kn%d" % i, "ropetab"], w=[kk + "b"])
        tt("pool", k3[:, :, 0:8], t1, t2, ALU.subtract, r=[kk + "a", kk + "b"], w=["kb%d" % i])
        tt("pool", t3, b, cb, ALU.mult, r=["kn%d" % i, "ropetab"], w=[kk + "c"])
        tt("pool", t4, a, sb_, ALU.mult, r=["kn%d" % i, "ropetab"], w=[kk + "d"])
        tt("pool", k3[:, :, 8:16], t3, t4, ALU.add, r=[kk + "c", kk + "d"], w=["kb%d" % i])

    cm3 = cmask[:, :].rearrange("p (t x) -> p t x", t=4)
    NM = NOWN if stop_after != "C1" else 2
    if mini:
        NM = 4 * mini_ns
    items = []
    for m in range(NM):
        for h in range(4):
            ngrp = (4 * m + 4) // 2
            for g in range(ngrp):
                items.append((m, h, g, ngrp))
    SBK = (2, 3, 4)
    pending = []
    LOOK = 2

    def emit_S(idx):
        m, h, g, ngrp = items[idx]
        bq = m % 2
        sbk = SBK[idx % 3]
        kbs = (2 * g, 2 * g + 1)
        mms([(ps[sbk][:, i * 256:(i + 1) * 256], KT[:, h, kb * 128:(kb + 1) * 128],
              Qbd[bq][:, h, :, :], True, True) for i, kb in enumerate(kbs)],
            r=[("KT", kbs[0]), ("KT", kbs[1]), "Qbd%d" % bq], w=["ps%d" % sbk])

    def emit_rest(idx):
        m, h, g, ngrp = items[idx]
        sbk = SBK[idx % 3]
        pti = idx % NPT
        ep = (m * 4 + h) % 2
        kbs = (2 * g, 2 * g + 1)
        act(PT[pti], ps[sbk][:, :], AF.Exp, r=["ps%d" % sbk, "m8c"], w=["PT%d" % pti],
            bias=m8c[:, 0:1], scale=0.125)
        if g >= ngrp - 2:
            gb = g - (ngrp - 2)
            tt("dve", PT[pti], PT[pti], cm3[:, 2 * gb:2 * gb + 2, :], ALU.mult,
               r=["PT%d" % pti, "cmask"], w=["PT%d" % pti])
        its = []
        for i, kb in enumerate(kbs):
            for c in range(2):
                its.append((ps[5 + c][:, 0:129], PT[pti][:, i * 256 + c * 128:i * 256 + (c + 1) * 128],
                            VA[:, kb, h, 0:129], (g == 0 and i == 0), (g == ngrp - 1 and i == 1)))
        mms(its, r=["PT%d" % pti, ("V", kbs[0]), ("V", kbs[1])], w=["ps5", "ps6"])
        if g == ngrp - 1:
            epilogue_a(m, h, ep)
            pending.append((idx + 2, lambda m=m, h=h, ep=ep: epilogue_a2(m, h, ep)))
            pending.append((idx + 4, lambda m=m, h=h, ep=ep: epilogue_b(m, h, ep)))

    def epilogue_a(m, h, ep):
        e_ = eps_[ep]
        ek = "eps%d" % ep
        act(e_[:, 5:6], ps[5][:, 128:129], AF.Copy, r=["ps5"], w=[ek])
        act(e_[:, 6:7], ps[6][:, 128:129], AF.Copy, r=["ps6"], w=[ek])
        act(ot[ep], ps[5][:, 0:128], AF.Copy, r=["ps5"], w=["ot%d" % ep])
        act(ot2[ep], ps[6][:, 0:128], AF.Copy, r=["ps6"], w=["ot2%d" % ep])
        S.add("dve", lambda e, e_=e_: e.reciprocal(e_[:, 0:2], e_[:, 5:7]), r=[ek], w=[ek])
        tt("dve", e_[:, 2:3], e_[:, 1:2], neglam, ALU.mult, r=[ek, "neglam"], w=[ek])
        tsc("dve", ot[ep], ot[ep], e_[:, 0:1], None, ALU.mult, None, r=["ot%d" % ep, ek], w=["ot%d" % ep])
        tsc("dve", ot2[ep], ot2[ep], e_[:, 2:3], None, ALU.mult, None, r=["ot2%d" % ep, ek], w=["ot2%d" % ep])
        tt("dve", ot[ep], ot[ep], ot2[ep], ALU.add, r=["ot%d" % ep, "ot2%d" % ep], w=["ot%d" % ep])

    def epilogue_a2(m, h, ep):
        e_ = eps_[ep]
        ek = "eps%d" % ep
        act(ot2[ep], ot[ep], AF.Square, r=["ot%d" % ep, "ot2%d" % ep], w=["ot2%d" % ep, ek + "s"], accum=e_[:, 3:4])
        rstd_from_ss(e_[:, 3:4], e_[:, 4:5], 1.0 / 128.0, ek + "s", ek + "r")
        act(ot2[ep], ot[ep], AF.Identity, r=["ot%d" % ep, ek + "r", "ot2%d" % ep], w=["ot2%d" % ep], scale=e_[:, 4:5])
        tt("dve", ob16[ep], ot2[ep], sg_rep, ALU.mult, r=["ot2%d" % ep, "sg_rep"], w=["ob%d" % ep])

    def epilogue_b(m, h, ep):
        tps([(psb(7)[:, ep * 128:(ep + 1) * 128], ob16[ep])], ident_b, r=["ob%d" % ep, "ident_b"], w=["ps7"])
        cp("dve", attnT[:, h, m * 128:(m + 1) * 128], psb(7)[:, ep * 128:(ep + 1) * 128], r=["ps7"], w=[("attnT", m, h)])

    for st_ in range(4):
        q_stage(0, st_)
    for i in range(min(LOOK, len(items))):
        emit_S(i)
    for idx in range(len(items)):
        m, h, g, ngrp = items[idx]
        if g == 0 and m + 1 < NM:
            q_stage(m + 1, h)
        if idx + LOOK < len(items):
            emit_S(idx + LOOK)
        while pending and pending[0][0] <= idx:
            pending.pop(0)[1]()
        emit_rest(idx)
    while pending:
        pending.pop(0)[1]()

    if stop_after in ("C", "C1"):
        dbg["attnT"] = nc.dram_tensor("dbg_attnT", [128, 4 * NOWN * 128], BF16, kind="ExternalOutput").ap()
        if NM < NOWN:
            S.add("dve", lambda e: e.memset(attnT[:, :, NM * 128:], 0.0), r=[], w=["attnpad"])
        allk = [("attnT", m, h) for m in range(NM) for h in range(4)] + ["attnpad"]
        dma(dbg["attnT"], attnT[:, :, :].rearrange("p a b -> p (a b)"), r=allk, w=[], tag="out0")
        S.emit()
        return nc

    S.barrier(bar_tile)
    A.reset(KV0)
    X = A.alloc([128, NOWN, D])
    wglu = A.alloc([128, KC, 1024], BF16)
    woutp = A.alloc([128, KC, 1024], BF16)
    D1W = A.mark()
    crep = A.alloc([128, KC, 128])
    abufs = ada_alloc()
    gta_rep = A.alloc([128, D])
    stg = [A.alloc([128, KC, 128]) for _ in range(2)]
    for s4 in range(4):
        S.bulk_tags.add("Xload%d" % s4)
        for i in range(4):
            m = 4 * s4 + i
            dma(X[:, m, :], xo[m * 128:(m + 1) * 128, :], r=[], w=[("X", m)], tag="Xload%d" % s4)
    cp("dve", crep, cact.unsqueeze(2).broadcast_to([128, KC, 128]), r=["cact"], w=["crep"])
    for g in (4, 5):
        ada_group(g, abufs[g % 2], 7 - 2 * (g % 2))
        cp("dve", gta_rep[:, (g - 4) * 512:(g - 3) * 512], abufs[g % 2][2], r=["ada_modrep%d" % (g % 2)], w=["gta_rep"])
    for pc in range(8):
        xi = pc % 2
        dma(stg[xi], w_in_v[:, :, 1536 + pc * 128:1536 + (pc + 1) * 128], r=[], w=["stg%d" % xi], tag="stg%d" % xi)
        if pc % 2 == 0:
            act(wglu[:, :, pc * 128:(pc + 1) * 128], stg[xi], AF.Copy, r=["stg%d" % xi], w=["wglu"])
        else:
            cp("pool", wglu[:, :, pc * 128:(pc + 1) * 128], stg[xi], r=["stg%d" % xi], w=["wglu"])
    w_out_v = w_out.rearrange("(k p) n -> p k n", p=128)
    for pc in range(8):
        xi = pc % 2
        dma(stg[xi], w_out_v[:, :, pc * 128:(pc + 1) * 128], r=[], w=["stg%d" % xi], tag="stg%d" % xi)
        tt("dve" if pc % 2 == 0 else "pool", woutp[:, :, pc * 128:(pc + 1) * 128], stg[xi],
           gta_rep[:, pc * 128:(pc + 1) * 128].unsqueeze(1).broadcast_to([128, KC, 128]), ALU.mult,
           r=["stg%d" % xi, "gta_rep"], w=["woutp"])
    S.barrier(bar_tile)
    A.reset(D1W)
    xhb = A.alloc([128, D])
    xn = [A.alloc([128, D]) for _ in range(2)]
    hT5 = A.alloc([128, KC, 640], BF16)
    uT = A.alloc([128, 4, 4, 160], BF16)
    sig = [A.alloc([128, 512]) for _ in range(2)]
    sigh = [A.alloc([128, 128]) for _ in range(2)]
    asb = [A.alloc([128, 512]) for _ in range(2)]
    asbh = [A.alloc([128, 128]) for _ in range(2)]
    yv = A.alloc([128, 4, 512])
    ysq = A.alloc([128, 4, 512])
    mean_sb = A.alloc([128, 512])
    m2 = A.alloc([128, 512])
    rstd_bc = A.alloc([128, 512])
    tmpv = [A.alloc([128, 512]) for _ in range(2)]
    convT = A.alloc([128, 4, 512], BF16)
    dg = [A.alloc([128, 128], BF16) for _ in range(8)]
    ssD = [A.alloc([128, 1]) for _ in range(2)]
    rsD = [A.alloc([128, 1]) for _ in range(2)]
    pass
    dgc = {"n": 0}

    NS = 4 if not mini else mini_ns
    hT5b = [hT5, A.alloc([128, KC, 640], BF16)]

    def d1_front(s4, i):
        bi = i % 2
        if i == 4:
            dma(xhb, xh[s4 * 128:(s4 + 1) * 128, :], r=[], w=["xhb"], tag="xhb")
        src, sk = (X[:, 4 * s4 + i, :], ("X", 4 * s4 + i)) if i < 4 else (xhb, "xhb")
        act(junkb, src, AF.Square, r=[sk], w=["junk_sb", "ssD%d" % bi], accum=ssD[bi])
        rstd_from_ss(ssD[bi], rsD[bi], 1.0 / D, "ssD%d" % bi, "ssD%dr" % bi)
        tsc("dve", xn[bi], src, rsD[bi], None, ALU.mult, None, r=[sk, "ssD%dr" % bi], w=["xn%d" % bi])

    def d1_back(s4, i):
        bi = i % 2
        hd = hT5b[s4 % 2][:, :, i * 128:(i + 1) * 128]
        for half in range(2):
            tps([(ps[half][:, kk * 128:(kk + 1) * 128], xn[bi][:, (half * 4 + kk) * 128:(half * 4 + kk + 1) * 128])
                 for kk in range(4)], ident_f, r=["xn%d" % bi, "ident_f"], w=["ps%d" % half])
            for kk in range(4):
                k = half * 4 + kk
                act(hd[:, k, :], ps[half][:, kk * 128:(kk + 1) * 128], AF.Identity,
                    r=["ps%d" % half, "gsc_a", "adacols"], w=["hT5_%d_%d_%d" % (s4 % 2, i, k)],
                    bias=sh_a[:, k:k + 1], scale=gsc_a[:, k:k + 1])

    for i in range(5):
        d1_front(0, i)
        d1_back(0, i)
    def d1_glu(s4):
        hT5 = hT5b[s4 % 2]
        nxt = s4 + 1 < NS
        allh = ["hT5_%d_%d_%d" % (s4 % 2, i, k) for i in range(5) for k in range(KC)]
        for cc in range(4):
            pb = cc % 2
            ba_, bg_ = bgluT[:, cc:cc + 1], bgluT[:, 4 + cc:5 + cc]
            pa, pg, ph = 2 + 3 * pb, 3 + 3 * pb, 4 + 3 * pb
            mms([(ps[pa][:, :], wglu[:, k, cc * 128:(cc + 1) * 128], hT5[:, k, 0:512], k == 0, k == KC - 1)
                 for k in range(KC)], r=allh + ["wglu"], w=["ps%d" % pa])
            mms([(ps[pg][:, :], wglu[:, k, 512 + cc * 128:512 + (cc + 1) * 128], hT5[:, k, 0:512], k == 0, k == KC - 1)
                 for k in range(KC)], r=allh + ["wglu"], w=["ps%d" % pg])
            mms([(ps[ph][:, 0:128], wglu[:, k, cc * 128:(cc + 1) * 128], hT5[:, k, 512:640], k == 0, k == KC - 1)
                 for k in range(KC)] +
                [(ps[ph][:, 128:256], wglu[:, k, 512 + cc * 128:512 + (cc + 1) * 128], hT5[:, k, 512:640], k == 0, k == KC - 1)
                 for k in range(KC)], r=allh + ["wglu"], w=["ps%d" % ph])
            act(sig[pb], ps[pg][:, :], AF.Sigmoid, r=["ps%d" % pg, "bgluT"], w=["sig%d" % pb], bias=bg_)
            act(sigh[pb], ps[ph][:, 128:256], AF.Sigmoid, r=["ps%d" % ph, "bgluT"], w=["sigh%d" % pb], bias=bg_)
            act(asb[pb], ps[pa][:, :], AF.Identity, r=["ps%d" % pa, "bgluT"], w=["asb%d" % pb], bias=ba_)
            act(asbh[pb], ps[ph][:, 0:128], AF.Identity, r=["ps%d" % ph, "bgluT"], w=["asbh%d" % pb], bias=ba_)
            tt("dve", uT[:, cc, :, 32:160], asb[pb][:, :].rearrange("p (i t) -> p i t", i=4),
               sig[pb][:, :].rearrange("p (i t) -> p i t", i=4), ALU.mult,
               r=["asb%d" % pb, "sig%d" % pb], w=["uT%d" % cc])
            tt("dve", uT[:, cc, :, 0:32], asbh[pb][:, :].rearrange("p (i t) -> p i t", i=4),
               sigh[pb][:, :].rearrange("p (i t) -> p i t", i=4), ALU.mult,
               r=["asbh%d" % pb, "sigh%d" % pb], w=["uT%d" % cc])
            if s4 == 0:
                tsc("dve", uT[:, cc, 0, 0:32], uT[:, cc, 0, 0:32], hmask[:, 0:1], None, ALU.mult, None,
                    r=["uT%d" % cc, "hmask"], w=["uT%d" % cc])

    def d1_conv(s4):
        hT5 = hT5b[s4 % 2]
        nxt = s4 + 1 < NS
        allh = ["hT5_%d_%d_%d" % (s4 % 2, i, k) for i in range(5) for k in range(KC)]
        if nxt:
            d1_front(s4 + 1, 0)
        for cc in range(4):
            cb_ = 2 + 3 * (cc % 2)
            for k in range(31):
                sl = dgc["n"] % 8
                dgc["n"] += 1
                tsc("dve", dg[sl], ident_b, wdwT[:, cc, k:k + 1], None, ALU.mult, None,
                    r=["ident_b", "wdwT"], w=["dg%d" % sl])
                mms([(ps[cb_][:, :], dg[sl], uT[:, cc, :, 2 + k:130 + k], k == 0, k == 30)],
                    r=["dg%d" % sl, "uT%d" % cc], w=["ps%d" % cb_])
            act(yv[:, cc, :], ps[cb_][:, :], AF.Identity, r=["ps%d" % cb_, "bdwT"], w=["yv%d" % cc], bias=bdwT[:, cc:cc + 1])
            act(ysq[:, cc, :], yv[:, cc, :], AF.Square, r=["yv%d" % cc], w=["ysq%d" % cc])
            if nxt:
                d1_back(s4 + 1, cc)
                d1_front(s4 + 1, cc + 1)

    def d1_ln(s4):
        hT5 = hT5b[s4 % 2]
        nxt = s4 + 1 < NS
        allh = ["hT5_%d_%d_%d" % (s4 % 2, i, k) for i in range(5) for k in range(KC)]
        mms([(ps[3][:, :], ones_ln, yv[:, cc, :], cc == 0, cc == 3) for cc in range(4)],
            r=["yv%d" % cc for cc in range(4)] + ["ones_ln"], w=["ps3"])
        mms([(ps[4][:, :], ones_ln, ysq[:, cc, :], cc == 0, cc == 3) for cc in range(4)],
            r=["ysq%d" % cc for cc in range(4)] + ["ones_ln"], w=["ps4"])
        if nxt:
            d1_back(s4 + 1, 4)
        act(mean_sb, ps[3][:, :], AF.Copy, r=["ps3"], w=["mean_sb"])
        tt("dve", m2, mean_sb, mean_sb, ALU.mult, r=["mean_sb"], w=["m2"])
        tt("dve", m2, ps[4][:, :], m2, ALU.subtract, r=["ps4", "m2"], w=["m2"])
        rstd_from_ss(m2, rstd_bc, 1.0, "m2", "rstd_bc")
        for cc in range(4):
            tb = cc % 2
            tt("dve", tmpv[tb], yv[:, cc, :], mean_sb, ALU.subtract, r=["yv%d" % cc, "mean_sb"], w=["tmpv%d" % tb])
            tt("dve", tmpv[tb], tmpv[tb], rstd_bc, ALU.mult, r=["tmpv%d" % tb, "rstd_bc"], w=["tmpv%d" % tb])
            act(convT[:, cc, :], tmpv[tb], AF.Silu, r=["tmpv%d" % tb, "lngT", "lnbT"], w=["convT%d" % cc],
                bias=lnbT[:, cc:cc + 1], scale=lngT[:, cc:cc + 1])

    def d1_out(s4):
        hT5 = hT5b[s4 % 2]
        nxt = s4 + 1 < NS
        allh = ["hT5_%d_%d_%d" % (s4 % 2, i, k) for i in range(5) for k in range(KC)]
        for i in range(4):
            m = 4 * s4 + i
            for half in range(2):
                ob_ = 5 + (i * 2 + half) % 3
                items = []
                for k in range(8):
                    lh = attnT[:, k, m * 128:(m + 1) * 128] if k < 4 else convT[:, k - 4, i * 128:(i + 1) * 128]
                    items.append((ps[ob_][:, :], lh, woutp[:, k, half * 512:(half + 1) * 512], k == 0, k == 7))
                mms(items, r=[("attnT", m, h) for h in range(4)] + ["convT%d" % c_ for c_ in range(4)] + ["woutp"],
                    w=["ps%d" % ob_])
                tt("dve", X[:, m, half * 512:(half + 1) * 512], ps[ob_][:, :], X[:, m, half * 512:(half + 1) * 512],
                   ALU.add, r=["ps%d" % ob_, ("X", m)], w=[("X", m)])


    d1_glu(0)
    for s4 in range(NS):
        d1_conv(s4)
        d1_ln(s4)
        if s4 + 1 < NS:
            d1_glu(s4 + 1)
        d1_out(s4)

    if stop_after in ("D1", "D1a"):
        dbg["X"] = nc.dram_tensor("dbg_X", [NOWN * 128, D], F32, kind="ExternalOutput").ap()
        for m in range(4 * NS):
            dma(dbg["X"][m * 128:(m + 1) * 128, :], X[:, m, :], r=[("X", m)], w=[], tag="outX")
        S.emit()
        return nc

    S.barrier(bar_tile)
    A.reset(D1W - 8192)
    h2T = A.alloc([128, KC, NOWN * 128], BF16)
    cmb = A.alloc([128, NOWN, 32])
    gtf_rep = A.alloc([128, D])
    D2W = A.mark()
    crep = A.alloc([128, KC, 128])
    abufs = ada_alloc()
    xn = [A.alloc([128, D]) for _ in range(2)]
    h2f = [A.alloc([128, KC, 128]) for _ in range(2)]
    rs_ = [A.alloc([128, 128]) for _ in range(2)]
    ssD = [A.alloc([128, 1]) for _ in range(2)]
    rsD = [A.alloc([128, 1]) for _ in range(2)]
    cp("dve", crep, cact.unsqueeze(2).broadcast_to([128, KC, 128]), r=["cact"], w=["crep"])
    for g in range(6, 10):
        ada_group(g, abufs[g % 2], 7 - 2 * (g % 2))
        ada_to_cols(abufs[g % 2], sh_f if g < 8 else sc_f, (g % 2) * 4, 6 - 2 * (g % 2))
    tsc("dve", gsc_f, sc_f, 1.0, None, ALU.add, None, r=["adacols"], w=["gsc_f"])
    tt("dve", gsc_f, gsc_f, gffnT, ALU.mult, r=["gsc_f", "gffnT"], w=["gsc_f"])
    for g in (10, 11):
        ada_group(g, abufs[g % 2], 7 - 2 * (g % 2))
        cp("dve", gtf_rep[:, (g - 10) * 512:(g - 9) * 512], abufs[g % 2][2], r=["ada_modrep%d" % (g % 2)], w=["gtf_rep"])

    NB2 = NOWN if not mini else 4
    NRS = 5
    rs_ = rs_ + [A.alloc([128, 128]) for _ in range(NRS - 2)]

    def rviews(m):
        R = rs_[m % NRS]
        v = dict(lg=R[:, 0:36], gmax=R[:, 36:37], ngmax=R[:, 37:38], gsum=R[:, 38:39], gp=R[:, 39:40],
                 ohg=R[:, 40:44], junk4=R[:, 44:48], esel=R[:, 48:80], ein=R[:, 80:88], top8=R[:, 88:96],
                 eq2=R[:, 112:120], cmb8=R[:, 120:128])
        for i_, nm in enumerate(("dcol", "ed", "den", "w1", "w2", "w1g", "w2g")):
            v[nm] = R[:, 96 + i_:97 + i_]
        return v, "rs%d" % (m % NRS)

    def d_n1(m):
        bi = m % 2
        act(junkb, X[:, m, :], AF.Square, r=[("X", m)], w=["junk_sb", "ssD%d" % bi], accum=ssD[bi])
        rstd_from_ss(ssD[bi], rsD[bi], 1.0 / D, "ssD%d" % bi, "ssD%dr" % bi)

    def d_n2(m):
        bi = m % 2
        tsc("dve", xn[bi], X[:, m, :], rsD[bi], None, ALU.mult, None, r=[("X", m), "ssD%dr" % bi], w=["xn%d" % bi])

    def d_n3(m):
        bi = m % 2
        for half in range(2):
            tps([(ps[half][:, kk * 128:(kk + 1) * 128], xn[bi][:, (half * 4 + kk) * 128:(half * 4 + kk + 1) * 128])
                 for kk in range(4)], ident_f, r=["xn%d" % bi, "ident_f"], w=["ps%d" % half])

    def d_n4(m):
        bi = m % 2
        for half in range(2):
            for kk in range(4):
                k = half * 4 + kk
                act(h2f[bi][:, k, :], ps[half][:, kk * 128:(kk + 1) * 128], AF.Identity,
                    r=["ps%d" % half, "gsc_f", "adacols"], w=["h2f%d_%d" % (bi, k)],
                    bias=sh_f[:, k:k + 1], scale=gsc_f[:, k:k + 1])

    def d_n5(m):
        bi = m % 2
        hks = ["h2f%d_%d" % (bi, k) for k in range(KC)]
        cp("pool", h2T[:, :, m * 128:(m + 1) * 128], h2f[bi], r=hks, w=[("h2T", m)])
        mms([(ps[2][:, 0:36], h2f[bi][:, k, :], wr[:, k, :], k == 0, k == KC - 1) for k in range(KC)],
            r=hks + ["wr"], w=["ps2"])

    def d_cA(m):
        v, rk_ = rviews(m)
        tt("dve", v["lg"], ps[2][:, 0:36], br_rep, ALU.add, r=["ps2", "br_rep"], w=[rk_])
        S.add("dve", lambda e, gmax=v["gmax"], lg=v["lg"]: e.tensor_reduce(gmax, lg[:, 0:4], AX.X, ALU.max), r=[rk_], w=[rk_])
        tsc("dve", v["ngmax"], v["gmax"], -1.0, None, ALU.mult, None, r=[rk_], w=[rk_])

    def d_cB(m):
        v, rk_ = rviews(m)
        act(v["junk4"], v["lg"][:, 0:4], AF.Exp, r=[rk_], w=[rk_], bias=v["ngmax"], scale=1.0, accum=v["gsum"])

    def d_cC(m):
        v, rk_ = rviews(m)
        lg, ohg, esel, ein, top8 = v["lg"], v["ohg"], v["esel"], v["ein"], v["top8"]
        S.add("dve", lambda e, gp=v["gp"], gsum=v["gsum"]: e.reciprocal(gp, gsum), r=[rk_], w=[rk_])
        tsc("dve", ohg, lg[:, 0:4], v["gmax"], None, ALU.is_ge, None, r=[rk_], w=[rk_])
        tt("dve", esel.rearrange("p (g e) -> p g e", g=4), lg[:, 4:36].rearrange("p (g e) -> p g e", g=4),
           ohg.unsqueeze(2).broadcast_to([128, 4, 8]), ALU.mult, r=[rk_], w=[rk_])
        S.add("dve", lambda e, ein=ein, esel=esel: e.tensor_reduce(ein, esel.rearrange("p (g e) -> p e g", g=4), AX.X, ALU.add),
              r=[rk_], w=[rk_])
        S.add("dve", lambda e, top8=top8, ein=ein: e.max(top8, ein), r=[rk_], w=[rk_])
        tt("dve", v["dcol"], top8[:, 1:2], top8[:, 0:1], ALU.subtract, r=[rk_], w=[rk_])

    def d_cD(m):
        v, rk_ = rviews(m)
        act(v["ed"], v["dcol"], AF.Exp, r=[rk_], w=[rk_])

    def d_cE(m):
        v, rk_ = rviews(m)
        ed, den, w1, w2, w1g, w2g, gp = v["ed"], v["den"], v["w1"], v["w2"], v["w1g"], v["w2g"], v["gp"]
        ein, top8, cmb8, eq2, ohg = v["ein"], v["top8"], v["cmb8"], v["eq2"], v["ohg"]
        tsc("dve", den, ed, 1.0, None, ALU.add, None, r=[rk_], w=[rk_])
        S.add("dve", lambda e, w1=w1, den=den: e.reciprocal(w1, den), r=[rk_], w=[rk_])
        tt("dve", w2, ed, w1, ALU.mult, r=[rk_], w=[rk_])
        tt("dve", w1g, w1, gp, ALU.mult, r=[rk_], w=[rk_])
        tt("dve", w2g, w2, gp, ALU.mult, r=[rk_], w=[rk_])
        tsc("dve", cmb8, ein, top8[:, 0:1], None, ALU.is_equal, None, r=[rk_], w=[rk_])
        tsc("dve", cmb8, cmb8, w1g, None, ALU.mult, None, r=[rk_], w=[rk_])
        tsc("dve", eq2, ein, top8[:, 1:2], None, ALU.is_equal, None, r=[rk_], w=[rk_])
        tsc("dve", eq2, eq2, w2g, None, ALU.mult, None, r=[rk_], w=[rk_])
        tt("dve", cmb8, cmb8, eq2, ALU.add, r=[rk_], w=[rk_])
        tt("dve", cmb[:, m, :].rearrange("p (g e) -> p g e", g=4), ohg.unsqueeze(2).broadcast_to([128, 4, 8]),
           cmb8.unsqueeze(1).broadcast_to([128, 4, 8]), ALU.mult, r=[rk_], w=[("cmb", m)])

    d_order = [(d_cE, 9), (d_cD, 8), (d_cC, 7), (d_cB, 6), (d_cA, 5), (d_n5, 4), (d_n4, 3), (d_n3, 2), (d_n2, 1), (d_n1, 0)]
    for t in range(NB2 + 9):
        for fn_, off_ in d_order:
            m = t - off_
            if 0 <= m < NB2:
                fn_(m)

    if stop_after == "D2":
        dbg["cmb"] = nc.dram_tensor("dbg_cmb", [128, NOWN * 32], F32, kind="ExternalOutput").ap()
        dbg["h2T"] = nc.dram_tensor("dbg_h2T", [128, KC * NOWN * 128], BF16, kind="ExternalOutput").ap()
        if mini:
            S.add("dve", lambda e: e.memset(cmb[:, NB2:, :], 0.0), r=[], w=["cmbpad"])
            S.add("dve", lambda e: e.memset(h2T[:, :, NB2 * 128:], 0.0), r=[], w=["h2pad"])
        dma(dbg["cmb"], cmb[:, :, :].rearrange("p a b -> p (a b)"), r=[("cmb", m) for m in range(NB2)] + ["cmbpad"], w=[], tag="out0")
        dma(dbg["h2T"], h2T[:, :, :].rearrange("p a b -> p (a b)"), r=[("h2T", m) for m in range(NB2)] + ["h2pad"], w=[], tag="out1")
        S.emit()
        return nc

    S.barrier(bar_tile)
    A.reset(D2W)
    Wg = [A.alloc([128, KC, 256], BF16) for _ in range(2)]
    Wu = [A.alloc([128, KC, 256], BF16) for _ in range(2)]
    Wd = [A.alloc([128, 2, D], BF16) for _ in range(2)]
    stE = [A.alloc([128, 2048]) for _ in range(4)]
    hid = [A.alloc([128, 2, 512], BF16) for _ in range(2)]
    sgl = [A.alloc([128, 512], BF16) for _ in range(2)]
    tacc = [A.alloc([128, 512]) for _ in range(4)]
    pass
    stc = {"n": 0}

    def load_expert(e_):
        eb = e_ % 2
        for which in range(3):
            sl = stc["n"] % 4
            stc["n"] += 1
            sk = "stE%d" % sl
            if which < 2:
                srcw = (w_gate if which == 0 else w_up)[e_].rearrange("(k p) f -> p k f", p=128)
                stv = stE[sl][:, :].rearrange("p (k f) -> p k f", k=KC)
                dma(stv, srcw, r=[], w=[sk], tag=sk)
                if which == 0:
                    cp("pool", Wg[eb], stv, r=[sk], w=["Wg%d" % eb])
                else:
                    act(Wu[eb], stv, AF.Copy, r=[sk], w=["Wu%d" % eb])
            else:
                srcw = w_down[e_].rearrange("(c p) n -> p c n", p=128)
                stv = stE[sl][:, :].rearrange("p (c n) -> p c n", c=2)
                dma(stv, srcw, r=[], w=[sk], tag=sk)
                tt("pool", Wd[eb], stv, gtf_rep.unsqueeze(1).broadcast_to([128, 2, D]), ALU.mult,
                   r=[sk, "gtf_rep"], w=["Wd%d" % eb])

    def gate_up(e_, s4, hb):
        eb = e_ % 2
        hk = [("h2T", 4 * s4 + i) for i in range(4)]
        for fc in range(2):
            mms([(ps[2 * fc][:, :], Wg[eb][:, k, fc * 128:(fc + 1) * 128], h2T[:, k, s4 * 512:(s4 + 1) * 512], k == 0, k == KC - 1)
                 for k in range(KC)], r=hk + ["Wg%d" % eb], w=["ps%d" % (2 * fc)])
            mms([(ps[2 * fc + 1][:, :], Wu[eb][:, k, fc * 128:(fc + 1) * 128], h2T[:, k, s4 * 512:(s4 + 1) * 512], k == 0, k == KC - 1)
                 for k in range(KC)], r=hk + ["Wu%d" % eb], w=["ps%d" % (2 * fc + 1)])
            act(sgl[fc], ps[2 * fc][:, :], AF.Silu, r=["ps%d" % (2 * fc)], w=["sgl%d" % fc])
            tt("dve", hid[hb][:, fc, :], ps[2 * fc + 1][:, :], sgl[fc], ALU.mult,
               r=["ps%d" % (2 * fc + 1), "sgl%d" % fc], w=["hid%d_%d" % (hb, fc)])

    def down(e_, s4, hb):
        eb = e_ % 2
        for i in range(4):
            m = 4 * s4 + i
            for half in range(2):
                ob_ = 4 + (i * 2 + half) % 4
                mms([(ps[ob_][:, :], hid[hb][:, fc, i * 128:(i + 1) * 128], Wd[eb][:, fc, half * 512:(half + 1) * 512], fc == 0, fc == 1)
                     for fc in range(2)], r=["hid%d_0" % hb, "hid%d_1" % hb, "Wd%d" % eb], w=["ps%d" % ob_])
                tb_ = (i * 2 + half) % 4
                act(tacc[tb_], ps[ob_][:, :], AF.Identity, r=["ps%d" % ob_, ("cmb", m)], w=["tacc%d" % tb_],
                    scale=cmb[:, m, e_:e_ + 1])
                tt("dve", X[:, m, half * 512:(half + 1) * 512], X[:, m, half * 512:(half + 1) * 512], tacc[tb_],
                   ALU.add, r=["tacc%d" % tb_, ("X", m)], w=[("X", m)])

    NE = NEXP
    load_expert(0)
    prev = None
    step = 0
    for e_ in range(NE):
        for s4 in range(4 if not mini else 1):
            hb = step % 2
            step += 1
            gate_up(e_, s4, hb)
            if prev is not None:
                down(*prev)
            prev = (e_, s4, hb)
            if s4 == 0 and e_ + 1 < NE:
                load_expert(e_ + 1)
    down(*prev)

    for m in range(NOWN if not mini else 4):
        dma(out_d[m * 128:(m + 1) * 128, :], X[:, m, :], r=[("X", m)], w=[], tag="outX")
    S.emit()
    return nc


def host_inputs(inputs, core):
    b, j = core // 4, core % 4
    f = np.float32
    x = np.asarray(inputs["x"], f)
    xb_ = np.ascontiguousarray(x[b])
    own_idx = np.concatenate([np.arange(512 * m + 128 * j, 512 * m + 128 * j + 128) for m in range(NOWN)])
    xo_ = np.ascontiguousarray(xb_[own_idx])
    xh_ = np.zeros((4 * 128, D), f)
    for m in range(NOWN):
        st = 512 * m + 128 * j - 32
        if st >= 0:
            xh_[m * 32:(m + 1) * 32] = xb_[st:st + 32]
    pos = np.asarray(inputs["positions"], np.int32)[b]
    posb_ = np.ascontiguousarray(pos.reshape(NBLK_B, 128).T)
    poso_ = np.ascontiguousarray(pos[own_idx].reshape(NOWN, 128).T)

    def colT(v, n):
        return np.ascontiguousarray(np.asarray(v, f).reshape(n, 128).T)

    kq = np.arange(128)
    cm = np.zeros((128, 4, 2, 128), f)
    for t in range(4):
        allowed = (t * 128 + kq[:, None]) <= (j * 128 + kq[None, :])
        cm[:, t, :, :] = allowed[:, None, :]
    d = {
        "xb": xb_, "xo": xo_, "xh": xh_,
        "cT": colT(inputs["c"][b], KC),
        "posb": posb_, "poso": poso_,
        "w_ada": np.ascontiguousarray(np.asarray(inputs["w_ada"], f)[0]),
        "b_ada": np.ascontiguousarray(np.asarray(inputs["b_ada"], f)[0:1]),
        "g_mixT": colT(inputs["g_mix"][0], KC),
        "g_ffnT": colT(inputs["g_ffn"][0], KC),
        "w_in": np.ascontiguousarray(np.asarray(inputs["w_in"], f)[0]),
        "q_norm_g": np.asarray(inputs["q_norm_g"], f)[0:1],
        "k_norm_g": np.asarray(inputs["k_norm_g"], f)[0:1],
        "lambda_q1": np.asarray(inputs["lambda_q1"], f)[0:1],
        "lambda_k1": np.asarray(inputs["lambda_k1"], f)[0:1],
        "lambda_q2": np.asarray(inputs["lambda_q2"], f)[0:1],
        "lambda_k2": np.asarray(inputs["lambda_k2"], f)[0:1],
        "subln_g": np.asarray(inputs["subln_g"], f)[0:1],
        "b_gluT": colT(inputs["b_glu"][0], 8),
        "w_dwT": np.ascontiguousarray(np.asarray(inputs["w_dw"], f)[0].T.reshape(4, 128, 31).transpose(1, 0, 2)),
        "b_dwT": colT(inputs["b_dw"][0], 4),
        "ln_gT": colT(inputs["conv_ln_g"][0], 4),
        "ln_bT": colT(inputs["conv_ln_b"][0], 4),
        "w_out": np.ascontiguousarray(np.asarray(inputs["w_out"], f)[0]),
        "w_rt": np.ascontiguousarray(np.concatenate([np.asarray(inputs["w_group"], f)[0],
                                                     np.asarray(inputs["w_router"], f)[0]], axis=1)),
        "b_rt": np.ascontiguousarray(np.concatenate([np.asarray(inputs["b_group"], f)[0],
                                                     np.asarray(inputs["b_router"], f)[0]])[None, :]),
        "w_gate": np.ascontiguousarray(np.asarray(inputs["w_gate"], f)[0]),
        "w_up": np.ascontiguousarray(np.asarray(inputs["w_up"], f)[0]),
        "w_down": np.ascontiguousarray(np.asarray(inputs["w_down"], f)[0]),
        "ident_f": np.eye(128, dtype=f),
        "ident_b": np.eye(128, dtype=f).astype(ml_dtypes.bfloat16),
        "inv_freq": (500000.0 ** (-np.arange(0, 16, 2, dtype=f) / 16.0)).astype(f)[None, :],
        "cmask": cm.reshape(128, 1024).astype(ml_dtypes.bfloat16),
        "hmask": np.full((128, 1), 0.0 if j == 0 else 1.0, f),
    }
    return d, own_idx


def kernel(**inputs):
    nc = build_program()
    in_maps = []
    owns = []
    for c in range(8):
        d, own_idx = host_inputs(inputs, c)
        in_maps.append(d)
        owns.append(own_idx)
    res = run_bass_kernel_spmd(nc, in_maps, core_ids=list(range(8)))
    out = np.zeros((2, SEQ, D), np.float32)
    for c in range(8):
        out[c // 4, owns[c]] = np.asarray(res.results[c]["out"], np.float32)
    return out
```

```python
import math
import numpy as np
import ml_dtypes
import concourse.bass as bass
import concourse.mybir as mybir
from concourse.bass_utils import run_bass_kernel_spmd

F32 = mybir.dt.float32
BF16 = mybir.dt.bfloat16
I32 = mybir.dt.int32
AF = mybir.ActivationFunctionType
ALU = mybir.AluOpType
AX = mybir.AxisListType

D = 1024
KC = 8
SEQ = 8192
NBLK_B = 64
NOWN = 16
EPS = 1e-6
LAMBDA_INIT = 0.8 - 0.6 * math.exp(-0.3 * 0)
NEXP = 32
TWO_PI = 2.0 * math.pi
C1 = 6.28125
C2 = TWO_PI - C1


class Sched:
    COMPUTE = ("pe", "act", "dve", "pool")

    def __init__(self, nc):
        self.nc = nc
        self.ops = []
        self.last_w = {}
        self.readers = {}
        self.tag_count = {}
        self.bulk_tags = set()
        self.epoch = None
        self.epoch_start = 0

    def add(self, eng, fn, r=(), w=(), tag=None):
        idx = len(self.ops)
        deps = set()
        for res in r:
            if res in self.last_w:
                deps.add(self.last_w[res])
        for res in w:
            if res in self.last_w:
                deps.add(self.last_w[res])
            for i in self.readers.get(res, {}).values():
                deps.add(i)
        if self.epoch is not None:
            deps.add(self.epoch)
        op = dict(eng=eng, fn=fn, deps=deps, tag=tag, signal=False, idx=idx)
        if tag is not None:
            self.tag_count[tag] = self.tag_count.get(tag, 0) + 1
            op["tagn"] = self.tag_count[tag]
        self.ops.append(op)
        for res in w:
            self.last_w[res] = idx
            self.readers[res] = {}
        for res in r:
            key = eng if tag is None else ("dma", idx)
            self.readers.setdefault(res, {})[key] = idx
        return idx

    def barrier(self, tile):
        deps = set()
        last = {}
        for op in self.ops[self.epoch_start:]:
            if op["tag"] is None:
                last[op["eng"]] = op["idx"]
            else:
                deps.add(op["idx"])
        deps |= set(last.values())
        idx = self.add("dve", lambda e: e.memset(tile, 0.0))
        self.ops[idx]["deps"] |= deps
        self.epoch = idx
        self.epoch_start = idx

    def emit(self):
        nc = self.nc
        ops = self.ops
        for op in ops:
            for d in op["deps"]:
                dop = ops[d]
                if dop["tag"] is None and dop["eng"] == "pe" and op["eng"] == "pe" and op["tag"] is None:
                    continue
                dop["signal"] = True
        sems = {e: nc.alloc_semaphore(name="sem_" + e) for e in self.COMPUTE}
        tagsem = {t: nc.alloc_semaphore(name="semt_" + str(t)) for t in self.tag_count}
        cnt = {e: 0 for e in self.COMPUTE}
        for op in ops:
            if op["tag"] is not None:
                t = op["tag"]
                n = self.tag_count[t] if t in self.bulk_tags else op["tagn"]
                op["token"] = (tagsem[t], 16 * n, ("t", t))
            elif op["signal"]:
                cnt[op["eng"]] += 1
                op["token"] = (sems[op["eng"]], cnt[op["eng"]], ("e", op["eng"]))
        streams = {e: [] for e in ("pe", "act", "dve", "pool", "sp")}
        for op in ops:
            streams[op["eng"]].append(op)
        out_tag_total = {t: 16 * n for t, n in self.tag_count.items()}

        def run(engname, e):
            waited = {}
            for op in streams[engname]:
                need = {}
                for d in op["deps"]:
                    dop = ops[d]
                    if dop["tag"] is None and dop["eng"] == "pe" and engname == "pe" and op["tag"] is None:
                        continue
                    sem, val, key = dop["token"]
                    if need.get(key, (None, 0))[1] < val:
                        need[key] = (sem, val)
                for key, (sem, val) in need.items():
                    if waited.get(key, 0) >= val:
                        continue
                    e.wait_ge(sem, val)
                    waited[key] = val
                ins = op["fn"](e)
                if op["tag"] is not None:
                    ins.then_inc(op["token"][0], 16)
                elif op["signal"]:
                    ins.then_inc(op["token"][0], 1)
            if engname == "sp":
                for t, tot in out_tag_total.items():
                    if str(t).startswith("out"):
                        e.wait_ge(tagsem[t], tot)

        with nc.Block() as block:
            @block.tensor
            def _(e):
                run("pe", e)

            @block.scalar
            def _(e):
                run("act", e)

            @block.vector
            def _(e):
                run("dve", e)

            @block.gpsimd
            def _(e):
                run("pool", e)

            @block.sync
            def _(e):
                run("sp", e)


class Arena:
    def __init__(self, nc, words):
        self.t = nc.alloc_sbuf_tensor("arena", [128, words], F32)
        self.words = words
        self.off = 0

    def mark(self):
        return self.off

    def reset(self, m):
        self.off = m

    def alloc(self, shape, dt=F32):
        n = 1
        for s in shape[1:]:
            n *= s
        w = n if dt in (F32, I32) else (n + 1) // 2
        w = (w + 7) // 8 * 8
        assert self.off + w <= self.words, ("arena overflow", self.off, w, self.words)
        v = self.t[:, self.off:self.off + w]
        self.off += w
        if dt != F32:
            v = v.bitcast(dt)
        v = v[:, 0:n]
        if len(shape) == 3:
            v = v.rearrange("p (a b) -> p a b", a=shape[1])
        elif len(shape) == 4:
            v = v.rearrange("p (a b c) -> p a b c", a=shape[1], b=shape[2])
        return v


def build_program(stop_after=None, mini=False, mini_ns=1):
    nc = bass.Bass("TRN2", target_bir_lowering=False)
    S = Sched(nc)
    S.bulk_tags.add("const")

    def din(name, shape, dt=F32):
        return nc.dram_tensor(name, list(shape), dt, kind="ExternalInput").ap()

    xb = din("xb", [SEQ, D])
    xo = din("xo", [NOWN * 128, D])
    xh = din("xh", [4 * 128, D])
    cT_d = din("cT", [128, KC])
    posb_d = din("posb", [128, NBLK_B], I32)
    poso_d = din("poso", [128, NOWN], I32)
    w_ada = din("w_ada", [D, 6 * D])
    b_ada = din("b_ada", [1, 6 * D])
    gmix_d = din("g_mixT", [128, KC])
    gffn_d = din("g_ffnT", [128, KC])
    w_in = din("w_in", [D, 2560])
    gq_d = din("q_norm_g", [1, 64])
    gk_d = din("k_norm_g", [1, 64])
    lq1_d = din("lambda_q1", [1, 64])
    lk1_d = din("lambda_k1", [1, 64])
    lq2_d = din("lambda_q2", [1, 64])
    lk2_d = din("lambda_k2", [1, 64])
    subg_d = din("subln_g", [1, 128])
    bglu_d = din("b_gluT", [128, 8])
    wdw_d = din("w_dwT", [128, 4, 31])
    bdw_d = din("b_dwT", [128, 4])
    lng_d = din("ln_gT", [128, 4])
    lnb_d = din("ln_bT", [128, 4])
    w_out = din("w_out", [D, D])
    wr_d = din("w_rt", [D, 36])
    br_d = din("b_rt", [1, 36])
    w_gate = din("w_gate", [NEXP, D, 256])
    w_up = din("w_up", [NEXP, D, 256])
    w_down = din("w_down", [NEXP, 256, D])
    identf_d = din("ident_f", [128, 128])
    identb_d = din("ident_b", [128, 128], BF16)
    invf_d = din("inv_freq", [1, 8])
    mask_d = din("cmask", [128, 4 * 256], BF16)
    hmask_d = din("hmask", [128, 1])
    out_d = nc.dram_tensor("out", [NOWN * 128, D], F32, kind="ExternalOutput").ap()
    dbg = {}

    A = Arena(nc, 53000)
    ps = [nc.alloc_psum_tensor("ps%d" % i, [128, 512], F32) for i in range(8)]

    def psb(i):
        return ps[i][:, :].bitcast(BF16)

    ident_f = A.alloc([128, 128])
    ident_b = A.alloc([128, 128], BF16)
    cT = A.alloc([128, KC])
    cact = A.alloc([128, KC])
    gmixT = A.alloc([128, KC])
    gffnT = A.alloc([128, KC])
    sh_a = A.alloc([128, KC]); sc_a = A.alloc([128, KC]); gsc_a = A.alloc([128, KC])
    sh_f = A.alloc([128, KC]); sc_f = A.alloc([128, KC]); gsc_f = A.alloc([128, KC])
    gq_rep = A.alloc([128, 64]); gk_rep = A.alloc([128, 64])
    lvec = A.alloc([128, 4, 64])
    sg_rep = A.alloc([128, 128])
    lam_t = A.alloc([128, 8])
    bgluT = A.alloc([128, 8])
    wdwT = A.alloc([128, 4, 31])
    bdwT = A.alloc([128, 4]); lngT = A.alloc([128, 4]); lnbT = A.alloc([128, 4])
    wr = A.alloc([128, KC, 36])
    br_rep = A.alloc([128, 36])
    invf = A.alloc([128, 8])
    cmask = A.alloc([128, 4 * 256], BF16)
    hmask = A.alloc([128, 1])
    cos_b = A.alloc([128, NBLK_B, 8]); sin_b = A.alloc([128, NBLK_B, 8])
    cos_o = A.alloc([128, NOWN, 8]); sin_o = A.alloc([128, NOWN, 8])
    epsc = A.alloc([128, 1]); m8c = A.alloc([128, 1])
    ones_ln = A.alloc([128, 128])
    ATTN_OFF = A.mark()
    attnT = A.alloc([128, 4, NOWN * 128], BF16)
    small = A.alloc([128, 64])
    junk_sb = A.alloc([128, D], BF16)
    P_END = A.mark()

    def dma(out, in_, r, w, tag):
        S.add("sp", lambda e, o=out, i=in_: e.dma_start(out=o, in_=i), r=r, w=w, tag=tag)

    def act(out, in_, func, r, w, bias=None, scale=None, accum=None):
        def f(e, out=out, in_=in_, func=func, bias=bias, scale=scale, accum=accum):
            kw = {}
            if bias is not None:
                kw["bias"] = bias
            if scale is not None:
                kw["scale"] = scale
            if accum is not None:
                kw["accum_out"] = accum
            return e.activation(out, in_, func, **kw)
        S.add("act", f, r=r, w=w)

    def tsc(eng, out, in0, s1, s2, op0, op1, r, w):
        def f(e, out=out, in0=in0, s1=s1, s2=s2, op0=op0, op1=op1):
            if op1 is None:
                return e.tensor_scalar(out, in0, s1, None, op0)
            return e.tensor_scalar(out, in0, s1, s2, op0, op1)
        S.add(eng, f, r=r, w=w)

    def tt(eng, out, in0, in1, op, r, w):
        S.add(eng, lambda e, o=out, a=in0, b=in1, op=op: e.tensor_tensor(o, a, b, op), r=r, w=w)

    def stt(out, in0, sc, in1, op0, op1, r, w):
        S.add("dve", lambda e, o=out, a=in0, s=sc, b=in1, o0=op0, o1=op1:
              e.scalar_tensor_tensor(o, a, s, b, o0, o1), r=r, w=w)

    def cp(eng, out, in_, r, w):
        S.add(eng, lambda e, o=out, i=in_: e.tensor_copy(o, i), r=r, w=w)

    def mms(items, r, w):
        def f(e, items=items):
            ins = None
            for (o, l, rh, st, sp_) in items:
                ins = e.matmul(o, l, rh, start=st, stop=sp_)
            return ins
        S.add("pe", f, r=r, w=w)

    def tps(items, ident, r, w):
        def f(e, items=items, ident=ident):
            ins = None
            for (o, i) in items:
                ins = e.transpose(o, i, ident)
            return ins
        S.add("pe", f, r=r, w=w)

    def rstd_from_ss(ss_ap, out_ap, inv_n, rk, wk):
        act(out_ap, ss_ap, AF.Ln, r=[rk, "epsc"], w=[wk], bias=epsc[:, 0:1], scale=inv_n)
        act(out_ap, out_ap, AF.Exp, r=[wk], w=[wk], scale=-0.5)

    def cload(dst, src, key):
        dma(dst, src, r=[], w=[key], tag="const")

    cload(ident_f, identf_d, "ident_f")
    cload(ident_b, identb_d, "ident_b")
    cload(cT, cT_d, "cT")
    cload(gmixT, gmix_d, "gmixT")
    cload(gffnT, gffn_d, "gffnT")
    cload(gq_rep, gq_d.broadcast_to([128, 64]), "gq_rep")
    cload(gk_rep, gk_d.broadcast_to([128, 64]), "gk_rep")
    for i, dd in enumerate((lq1_d, lk1_d, lq2_d, lk2_d)):
        cload(lvec[:, i, :], dd.broadcast_to([128, 64]), "lvec%d" % i)
    cload(sg_rep, subg_d.broadcast_to([128, 128]), "sg_rep")
    cload(bgluT, bglu_d, "bgluT")
    cload(wdwT, wdw_d, "wdwT")
    cload(bdwT, bdw_d, "bdwT")
    cload(lngT, lng_d, "lngT")
    cload(lnbT, lnb_d, "lnbT")
    cload(wr, wr_d.rearrange("(k p) n -> p k n", p=128), "wr")
    cload(br_rep, br_d.broadcast_to([128, 36]), "br_rep")
    cload(invf, invf_d.broadcast_to([128, 8]), "invf")
    cload(cmask, mask_d, "cmask")
    cload(hmask, hmask_d, "hmask")
    posb_i = A.alloc([128, NBLK_B], I32)
    poso_i = A.alloc([128, NOWN], I32)
    cload(posb_i, posb_d, "posb_i")
    cload(poso_i, poso_d, "poso_i")

    S.add("dve", lambda e: e.memset(lam_t, 0.0), r=[], w=["lam0", "lam1", "lam23", "lam4", "neglam"])
    S.add("dve", lambda e: e.memset(small, 0.0), r=[], w=["small", "junk64", "ropetab", "gains"])
    S.add("dve", lambda e: e.memset(epsc, EPS), r=[], w=["epsc"])
    S.add("dve", lambda e: e.memset(m8c, -8.0), r=[], w=["m8c"])
    S.add("dve", lambda e: e.memset(ones_ln, 1.0 / 512.0), r=[], w=["ones_ln"])

    act(cact, cT, AF.Silu, r=["cT"], w=["cact"])
    tsc("dve", sg_rep, sg_rep, 1.0 - LAMBDA_INIT, None, ALU.mult, None, r=["sg_rep"], w=["sg_rep"])

    junk64 = small[:, 0:64]
    tt("dve", junk64, lvec[:, 0, :], lvec[:, 1, :], ALU.mult, r=["lvec0", "lvec1"], w=["junk64"])
    S.add("dve", lambda e: e.tensor_reduce(lam_t[:, 0:1], junk64, AX.X, ALU.add), r=["junk64"], w=["lam0"])
    tt("dve", junk64, lvec[:, 2, :], lvec[:, 3, :], ALU.mult, r=["lvec2", "lvec3", "lam0"], w=["junk64"])
    S.add("dve", lambda e: e.tensor_reduce(lam_t[:, 1:2], junk64, AX.X, ALU.add), r=["junk64"], w=["lam1"])
    act(lam_t[:, 2:4], lam_t[:, 0:2], AF.Exp, r=["lam0", "lam1"], w=["lam23"])
    tt("dve", lam_t[:, 4:5], lam_t[:, 3:4], lam_t[:, 2:3], ALU.subtract, r=["lam23"], w=["lam4"])
    tsc("dve", lam_t[:, 7:8], lam_t[:, 4:5], -LAMBDA_INIT, None, ALU.add, None, r=["lam4"], w=["neglam"])
    neglam = lam_t[:, 7:8]

    def rope_tables(pos_i, nblk, cos_t, sin_t, pfx):
        m0 = A.mark()
        posf = A.alloc([128, nblk])
        ang = A.alloc([128, nblk, 8])
        tq = A.alloc([128, nblk, 8])
        ki = A.alloc([128, nblk, 8], I32)
        kf = A.alloc([128, nblk, 8])
        rr = A.alloc([128, nblk, 8])
        mk = A.alloc([128, nblk, 8])
        cp("dve", posf, pos_i, r=[pfx + "pos_i"], w=[pfx + "posf"])
        tt("dve", ang, posf.unsqueeze(2).broadcast_to([128, nblk, 8]),
           invf.unsqueeze(1).broadcast_to([128, nblk, 8]), ALU.mult, r=[pfx + "posf", "invf"], w=[pfx + "ang"])
        for which, dst in (("s", sin_t), ("c", cos_t)):
            k0 = pfx + which
            src = ang
            if which == "c":
                tsc("dve", rr, ang, math.pi / 2.0, None, ALU.add, None, r=[pfx + "ang", pfx + "rr"], w=[pfx + "rr"])
                tsc("dve", tq, rr, 1.0 / TWO_PI, None, ALU.mult, None, r=[pfx + "rr", pfx + "tq"], w=[pfx + "tq"])
                base = rr
            else:
                tsc("dve", tq, ang, 1.0 / TWO_PI, None, ALU.mult, None, r=[pfx + "ang"], w=[pfx + "tq"])
                base = ang
            cp("dve", ki, tq, r=[pfx + "tq"], w=[pfx + "ki"])
            cp("dve", kf, ki, r=[pfx + "ki"], w=[pfx + "kf"])
            stt(rr, kf, -C1, base, ALU.mult, ALU.add, r=[pfx + "kf", pfx + "ang", pfx + "rr"], w=[pfx + "rr"])
            stt(rr, kf, -C2, rr, ALU.mult, ALU.add, r=[pfx + "kf", pfx + "rr"], w=[pfx + "rr"])
            tsc("dve", mk, rr, math.pi, -TWO_PI, ALU.is_gt, ALU.mult, r=[pfx + "rr", pfx + "mk"], w=[pfx + "mk"])
            tt("dve", rr, rr, mk, ALU.add, r=[pfx + "rr", pfx + "mk"], w=[pfx + "rr"])
            tsc("dve", mk, rr, -math.pi, TWO_PI, ALU.is_lt, ALU.mult, r=[pfx + "rr", pfx + "mk"], w=[pfx + "mk"])
            tt("dve", rr, rr, mk, ALU.add, r=[pfx + "rr", pfx + "mk"], w=[pfx + "rr"])
            act(dst, rr, AF.Sin, r=[pfx + "rr"], w=[pfx + "tab" + which])
        return [pfx + "tabs", pfx + "tabc"]

    KV0 = A.mark()
    KT = A.alloc([128, 4, SEQ], BF16)
    VA = A.alloc([128, NBLK_B, 4, 130], BF16)
    WORK0 = A.mark()
    rb = rope_tables(posb_i, NBLK_B, cos_b, sin_b, "rb_")
    ro = rope_tables(poso_i, NOWN, cos_o, sin_o, "ro_")
    rope_keys = ["rb_rr", "rb_mk", "rb_kf", "rb_ki", "rb_tq", "rb_ang", "rb_posf",
                 "ro_rr", "ro_mk", "ro_kf", "ro_ki", "ro_tq", "ro_ang", "ro_posf"]

    def ada_alloc():
        return [(A.alloc([128, KC, 512]), A.alloc([128, 512]), A.alloc([128, 512]), str(i)) for i in range(2)]

    def ada_group(g, ab, pbank):
        stage, brep, modrep, sfx = ab
        dma(stage, w_ada.rearrange("(k p) n -> p k n", p=128)[:, :, g * 512:(g + 1) * 512],
            r=[], w=["ada_stage" + sfx], tag="ada" + sfx)
        dma(brep, b_ada[0:1, g * 512:(g + 1) * 512].broadcast_to([128, 512]), r=[], w=["ada_brep" + sfx], tag="adab" + sfx)
        mms([(ps[pbank][:, :], crep[:, k, :], stage[:, k, :], k == 0, k == KC - 1) for k in range(KC)],
            r=["ada_stage" + sfx, "crep"], w=["ps%d" % pbank])
        tt("dve", modrep, ps[pbank][:, :], brep, ALU.add, r=["ps%d" % pbank, "ada_brep" + sfx], w=["ada_modrep" + sfx])

    def ada_to_cols(ab, dst, col0, pbank):
        modrep, sfx = ab[2], ab[3]
        tps([(ps[pbank][:, jj * 128:(jj + 1) * 128], modrep[:, jj * 128:(jj + 1) * 128]) for jj in range(4)],
            ident_f, r=["ada_modrep" + sfx, "ident_f"], w=["ps%d" % pbank])
        cp("dve", dst[:, col0:col0 + 4], ps[pbank][:, :].rearrange("p (j c) -> p j c", c=128)[:, :, 0],
           r=["ps%d" % pbank], w=["adacols"])

    S.barrier(small[:, 62:63])
    A.reset(WORK0)
    m_ada = A.mark()
    crep = A.alloc([128, KC, 128])
    cp("dve", crep, cact.unsqueeze(2).broadcast_to([128, KC, 128]), r=["cact"], w=["crep"])
    abufs = ada_alloc()
    for g in range(4):
        ada_group(g, abufs[g % 2], 7 - 2 * (g % 2))
        ada_to_cols(abufs[g % 2], sh_a if g < 2 else sc_a, (g % 2) * 4, 6 - 2 * (g % 2))
    tsc("dve", gsc_a, sc_a, 1.0, None, ALU.add, None, r=["adacols"], w=["gsc_a"])
    tt("dve", gsc_a, gsc_a, gmixT, ALU.mult, r=["gsc_a", "gmixT"], w=["gsc_a"])

    S.barrier(small[:, 62:63])
    A.reset(WORK0)
    WORK = A.mark()
    NXT = 3
    xt = [A.alloc([128, D]) for _ in range(NXT)]
    _cur = A.mark()
    A.reset(ATTN_OFF)
    xnb = [A.alloc([128, D], BF16) for _ in range(2)]
    hT = [A.alloc([128, KC, 128], BF16) for _ in range(2)]
    shrep = A.alloc([128, KC, 128])
    assert A.mark() <= ATTN_OFF + 4096
    A.reset(_cur)
    wkv = A.alloc([128, KC, 1024], BF16)
    shW_bf = A.alloc([128, 1024], BF16)
    c128 = A.alloc([128, 128], BF16)
    junkb = junk_sb
    ssA = [A.alloc([128, 1]) for _ in range(2)]
    rsA = [A.alloc([128, 1]) for _ in range(2)]
    ksq = [A.alloc([128, 512]) for _ in range(2)]
    kn = [A.alloc([128, 8, 64]) for _ in range(2)]
    kb16 = [A.alloc([128, 512], BF16) for _ in range(2)]
    ssk = [A.alloc([128, 8]) for _ in range(2)]
    rk = [A.alloc([128, 8]) for _ in range(2)]
    rt = [[A.alloc([128, 8, 8]) for _ in range(4)] for _ in range(2)]

    S.add("pool", lambda e: e.memset(VA[:, :, :, 128:130], 1.0), r=[], w=["VAones"])
    S.add("pool", lambda e: e.memset(c128, 1.0 / 128.0), r=[], w=["c128"])
    cp("dve", shrep, sh_a.unsqueeze(2).broadcast_to([128, KC, 128]), r=["adacols"], w=["shrep"])

    w_in_v = w_in.rearrange("(k p) n -> p k n", p=128)
    for pc in range(8):
        xi = pc % NXT
        st = xt[xi][:, :].rearrange("p (k n) -> p k n", k=KC)
        dma(st, w_in_v[:, :, 512 + pc * 128: 512 + (pc + 1) * 128], r=[], w=["xt%d" % xi], tag="xt%d" % xi)
        tt("pool", wkv[:, :, pc * 128:(pc + 1) * 128], st, gsc_a.unsqueeze(2).broadcast_to([128, KC, 128]), ALU.mult,
           r=["xt%d" % xi, "gsc_a"], w=["wkv"])
        bk = pc // 4
        mms([(ps[bk][:, (pc % 4) * 128:(pc % 4 + 1) * 128], shrep[:, k, :], st[:, k, :], k == 0, k == KC - 1)
             for k in range(KC)], r=["xt%d" % xi, "shrep"], w=["ps%d" % bk])
    cp("dve", shW_bf[:, 0:512], ps[0][:, :], r=["ps0"], w=["shW"])
    cp("dve", shW_bf[:, 512:1024], ps[1][:, :], r=["ps1"], w=["shW"])

    def qk_chain(pkey, psrc, gain_rep, cos_ap, sin_ap, sidx, evac_fn, tbank):
        i = sidx
        p3 = psrc.rearrange("p (g d) -> p g d", d=64)
        act(ksq[i], psrc, AF.Square, r=[pkey], w=["ksq"])
        S.add("dve", lambda e, o=ssk[i], a=ksq[i][:, :].rearrange("p (g d) -> p g d", d=64): e.tensor_reduce(o, a, AX.X, ALU.add),
              r=["ksq"], w=["ssk%d" % i])
        rstd_from_ss(ssk[i], rk[i], 1.0 / 64.0, "ssk%d" % i, "rk%d" % i)
        tt("dve", kn[i], p3, rk[i].unsqueeze(2).broadcast_to([128, 8, 64]), ALU.mult,
           r=[pkey, "rk%d" % i], w=["kn%d" % i])
        tt("dve", kn[i], kn[i], gain_rep.unsqueeze(1).broadcast_to([128, 8, 64]), ALU.mult,
           r=["kn%d" % i, "gains"], w=["kn%d" % i])
        k3 = kb16[i][:, :].rearrange("p (g d) -> p g d", d=64)
        cp("pool", k3, kn[i], r=["kn%d" % i], w=["kb%d" % i])
        a = kn[i][:, :, 0:8]
        b = kn[i][:, :, 8:16]
        cb = cos_ap.unsqueeze(1).broadcast_to([128, 8, 8])
        sb_ = sin_ap.unsqueeze(1).broadcast_to([128, 8, 8])
        t1, t2, t3, t4 = rt[i]
        kk = "rt%d" % i
        tt("pool", t1, a, cb, ALU.mult, r=["kn%d" % i, "ropetab"], w=[kk + "a"])
        tt("pool", t2, b, sb_, ALU.mult, r=["kn%d" % i, "ropetab"], w=[kk + "b"])
        tt("pool", k3[:, :, 0:8], t1, t2, ALU.subtract, r=[kk + "a", kk + "b"], w=["kb%d" % i])
        tt("pool", t3, b, cb, ALU.mult, r=["kn%d" % i, "ropetab"], w=[kk + "c"])
        tt("pool", t4, a, sb_, ALU.mult, r=["kn%d" % i, "ropetab"], w=[kk + "d"])
        tt("pool", k3[:, :, 8:16], t3, t4, ALU.add, r=[kk + "c", kk + "d"], w=["kb%d" % i])
        pT = psb(tbank)
        tps([(pT[:, h * 128:(h + 1) * 128], kb16[i][:, h * 128:(h + 1) * 128]) for h in range(4)],
            ident_b, r=["kb%d" % i, "ident_b"], w=["ps%d" % tbank])
        evac_fn(pT[:, 0:512])

    S.add("pool", lambda e: e.memset(small[:, 60:61], 0.0), r=rb + ro, w=["ropetab"])
    S.add("pool", lambda e: e.memset(small[:, 61:62], 0.0), r=["gq_rep", "gk_rep"], w=["gains"])

    def norm_transpose(xsrc, xk, ssv, rsv, sskey, hdst, hkeys, gsc, sh, gkeys, xout=None, xoutk=None, junk=None, junkk="ps7"):
        act(junkb, xsrc, AF.Square, r=[xk], w=["junk_sb", sskey], accum=ssv)
        rstd_from_ss(ssv, rsv, 1.0 / D, sskey, sskey + "r")
        if xout is None:
            xout, xoutk = xsrc, xk
            tsc("dve", xout, xsrc, rsv, None, ALU.mult, None, r=[xk, sskey + "r"], w=[xk])
        else:
            tsc("dve", xout, xsrc, rsv, None, ALU.mult, None, r=[xk, sskey + "r"], w=[xoutk])
        for half in range(2):
            tps([(ps[half][:, kk * 128:(kk + 1) * 128], xout[:, (half * 4 + kk) * 128:(half * 4 + kk + 1) * 128])
                 for kk in range(4)], ident_f, r=[xoutk, "ident_f"], w=["ps%d" % half])
            for kk in range(4):
                k = half * 4 + kk
                src = ps[half][:, kk * 128:(kk + 1) * 128]
                if True:
                    act(hdst[:, k, :], src, AF.Identity, r=["ps%d" % half] + gkeys, w=[hkeys[k]],
                        bias=sh[:, k:k + 1], scale=gsc[:, k:k + 1])
                else:
                    tsc("dve", hdst[:, k, :], src, gsc[:, k:k + 1], sh[:, k:k + 1], ALU.mult, ALU.add,
                        r=["ps%d" % half] + gkeys, w=[hkeys[k]])

    NT_A = NBLK_B
    if stop_after == "pro":
        NT_A = 0
    if stop_after == "A4":
        NT_A = 4
    if stop_after == "C1":
        NT_A = 8
    if mini:
        NT_A = 16 * mini_ns
    import os
    PST = (0, 1)
    PSK = (2, 3, 4)
    PSV = (5, 6)
    PST2 = 7

    def a_d0(b):
        xi = b % NXT
        dma(xt[xi], xb[b * 128:(b + 1) * 128, :], r=[], w=["xt%d" % xi], tag="xt%d" % xi)

    def a_a12(b):
        xi, i2 = b % NXT, b % 2
        act(junkb, xt[xi], AF.Square, r=["xt%d" % xi], w=["junk_sb", "ssA%d" % i2], accum=ssA[i2])
        rstd_from_ss(ssA[i2], rsA[i2], 1.0 / D, "ssA%d" % i2, "rsA%d" % i2)

    def a_v1(b):
        xi, i2 = b % NXT, b % 2
        act(xnb[i2], xt[xi], AF.Identity, r=["xt%d" % xi, "rsA%d" % i2], w=["xnb%d" % i2], scale=rsA[i2])

    def a_p1(b):
        i2 = b % 2
        pT = psb(PST[i2])
        tps([(pT[:, k * 128:(k + 1) * 128], xnb[i2][:, k * 128:(k + 1) * 128]) for k in range(KC)],
            ident_b, r=["xnb%d" % i2, "ident_b"], w=["ps%d" % PST[i2]])

    def a_ev(b):
        i2 = b % 2
        pT = psb(PST[i2])
        for half in range(2):
            cp("dve", hT[i2][:, half * 4:(half + 1) * 4, :],
               pT[:, half * 512:(half + 1) * 512].rearrange("p (k q) -> p k q", q=128),
               r=["ps%d" % PST[i2]], w=["hT%d_%d" % (i2, half)])

    def a_p2(b):
        i2 = b % 2
        pK = PSK[b % 3]
        pV = PSV[i2]
        hks = ["hT%d_0" % i2, "hT%d_1" % i2]
        mms([(ps[pK][:, :], hT[i2][:, k, :], wkv[:, k, 0:512], k == 0, False) for k in range(KC)] +
            [(ps[pK][:, :], c128, shW_bf[:, 0:512], False, True)],
            r=hks + ["wkv", "shW", "c128"], w=["ps%d" % pK])
        mms([(ps[pV][:, :], hT[i2][:, k, :], wkv[:, k, 512:1024], k == 0, False) for k in range(KC)] +
            [(ps[pV][:, :], c128, shW_bf[:, 512:1024], False, True)],
            r=hks + ["wkv", "shW", "c128"], w=["ps%d" % pV])

    def a_a45(b):
        i2 = b % 2
        pK = PSK[b % 3]
        pV = PSV[i2]
        act(ksq[i2], ps[pK][:, :], AF.Square, r=["ps%d" % pK], w=["ksq%d" % i2])
        act(VA[:, b, :, 0:128], ps[pV][:, :].rearrange("p (h e) -> p h e", e=128), AF.Copy,
            r=["ps%d" % pV, "VAones"], w=[("V", b)])

    def a_v3(b):
        i2 = b % 2
        S.add("dve", lambda e, o=ssk[i2], a=ksq[i2][:, :].rearrange("p (g d) -> p g d", d=64): e.tensor_reduce(o, a, AX.X, ALU.add),
              r=["ksq%d" % i2], w=["ssk%d" % i2])

    def a_a6(b):
        i2 = b % 2
        rstd_from_ss(ssk[i2], rk[i2], 1.0 / 64.0, "ssk%d" % i2, "rk%d" % i2)

    def a_v4(b):
        i2 = b % 2
        pK = PSK[b % 3]
        p3 = ps[pK][:, :].rearrange("p (g d) -> p g d", d=64)
        tt("dve", kn[i2], p3, rk[i2].unsqueeze(2).broadcast_to([128, 8, 64]), ALU.mult,
           r=["ps%d" % pK, "rk%d" % i2], w=["kn%d" % i2])
        tt("dve", kn[i2], kn[i2], gk_rep.unsqueeze(1).broadcast_to([128, 8, 64]), ALU.mult,
           r=["kn%d" % i2, "gains"], w=["kn%d" % i2])

    def a_g1(b):
        i = b % 2
        k3 = kb16[i][:, :].rearrange("p (g d) -> p g d", d=64)
        cp("pool", k3, kn[i], r=["kn%d" % i], w=["kb%d" % i])
        a = kn[i][:, :, 0:8]
        b_ = kn[i][:, :, 8:16]
        cb = cos_b[:, b, :].unsqueeze(1).broadcast_to([128, 8, 8])
        sb_ = sin_b[:, b, :].unsqueeze(1).broadcast_to([128, 8, 8])
        t1, t2, t3, t4 = rt[i]
        kk = "rt%d" % i
        tt("pool", t1, a, cb, ALU.mult, r=["kn%d" % i, "ropetab"], w=[kk + "a"])
        tt("pool", t2, b_, sb_, ALU.mult, r=["kn%d" % i, "ropetab"], w=[kk + "b"])
        tt("pool", k3[:, :, 0:8], t1, t2, ALU.subtract, r=[kk + "a", kk + "b"], w=["kb%d" % i])
        tt("pool", t3, b_, cb, ALU.mult, r=["kn%d" % i, "ropetab"], w=[kk + "c"])
        tt("pool", t4, a, sb_, ALU.mult, r=["kn%d" % i, "ropetab"], w=[kk + "d"])
        tt("pool", k3[:, :, 8:16], t3, t4, ALU.add, r=[kk + "c", kk + "d"], w=["kb%d" % i])

    def a_p3(b):
        i = b % 2
        pT = psb(PST2)
        tps([(pT[:, h * 128:(h + 1) * 128], kb16[i][:, h * 128:(h + 1) * 128]) for h in range(4)],
            ident_b, r=["kb%d" % i, "ident_b"], w=["ps%d" % PST2])

    def a_v5(b):
        pT = psb(PST2)
        cp("dve", KT[:, :, b * 128:(b + 1) * 128], pT[:, 0:512].rearrange("p (h q) -> p h q", q=128),
           r=["ps%d" % PST2], w=[("KT", b)])

    a_order = [(a_v5, 11), (a_p3, 10), (a_g1, 9), (a_a6, 8), (a_a45, 7), (a_v4, 8), (a_p2, 6), (a_ev, 5),
               (a_p1, 4), (a_v1, 3), (a_a12, 2), (a_v3, 7), (a_d0, 0)]
    for t in range(NT_A + 12):
        for fn_, off_ in a_order:
            b = t - off_
            if 0 <= b < NT_A:
                fn_(b)


    if stop_after in ("pro", "A", "A4"):
        if os.environ.get("MK_CUT"):
            cut = int(os.environ["MK_CUT"])
            S.ops = S.ops[:cut]
            S.last_w = {k: v for k, v in S.last_w.items() if v < cut}
            S.readers = {k: {e: i for e, i in d_.items() if i < cut} for k, d_ in S.readers.items()}
            S.tag_count = {}
            for op in S.ops:
                if op["tag"] is not None:
                    S.tag_count[op["tag"]] = S.tag_count.get(op["tag"], 0) + 1
            stop_after = "pro"
        dbg["KT"] = nc.dram_tensor("dbg_KT", [128, 4 * SEQ], BF16, kind="ExternalOutput").ap()
        dbg["VA"] = nc.dram_tensor("dbg_VA", [128, NBLK_B * 4 * 130], BF16, kind="ExternalOutput").ap()
        dbg["misc"] = nc.dram_tensor("dbg_misc", [128, 64], F32, kind="ExternalOutput").ap()
        allk = [("KT", b) for b in range(NT_A)] + [("V", b) for b in range(NT_A)]
        if stop_after in ("A", "A4"):
            S.add("dve", lambda e: e.memset(KT[:, :, NT_A * 128:], 0.0), r=[], w=["ktpad"])
            S.add("dve", lambda e: e.memset(VA[:, NT_A:, :, 0:128], 0.0), r=[], w=["vapad"])
            allk = allk + ["ktpad", "vapad"]
            dma(dbg["KT"], KT[:, :, :].rearrange("p a b -> p (a b)"), r=allk, w=[], tag="out0")
            dma(dbg["VA"], VA[:, :, :, :].rearrange("p a b c -> p (a b c)"), r=allk + ["VAones"], w=[], tag="out1")
        cp("dve", small[:, 0:8], sh_a, r=["adacols", "junk64", "neglam"], w=["small"])
        cp("dve", small[:, 8:16], gsc_a, r=["gsc_a"], w=["small"])
        cp("dve", small[:, 16:24], cos_b[:, 0, :], r=rb, w=["small"])
        cp("dve", small[:, 24:32], sin_b[:, 63, :], r=rb, w=["small"])
        cp("dve", small[:, 32:40], lam_t, r=["neglam"], w=["small"])
        dma(dbg["misc"][:, 0:40], small[:, 0:40], r=["small"], w=[], tag="out2")
        S.emit()
        return nc

    bar_tile = small[:, 62:63]
    S.barrier(bar_tile)
    A.reset(WORK)
    xq = [A.alloc([128, D]) for _ in range(2)]
    hTq = [A.alloc([128, KC, 128], BF16) for _ in range(2)]
    wq = A.alloc([128, KC, 512], BF16)
    Qbd = [A.alloc([128, 4, 2, 128], BF16) for _ in range(2)]
    NPT = 4
    PT = [A.alloc([128, 512], BF16) for _ in range(NPT)]
    ssQ = [A.alloc([128, 1]) for _ in range(2)]
    rsQ = [A.alloc([128, 1]) for _ in range(2)]
    ksq = [A.alloc([128, 512])] * 2
    kn = [A.alloc([128, 8, 64]) for _ in range(2)]
    kb16 = [A.alloc([128, 512], BF16) for _ in range(2)]
    ssk = [A.alloc([128, 8]) for _ in range(2)]
    rk = [A.alloc([128, 8]) for _ in range(2)]
    rt = [[A.alloc([128, 8, 8]) for _ in range(4)] for _ in range(2)]
    ot = [A.alloc([128, 128]) for _ in range(2)]
    ot2 = [A.alloc([128, 128]) for _ in range(2)]
    ob16 = [A.alloc([128, 128], BF16) for _ in range(2)]
    eps_ = [A.alloc([128, 8]) for _ in range(2)]
    pass

    for bq in range(2):
        S.add("pool", lambda e, q=Qbd[bq]: e.memset(q, 0.0), r=[], w=["Qbd%d" % bq])
    for pc in range(4):
        xi = pc % 2
        st = xq[xi][:, :].rearrange("p (k n) -> p k n", k=KC)
        dma(st, w_in_v[:, :, pc * 128:(pc + 1) * 128], r=[], w=["xq%d" % xi], tag="xq%d" % xi)
        cp("pool", wq[:, :, pc * 128:(pc + 1) * 128], st, r=["xq%d" % xi], w=["wq"])

    def q_stage(m, stage):
        bq = m % 2
        xk = "xq%d" % bq
        hks = ["hTq%d_%d" % (bq, k) for k in range(KC)]
        if stage == 0:
            dma(xq[bq], xo[m * 128:(m + 1) * 128, :], r=[], w=[xk], tag=xk)
            act(junkb, xq[bq], AF.Square, r=[xk], w=["junk_sb", "ssQ%d" % bq], accum=ssQ[bq])
            rstd_from_ss(ssQ[bq], rsQ[bq], 1.0 / D, "ssQ%d" % bq, "ssQ%dr" % bq)
            tsc("dve", xq[bq], xq[bq], rsQ[bq], None, ALU.mult, None, r=[xk, "ssQ%dr" % bq], w=[xk])
        elif stage == 1:
            for half in range(2):
                tps([(ps[half][:, kk * 128:(kk + 1) * 128], xq[bq][:, (half * 4 + kk) * 128:(half * 4 + kk + 1) * 128])
                     for kk in range(4)], ident_f, r=[xk, "ident_f"], w=["ps%d" % half])
            for half in range(2):
                for kk in range(4):
                    k = half * 4 + kk
                    src_ = ps[half][:, kk * 128:(kk + 1) * 128]
                    act(hTq[bq][:, k, :], src_, AF.Identity, r=["ps%d" % half, "gsc_a", "adacols"], w=[hks[k]],
                        bias=sh_a[:, k:k + 1], scale=gsc_a[:, k:k + 1])
        elif stage == 2:
            mms([(ps[0][:, :], hTq[bq][:, k, :], wq[:, k, :], k == 0, k == KC - 1) for k in range(KC)],
                r=hks + ["wq"], w=["ps0"])
            qk_front("ps0", ps[0][:, :], gq_rep, cos_o[:, m, :], sin_o[:, m, :], bq)
        else:
            pT = psb(1)
            tps([(pT[:, h * 128:(h + 1) * 128], kb16[bq][:, h * 128:(h + 1) * 128]) for h in range(4)],
                ident_b, r=["kb%d" % bq, "ident_b"], w=["ps1"])
            p3 = pT[:, 0:512].rearrange("p (h q) -> p h q", q=128)
            cp("dve", Qbd[bq][0:64, :, 0, :], p3[0:64], r=["ps1"], w=["Qbd%d" % bq])
            cp("dve", Qbd[bq][64:128, :, 1, :], p3[64:128], r=["ps1"], w=["Qbd%d" % bq])

    def qk_front(pkey, psrc, gain_rep, cos_ap, sin_ap, i):
        p3 = psrc.rearrange("p (g d) -> p g d", d=64)
        act(ksq[i], psrc, AF.Square, r=[pkey], w=["ksq"])
        S.add("dve", lambda e, o=ssk[i], a=ksq[i][:, :].rearrange("p (g d) -> p g d", d=64): e.tensor_reduce(o, a, AX.X, ALU.add),
              r=["ksq"], w=["ssk%d" % i])
        rstd_from_ss(ssk[i], rk[i], 1.0 / 64.0, "ssk%d" % i, "rk%d" % i)
        tt("dve", kn[i], p3, rk[i].unsqueeze(2).broadcast_to([128, 8, 64]), ALU.mult,
           r=[pkey, "rk%d" % i], w=["kn%d" % i])
        tt("dve", kn[i], kn[i], gain_rep.unsqueeze(1).broadcast_to([128, 8, 64]), ALU.mult,
           r=["kn%d" % i, "gains"], w=["kn%d" % i])
        k3 = kb16[i][:, :].rearrange("p (g d) -> p g d", d=64)
        cp("pool", k3, kn[i], r=["kn%d" % i], w=["kb%d" % i])
        a = kn[i][:, :, 0:8]
        b = kn[i][:, :, 8:16]
        cb = cos_ap.unsqueeze(1).broadcast_to([128, 8, 8])
        sb_ = sin_ap.unsqueeze(1).broadcast_to([128, 8, 8])
        t1, t2, t3, t4 = rt[i]
        kk = "rt%d" % i
        tt("pool", t1, a, cb, ALU.mult, r=["kn%d" % i, "ropetab"], w=[kk + "a"])
        tt("pool", t2, b, sb_, ALU.mult, r=["kn%d" % i, "ropetab"], w=[kk + "b"])
        tt("pool", k3[:, :, 0:8], t1, t2, ALU.subtract, r=[kk + "a", kk + "b"], w=["kb%d" % i])
        tt("pool", t3, b, cb, ALU.mult, r=["kn%d" % i, "ropetab"], w=[kk + "c"])
        tt("pool", t4, a, sb_, ALU.mult, r=["kn%d" % i, "ropetab"], w=[kk + "d"])
        tt("pool", k3[:, :, 8:16], t3, t4, ALU.add, r=[kk + "c", kk + "d"], w=["kb%d" % i])

    cm3 = cmask[:, :].rearrange("p (t x) -> p t x", t=4)
    NM = NOWN if stop_after != "C1" else 2
    if mini:
        NM = 4 * mini_ns
    items = []
    for m in range(NM):
        for h in range(4):
            ngrp = (4 * m + 4) // 2
            for g in range(ngrp):
                items.append((m, h, g, ngrp))
    SBK = (2, 3, 4)
    pending = []
    LOOK = 2

    def emit_S(idx):
        m, h, g, ngrp = items[idx]
        bq = m % 2
        sbk = SBK[idx % 3]
        kbs = (2 * g, 2 * g + 1)
        mms([(ps[sbk][:, i * 256:(i + 1) * 256], KT[:, h, kb * 128:(kb + 1) * 128],
              Qbd[bq][:, h, :, :], True, True) for i, kb in enumerate(kbs)],
            r=[("KT", kbs[0]), ("KT", kbs[1]), "Qbd%d" % bq], w=["ps%d" % sbk])

    def emit_rest(idx):
        m, h, g, ngrp = items[idx]
        sbk = SBK[idx % 3]
        pti = idx % NPT
        ep = (m * 4 + h) % 2
        kbs = (2 * g, 2 * g + 1)
        act(PT[pti], ps[sbk][:, :], AF.Exp, r=["ps%d" % sbk, "m8c"], w=["PT%d" % pti],
            bias=m8c[:, 0:1], scale=0.125)
        if g >= ngrp - 2:
            gb = g - (ngrp - 2)
            tt("dve", PT[pti], PT[pti], cm3[:, 2 * gb:2 * gb + 2, :], ALU.mult,
               r=["PT%d" % pti, "cmask"], w=["PT%d" % pti])
        its = []
        for i, kb in enumerate(kbs):
            for c in range(2):
                its.append((ps[5 + c][:, 0:129], PT[pti][:, i * 256 + c * 128:i * 256 + (c + 1) * 128],
                            VA[:, kb, h, 0:129], (g == 0 and i == 0), (g == ngrp - 1 and i == 1)))
        mms(its, r=["PT%d" % pti, ("V", kbs[0]), ("V", kbs[1])], w=["ps5", "ps6"])
        if g == ngrp - 1:
            epilogue_a(m, h, ep)
            pending.append((idx + 2, lambda m=m, h=h, ep=ep: epilogue_a2(m, h, ep)))
            pending.append((idx + 4, lambda m=m, h=h, ep=ep: epilogue_b(m, h, ep)))

    def epilogue_a(m, h, ep):
        e_ = eps_[ep]
        ek = "eps%d" % ep
        act(e_[:, 5:6], ps[5][:, 128:129], AF.Copy, r=["ps5"], w=[ek])
        act(e_[:, 6:7], ps[6][:, 128:129], AF.Copy, r=["ps6"], w=[ek])
        act(ot[ep], ps[5][:, 0:128], AF.Copy, r=["ps5"], w=["ot%d" % ep])
        act(ot2[ep], ps[6][:, 0:128], AF.Copy, r=["ps6"], w=["ot2%d" % ep])
        S.add("dve", lambda e, e_=e_: e.reciprocal(e_[:, 0:2], e_[:, 5:7]), r=[ek], w=[ek])
        tt("dve", e_[:, 2:3], e_[:, 1:2], neglam, ALU.mult, r=[ek, "neglam"], w=[ek])
        tsc("dve", ot[ep], ot[ep], e_[:, 0:1], None, ALU.mult, None, r=["ot%d" % ep, ek], w=["ot%d" % ep])
        tsc("dve", ot2[ep], ot2[ep], e_[:, 2:3], None, ALU.mult, None, r=["ot2%d" % ep, ek], w=["ot2%d" % ep])
        tt("dve", ot[ep], ot[ep], ot2[ep], ALU.add, r=["ot%d" % ep, "ot2%d" % ep], w=["ot%d" % ep])

    def epilogue_a2(m, h, ep):
        e_ = eps_[ep]
        ek = "eps%d" % ep
        act(ot2[ep], ot[ep], AF.Square, r=["ot%d" % ep, "ot2%d" % ep], w=["ot2%d" % ep, ek + "s"], accum=e_[:, 3:4])
        rstd_from_ss(e_[:, 3:4], e_[:, 4:5], 1.0 / 128.0, ek + "s", ek + "r")
        act(ot2[ep], ot[ep], AF.Identity, r=["ot%d" % ep, ek + "r", "ot2%d" % ep], w=["ot2%d" % ep], scale=e_[:, 4:5])
        tt("dve", ob16[ep], ot2[ep], sg_rep, ALU.mult, r=["ot2%d" % ep, "sg_rep"], w=["ob%d" % ep])

    def epilogue_b(m, h, ep):
        tps([(psb(7)[:, ep * 128:(ep + 1) * 128], ob16[ep])], ident_b, r=["ob%d" % ep, "ident_b"], w=["ps7"])
        cp("dve", attnT[:, h, m * 128:(m + 1) * 128], psb(7)[:, ep * 128:(ep + 1) * 128], r=["ps7"], w=[("attnT", m, h)])

    for st_ in range(4):
        q_stage(0, st_)
    for i in range(min(LOOK, len(items))):
        emit_S(i)
    for idx in range(len(items)):
        m, h, g, ngrp = items[idx]
        if g == 0 and m + 1 < NM:
            q_stage(m + 1, h)
        if idx + LOOK < len(items):
            emit_S(idx + LOOK)
        while pending and pending[0][0] <= idx:
            pending.pop(0)[1]()
        emit_rest(idx)
    while pending:
        pending.pop(0)[1]()

    if stop_after in ("C", "C1"):
        dbg["attnT"] = nc.dram_tensor("dbg_attnT", [128, 4 * NOWN * 128], BF16, kind="ExternalOutput").ap()
        if NM < NOWN:
            S.add("dve", lambda e: e.memset(attnT[:, :, NM * 128:], 0.0), r=[], w=["attnpad"])
        allk = [("attnT", m, h) for m in range(NM) for h in range(4)] + ["attnpad"]
        dma(dbg["attnT"], attnT[:, :, :].rearrange("p a b -> p (a b)"), r=allk, w=[], tag="out0")
        S.emit()
        return nc

    S.barrier(bar_tile)
    A.reset(KV0)
    X = A.alloc([128, NOWN, D])
    wglu = A.alloc([128, KC, 1024], BF16)
    woutp = A.alloc([128, KC, 1024], BF16)
    D1W = A.mark()
    crep = A.alloc([128, KC, 128])
    abufs = ada_alloc()
    gta_rep = A.alloc([128, D])
    stg = [A.alloc([128, KC, 128]) for _ in range(2)]
    for s4 in range(4):
        S.bulk_tags.add("Xload%d" % s4)
        for i in range(4):
            m = 4 * s4 + i
            dma(X[:, m, :], xo[m * 128:(m + 1) * 128, :], r=[], w=[("X", m)], tag="Xload%d" % s4)
    cp("dve", crep, cact.unsqueeze(2).broadcast_to([128, KC, 128]), r=["cact"], w=["crep"])
    for g in (4, 5):
        ada_group(g, abufs[g % 2], 7 - 2 * (g % 2))
        cp("dve", gta_rep[:, (g - 4) * 512:(g - 3) * 512], abufs[g % 2][2], r=["ada_modrep%d" % (g % 2)], w=["gta_rep"])
    for pc in range(8):
        xi = pc % 2
        dma(stg[xi], w_in_v[:, :, 1536 + pc * 128:1536 + (pc + 1) * 128], r=[], w=["stg%d" % xi], tag="stg%d" % xi)
        if pc % 2 == 0:
            act(wglu[:, :, pc * 128:(pc + 1) * 128], stg[xi], AF.Copy, r=["stg%d" % xi], w=["wglu"])
        else:
            cp("pool", wglu[:, :, pc * 128:(pc + 1) * 128], stg[xi], r=["stg%d" % xi], w=["wglu"])
    w_out_v = w_out.rearrange("(k p) n -> p k n", p=128)
    for pc in range(8):
        xi = pc % 2
        dma(stg[xi], w_out_v[:, :, pc * 128:(pc + 1) * 128], r=[], w=["stg%d" % xi], tag="stg%d" % xi)
        tt("dve" if pc % 2 == 0 else "pool", woutp[:, :, pc * 128:(pc + 1) * 128], stg[xi],
           gta_rep[:, pc * 128:(pc + 1) * 128].unsqueeze(1).broadcast_to([128, KC, 128]), ALU.mult,
           r=["stg%d" % xi, "gta_rep"], w=["woutp"])
    S.barrier(bar_tile)
    A.reset(D1W)
    xhb = A.alloc([128, D])
    xn = [A.alloc([128, D]) for _ in range(2)]
    hT5 = A.alloc([128, KC, 640], BF16)
    uT = A.alloc([128, 4, 4, 160], BF16)
    sig = [A.alloc([128, 512]) for _ in range(2)]
    sigh = [A.alloc([128, 128]) for _ in range(2)]
    asb = [A.alloc([128, 512]) for _ in range(2)]
    asbh = [A.alloc([128, 128]) for _ in range(2)]
    yv = A.alloc([128, 4, 512])
    ysq = A.alloc([128, 4, 512])
    mean_sb = A.alloc([128, 512])
    m2 = A.alloc([128, 512])
    rstd_bc = A.alloc([128, 512])
    tmpv = [A.alloc([128, 512]) for _ in range(2)]
    convT = A.alloc([128, 4, 512], BF16)
    dg = [A.alloc([128, 128], BF16) for _ in range(8)]
    ssD = [A.alloc([128, 1]) for _ in range(2)]
    rsD = [A.alloc([128, 1]) for _ in range(2)]
    pass
    dgc = {"n": 0}

    NS = 4 if not mini else mini_ns
    hT5b = [hT5, A.alloc([128, KC, 640], BF16)]

    def d1_front(s4, i):
        bi = i % 2
        if i == 4:
            dma(xhb, xh[s4 * 128:(s4 + 1) * 128, :], r=[], w=["xhb"], tag="xhb")
        src, sk = (X[:, 4 * s4 + i, :], ("X", 4 * s4 + i)) if i < 4 else (xhb, "xhb")
        act(junkb, src, AF.Square, r=[sk], w=["junk_sb", "ssD%d" % bi], accum=ssD[bi])
        rstd_from_ss(ssD[bi], rsD[bi], 1.0 / D, "ssD%d" % bi, "ssD%dr" % bi)
        tsc("dve", xn[bi], src, rsD[bi], None, ALU.mult, None, r=[sk, "ssD%dr" % bi], w=["xn%d" % bi])

    def d1_back(s4, i):
        bi = i % 2
        hd = hT5b[s4 % 2][:, :, i * 128:(i + 1) * 128]
        for half in range(2):
            tps([(ps[half][:, kk * 128:(kk + 1) * 128], xn[bi][:, (half * 4 + kk) * 128:(half * 4 + kk + 1) * 128])
                 for kk in range(4)], ident_f, r=["xn%d" % bi, "ident_f"], w=["ps%d" % half])
            for kk in range(4):
                k = half * 4 + kk
                act(hd[:, k, :], ps[half][:, kk * 128:(kk + 1) * 128], AF.Identity,
                    r=["ps%d" % half, "gsc_a", "adacols"], w=["hT5_%d_%d_%d" % (s4 % 2, i, k)],
                    bias=sh_a[:, k:k + 1], scale=gsc_a[:, k:k + 1])

    for i in range(5):
        d1_front(0, i)
        d1_back(0, i)
    def d1_glu(s4):
        hT5 = hT5b[s4 % 2]
        nxt = s4 + 1 < NS
        allh = ["hT5_%d_%d_%d" % (s4 % 2, i, k) for i in range(5) for k in range(KC)]
        for cc in range(4):
            pb = cc % 2
            ba_, bg_ = bgluT[:, cc:cc + 1], bgluT[:, 4 + cc:5 + cc]
            pa, pg, ph = 2 + 3 * pb, 3 + 3 * pb, 4 + 3 * pb
            mms([(ps[pa][:, :], wglu[:, k, cc * 128:(cc + 1) * 128], hT5[:, k, 0:512], k == 0, k == KC - 1)
                 for k in range(KC)], r=allh + ["wglu"], w=["ps%d" % pa])
            mms([(ps[pg][:, :], wglu[:, k, 512 + cc * 128:512 + (cc + 1) * 128], hT5[:, k, 0:512], k == 0, k == KC - 1)
                 for k in range(KC)], r=allh + ["wglu"], w=["ps%d" % pg])
            mms([(ps[ph][:, 0:128], wglu[:, k, cc * 128:(cc + 1) * 128], hT5[:, k, 512:640], k == 0, k == KC - 1)
                 for k in range(KC)] +
                [(ps[ph][:, 128:256], wglu[:, k, 512 + cc * 128:512 + (cc + 1) * 128], hT5[:, k, 512:640], k == 0, k == KC - 1)
                 for k in range(KC)], r=allh + ["wglu"], w=["ps%d" % ph])
            act(sig[pb], ps[pg][:, :], AF.Sigmoid, r=["ps%d" % pg, "bgluT"], w=["sig%d" % pb], bias=bg_)
            act(sigh[pb], ps[ph][:, 128:256], AF.Sigmoid, r=["ps%d" % ph, "bgluT"], w=["sigh%d" % pb], bias=bg_)
            act(asb[pb], ps[pa][:, :], AF.Identity, r=["ps%d" % pa, "bgluT"], w=["asb%d" % pb], bias=ba_)
            act(asbh[pb], ps[ph][:, 0:128], AF.Identity, r=["ps%d" % ph, "bgluT"], w=["asbh%d" % pb], bias=ba_)
            tt("dve", uT[:, cc, :, 32:160], asb[pb][:, :].rearrange("p (i t) -> p i t", i=4),
               sig[pb][:, :].rearrange("p (i t) -> p i t", i=4), ALU.mult,
               r=["asb%d" % pb, "sig%d" % pb], w=["uT%d" % cc])
            tt("dve", uT[:, cc, :, 0:32], asbh[pb][:, :].rearrange("p (i t) -> p i t", i=4),
               sigh[pb][:, :].rearrange("p (i t) -> p i t", i=4), ALU.mult,
               r=["asbh%d" % pb, "sigh%d" % pb], w=["uT%d" % cc])
            if s4 == 0:
                tsc("dve", uT[:, cc, 0, 0:32], uT[:, cc, 0, 0:32], hmask[:, 0:1], None, ALU.mult, None,
                    r=["uT%d" % cc, "hmask"], w=["uT%d" % cc])

    def d1_conv(s4):
        hT5 = hT5b[s4 % 2]
        nxt = s4 + 1 < NS
        allh = ["hT5_%d_%d_%d" % (s4 % 2, i, k) for i in range(5) for k in range(KC)]
        if nxt:
            d1_front(s4 + 1, 0)
        for cc in range(4):
            cb_ = 2 + 3 * (cc % 2)
            for k in range(31):
                sl = dgc["n"] % 8
                dgc["n"] += 1
                tsc("dve", dg[sl], ident_b, wdwT[:, cc, k:k + 1], None, ALU.mult, None,
                    r=["ident_b", "wdwT"], w=["dg%d" % sl])
                mms([(ps[cb_][:, :], dg[sl], uT[:, cc, :, 2 + k:130 + k], k == 0, k == 30)],
                    r=["dg%d" % sl, "uT%d" % cc], w=["ps%d" % cb_])
            act(yv[:, cc, :], ps[cb_][:, :], AF.Identity, r=["ps%d" % cb_, "bdwT"], w=["yv%d" % cc], bias=bdwT[:, cc:cc + 1])
            act(ysq[:, cc, :], yv[:, cc, :], AF.Square, r=["yv%d" % cc], w=["ysq%d" % cc])
            if nxt:
                d1_back(s4 + 1, cc)
                d1_front(s4 + 1, cc + 1)

    def d1_ln(s4):
        hT5 = hT5b[s4 % 2]
        nxt = s4 + 1 < NS
        allh = ["hT5_%d_%d_%d" % (s4 % 2, i, k) for i in range(5) for k in range(KC)]
        mms([(ps[3][:, :], ones_ln, yv[:, cc, :], cc == 0, cc == 3) for cc in range(4)],
            r=["yv%d" % cc for cc in range(4)] + ["ones_ln"], w=["ps3"])
        mms([(ps[4][:, :], ones_ln, ysq[:, cc, :], cc == 0, cc == 3) for cc in range(4)],
            r=["ysq%d" % cc for cc in range(4)] + ["ones_ln"], w=["ps4"])
        if nxt:
            d1_back(s4 + 1, 4)
        act(mean_sb, ps[3][:, :], AF.Copy, r=["ps3"], w=["mean_sb"])
        tt("dve", m2, mean_sb, mean_sb, ALU.mult, r=["mean_sb"], w=["m2"])
        tt("dve", m2, ps[4][:, :], m2, ALU.subtract, r=["ps4", "m2"], w=["m2"])
        rstd_from_ss(m2, rstd_bc, 1.0, "m2", "rstd_bc")
        for cc in range(4):
            tb = cc % 2
            tt("dve", tmpv[tb], yv[:, cc, :], mean_sb, ALU.subtract, r=["yv%d" % cc, "mean_sb"], w=["tmpv%d" % tb])
            tt("dve", tmpv[tb], tmpv[tb], rstd_bc, ALU.mult, r=["tmpv%d" % tb, "rstd_bc"], w=["tmpv%d" % tb])
            act(convT[:, cc, :], tmpv[tb], AF.Silu, r=["tmpv%d" % tb, "lngT", "lnbT"], w=["convT%d" % cc],
                bias=lnbT[:, cc:cc + 1], scale=lngT[:, cc:cc + 1])

    def d1_out(s4):
        hT5 = hT5b[s4 % 2]
        nxt = s4 + 1 < NS
        allh = ["hT5_%d_%d_%d" % (s4 % 2, i, k) for i in range(5) for k in range(KC)]
        for i in range(4):
            m = 4 * s4 + i
            for half in range(2):
                ob_ = 5 + (i * 2 + half) % 3
                items = []
                for k in range(8):
                    lh = attnT[:, k, m * 128:(m + 1) * 128] if k < 4 else convT[:, k - 4, i * 128:(i + 1) * 128]
                    items.append((ps[ob_][:, :], lh, woutp[:, k, half * 512:(half + 1) * 512], k == 0, k == 7))
                mms(items, r=[("attnT", m, h) for h in range(4)] + ["convT%d" % c_ for c_ in range(4)] + ["woutp"],
                    w=["ps%d" % ob_])
                tt("dve", X[:, m, half * 512:(half + 1) * 512], ps[ob_][:, :], X[:, m, half * 512:(half + 1) * 512],
                   ALU.add, r=["ps%d" % ob_, ("X", m)], w=[("X", m)])


    d1_glu(0)
    for s4 in range(NS):
        d1_conv(s4)
        d1_ln(s4)
        if s4 + 1 < NS:
            d1_glu(s4 + 1)
        d1_out(s4)

    if stop_after in ("D1", "D1a"):
        dbg["X"] = nc.dram_tensor("dbg_X", [NOWN * 128, D], F32, kind="ExternalOutput").ap()
        for m in range(4 * NS):
            dma(dbg["X"][m * 128:(m + 1) * 128, :], X[:, m, :], r=[("X", m)], w=[], tag="outX")
        S.emit()
        return nc

    S.barrier(bar_tile)
    A.reset(D1W - 8192)
    h2T = A.alloc([128, KC, NOWN * 128], BF16)
    cmb = A.alloc([128, NOWN, 32])
    gtf_rep = A.alloc([128, D])
    D2W = A.mark()
    crep = A.alloc([128, KC, 128])
    abufs = ada_alloc()
    xn = [A.alloc([128, D]) for _ in range(2)]
    h2f = [A.alloc([128, KC, 128]) for _ in range(2)]
    rs_ = [A.alloc([128, 128]) for _ in range(2)]
    ssD = [A.alloc([128, 1]) for _ in range(2)]
    rsD = [A.alloc([128, 1]) for _ in range(2)]
    cp("dve", crep, cact.unsqueeze(2).broadcast_to([128, KC, 128]), r=["cact"], w=["crep"])
    for g in range(6, 10):
        ada_group(g, abufs[g % 2], 7 - 2 * (g % 2))
        ada_to_cols(abufs[g % 2], sh_f if g < 8 else sc_f, (g % 2) * 4, 6 - 2 * (g % 2))
    tsc("dve", gsc_f, sc_f, 1.0, None, ALU.add, None, r=["adacols"], w=["gsc_f"])
    tt("dve", gsc_f, gsc_f, gffnT, ALU.mult, r=["gsc_f", "gffnT"], w=["gsc_f"])
    for g in (10, 11):
        ada_group(g, abufs[g % 2], 7 - 2 * (g % 2))
        cp("dve", gtf_rep[:, (g - 10) * 512:(g - 9) * 512], abufs[g % 2][2], r=["ada_modrep%d" % (g % 2)], w=["gtf_rep"])

    NB2 = NOWN if not mini else 4
    NRS = 5
    rs_ = rs_ + [A.alloc([128, 128]) for _ in range(NRS - 2)]

    def rviews(m):
        R = rs_[m % NRS]
        v = dict(lg=R[:, 0:36], gmax=R[:, 36:37], ngmax=R[:, 37:38], gsum=R[:, 38:39], gp=R[:, 39:40],
                 ohg=R[:, 40:44], junk4=R[:, 44:48], esel=R[:, 48:80], ein=R[:, 80:88], top8=R[:, 88:96],
                 eq2=R[:, 112:120], cmb8=R[:, 120:128])
        for i_, nm in enumerate(("dcol", "ed", "den", "w1", "w2", "w1g", "w2g")):
            v[nm] = R[:, 96 + i_:97 + i_]
        return v, "rs%d" % (m % NRS)

    def d_n1(m):
        bi = m % 2
        act(junkb, X[:, m, :], AF.Square, r=[("X", m)], w=["junk_sb", "ssD%d" % bi], accum=ssD[bi])
        rstd_from_ss(ssD[bi], rsD[bi], 1.0 / D, "ssD%d" % bi, "ssD%dr" % bi)

    def d_n2(m):
        bi = m % 2
        tsc("dve", xn[bi], X[:, m, :], rsD[bi], None, ALU.mult, None, r=[("X", m), "ssD%dr" % bi], w=["xn%d" % bi])

    def d_n3(m):
        bi = m % 2
        for half in range(2):
            tps([(ps[half][:, kk * 128:(kk + 1) * 128], xn[bi][:, (half * 4 + kk) * 128:(half * 4 + kk + 1) * 128])
                 for kk in range(4)], ident_f, r=["xn%d" % bi, "ident_f"], w=["ps%d" % half])

    def d_n4(m):
        bi = m % 2
        for half in range(2):
            for kk in range(4):
                k = half * 4 + kk
                act(h2f[bi][:, k, :], ps[half][:, kk * 128:(kk + 1) * 128], AF.Identity,
                    r=["ps%d" % half, "gsc_f", "adacols"], w=["h2f%d_%d" % (bi, k)],
                    bias=sh_f[:, k:k + 1], scale=gsc_f[:, k:k + 1])

    def d_n5(m):
        bi = m % 2
        hks = ["h2f%d_%d" % (bi, k) for k in range(KC)]
        cp("pool", h2T[:, :, m * 128:(m + 1) * 128], h2f[bi], r=hks, w=[("h2T", m)])
        mms([(ps[2][:, 0:36], h2f[bi][:, k, :], wr[:, k, :], k == 0, k == KC - 1) for k in range(KC)],
            r=hks + ["wr"], w=["ps2"])

    def d_cA(m):
        v, rk_ = rviews(m)
        tt("dve", v["lg"], ps[2][:, 0:36], br_rep, ALU.add, r=["ps2", "br_rep"], w=[rk_])
        S.add("dve", lambda e, gmax=v["gmax"], lg=v["lg"]: e.tensor_reduce(gmax, lg[:, 0:4], AX.X, ALU.max), r=[rk_], w=[rk_])
        tsc("dve", v["ngmax"], v["gmax"], -1.0, None, ALU.mult, None, r=[rk_], w=[rk_])

    def d_cB(m):
        v, rk_ = rviews(m)
        act(v["junk4"], v["lg"][:, 0:4], AF.Exp, r=[rk_], w=[rk_], bias=v["ngmax"], scale=1.0, accum=v["gsum"])

    def d_cC(m):
        v, rk_ = rviews(m)
        lg, ohg, esel, ein, top8 = v["lg"], v["ohg"], v["esel"], v["ein"], v["top8"]
        S.add("dve", lambda e, gp=v["gp"], gsum=v["gsum"]: e.reciprocal(gp, gsum), r=[rk_], w=[rk_])
        tsc("dve", ohg, lg[:, 0:4], v["gmax"], None, ALU.is_ge, None, r=[rk_], w=[rk_])
        tt("dve", esel.rearrange("p (g e) -> p g e", g=4), lg[:, 4:36].rearrange("p (g e) -> p g e", g=4),
           ohg.unsqueeze(2).broadcast_to([128, 4, 8]), ALU.mult, r=[rk_], w=[rk_])
        S.add("dve", lambda e, ein=ein, esel=esel: e.tensor_reduce(ein, esel.rearrange("p (g e) -> p e g", g=4), AX.X, ALU.add),
              r=[rk_], w=[rk_])
        S.add("dve", lambda e, top8=top8, ein=ein: e.max(top8, ein), r=[rk_], w=[rk_])
        tt("dve", v["dcol"], top8[:, 1:2], top8[:, 0:1], ALU.subtract, r=[rk_], w=[rk_])

    def d_cD(m):
        v, rk_ = rviews(m)
        act(v["ed"], v["dcol"], AF.Exp, r=[rk_], w=[rk_])

    def d_cE(m):
        v, rk_ = rviews(m)
        ed, den, w1, w2, w1g, w2g, gp = v["ed"], v["den"], v["w1"], v["w2"], v["w1g"], v["w2g"], v["gp"]
        ein, top8, cmb8, eq2, ohg = v["ein"], v["top8"], v["cmb8"], v["eq2"], v["ohg"]
        tsc("dve", den, ed, 1.0, None, ALU.add, None, r=[rk_], w=[rk_])
        S.add("dve", lambda e, w1=w1, den=den: e.reciprocal(w1, den), r=[rk_], w=[rk_])
        tt("dve", w2, ed, w1, ALU.mult, r=[rk_], w=[rk_])
        tt("dve", w1g, w1, gp, ALU.mult, r=[rk_], w=[rk_])
        tt("dve", w2g, w2, gp, ALU.mult, r=[rk_], w=[rk_])
        tsc("dve", cmb8, ein, top8[:, 0:1], None, ALU.is_equal, None, r=[rk_], w=[rk_])
        tsc("dve", cmb8, cmb8, w1g, None, ALU.mult, None, r=[rk_], w=[rk_])
        tsc("dve", eq2, ein, top8[:, 1:2], None, ALU.is_equal, None, r=[rk_], w=[rk_])
        tsc("dve", eq2, eq2, w2g, None, ALU.mult, None, r=[rk_], w=[rk_])
        tt("dve", cmb8, cmb8, eq2, ALU.add, r=[rk_], w=[rk_])
        tt("dve", cmb[:, m, :].rearrange("p (g e) -> p g e", g=4), ohg.unsqueeze(2).broadcast_to([128, 4, 8]),
           cmb8.unsqueeze(1).broadcast_to([128, 4, 8]), ALU.mult, r=[rk_], w=[("cmb", m)])

    d_order = [(d_cE, 9), (d_cD, 8), (d_cC, 7), (d_cB, 6), (d_cA, 5), (d_n5, 4), (d_n4, 3), (d_n3, 2), (d_n2, 1), (d_n1, 0)]
    for t in range(NB2 + 9):
        for fn_, off_ in d_order:
            m = t - off_
            if 0 <= m < NB2:
                fn_(m)

    if stop_after == "D2":
        dbg["cmb"] = nc.dram_tensor("dbg_cmb", [128, NOWN * 32], F32, kind="ExternalOutput").ap()
        dbg["h2T"] = nc.dram_tensor("dbg_h2T", [128, KC * NOWN * 128], BF16, kind="ExternalOutput").ap()
        if mini:
            S.add("dve", lambda e: e.memset(cmb[:, NB2:, :], 0.0), r=[], w=["cmbpad"])
            S.add("dve", lambda e: e.memset(h2T[:, :, NB2 * 128:], 0.0), r=[], w=["h2pad"])
        dma(dbg["cmb"], cmb[:, :, :].rearrange("p a b -> p (a b)"), r=[("cmb", m) for m in range(NB2)] + ["cmbpad"], w=[], tag="out0")
        dma(dbg["h2T"], h2T[:, :, :].rearrange("p a b -> p (a b)"), r=[("h2T", m) for m in range(NB2)] + ["h2pad"], w=[], tag="out1")
        S.emit()
        return nc

    S.barrier(bar_tile)
    A.reset(D2W)
    Wg = [A.alloc([128, KC, 256], BF16) for _ in range(2)]
    Wu = [A.alloc([128, KC, 256], BF16) for _ in range(2)]
    Wd = [A.alloc([128, 2, D], BF16) for _ in range(2)]
    stE = [A.alloc([128, 2048]) for _ in range(4)]
    hid = [A.alloc([128, 2, 512], BF16) for _ in range(2)]
    sgl = [A.alloc([128, 512], BF16) for _ in range(4)]
    tacc = [A.alloc([128, 512]) for _ in range(4)]
    pass
    stc = {"n": 0}

    def load_expert(e_):
        eb = e_ % 2
        for which in range(3):
            sl = stc["n"] % 4
            stc["n"] += 1
            sk = "stE%d" % sl
            if which < 2:
                srcw = (w_gate if which == 0 else w_up)[e_].rearrange("(k p) f -> p k f", p=128)
                stv = stE[sl][:, :].rearrange("p (k f) -> p k f", k=KC)
                dma(stv, srcw, r=[], w=[sk], tag=sk)
                if which == 0:
                    cp("pool", Wg[eb], stv, r=[sk], w=["Wg%d" % eb])
                else:
                    act(Wu[eb], stv, AF.Copy, r=[sk], w=["Wu%d" % eb])
            else:
                srcw = w_down[e_].rearrange("(c p) n -> p c n", p=128)
                stv = stE[sl][:, :].rearrange("p (c n) -> p c n", c=2)
                dma(stv, srcw, r=[], w=[sk], tag=sk)
                tt("pool", Wd[eb], stv, gtf_rep.unsqueeze(1).broadcast_to([128, 2, D]), ALU.mult,
                   r=[sk, "gtf_rep"], w=["Wd%d" % eb])

    def gate_up(e_, s4, hb):
        eb = e_ % 2
        hk = [("h2T", 4 * s4 + i) for i in range(4)]
        for fc in range(2):
            mms([(ps[2 * fc][:, :], Wg[eb][:, k, fc * 128:(fc + 1) * 128], h2T[:, k, s4 * 512:(s4 + 1) * 512], k == 0, k == KC - 1)
                 for k in range(KC)], r=hk + ["Wg%d" % eb], w=["ps%d" % (2 * fc)])
            mms([(ps[2 * fc + 1][:, :], Wu[eb][:, k, fc * 128:(fc + 1) * 128], h2T[:, k, s4 * 512:(s4 + 1) * 512], k == 0, k == KC - 1)
                 for k in range(KC)], r=hk + ["Wu%d" % eb], w=["ps%d" % (2 * fc + 1)])
            si = hb * 2 + fc
            act(sgl[si], ps[2 * fc][:, :], AF.Silu, r=["ps%d" % (2 * fc)], w=["sgl%d" % si])
            tt("dve", hid[hb][:, fc, :], ps[2 * fc + 1][:, :], sgl[si], ALU.mult,
               r=["ps%d" % (2 * fc + 1), "sgl%d" % si], w=["hid%d_%d" % (hb, fc)])

    def down(e_, s4, hb):
        eb = e_ % 2
        for i in range(4):
            m = 4 * s4 + i
            for half in range(2):
                ob_ = 4 + (i * 2 + half) % 4
                mms([(ps[ob_][:, :], hid[hb][:, fc, i * 128:(i + 1) * 128], Wd[eb][:, fc, half * 512:(half + 1) * 512], fc == 0, fc == 1)
                     for fc in range(2)], r=["hid%d_0" % hb, "hid%d_1" % hb, "Wd%d" % eb], w=["ps%d" % ob_])
                tb_ = (i * 2 + half) % 4
                act(tacc[tb_], ps[ob_][:, :], AF.Identity, r=["ps%d" % ob_, ("cmb", m)], w=["tacc%d" % tb_],
                    scale=cmb[:, m, e_:e_ + 1])
                tt("dve", X[:, m, half * 512:(half + 1) * 512], X[:, m, half * 512:(half + 1) * 512], tacc[tb_],
                   ALU.add, r=["tacc%d" % tb_, ("X", m)], w=[("X", m)])

    NE = NEXP
    load_expert(0)
    prev = None
    step = 0
    for e_ in range(NE):
        for s4 in range(4 if not mini else 1):
            hb = step % 2
            step += 1
            gate_up(e_, s4, hb)
            if prev is not None:
                down(*prev)
            prev = (e_, s4, hb)
            if s4 == 0 and e_ + 1 < NE:
                load_expert(e_ + 1)
    down(*prev)

    for m in range(NOWN if not mini else 4):
        dma(out_d[m * 128:(m + 1) * 128, :], X[:, m, :], r=[("X", m)], w=[], tag="outX")
    S.emit()
    return nc


def host_inputs(inputs, core):
    b, j = core // 4, core % 4
    f = np.float32
    x = np.asarray(inputs["x"], f)
    xb_ = np.ascontiguousarray(x[b])
    own_idx = np.concatenate([np.arange(512 * m + 128 * j, 512 * m + 128 * j + 128) for m in range(NOWN)])
    xo_ = np.ascontiguousarray(xb_[own_idx])
    xh_ = np.zeros((4 * 128, D), f)
    for m in range(NOWN):
        st = 512 * m + 128 * j - 32
        if st >= 0:
            xh_[m * 32:(m + 1) * 32] = xb_[st:st + 32]
    pos = np.asarray(inputs["positions"], np.int32)[b]
    posb_ = np.ascontiguousarray(pos.reshape(NBLK_B, 128).T)
    poso_ = np.ascontiguousarray(pos[own_idx].reshape(NOWN, 128).T)

    def colT(v, n):
        return np.ascontiguousarray(np.asarray(v, f).reshape(n, 128).T)

    kq = np.arange(128)
    cm = np.zeros((128, 4, 2, 128), f)
    for t in range(4):
        allowed = (t * 128 + kq[:, None]) <= (j * 128 + kq[None, :])
        cm[:, t, :, :] = allowed[:, None, :]
    d = {
        "xb": xb_, "xo": xo_, "xh": xh_,
        "cT": colT(inputs["c"][b], KC),
        "posb": posb_, "poso": poso_,
        "w_ada": np.ascontiguousarray(np.asarray(inputs["w_ada"], f)[0]),
        "b_ada": np.ascontiguousarray(np.asarray(inputs["b_ada"], f)[0:1]),
        "g_mixT": colT(inputs["g_mix"][0], KC),
        "g_ffnT": colT(inputs["g_ffn"][0], KC),
        "w_in": np.ascontiguousarray(np.asarray(inputs["w_in"], f)[0]),
        "q_norm_g": np.asarray(inputs["q_norm_g"], f)[0:1],
        "k_norm_g": np.asarray(inputs["k_norm_g"], f)[0:1],
        "lambda_q1": np.asarray(inputs["lambda_q1"], f)[0:1],
        "lambda_k1": np.asarray(inputs["lambda_k1"], f)[0:1],
        "lambda_q2": np.asarray(inputs["lambda_q2"], f)[0:1],
        "lambda_k2": np.asarray(inputs["lambda_k2"], f)[0:1],
        "subln_g": np.asarray(inputs["subln_g"], f)[0:1],
        "b_gluT": colT(inputs["b_glu"][0], 8),
        "w_dwT": np.ascontiguousarray(np.asarray(inputs["w_dw"], f)[0].T.reshape(4, 128, 31).transpose(1, 0, 2)),
        "b_dwT": colT(inputs["b_dw"][0], 4),
        "ln_gT": colT(inputs["conv_ln_g"][0], 4),
        "ln_bT": colT(inputs["conv_ln_b"][0], 4),
        "w_out": np.ascontiguousarray(np.asarray(inputs["w_out"], f)[0]),
        "w_rt": np.ascontiguousarray(np.concatenate([np.asarray(inputs["w_group"], f)[0],
                                                     np.asarray(inputs["w_router"], f)[0]], axis=1)),
        "b_rt": np.ascontiguousarray(np.concatenate([np.asarray(inputs["b_group"], f)[0],
                                                     np.asarray(inputs["b_router"], f)[0]])[None, :]),
        "w_gate": np.ascontiguousarray(np.asarray(inputs["w_gate"], f)[0]),
        "w_up": np.ascontiguousarray(np.asarray(inputs["w_up"], f)[0]),
        "w_down": np.ascontiguousarray(np.asarray(inputs["w_down"], f)[0]),
        "ident_f": np.eye(128, dtype=f),
        "ident_b": np.eye(128, dtype=f).astype(ml_dtypes.bfloat16),
        "inv_freq": (500000.0 ** (-np.arange(0, 16, 2, dtype=f) / 16.0)).astype(f)[None, :],
        "cmask": cm.reshape(128, 1024).astype(ml_dtypes.bfloat16),
        "hmask": np.full((128, 1), 0.0 if j == 0 else 1.0, f),
    }
    return d, own_idx


def kernel(**inputs):
    nc = build_program()
    in_maps = []
    owns = []
    for c in range(8):
        d, own_idx = host_inputs(inputs, c)
        in_maps.append(d)
        owns.append(own_idx)
    res = run_bass_kernel_spmd(nc, in_maps, core_ids=list(range(8)))
    out = np.zeros((2, SEQ, D), np.float32)
    for c in range(8):
        out[c // 4, owns[c]] = np.asarray(res.results[c]["out"], np.float32)
    return out
```
